# Optimizing a Trainium2 kernel written in Bass

```python
import math
import jax, jax.numpy as jnp
from jax import lax
import numpy as np

D_MODEL = 1024
BATCH = 2
SEQ = 8192
DEPTH = 2

N_A_LAYERS = DEPTH // 2
N_B_LAYERS = DEPTH - N_A_LAYERS
RET_HEADS = 4
RET_QK_DIM = D_MODEL // RET_HEADS
RET_V_DIM = 2 * RET_QK_DIM
RET_CHUNK = 128
RET_ROT_BASE = 10000.0
ATT_HEADS = 8
ATT_HEAD_DIM = D_MODEL // ATT_HEADS
ROPE_DIM = ATT_HEAD_DIM // 4
ROPE_THETA = 500000.0
MOBA_BLOCK = 256
MOBA_TOPK = 3
MOBA_Q_CHUNK = 32
N_GROUPS = 4
EXPERTS_PER_GROUP = 4
D_EXPERT = D_MODEL // 2
EXPERT_TOPK = 2
NORM_EPS = 1e-6

kernel_name = 'yoco_retention_moba_hmoe'


def rms_norm(x, g):
    x32 = x.astype(jnp.float32)
    y = x32 * lax.rsqrt(jnp.mean(x32 * x32, axis=-1, keepdims=True) + NORM_EPS)
    return (y * g.astype(jnp.float32)).astype(x.dtype)


def rotary(x, pos, rot_dim, theta):
    half = rot_dim // 2
    inv = 1.0 / (theta ** (jnp.arange(half, dtype=jnp.float32) / half))
    ang = pos.astype(jnp.float32)[:, None] * inv[None, :]
    cos = jnp.cos(ang)[:, None, :]
    sin = jnp.sin(ang)[:, None, :]
    xr = x[..., :rot_dim].astype(jnp.float32)
    x1, x2 = xr[..., :half], xr[..., half:]
    rot = jnp.concatenate([x1 * cos - x2 * sin, x2 * cos + x1 * sin], axis=-1).astype(x.dtype)
    return jnp.concatenate([rot, x[..., rot_dim:]], axis=-1)


def retention(x, w_in, w_out):
    B, S, _ = x.shape
    H, dk, dv, C = RET_HEADS, RET_QK_DIM, RET_V_DIM, RET_CHUNK
    f32 = jnp.float32
    proj = x @ w_in
    q, k, v, g = jnp.split(proj, [H * dk, 2 * H * dk, 2 * H * dk + H * dv], axis=-1)
    pos = jnp.arange(S)
    q = rotary(q.reshape(B, S, H, dk), pos, dk, RET_ROT_BASE)
    k = rotary(k.reshape(B, S, H, dk), pos, dk, RET_ROT_BASE) * (dk ** -0.5)
    v = v.reshape(B, S, H, dv)
    n_c = S // C
    qc = q.astype(f32).reshape(B, n_c, C, H, dk).transpose(1, 0, 3, 2, 4)
    kc = k.astype(f32).reshape(B, n_c, C, H, dk).transpose(1, 0, 3, 2, 4)
    vc = v.astype(f32).reshape(B, n_c, C, H, dv).transpose(1, 0, 3, 2, 4)
    log_gamma = jnp.log1p(-jnp.power(2.0, -5.0 - jnp.arange(H, dtype=f32)))
    idx = jnp.arange(C, dtype=f32)
    diff = idx[:, None] - idx[None, :]
    decay_mask = jnp.where(diff >= 0, jnp.exp(log_gamma[:, None, None] * jnp.maximum(diff, 0.0)), 0.0)
    q_decay = jnp.exp(log_gamma[:, None] * (idx + 1.0))
    k_decay = jnp.exp(log_gamma[:, None] * (C - 1.0 - idx))
    chunk_decay = jnp.exp(log_gamma * C)
    scores = jnp.einsum('nbhcd,nbhmd->nbhcm', qc, kc) * decay_mask[None, None]
    o_intra = jnp.einsum('nbhcm,nbhme->nbhce', scores, vc)

    def step(state, inp):
        qi, ki, vi = inp
        o = jnp.einsum('bhcd,bhde->bhce', qi * q_decay[None, :, :, None], state)
        state = state * chunk_decay[None, :, None, None] + jnp.einsum(
            'bhcd,bhce->bhde', ki * k_decay[None, :, :, None], vi)
        return state, o

    state0 = jnp.zeros((B, H, dk, dv), f32)
    _, o_cross = lax.scan(step, state0, (qc, kc, vc))
    o = (o_intra + o_cross).transpose(1, 0, 3, 2, 4).reshape(B, S, H, dv)
    o = o * lax.rsqrt(jnp.mean(o * o, axis=-1, keepdims=True) + NORM_EPS)
    y = jax.nn.silu(g.astype(f32)) * o.reshape(B, S, H * dv)
    return y.astype(x.dtype) @ w_out


def shared_kv(h, kv_norm, w_kv):
    B, S, _ = h.shape
    H, dh, BLK = ATT_HEADS, ATT_HEAD_DIM, MOBA_BLOCK
    hn = rms_norm(h, kv_norm)
    k, v = jnp.split(hn @ w_kv, 2, axis=-1)
    k = rotary(k.reshape(B, S, H, dh), jnp.arange(S), ROPE_DIM, ROPE_THETA)
    v = v.reshape(B, S, H, dh)
    n_blk = -(-S // BLK)
    pad = n_blk * BLK - S
    k = jnp.pad(k, ((0, 0), (0, pad), (0, 0), (0, 0))).transpose(0, 2, 1, 3).reshape(B, H, n_blk, BLK, dh)
    v = jnp.pad(v, ((0, 0), (0, pad), (0, 0), (0, 0))).transpose(0, 2, 1, 3).reshape(B, H, n_blk, BLK, dh)
    k_mean = jnp.mean(k.astype(jnp.float32), axis=3).astype(k.dtype)
    return k, v, k_mean


def moba_attention(x, w_q, w_o, kb, vb, k_mean):
    B, S, _ = x.shape
    H, dh, BLK, QC = ATT_HEADS, ATT_HEAD_DIM, MOBA_BLOCK, MOBA_Q_CHUNK
    n_blk = kb.shape[2]
    s_pad = n_blk * BLK
    topk = min(MOBA_TOPK, n_blk)
    q = (x @ w_q).reshape(B, S, H, dh)
    q = rotary(q, jnp.arange(S), ROPE_DIM, ROPE_THETA) * (dh ** -0.5)
    q = jnp.pad(q, ((0, 0), (0, s_pad - S), (0, 0), (0, 0))).transpose(0, 2, 1, 3)
    n_qc = s_pad // QC
    qs = q.reshape(B, H, n_qc, QC, dh).transpose(2, 0, 1, 3, 4)
    b_idx = jnp.arange(B)[:, None, None, None]
    h_idx = jnp.arange(H)[None, :, None, None]
    blk_ids = jnp.arange(n_blk)
    neg_inf = -jnp.inf

    def attend(args):
        qi, ci = args
        q0 = ci * QC
        blk = q0 // BLK
        gate = jnp.einsum('bhqd,bhnd->bhqn', qi, k_mean).astype(jnp.float32)
        gate = jnp.where(blk_ids < blk, gate, neg_inf)
        _, sel = lax.top_k(gate, topk)
        slot_ok = jnp.arange(topk) < blk
        ks = kb[b_idx, h_idx, sel]
        vs = vb[b_idx, h_idx, sel]
        s_sel = jnp.einsum('bhqd,bhqnkd->bhqnk', qi, ks).astype(jnp.float32)
        s_sel = jnp.where(slot_ok[:, None], s_sel, neg_inf).reshape(B, H, QC, topk * BLK)
        k_own = lax.dynamic_index_in_dim(kb, blk, axis=2, keepdims=False)
        v_own = lax.dynamic_index_in_dim(vb, blk, axis=2, keepdims=False)
        s_own = jnp.einsum('bhqd,bhkd->bhqk', qi, k_own).astype(jnp.float32)
        q_pos = q0 + jnp.arange(QC)
        k_pos = blk * BLK + jnp.arange(BLK)
        s_own = jnp.where(k_pos[None, :] <= q_pos[:, None], s_own, neg_inf)
        p = jax.nn.softmax(jnp.concatenate([s_sel, s_own], axis=-1), axis=-1).astype(vb.dtype)
        p_sel = p[..., :topk * BLK].reshape(B, H, QC, topk, BLK)
        p_own = p[..., topk * BLK:]
        return (jnp.einsum('bhqnk,bhqnkd->bhqd', p_sel, vs)
                + jnp.einsum('bhqk,bhkd->bhqd', p_own, v_own))

    o = lax.map(attend, (qs, jnp.arange(n_qc)))
    o = o.transpose(1, 0, 3, 2, 4).reshape(B, s_pad, H * dh)[:, :S]
    return o @ w_o


def hier_moe(x, w_rg, b_rg, w_re, b_re, w_gate, w_up, w_down):
    B, S, D = x.shape
    G, E = N_GROUPS, EXPERTS_PER_GROUP
    t = x.reshape(-1, D)
    g_prob = jax.nn.softmax((t @ w_rg).astype(jnp.float32) + b_rg, axis=-1)
    g_val, g_idx = lax.top_k(g_prob, 1)
    e_logits_all = jnp.einsum('td,gde->tge', t, w_re).astype(jnp.float32) + b_re
    e_logits = jnp.take_along_axis(e_logits_all, g_idx[:, :, None], axis=1)[:, 0]
    e_val, e_idx = lax.top_k(e_logits, EXPERT_TOPK)
    e_w = jax.nn.softmax(e_val, axis=-1)
    within = jnp.sum(jax.nn.one_hot(e_idx, E) * e_w[..., None], axis=1)
    group_w = jax.nn.one_hot(g_idx[:, 0], G) * g_val
    combine = (group_w[:, :, None] * within[:, None, :]).astype(x.dtype)
    y = jnp.zeros_like(t)
    for g in range(G):
        hg = jax.nn.silu(jnp.einsum('td,edf->tef', t, w_gate[g])) * jnp.einsum('td,edf->tef', t, w_up[g])
        y = y + jnp.einsum('tef,efd->td', hg * combine[:, g, :, None], w_down[g])
    return y.reshape(B, S, D)


def setup_inputs(seed: int = 0) -> dict:
    key = jax.random.key(seed)
    ks = jax.random.split(key, 18)
    f32 = jnp.float32

    def normal(k, shape, scale):
        return jax.random.normal(k, shape, f32) * scale

    def gain(k, shape):
        return 1.0 + 0.05 * jax.random.normal(k, shape, f32)

    out_scale = (2.0 * DEPTH) ** -0.5
    ret_in_cols = 2 * RET_HEADS * RET_QK_DIM + 2 * RET_HEADS * RET_V_DIM
    att_w = ATT_HEADS * ATT_HEAD_DIM
    return {
        'x': normal(ks[0], (BATCH, SEQ, D_MODEL), 1.0),
        'ret_norm': gain(ks[1], (N_A_LAYERS, D_MODEL)),
        'ret_w_in': normal(ks[2], (N_A_LAYERS, D_MODEL, ret_in_cols), D_MODEL ** -0.5),
        'ret_w_out': normal(ks[3], (N_A_LAYERS, RET_HEADS * RET_V_DIM, D_MODEL), (RET_HEADS * RET_V_DIM) ** -0.5 * out_scale),
        'kv_norm': gain(ks[4], (D_MODEL,)),
        'w_kv': normal(ks[5], (D_MODEL, 2 * att_w), D_MODEL ** -0.5),
        'attn_norm': gain(ks[6], (N_B_LAYERS, D_MODEL)),
        'w_q': normal(ks[7], (N_B_LAYERS, D_MODEL, att_w), D_MODEL ** -0.5),
        'w_o': normal(ks[8], (N_B_LAYERS, att_w, D_MODEL), att_w ** -0.5 * out_scale),
        'ffn_norm': gain(ks[9], (DEPTH, D_MODEL)),
        'router_group_w': normal(ks[10], (DEPTH, D_MODEL, N_GROUPS), D_MODEL ** -0.5),
        'router_group_b': normal(ks[11], (DEPTH, N_GROUPS), 0.01),
        'router_expert_w': normal(ks[12], (DEPTH, N_GROUPS, D_MODEL, EXPERTS_PER_GROUP), D_MODEL ** -0.5),
        'router_expert_b': normal(ks[13], (DEPTH, N_GROUPS, EXPERTS_PER_GROUP), 0.01),
        'expert_w_gate': normal(ks[14], (DEPTH, N_GROUPS, EXPERTS_PER_GROUP, D_MODEL, D_EXPERT), D_MODEL ** -0.5),
        'expert_w_up': normal(ks[15], (DEPTH, N_GROUPS, EXPERTS_PER_GROUP, D_MODEL, D_EXPERT), D_MODEL ** -0.5),
        'expert_w_down': normal(ks[16], (DEPTH, N_GROUPS, EXPERTS_PER_GROUP, D_EXPERT, D_MODEL), D_EXPERT ** -0.5 * out_scale),
        'final_norm': gain(ks[17], (D_MODEL,)),
    }


def reference(x, ret_norm, ret_w_in, ret_w_out, kv_norm, w_kv, attn_norm, w_q, w_o,
              ffn_norm, router_group_w, router_group_b, router_expert_w, router_expert_b,
              expert_w_gate, expert_w_up, expert_w_down, final_norm):
    h = x
    kb = vb = k_mean = None
    for layer in range(DEPTH):
        if layer < N_A_LAYERS:
            h = h + retention(rms_norm(h, ret_norm[layer]), ret_w_in[layer], ret_w_out[layer])
        else:
            if layer == N_A_LAYERS:
                kb, vb, k_mean = shared_kv(h, kv_norm, w_kv)
            j = layer - N_A_LAYERS
            h = h + moba_attention(rms_norm(h, attn_norm[j]), w_q[j], w_o[j], kb, vb, k_mean)
        h = h + hier_moe(rms_norm(h, ffn_norm[layer]), router_group_w[layer], router_group_b[layer],
                         router_expert_w[layer], router_expert_b[layer], expert_w_gate[layer],
                         expert_w_up[layer], expert_w_down[layer])
    return rms_norm(h, final_norm)
```

```python
import numpy as np
import ml_dtypes
import concourse.bass as bass
import concourse.mybir as mybir
from concourse.bass_utils import run_bass_kernel_spmd

F32 = mybir.dt.float32
BF16 = mybir.dt.bfloat16
AF = mybir.ActivationFunctionType
ALU = mybir.AluOpType
AX = mybir.AxisListType

EPS = 1e-6
ENGS = ("pe", "act", "dve", "pool", "sp")


class _First:
    def __init__(self, eng, w):
        self._e, self._w = eng, w

    def __getattr__(self, name):
        real = getattr(self._e, name)
        if not callable(real):
            return real

        def call(*a, **k):
            ins = real(*a, **k)
            if self._w is not None and hasattr(ins, "_wait_ge"):
                ins._wait_ge(*self._w)
                self._w = None
            return ins
        return call


class _Dummy:
    def then_inc(self, *a, **k):
        return self

    def _wait_ge(self, *a, **k):
        return self


class _Rec:
    def __init__(self):
        self.calls = []

    def __getattr__(self, name):
        def call(*a, **k):
            self.calls.append((name, a, k))
            return _Dummy()
        return call


class Prog:
    def __init__(self, nc):
        self.nc = nc
        self.ops = []
        self.want_pid = False
        self.reorder = True
        self.debug = None
        self.fifo = FIFO_Q
        self.psum = set()

    def op(self, eng, fn, reads=(), writes=()):
        self.ops.append(dict(eng=eng, fn=fn, r=tuple(reads), w=tuple(writes), dma=None))

    def dma(self, eng, fn, reads=(), writes=(), lane=None):
        assert lane is not None
        self.ops.append(dict(eng=eng, fn=fn, r=tuple(reads), w=tuple(writes), dma=lane))

    def cc(self, fn, reads=(), writes=(), lane=None):
        self.ops.append(dict(eng="pool", fn=fn, r=tuple(reads), w=tuple(writes), dma=lane, cc=True))

    def barrier(self):
        self.ops.append(dict(eng="barrier", fn=None, r=(), w=(), dma=None))

    def finish(self):
        self.ops.append(dict(eng="sp", fn=lambda e: e.nop(), r=(), w=(), dma=None, fin=True))

    def _cost(self, o):
        E = o["eng"]
        rec = _Rec()
        try:
            o["fn"](rec)
        except Exception:
            return (50.0, 2000.0) if o["dma"] is not None else (500.0, 0.0)
        busy, lat = 0.0, 0.0
        for name, a, k in rec.calls:
            out = k.get("out", a[0] if a else None)
            if name == "dma_start":
                src = k.get("in_")
                nb = 1
                for d_ in out.shape:
                    nb *= d_
                nb *= max(mybir.dt.size(out.dtype), mybir.dt.size(src.dtype))
                busy += 50.0
                lat += nb / 250.0
            elif name in ("matmul", "transpose"):
                src = k.get("lhsT") if name == "matmul" else k.get("in_")
                f = 4.0 if src.dtype == F32 else 1.0
                busy += (16.0 + 0.46 * out.free_size()) * f
            elif name == "collective_compute":
                busy += 100.0
            elif out is None or not hasattr(out, "free_size"):
                busy += 50.0
            else:
                nf = out.free_size()
                busy += {"act": 200.0 + 1.05 * nf, "dve": 100.0 + 1.4 * nf, "pool": 150.0 + 4.6 * nf}.get(E, 100.0 + nf)
        return busy, lat

    def _sched_seg(self, seg):
        n = len(seg)
        if n < 3:
            return seg
        deps = [set() for _ in range(n)]
        last_w, readers = {}, {}
        for i, o in enumerate(seg):
            d = deps[i]
            for b in o["r"]:
                if b in last_w:
                    d.add(last_w[b])
                if b in self.psum:
                    for r in readers.get(b, ()):
                        if seg[r]["eng"] != o["eng"]:
                            d.add(r)
            for b in o["w"]:
                if b in last_w:
                    d.add(last_w[b])
                d.update(readers.get(b, ()))
            d.discard(i)
            for b in o["w"]:
                last_w[b] = i
                readers[b] = []
            for b in o["r"]:
                readers.setdefault(b, []).append(i)
        succ = [[] for _ in range(n)]
        indeg = [len(d) for d in deps]
        for i, d in enumerate(deps):
            for x in d:
                succ[x].append(i)
        cost = [self._cost(o) for o in seg]
        free_at = dict.fromkeys(ENGS, 0.0)
        dma_free = 0.0
        ready = [0.0] * n
        ext = getattr(self, "_ext", {})
        for i, o in enumerate(seg):
            for b in o["r"]:
                if b in ext:
                    ready[i] = max(ready[i], ext[b])
        avail = {E: [] for E in ENGS}
        for i in range(n):
            if indeg[i] == 0:
                avail[seg[i]["eng"]].append(i)
        order = []
        SLACK = 150.0
        while len(order) < n:
            best = None
            for E in ENGS:
                av = avail[E]
                if not av:
                    continue
                t0 = max(free_at[E], min(ready[i] for i in av))
                if self.fifo:
                    cand = min((i for i in av if ready[i] <= t0 + SLACK), key=lambda i: (int(ready[i] / self.fifo), i))
                else:
                    cand = min(i for i in av if ready[i] <= t0 + SLACK)
                st = max(free_at[E], ready[cand])
                if best is None or (st, cand) < best[:2]:
                    best = (st, cand, E)
            st, i, E = best
            avail[E].remove(i)
            busy, lat = cost[i]
            free_at[E] = st + busy
            if seg[i].get("cc"):
                fin = st + 150000.0
            elif seg[i]["dma"] is not None:
                dma_free = max(st + busy, dma_free) + lat
                fin = dma_free + 2000.0
            else:
                fin = st + busy + 100.0
            order.append(i)
            if self.debug is not None:
                self.debug.append((seg[i], st, fin, E))
            for j in succ[i]:
                if fin > ready[j]:
                    ready[j] = fin
                indeg[j] -= 1
                if indeg[j] == 0:
                    avail[seg[j]["eng"]].append(j)
        self.sim_time = getattr(self, "sim_time", 0.0) + max(max(free_at.values()), dma_free)
        self._ext = {}
        k = 0
        for i in order:
            if seg[i].get("cc"):
                k += 1
        j = 0
        for i in order:
            if seg[i].get("cc"):
                j += 1
                late = max(0.0, 60000.0 * (j - (k - 4))) if j > k - 4 else 0.0
                for b in seg[i]["w"]:
                    self._ext[b] = late
        return [seg[i] for i in order]

    def _schedule(self, ops):
        out, seg = [], []
        for o in ops:
            if o["eng"] == "barrier" or o.get("fin"):
                out += self._sched_seg(seg)
                seg = []
                out.append(o)
            else:
                seg.append(o)
        return out + self._sched_seg(seg)

    def build(self):
        nc = self.nc
        if self.reorder:
            self.ops = self._schedule(self.ops)
        ops = self.ops
        n = len(ops)
        last_w = {}
        readers = {}
        deps = [None] * n
        bar_deps = set()
        last_on_eng = {}
        lanes_last = {}
        first_after_bar = {}
        for i, o in enumerate(ops):
            if o["eng"] == "barrier":
                bar_deps = set(last_on_eng.values()) | {v for k, v in lanes_last.items() if not k.startswith("CC:")}
                first_after_bar = {}
                last_w = {k: v for k, v in last_w.items() if k.startswith("d:")}
                readers = {k: v for k, v in readers.items() if k.startswith("d:")}
                deps[i] = set()
                continue
            raw = set()
            oth = set()
            for b in o["r"]:
                if b in last_w:
                    raw.add(last_w[b])
                if b in self.psum:
                    for r in readers.get(b, ()):
                        if ops[r]["eng"] != o["eng"]:
                            raw.add(r)
            for b in o["w"]:
                if b in last_w:
                    oth.add(last_w[b])
                for r in readers.get(b, ()):
                    oth.add(r)
            E = o["eng"]
            d = set()
            for x in raw | oth:
                if x == i:
                    continue
                ox = ops[x]
                if ox["dma"] is None and o["dma"] is None and ox["eng"] == E:
                    if E == "pe":
                        continue
                    if x not in raw:
                        continue
                d.add(x)
            if o.get("fin"):
                d |= set(last_on_eng.values()) | set(lanes_last.values())
            if bar_deps and E not in first_after_bar:
                d |= bar_deps
                first_after_bar[E] = i
            deps[i] = d
            for b in o["w"]:
                last_w[b] = i
                readers[b] = []
            for b in o["r"]:
                lst = readers.setdefault(b, [])
                if o["dma"] is None:
                    lst[:] = [r for r in lst if not (ops[r]["dma"] is None and ops[r]["eng"] == E)]
                lst.append(i)
            if o["dma"] is None:
                last_on_eng[E] = i
            else:
                lanes_last[o["dma"]] = i

        pos = [0] * n
        eng_count = {e: 0 for e in ENGS}
        lane_count = {}
        seen = {e: {} for e in ENGS}
        snap = [None] * n
        waits = [None] * n
        signals = [False] * n
        for i, o in enumerate(ops):
            if o["eng"] == "barrier":
                continue
            E = o["eng"]
            w = []
            for d in sorted(deps[i], reverse=True):
                od = ops[d]
                key = ("L", od["dma"]) if od["dma"] is not None else ("E", od["eng"])
                need = pos[d]
                if od["dma"] is not None and od["dma"].startswith("G:"):
                    need = lane_count[od["dma"]]
                if seen[E].get(key, 0) >= need:
                    continue
                w.append((key, d, need))
                if od["dma"] is None:
                    signals[d] = True
                seen[E][key] = need
                for k2, v2 in snap[d].items():
                    if seen[E].get(k2, 0) < v2:
                        seen[E][k2] = v2
            waits[i] = w
            if o["dma"] is not None:
                lane_count[o["dma"]] = lane_count.get(o["dma"], 0) + 1
                pos[i] = lane_count[o["dma"]]
            else:
                eng_count[E] += 1
                pos[i] = eng_count[E]
            snap[i] = dict(seen[E])

        sigval = [0] * n
        cnt = {e: 0 for e in ENGS}
        for i, o in enumerate(ops):
            if o["eng"] == "barrier" or o["dma"] is not None:
                continue
            if signals[i]:
                cnt[o["eng"]] += 1
            sigval[i] = cnt[o["eng"]]

        sems = {e: nc.alloc_semaphore(f"s_{e}") for e in ENGS}
        lane_sems = {ln: nc.alloc_semaphore(f"l{j}") for j, ln in enumerate(sorted(lane_count))}
        per_eng = {e: [i for i, o in enumerate(ops) if o["eng"] == e] for e in ENGS}
        self.stats = dict(n_ops=n, lanes=len(lane_sems), sig=dict(cnt),
                          nwaits=sum(len(w) for w in waits if w))

        self.pid = {}

        def run(E, eng):
            if E in ("sp", "pool") and self.want_pid:
                r = eng.alloc_register("qoff")
                eng.reg_mod(r, eng.partition_id(), 4)
                eng.reg_mul(r, r, 8192)
                self.pid[E] = eng.snap(r, min_val=0, max_val=3 * 8192)
            for i in per_eng[E]:
                o = ops[i]
                wl = []
                for key, d, need in waits[i]:
                    if key[0] == "L":
                        wl.append((lane_sems[key[1]], 1 if ops[d].get("cc") else 16 * need))
                    else:
                        wl.append((sems[key[1]], sigval[d]))
                attach = bool(wl) and not o.get("cc") and not o.get("fin")
                for sem_, val_ in (wl[:-1] if attach else wl):
                    eng.wait_ge(sem_, val_)
                ins = o["fn"](_First(eng, wl[-1]) if attach else eng)
                if o.get("cc"):
                    ins.then_inc(lane_sems[o["dma"]])
                elif o["dma"] is not None:
                    ins.then_inc(lane_sems[o["dma"]], 16)
                elif signals[i]:
                    ins.then_inc(sems[E], 1)

        with nc.Block() as block:
            @block.tensor
            def _(e):
                run("pe", e)

            @block.scalar
            def _(e):
                run("act", e)

            @block.vector
            def _(e):
                run("dve", e)

            @block.gpsimd
            def _(e):
                run("pool", e)

            @block.sync
            def _(e):
                run("sp", e)


class Ctx:
    def __init__(self, nc, P, pfx):
        self.nc, self.P, self.pfx = nc, P, pfx

    def sb(self, name, shape, dt):
        return self.nc.alloc_sbuf_tensor(f"{self.pfx}{name}", list(shape), dt)

    def ps(self, name, shape, dt=F32):
        return self.nc.alloc_psum_tensor(f"{self.pfx}{name}", list(shape), dt)


def rstd_ops(P, ss, tmp, rstd, n, rd, wr):
    P.op("act", lambda e: e.activation(out=tmp, in_=ss, func=AF.Ln, scale=1.0 / n, bias=EPS),
         reads=rd, writes=[wr + "_t"])
    P.op("act", lambda e: e.activation(out=rstd, in_=tmp, func=AF.Exp, scale=-0.5),
         reads=[wr + "_t"], writes=[wr])


A_ENGS = ("dve", "pool", "pool")
FIFO_Q = 0.0
NXT = 6


def phase_ret(P, nc, D, NT=8192, NS=2):
    C = Ctx(nc, P, "A_")
    RENG, QENG, GENG = A_ENGS
    NG = NT // 512
    ident = C.sb("ident", [128, 128], BF16)
    w_bf = C.sb("w_bf", [128, 8, 1536], BF16)
    wst = [C.sb(f"wst{i}", [128, 1536], F32) for i in range(2)]
    gain = C.sb("gain", [128, 8], F32)
    dmask = C.sb("dmask", [128, 128], F32)
    qdec4 = C.sb("qdec4", [128, 512], F32)
    kdec = C.sb("kdec", [128, 1], F32)
    cdec = C.sb("cdec", [128, 1], F32)
    xt = [C.sb(f"xt{i}", [128, 1024], F32) for i in range(NXT)]
    junk = C.sb("junk", [128, 1024], BF16)
    xn = [C.sb(f"xn{i}", [128, 1024], BF16) for i in range(2)]
    xnT = [C.sb(f"xnT{i}", [128, 8, 512], BF16) for i in range(NS)]
    cosg = [C.sb(f"cos{i}", [128, 512], F32) for i in range(NS)]
    sing = [C.sb(f"sin{i}", [128, 512], F32) for i in range(NS)]
    qrot = [C.sb(f"qrot{i}", [128, 2, 512], BF16) for i in range(NS)]
    qd = [C.sb(f"qd{i}", [128, 2, 512], BF16) for i in range(NS)]
    krot = [C.sb(f"krot{i}", [128, 2, 512], BF16) for i in range(NS)]
    vv = [C.sb(f"v{i}", [128, 4, 512], BF16) for i in range(NS)]
    sg = [C.sb(f"sg{i}", [128, 4, 512], F32) for i in range(NS)]
    ktm = [C.sb(f"ktm{i}", [128, 4, 256], BF16) for i in range(NS)]
    tmp = [C.sb(f"tmp{i}", [128, 512], F32) for i in range(4)]
    st32 = C.sb("st32", [128, 2, 512], F32)
    stbf = [C.sb(f"stbf{i}", [128, 2, 512], BF16) for i in range(2)]
    stm = [C.sb(f"stm{i}", [128, 128], BF16) for i in range(2)]
    ybuf = [C.sb(f"ybuf{i}", [128, 512], BF16) for i in range(4)]
    stat = C.sb("stat", [128, 24], F32)
    junk2 = C.sb("junk2", [128, 512], BF16)
    ge = C.sb("ge", [128, 512], F32)
    gc = C.sb("gc", [128, 512], F32)

    psT = C.ps("psT", [128, 8, 128], BF16)
    pq = [C.ps(f"pq{i}", [128, 512]) for i in range(2)]
    pv = C.ps("pv", [128, 512])
    pg = C.ps("pg", [128, 512])
    pst = C.ps("pst", [128, 128])
    po = C.ps("po", [128, 512])
    pu = C.ps("pu", [128, 512])

    P.psum |= {"A_psT", "A_pq0", "A_pq1", "A_pv", "A_pg", "A_pst", "A_po", "A_pu"}

    def ld(dst, src, name, eng="sp", lane=None):
        P.dma(eng, lambda e: e.dma_start(out=dst, in_=src), writes=[name], lane=lane or ("L:" + name))

    ld(ident[:], D["ident"], "A_ident", lane="G:const")
    ld(gain[:], D["gain"], "A_gain", lane="G:const")
    ld(dmask[:], D["dmask"], "A_dmask", lane="G:const")
    ld(qdec4[:], D["qdec4"], "A_qdec4", lane="G:const")
    ld(kdec[:], D["kdec"], "A_kdec", lane="G:const")
    ld(cdec[:], D["cdec"], "A_cdec", lane="G:const")
    wv_ = D["w"].rearrange("(kc p) f -> p kc f", p=128)
    for kc in range(8):
        s = kc % 2
        ld(wst[s][:], wv_[:, kc, :], f"A_wst{s}")
        P.op("dve", lambda e, kc=kc, s=s: e.tensor_scalar(
            out=w_bf[:, kc, :], in0=wst[s][:], scalar1=gain[:, kc:kc + 1], scalar2=None, op0=ALU.mult),
            reads=[f"A_wst{s}", "A_gain"], writes=[f"A_w{kc}"])
    WN = [f"A_w{kc}" for kc in range(8)]
    P.op("dve", lambda e: e.memset(st32[:], 0.0), writes=["A_st32_0", "A_st32_1"])
    P.op("pool", lambda e: e.memset(stbf[0][:], 0.0), writes=["A_stbf0_0", "A_stbf0_1"])

    xb = D["xb"]
    for tg in range(NG):
        s = tg % NS
        t0 = tg * 512
        ld(cosg[s][:], D["cosT"][:, t0:t0 + 512], f"A_cos{s}")
        ld(sing[s][:], D["sinT"][:, t0:t0 + 512], f"A_sin{s}")
        for tt in range(4):
            xs = (tg * 4 + tt) % NXT
            r0 = t0 + tt * 128
            ld(xt[xs][:], xb[r0:r0 + 128, :], f"A_xt{xs}")
            c0 = 3 * tt
            P.op("act", lambda e, xs=xs, c0=c0: e.activation(out=junk[:], in_=xt[xs][:], func=AF.Square,
                                                            accum_out=stat[:, c0:c0 + 1]),
                 reads=[f"A_xt{xs}"], writes=[f"A_ss{tt}"])
            rstd_ops(P, stat[:, c0:c0 + 1], stat[:, c0 + 1:c0 + 2], stat[:, c0 + 2:c0 + 3], 1024, [f"A_ss{tt}"],
                     f"A_rstd{tt}")
            xq = tt % 2
            P.op("act", lambda e, xs=xs, xq=xq, c0=c0: e.activation(out=xn[xq][:], in_=xt[xs][:], func=AF.Copy,
                                                                  scale=stat[:, c0 + 2:c0 + 3]),
                 reads=[f"A_xt{xs}", f"A_rstd{tt}"], writes=[f"A_xn{xq}"])

            def tr(e, xq=xq):
                for kc in range(8):
                    ins = e.transpose(out=psT[:, kc, :], in_=xn[xq][:, kc * 128:(kc + 1) * 128],
                                      identity=ident[:])
                return ins
            P.op("pe", tr, reads=[f"A_xn{xq}", "A_ident"], writes=["A_psT"])
            P.op("dve", lambda e, s=s, tt=tt: e.tensor_copy(out=xnT[s][:, :, tt * 128:(tt + 1) * 128],
                                                            in_=psT[:]),
                 reads=["A_psT"], writes=[f"A_xnT{s}_{tt}"])
        XN = [f"A_xnT{s}_{tt}" for tt in range(4)]

        for which, dst, c0 in (("q", qrot, 0), ("k", krot, 256)):
            for hh in range(2):
                def mm(e, hh=hh, c0=c0, s=s):
                    for kc in range(8):
                        ins = e.matmul(out=pq[hh][:], lhsT=w_bf[:, kc, c0 + hh * 128:c0 + (hh + 1) * 128],
                                       rhs=xnT[s][:, kc, :], start=(kc == 0), stop=(kc == 7))
                    return ins
                P.op("pe", mm, reads=WN + XN, writes=[f"A_pq{hh}"])
            cs, sn = cosg[s], sing[s]
            P.op("dve", lambda e, cs=cs: e.tensor_tensor(out=tmp[0][:], in0=pq[0][:], in1=cs[:], op=ALU.mult),
                 reads=["A_pq0", f"A_cos{s}"], writes=["A_tmp0"])
            P.op("dve", lambda e, sn=sn: e.tensor_tensor(out=tmp[1][:], in0=pq[1][:], in1=sn[:], op=ALU.mult),
                 reads=["A_pq1", f"A_sin{s}"], writes=["A_tmp1"])
            P.op("dve", lambda e, cs=cs: e.tensor_tensor(out=tmp[2][:], in0=pq[1][:], in1=cs[:], op=ALU.mult),
                 reads=["A_pq1", f"A_cos{s}"], writes=["A_tmp2"])
            P.op("dve", lambda e, sn=sn: e.tensor_tensor(out=tmp[3][:], in0=pq[0][:], in1=sn[:], op=ALU.mult),
                 reads=["A_pq0", f"A_sin{s}"], writes=["A_tmp3"])
            P.op(RENG, lambda e, dst=dst, s=s: e.tensor_tensor(out=dst[s][:, 0, :], in0=tmp[0][:], in1=tmp[1][:],
                                                                 op=ALU.subtract),
                 reads=["A_tmp0", "A_tmp1"], writes=[f"A_{which}rot{s}_0"])
            P.op(RENG, lambda e, dst=dst, s=s: e.tensor_tensor(out=dst[s][:, 1, :], in0=tmp[2][:], in1=tmp[3][:],
                                                                 op=ALU.add),
                 reads=["A_tmp2", "A_tmp3"], writes=[f"A_{which}rot{s}_1"])
            if which == "q":
                for dc in range(2):
                    P.op(QENG, lambda e, dc=dc, s=s: e.tensor_tensor(out=qd[s][:, dc, :], in0=qrot[s][:, dc, :],
                                                                       in1=qdec4[:], op=ALU.mult),
                         reads=[f"A_qrot{s}_{dc}", "A_qdec4"], writes=[f"A_qd{s}_{dc}"])

        for tt in range(4):
            def mv(e, tt=tt, s=s):
                for kc in range(8):
                    ins = e.matmul(out=pv[:], lhsT=xnT[s][:, kc, tt * 128:(tt + 1) * 128],
                                   rhs=w_bf[:, kc, 512:1024], start=(kc == 0), stop=(kc == 7))
                return ins
            P.op("pe", mv, reads=WN + [XN[tt]], writes=["A_pv"])
            P.op("act", lambda e, tt=tt, s=s: e.activation(out=vv[s][:, tt, :], in_=pv[:], func=AF.Copy),
                 reads=["A_pv"], writes=[f"A_v{s}_{tt}"])

            def mg(e, tt=tt, s=s):
                for kc in range(8):
                    ins = e.matmul(out=pg[:], lhsT=xnT[s][:, kc, tt * 128:(tt + 1) * 128],
                                   rhs=w_bf[:, kc, 1024:1536], start=(kc == 0), stop=(kc == 7))
                return ins
            P.op("pe", mg, reads=WN + [XN[tt]], writes=["A_pg"])
            P.op("act", lambda e: e.activation(out=ge[:], in_=pg[:], func=AF.Exp, scale=-1.0),
                 reads=["A_pg"], writes=["A_ge"])
            P.op("act", lambda e: e.activation(out=gc[:], in_=pg[:], func=AF.Copy),
                 reads=["A_pg"], writes=["A_gc"])
            P.op("dve", lambda e: e.tensor_scalar(out=ge[:], in0=ge[:], scalar1=1.0, scalar2=None, op0=ALU.add),
                 reads=["A_ge"], writes=["A_ge"])
            P.op("dve", lambda e: e.reciprocal(out=ge[:], in_=ge[:]), reads=["A_ge"], writes=["A_ge"])
            P.op(GENG, lambda e, tt=tt, s=s: e.tensor_tensor(out=sg[s][:, tt, :], in0=gc[:], in1=ge[:], op=ALU.mult),
                 reads=["A_ge", "A_gc"], writes=[f"A_sg{s}_{tt}"])

            def tk(e, tt=tt, s=s):
                for dc in range(2):
                    ins = e.transpose(out=psT[:, dc, :], in_=krot[s][:, dc, tt * 128:(tt + 1) * 128],
                                      identity=ident[:])
                return ins
            P.op("pe", tk, reads=[f"A_krot{s}_0", f"A_krot{s}_1", "A_ident"], writes=["A_psT"])
            P.op("dve", lambda e, tt=tt, s=s: e.tensor_scalar(
                out=ktm[s][:, tt, :].rearrange("p (a b) -> p a b", a=2), in0=psT[:, 0:2, :],
                scalar1=kdec[:, 0:1], scalar2=None, op0=ALU.mult),
                reads=["A_psT", "A_kdec"], writes=[f"A_ktm{s}_{tt}"])

        for tt in range(4):
            c = tg * 4 + tt
            sp_ = c % 2
            sl = slice(tt * 128, (tt + 1) * 128)

            def ms(e, s=s, sl=sl):
                for dc in range(2):
                    ins = e.matmul(out=pst[:], lhsT=krot[s][:, dc, sl], rhs=qrot[s][:, dc, sl],
                                   start=(dc == 0), stop=(dc == 1))
                return ins
            P.op("pe", ms, reads=[f"A_krot{s}_0", f"A_krot{s}_1", f"A_qrot{s}_0", f"A_qrot{s}_1"],
                 writes=["A_pst"])
            P.op("dve", lambda e, sp_=sp_: e.tensor_tensor(out=stm[sp_][:], in0=pst[:], in1=dmask[:], op=ALU.mult),
                 reads=["A_pst", "A_dmask"], writes=[f"A_stm{sp_}"])

            def mo(e, s=s, sl=sl, sp_=sp_, tt=tt):
                e.matmul(out=po[:], lhsT=stm[sp_][:], rhs=vv[s][:, tt, :], start=True, stop=False)
                for dc in range(2):
                    ins = e.matmul(out=po[:], lhsT=qd[s][:, dc, sl], rhs=stbf[sp_][:, dc, :],
                                   start=False, stop=(dc == 1))
                return ins
            P.op("pe", mo, reads=[f"A_stm{sp_}", f"A_v{s}_{tt}", f"A_qd{s}_0", f"A_qd{s}_1",
                                  f"A_stbf{sp_}_0", f"A_stbf{sp_}_1"], writes=["A_po"])
            for dc in range(2):
                P.op("pe", lambda e, s=s, tt=tt, dc=dc: e.matmul(
                    out=pu[:], lhsT=ktm[s][:, tt, dc * 128:(dc + 1) * 128], rhs=vv[s][:, tt, :],
                    start=True, stop=True),
                    reads=[f"A_ktm{s}_{tt}", f"A_v{s}_{tt}"], writes=["A_pu"])
                P.op("dve", lambda e, dc=dc: e.scalar_tensor_tensor(
                    out=st32[:, dc, :], in0=st32[:, dc, :], scalar=cdec[:, 0:1], in1=pu[:],
                    op0=ALU.mult, op1=ALU.add),
                    reads=["A_pu", "A_cdec", f"A_st32_{dc}"], writes=[f"A_st32_{dc}"])
                P.op("act", lambda e, dc=dc, sp_=sp_: e.activation(out=stbf[1 - sp_][:, dc, :], in_=st32[:, dc, :],
                                                                   func=AF.Copy),
                     reads=[f"A_st32_{dc}"], writes=[f"A_stbf{1 - sp_}_{dc}"])
            g0 = 12 + 3 * (c % 2)
            P.op("act", lambda e, g0=g0: e.activation(out=junk2[:], in_=po[:], func=AF.Square,
                                                      accum_out=stat[:, g0:g0 + 1]),
                 reads=["A_po"], writes=[f"A_ssq{c % 2}"])
            rstd_ops(P, stat[:, g0:g0 + 1], stat[:, g0 + 1:g0 + 2], stat[:, g0 + 2:g0 + 3], 512, [f"A_ssq{c % 2}"],
                     f"A_rs{c % 2}")
            yb = c % 4
            P.op("dve", lambda e, yb=yb, s=s, tt=tt, g0=g0: e.scalar_tensor_tensor(
                out=ybuf[yb][:], in0=po[:], scalar=stat[:, g0 + 2:g0 + 3], in1=sg[s][:, tt, :],
                op0=ALU.mult, op1=ALU.mult),
                reads=["A_po", f"A_rs{c % 2}", f"A_sg{s}_{tt}"], writes=[f"A_ybuf{yb}"])
            P.dma("sp", lambda e, yb=yb, c=c: e.dma_start(out=D["y_out"][c * 128:(c + 1) * 128, :], in_=ybuf[yb][:]),
                  reads=[f"A_ybuf{yb}"], writes=[f"d:A_y{c // 8}"], lane=f"S:A_ybuf{yb}")
        if "after_group" in D:
            D["after_group"](tg)


def phase_ffn(P, nc, D, F, final, pfx, NTOK=2048, NE=16, stage=3):
    C = Ctx(nc, P, pfx)
    assert F <= 2048
    N = lambda s: pfx + s
    NTILE = NTOK // 128
    FC = F // 128
    NGRP = NTOK // 512
    ident = C.sb("ident", [128, 128], BF16)
    identf = C.sb("identf", [128, 128], F32)
    h = C.sb("h", [128, NTILE, 1024], F32)
    hnT = C.sb("hnT", [128, 8, NTOK], BF16)
    wbuf = C.sb("wbuf", [128, 24576], BF16)
    wo_v = wbuf[:, 0:FC * 1024].rearrange("p (f n) -> p f n", f=FC)

    def wslot(s):
        b = s * 12288
        return (wbuf[:, b:b + 4096].rearrange("p (k f) -> p k f", k=8),
                wbuf[:, b + 4096:b + 8192].rearrange("p (k f) -> p k f", k=8),
                wbuf[:, b + 8192:b + 12288].rearrange("p (k f) -> p k f", k=4))
    yt = [C.sb(f"yt{i}", [128, F], BF16) for i in range(2)]
    yT = [C.sb(f"yT{i}", [128, FC, 128], BF16) for i in range(2)]
    hn32 = [C.sb(f"hn32_{i}", [128, 1024], F32) for i in range(2)]
    hnT32 = [C.sb(f"hnT32_{i}", [128, 8, 128], F32) for i in range(2)]
    fgain = C.sb("fgain", [128, 1024], F32)
    wr = C.sb("wr", [128, 8, 20], F32)
    rbias = C.sb("rbias", [128, 20], F32)
    comb = C.sb("comb", [128, NTILE, 16], F32)
    rts = [C.sb(f"rt{i}", [128, 64], F32) for i in range(4)]
    st = C.sb("st", [128, 3, NTILE], F32)
    junk = C.sb("junk", [128, 1024], BF16)
    sgl = [C.sb(f"sgl{i}", [128, 512], F32) for i in range(2)]
    hT = [C.sb(f"hT{i}", [128, 4, 512], BF16) for i in range(2)]
    if final:
        ngain = C.sb("ngain", [128, 1024], F32)
        ob = [C.sb(f"ob{i}", [128, 1024], F32) for i in range(2)]

    pyT = C.ps("pyT", [128, 8, 128], BF16)
    pT32 = C.ps("pT32", [128, 8, 128], F32)
    pT32v = pT32[:].rearrange("p a b -> p (a b)")
    pg0 = C.ps("pg", [128, 512])
    pg = [pg0[:], pT32v[:, 0:512]]
    pyTs = [pyT[:], pg0[:].bitcast(BF16).rearrange("p (a b) -> p a b", a=8)]
    pyTn = [N("pyT"), N("pg")]
    pu = [C.ps("pu", [128, 512])[:], pT32v[:, 512:1024]]
    pgn = [N("pg"), N("pT32a")]
    pun = [N("pu"), N("pT32b")]
    pd = C.ps("pd", [128, 2, 512])
    pr = C.ps("pr", [128, 32])

    P.psum |= {N(x) for x in ("pyT", "pT32a", "pT32b", "pg", "pu", "pd0", "pd1", "pr")}

    def ld(dst, src, name, eng="sp", lane=None):
        P.dma(eng, lambda e: e.dma_start(out=dst, in_=src), writes=[name], lane=lane or ("L:" + name))

    ld(ident[:], D["ident"], N("ident"), lane="G:const")
    ld(identf[:], D["identf"], N("identf"), lane="G:const")
    ld(fgain[:], D["fgain"], N("fgain"), lane="G:const")
    ld(wr[:], D["wr"].rearrange("(kc p) n -> p kc n", p=128), N("wr"), lane="G:const")
    ld(rbias[:], D["rbias"], N("rbias"), lane="G:const")
    if final:
        ld(ngain[:], D["ngain"], N("ngain"), lane="G:const")
    xs_v = D["xs"].rearrange("(t p) f -> p t f", p=128)
    def load_hq(q):
        tq = NTILE // 4
        P.dma("sp", lambda e, q=q, tq=tq: e.dma_start(out=h[:, q * tq:(q + 1) * tq, :], in_=xs_v[:, q * tq:(q + 1) * tq, :]),
              reads=D.get("xs_reads", ()), writes=[N(f"hq{q}")], lane="G:hq")
    HQ = lambda t: N(f"hq{t // (NTILE // 4)}")
    wo_d = D["wo"].rearrange("(fc p) n -> p fc n", p=128)
    nq = FC // 4
    for q in range(nq):
        P.dma("pool", lambda e, q=q: e.dma_start(out=wo_v[:, q * 4:(q + 1) * 4, :], in_=wo_d[:, q * 4:(q + 1) * 4, :]),
              writes=[N(f"wo{q}")], lane="G:wo")
    WO = [N(f"wo{q}") for q in range(nq)]
    SL = [N("ws0"), N("ws1")]

    for t in range(NTILE):
        s = t % 2
        if "ys_fn" in D:
            P.dma(D.get("ys_eng", "sp"), lambda e, t=t, s=s: D["ys_fn"](e, t, yt[s]), reads=D["ys_reads"](t), writes=[N(f"yt{s}")],
                  lane="L:" + N(f"yt{s}"))
        else:
            ld(yt[s][:], D["ys"][t * 128:(t + 1) * 128, :], N(f"yt{s}"))
        if t % (NTILE // 4) == 0:
            load_hq(t // (NTILE // 4))
        for half in range(FC // 8):
            pb = (t * (FC // 8) + half) % 2

            def tr(e, s=s, half=half, pb=pb):
                for j in range(8):
                    fc = half * 8 + j
                    ins = e.transpose(out=pyTs[pb][:, j, :], in_=yt[s][:, fc * 128:(fc + 1) * 128], identity=ident[:])
                return ins
            P.op("pe", tr, reads=[N(f"yt{s}"), N("ident")], writes=[pyTn[pb]])
            P.op("act", lambda e, half=half, s=s, pb=pb: e.activation(out=yT[s][:, half * 8:(half + 1) * 8, :], in_=pyTs[pb],
                                                                func=AF.Copy),
                 reads=[pyTn[pb]], writes=[N(f"yT{s}_{half}")])
        for hh in range(2):
            def mm(e, hh=hh, s=s):
                for fc in range(FC):
                    ins = e.matmul(out=pd[:, hh, :], lhsT=yT[s][:, fc, :], rhs=wo_v[:, fc, hh * 512:(hh + 1) * 512],
                                   start=(fc == 0), stop=(fc == FC - 1))
                return ins
            P.op("pe", mm, reads=[N(f"yT{s}_{i}") for i in range(FC // 8)] + WO + SL, writes=[N(f"pd{hh}")])
            P.op("dve", lambda e, t=t, hh=hh: e.tensor_tensor(out=h[:, t, hh * 512:(hh + 1) * 512],
                                                              in0=pd[:, hh, :], in1=h[:, t, hh * 512:(hh + 1) * 512],
                                                              op=ALU.add),
                 reads=[N(f"pd{hh}"), HQ(t), N(f"h{t}")], writes=[N(f"h{t}")])

    for t in range(NTILE if stage >= 2 else 0):
        P.op("act", lambda e, t=t: e.activation(out=junk[:], in_=h[:, t, :], func=AF.Square, accum_out=st[:, 0, t:t + 1]),
             reads=[N(f"h{t}")], writes=[N(f"ss{t}")])
    for t in range(NTILE if stage >= 2 else 0):
        rstd_ops(P, st[:, 0, t:t + 1], st[:, 1, t:t + 1], st[:, 2, t:t + 1], 1024, [N(f"ss{t}")], N(f"rstd{t}"))
    for t in range(NTILE if stage >= 2 else 0):
        u = t % 2
        P.op("dve", lambda e, t=t, u=u: e.scalar_tensor_tensor(out=hn32[u][:], in0=h[:, t, :], scalar=st[:, 2, t:t + 1],
                                                               in1=fgain[:], op0=ALU.mult, op1=ALU.mult),
             reads=[N(f"h{t}"), N(f"rstd{t}"), N("fgain")], writes=[N(f"hn32_{u}")])

        def trf(e, u=u):
            for kc in range(8):
                ins = e.transpose(out=pT32[:, kc, :], in_=hn32[u][:, kc * 128:(kc + 1) * 128], identity=identf[:])
            return ins
        P.op("pe", trf, reads=[N(f"hn32_{u}"), N("identf")], writes=[N("pT32a"), N("pT32b")])
        P.op("act", lambda e, t=t: e.activation(out=hnT[:, :, t * 128:(t + 1) * 128], in_=pT32[:], func=AF.Copy),
             reads=[N("pT32a"), N("pT32b")], writes=[N(f"hnT{t}")])
        P.op("dve", lambda e, u=u: e.tensor_copy(out=hnT32[u][:], in_=pT32[:]),
             reads=[N("pT32a"), N("pT32b")], writes=[N(f"hnT32_{u}")])

        def mr(e, u=u):
            for kc in range(8):
                ins = e.matmul(out=pr[:, 0:20], lhsT=hnT32[u][:, kc, :], rhs=wr[:, kc, :], start=(kc == 0), stop=(kc == 7))
            return ins
        P.op("pe", mr, reads=[N(f"hnT32_{u}"), N("wr")], writes=[N("pr")])
        rt = rts[t % 4]
        R = N(f"rt{t % 4}")
        k = [0]

        def dv(fn, rd=(), eng="dve"):
            P.op(eng, fn, reads=[R] + list(rd), writes=[R])
        dv(lambda e, rt=rt: e.tensor_tensor(out=rt[:, 0:20], in0=pr[:, 0:20], in1=rbias[:], op=ALU.add), [N("pr"), N("rbias")])
        dv(lambda e, rt=rt: e.tensor_reduce(out=rt[:, 20:21], in_=rt[:, 0:4], axis=AX.X, op=ALU.max))
        dv(lambda e, rt=rt: e.tensor_scalar(out=rt[:, 24:28], in0=rt[:, 0:4], scalar1=rt[:, 20:21], scalar2=None, op0=ALU.is_equal))
        dv(lambda e, rt=rt: e.tensor_scalar(out=rt[:, 28:32], in0=rt[:, 0:4], scalar1=rt[:, 20:21], scalar2=None, op0=ALU.subtract))
        dv(lambda e, rt=rt: e.activation(out=rt[:, 28:32], in_=rt[:, 28:32], func=AF.Exp, accum_out=rt[:, 21:22]), eng="act")
        dv(lambda e, rt=rt: e.reciprocal(out=rt[:, 22:23], in_=rt[:, 21:22]))
        dv(lambda e, rt=rt: e.tensor_scalar(out=rt[:, 32:36], in0=rt[:, 4:8], scalar1=rt[:, 24:25], scalar2=None, op0=ALU.mult))
        for g in range(1, 4):
            dv(lambda e, g=g, rt=rt: e.scalar_tensor_tensor(out=rt[:, 32:36], in0=rt[:, 4 + 4 * g:8 + 4 * g],
                                                     scalar=rt[:, 24 + g:25 + g], in1=rt[:, 32:36],
                                                     op0=ALU.mult, op1=ALU.add))
        dv(lambda e, rt=rt: e.tensor_reduce(out=rt[:, 36:37], in_=rt[:, 32:36], axis=AX.X, op=ALU.max))
        dv(lambda e, rt=rt: e.tensor_scalar(out=rt[:, 40:44], in0=rt[:, 32:36], scalar1=rt[:, 36:37], scalar2=None, op0=ALU.is_equal))
        dv(lambda e, rt=rt: e.scalar_tensor_tensor(out=rt[:, 44:48], in0=rt[:, 40:44], scalar=-1e30, in1=rt[:, 32:36],
                                            op0=ALU.mult, op1=ALU.add))
        dv(lambda e, rt=rt: e.tensor_reduce(out=rt[:, 37:38], in_=rt[:, 44:48], axis=AX.X, op=ALU.max))
        dv(lambda e, rt=rt: e.tensor_scalar(out=rt[:, 48:52], in0=rt[:, 44:48], scalar1=rt[:, 37:38], scalar2=None, op0=ALU.is_equal))
        dv(lambda e, rt=rt: e.tensor_tensor(out=rt[:, 38:39], in0=rt[:, 37:38], in1=rt[:, 36:37], op=ALU.subtract))
        dv(lambda e, rt=rt: e.activation(out=rt[:, 39:40], in_=rt[:, 38:39], func=AF.Exp), eng="act")
        dv(lambda e, rt=rt: e.tensor_scalar(out=rt[:, 52:53], in0=rt[:, 39:40], scalar1=1.0, scalar2=None, op0=ALU.add))
        dv(lambda e, rt=rt: e.reciprocal(out=rt[:, 52:53], in_=rt[:, 52:53]))
        dv(lambda e, rt=rt: e.tensor_tensor(out=rt[:, 53:54], in0=rt[:, 39:40], in1=rt[:, 52:53], op=ALU.mult))
        dv(lambda e, rt=rt: e.tensor_scalar(out=rt[:, 56:60], in0=rt[:, 40:44], scalar1=rt[:, 52:53], scalar2=None, op0=ALU.mult))
        dv(lambda e, rt=rt: e.scalar_tensor_tensor(out=rt[:, 56:60], in0=rt[:, 48:52], scalar=rt[:, 53:54], in1=rt[:, 56:60],
                                            op0=ALU.mult, op1=ALU.add))
        dv(lambda e, rt=rt: e.tensor_scalar(out=rt[:, 60:64], in0=rt[:, 24:28], scalar1=rt[:, 22:23], scalar2=None, op0=ALU.mult))
        for g in range(4):
            P.op("dve", lambda e, g=g, t=t, rt=rt: e.tensor_scalar(out=comb[:, t, 4 * g:4 * g + 4], in0=rt[:, 56:60],
                                                            scalar1=rt[:, 60 + g:61 + g], scalar2=None, op0=ALU.mult),
                 reads=[R], writes=[N(f"comb{t}")])

    HNT = [N(f"hnT{t}") for t in range(NTILE)]
    pend = None
    it = 0
    wl_cnt = [0]
    passes = D.get("tg_passes", [list(range(NGRP))])
    for ex, tgs in [(ex, tgs) for tgs in (passes if stage >= 3 else []) for ex in range(NE)]:
        s = wl_cnt[0] % 2
        wl_cnt[0] += 1
        wg_s, wu_s, wd_s = wslot(s)
        wg_d = D["wg"][ex].rearrange("(kc p) f -> p kc f", p=128)
        wu_d = D["wu"][ex].rearrange("(kc p) f -> p kc f", p=128)
        wd_d = D["wd"][ex].rearrange("(fc p) n -> p fc n", p=128)
        for (dst, src) in ((wg_s, wg_d), (wu_s, wu_d), (wd_s, wd_d)):
            P.dma("pool", lambda e, dst=dst, src=src: e.dma_start(out=dst, in_=src),
                  writes=[SL[s]], lane="L:" + SL[s])
        for tg in tgs:
            b = it % 2
            for fc in range(4):
                pb = (it * 4 + fc) % 2

                def mg(e, fc=fc, tg=tg, pb=pb, wg_s=wg_s):
                    for kc in range(8):
                        ins = e.matmul(out=pg[pb], lhsT=wg_s[:, kc, fc * 128:(fc + 1) * 128],
                                       rhs=hnT[:, kc, tg * 512:(tg + 1) * 512], start=(kc == 0), stop=(kc == 7))
                    return ins

                def mu(e, fc=fc, tg=tg, pb=pb, wu_s=wu_s):
                    for kc in range(8):
                        ins = e.matmul(out=pu[pb], lhsT=wu_s[:, kc, fc * 128:(fc + 1) * 128],
                                       rhs=hnT[:, kc, tg * 512:(tg + 1) * 512], start=(kc == 0), stop=(kc == 7))
                    return ins
                hn_names = HNT[tg * 4:(tg + 1) * 4]
                P.op("pe", mg, reads=[SL[s]] + hn_names, writes=[pgn[pb]])
                P.op("pe", mu, reads=[SL[s]] + hn_names, writes=[pun[pb]])
                P.op("act", lambda e, pb=pb: e.activation(out=sgl[pb][:], in_=pg[pb], func=AF.Silu),
                     reads=[pgn[pb]], writes=[N(f"sgl{pb}")])
                P.op("dve", lambda e, pb=pb, b=b, fc=fc: e.tensor_tensor(out=hT[b][:, fc, :], in0=pu[pb], in1=sgl[pb][:],
                                                                       op=ALU.mult),
                     reads=[pun[pb], N(f"sgl{pb}")], writes=[N(f"hT{b}_{fc}")])

            def down(ex=ex, tg=tg, b=b, s=s, wd_s=wd_s):
                for tt in range(4):
                    t = tg * 4 + tt
                    for hh in range(2):
                        def md(e, tt=tt, hh=hh):
                            for fc in range(4):
                                ins = e.matmul(out=pd[:, hh, :], lhsT=hT[b][:, fc, tt * 128:(tt + 1) * 128],
                                               rhs=wd_s[:, fc, hh * 512:(hh + 1) * 512], start=(fc == 0), stop=(fc == 3))
                            return ins
                        P.op("pe", md, reads=[SL[s]] + [N(f"hT{b}_{fc}") for fc in range(4)], writes=[N(f"pd{hh}")])
                        P.op("dve", lambda e, t=t, hh=hh: e.scalar_tensor_tensor(
                            out=h[:, t, hh * 512:(hh + 1) * 512], in0=pd[:, hh, :], scalar=comb[:, t, ex:ex + 1],
                            in1=h[:, t, hh * 512:(hh + 1) * 512], op0=ALU.mult, op1=ALU.add),
                            reads=[N(f"pd{hh}"), N(f"comb{t}"), N(f"h{t}")], writes=[N(f"h{t}")])
            if pend is not None:
                pend()
            pend = down
            it += 1
    if pend is not None:
        pend()

    if not final:
        for t in range(NTILE):
            P.dma("sp", lambda e, t=t: e.dma_start(out=D["h_out"][t * 128:(t + 1) * 128, :], in_=h[:, t, :]),
                  reads=[N(f"h{t}")], writes=["d:" + N("h_out")], lane="S:" + N(f"h{t % 4}"))
        if "hn_out" in D:
            for t in range(NTILE):
                P.op("act", lambda e, t=t: e.activation(out=junk[:], in_=h[:, t, :], func=AF.Square,
                                                       accum_out=st[:, 0, t:t + 1]),
                     reads=[N(f"h{t}")], writes=[N(f"nss{t}")])
            rstd_ops(P, st[:, 0, :], st[:, 1, :], st[:, 2, :], 1024, [N(f"nss{t}") for t in range(NTILE)], N("nrstd"))
            for t in range(NTILE):
                s2 = t % 2
                P.op("act", lambda e, t=t, s2=s2: e.activation(out=yt[s2][:, 0:1024], in_=h[:, t, :], func=AF.Copy,
                                                              scale=st[:, 2, t:t + 1]),
                     reads=[N(f"h{t}"), N("nrstd")], writes=[N(f"yt{s2}")])
                P.dma("sp", lambda e, t=t, s2=s2: e.dma_start(out=D["hn_out"][t * 128:(t + 1) * 128, :],
                                                             in_=yt[s2][:, 0:1024]),
                      reads=[N(f"yt{s2}")], writes=["d:" + N(f"hn{t // 4}")], lane="S:" + N(f"yt{s2}"))
    else:
        for t in range(NTILE):
            P.op("act", lambda e, t=t: e.activation(out=junk[:], in_=h[:, t, :], func=AF.Square, accum_out=st[:, 0, t:t + 1]),
                 reads=[N(f"h{t}")], writes=[N(f"fss{t}")])
        for t in range(NTILE):
            rstd_ops(P, st[:, 0, t:t + 1], st[:, 1, t:t + 1], st[:, 2, t:t + 1], 1024, [N(f"fss{t}")], N(f"frstd{t}"))
        for t in range(NTILE):
            s = t % 2
            P.op("dve", lambda e, t=t, s=s: e.scalar_tensor_tensor(out=ob[s][:], in0=h[:, t, :], scalar=st[:, 2, t:t + 1],
                                                                  in1=ngain[:], op0=ALU.mult, op1=ALU.mult),
                 reads=[N(f"h{t}"), N(f"frstd{t}"), N("ngain")], writes=[N(f"ob{s}")])
            P.dma("sp", lambda e, t=t, s=s: e.dma_start(out=D["h_out"][t * 128:(t + 1) * 128, :], in_=ob[s][:]),
                  reads=[N(f"ob{s}")], writes=["d:" + N("h_out")], lane="S:" + N(f"ob{s}"))


def build_ffn(F, final, NTOK=2048, NE=16, stage=3):
    nc = bass.Bass("TRN2", target_bir_lowering=False)
    P = Prog(nc)
    D = {}

    def inp(name, shape, dt=F32):
        D[name] = nc.dram_tensor(name, list(shape), dt, kind="ExternalInput").ap()
    inp("xs", [NTOK, 1024]); inp("ys", [NTOK, F], BF16); inp("wo", [F, 1024]); inp("fgain", [128, 1024])
    inp("wr", [1024, 20]); inp("rbias", [128, 20]); inp("wg", [NE, 1024, 512]); inp("wu", [NE, 1024, 512])
    inp("wd", [NE, 512, 1024]); inp("ident", [128, 128], BF16); inp("identf", [128, 128])
    if final:
        inp("ngain", [128, 1024])
    D["h_out"] = nc.dram_tensor("h_out", [NTOK, 1024], F32, kind="ExternalOutput").ap()
    phase_ffn(P, nc, D, F, final, "B_", NTOK, NE, stage)
    finish(P, ["d:h_out"])
    P.build()
    return nc, P


def ffn_maps(xs_list, ys_list, wo, fgain, rgw, rgb, rew, reb, wg, wu, wd, ngain=None):
    ident = np.eye(128, dtype=np.float32).astype(ml_dtypes.bfloat16)
    identf = np.eye(128, dtype=np.float32)
    wr = np.ascontiguousarray(np.concatenate([rgw] + [rew[g] for g in range(4)], axis=1))
    rb = np.concatenate([rgb, reb.reshape(-1)])[None, :]
    common = dict(wo=np.ascontiguousarray(wo), fgain=np.ascontiguousarray(np.broadcast_to(fgain[None, :], (128, 1024))),
                  wr=wr, rbias=np.ascontiguousarray(np.broadcast_to(rb, (128, 20))),
                  wg=np.ascontiguousarray(wg.reshape(16, 1024, 512)), wu=np.ascontiguousarray(wu.reshape(16, 1024, 512)),
                  wd=np.ascontiguousarray(wd.reshape(16, 512, 1024)), ident=ident, identf=identf)
    if ngain is not None:
        common["ngain"] = np.ascontiguousarray(np.broadcast_to(ngain[None, :], (128, 1024)))
    return [dict(common, xs=np.ascontiguousarray(xs_list[c]), ys=np.ascontiguousarray(ys_list[c])) for c in range(8)]


def phase_moba(P, nc, D, NT=8192):
    C = Ctx(nc, P, "C_")
    N = lambda s: "C_" + s
    NG = NT // 512
    NQT = NT // 128
    NB = NT // 256
    ident = C.sb("ident", [128, 128], BF16)
    cmask = C.sb("cmask", [128, 2, 256], BF16)
    gq = C.sb("gq", [128, 8], F32)
    gkv = C.sb("gkv", [128, 8], F32)
    wst = C.sb("wst", [128, 8, 256], F32)
    wq_bf = C.sb("wq_bf", [128, 8, 256], BF16)
    wk_bf = C.sb("wk_bf", [128, 8, 256], BF16)
    wv_bf = C.sb("wv_bf", [128, 8, 256], BF16)
    wq_rot = C.sb("wq_rot", [128, 8, 2, 32], BF16)
    wk_rot = C.sb("wk_rot", [128, 8, 2, 32], BF16)
    QT = [C.sb(f"QT{i}", [128, NT], BF16) for i in range(2)]
    KT = [C.sb(f"KT{i}", [128, NT], BF16) for i in range(2)]
    Vext = C.sb("Vext", [128, 2, NQT, 130], BF16)
    Msel = C.sb("Msel", [128, 2, NQT, 32], F32)
    km32 = C.sb("km32", [128, 2, 32], F32)
    kmT = C.sb("kmT", [128, 2, 32], BF16)
    ht = [C.sb(f"ht{i}", [128, 1024], F32) for i in range(2)]
    junk = C.sb("junk", [128, 1024], BF16)
    hn = [C.sb(f"hn{i}", [128, 1024], BF16) for i in range(2)]
    hnT = [C.sb(f"hnT{i}", [128, 8, 512], BF16) for i in range(2)]
    cosg = [C.sb(f"cos{i}", [32, 512], F32) for i in range(2)]
    sing = [C.sb(f"sin{i}", [32, 512], F32) for i in range(2)]
    ta = C.sb("ta", [32, 512], F32)
    tb = C.sb("tb", [32, 512], F32)
    st = C.sb("st", [128, 3, 4], F32)
    gt = C.sb("gt", [128, 32], F32)
    mx = C.sb("mx", [128, 8], F32)
    PT = [C.sb(f"PT{i}", [128, 2, 256], BF16) for i in range(3)]
    acc = [C.sb(f"acc{i}", [128, 130], F32) for i in range(2)]
    rec = C.sb("rec", [128, 2], F32)
    ob = [C.sb(f"ob{i}", [128, 128], BF16) for i in range(4)]

    psT = C.ps("psT", [128, 8, 128], BF16)
    pm = C.ps("pm", [128, 512])
    prot = C.ps("prot", [128, 512])
    pv = C.ps("pv", [128, 512])
    pS = [C.ps(f"pS{i}", [128, 2, 256]) for i in range(2)]
    pO = [C.ps(f"pO{i}", [128, 512]) for i in range(2)]
    P.psum |= {N(x) for x in ("psT", "pm", "prot", "pv", "pS0", "pS1", "pO0", "pO1")}

    def ld(dst, src, name, eng="sp", lane=None):
        P.dma(eng, lambda e: e.dma_start(out=dst, in_=src), writes=[name], lane=lane or ("L:" + name))

    ld(ident[:], D["ident"], N("ident"), lane="G:const")
    ld(cmask[:], D["cmask"], N("cmask"), lane="G:const")
    ld(gq[:], D["gq"], N("gq"), lane="G:const")
    ld(gkv[:], D["gkv"], N("gkv"), lane="G:const")
    qscale = float(128 ** -0.5)
    for (wname, wdst, g, sc) in (("wq", wq_bf, gq, qscale), ("wk", wk_bf, gkv, 1.0), ("wv", wv_bf, gkv, 1.0)):
        ld(wst[:], D[wname].rearrange("(kc p) f -> p kc f", p=128), N("wst"))
        for kc in range(8):
            P.op("dve", lambda e, kc=kc, wdst=wdst, g=g, sc=sc: e.tensor_scalar(
                out=wdst[:, kc, :], in0=wst[:, kc, :], scalar1=g[:, kc:kc + 1], scalar2=sc, op0=ALU.mult, op1=ALU.mult),
                reads=[N("wst"), N("gq"), N("gkv")], writes=[N(wname + "_bf")])
    for (wsrc, wrot, nm) in ((wq_bf, wq_rot, "wq"), (wk_bf, wk_rot, "wk")):
        for hh in range(2):
            P.op("dve", lambda e, wsrc=wsrc, wrot=wrot, hh=hh: e.tensor_scalar(
                out=wrot[:, :, hh, 0:16], in0=wsrc[:, :, hh * 128 + 16:hh * 128 + 32], scalar1=-1.0, scalar2=None,
                op0=ALU.mult), reads=[N(nm + "_bf")], writes=[N(nm + "_rot")])
            P.op("dve", lambda e, wsrc=wsrc, wrot=wrot, hh=hh: e.tensor_copy(
                out=wrot[:, :, hh, 16:32], in_=wsrc[:, :, hh * 128:hh * 128 + 16]),
                reads=[N(nm + "_bf")], writes=[N(nm + "_rot")])
    P.op("pool", lambda e: e.memset(Vext[:], 1.0), writes=[N("Vext_init")])
    P.op("pool", lambda e: e.memset(Msel[:], 0.0), writes=[N("Msel_init")])
    P.op("pool", lambda e: e.memset(kmT[:], 0.0), writes=[N("kmT_init")])

    hb = D.get("hb")
    for tg in range(NG):
        s = tg % 2
        t0 = tg * 512
        ld(cosg[s][:], D["cos32"][:, t0:t0 + 512], N(f"cos{s}"))
        ld(sing[s][:], D["sin32"][:, t0:t0 + 512], N(f"sin{s}"))
        for tt in range(4):
            xs = tt % 2
            r0 = t0 + tt * 128
            if "hn_src" in D:
                P.dma("sp", lambda e, xs=xs, r0=r0: e.dma_start(out=hn[xs][:], in_=D["hn_src"](r0)),
                      reads=D["hn_reads"](r0), writes=[N(f"hn{xs}")], lane="L:" + N(f"hn{xs}"))
            else:
                ld(ht[xs][:], hb[r0:r0 + 128, :], N(f"ht{xs}"))
                P.op("act", lambda e, xs=xs, tt=tt: e.activation(out=junk[:], in_=ht[xs][:], func=AF.Square,
                                                                accum_out=st[:, 0, tt:tt + 1]),
                     reads=[N(f"ht{xs}")], writes=[N("ss")])
                rstd_ops(P, st[:, 0, tt:tt + 1], st[:, 1, tt:tt + 1], st[:, 2, tt:tt + 1], 1024, [N("ss")], N("rstd"))
                P.op("act", lambda e, xs=xs, tt=tt: e.activation(out=hn[xs][:], in_=ht[xs][:], func=AF.Copy,
                                                                scale=st[:, 2, tt:tt + 1]),
                     reads=[N(f"ht{xs}"), N("rstd")], writes=[N(f"hn{xs}")])

            def tr(e, xs=xs):
                for kc in range(8):
                    ins = e.transpose(out=psT[:, kc, :], in_=hn[xs][:, kc * 128:(kc + 1) * 128], identity=ident[:])
                return ins
            P.op("pe", tr, reads=[N(f"hn{xs}"), N("ident")], writes=[N("psT")])
            P.op("dve", lambda e, s=s, tt=tt: e.tensor_copy(out=hnT[s][:, :, tt * 128:(tt + 1) * 128], in_=psT[:]),
                 reads=[N("psT")], writes=[N(f"hnT{s}_{tt}")])
        XN = [N(f"hnT{s}_{tt}") for tt in range(4)]
        for hh in range(2):
            for (w_bf, w_rot, dst, nm) in ((wq_bf, wq_rot, QT, "Q"), (wk_bf, wk_rot, KT, "K")):
                wn = "wq" if nm == "Q" else "wk"

                def mm(e, w_bf=w_bf, hh=hh, s=s):
                    for kc in range(8):
                        ins = e.matmul(out=pm[:], lhsT=w_bf[:, kc, hh * 128:(hh + 1) * 128], rhs=hnT[s][:, kc, :],
                                       start=(kc == 0), stop=(kc == 7))
                    return ins

                def mr(e, w_rot=w_rot, hh=hh, s=s):
                    for kc in range(8):
                        ins = e.matmul(out=prot[0:32, :], lhsT=w_rot[:, kc, hh, :], rhs=hnT[s][:, kc, :],
                                       start=(kc == 0), stop=(kc == 7))
                    return ins
                P.op("pe", mm, reads=[N(wn + "_bf")] + XN, writes=[N("pm")])
                P.op("pe", mr, reads=[N(wn + "_rot")] + XN, writes=[N("prot")])
                dname = N(f"{nm}T{hh}_{tg}")
                P.op("act", lambda e, dst=dst, hh=hh, t0=t0: e.activation(out=dst[hh][32:64, t0:t0 + 512],
                                                                         in_=pm[32:64, :], func=AF.Copy),
                     reads=[N("pm")], writes=[dname + "mid"])
                P.op("act", lambda e, dst=dst, hh=hh, t0=t0: e.activation(out=dst[hh][64:128, t0:t0 + 512],
                                                                         in_=pm[64:128, :], func=AF.Copy),
                     reads=[N("pm")], writes=[dname + "hi"])
                P.op("dve", lambda e, s=s: e.tensor_tensor(out=ta[:], in0=pm[0:32, :], in1=cosg[s][:], op=ALU.mult),
                     reads=[N("pm"), N(f"cos{s}")], writes=[N("ta")])
                P.op("dve", lambda e, s=s: e.tensor_tensor(out=tb[:], in0=prot[0:32, :], in1=sing[s][:], op=ALU.mult),
                     reads=[N("prot"), N(f"sin{s}")], writes=[N("tb")])
                P.op("pool", lambda e, dst=dst, hh=hh, t0=t0: e.tensor_tensor(out=dst[hh][0:32, t0:t0 + 512], in0=ta[:],
                                                                             in1=tb[:], op=ALU.add),
                     reads=[N("ta"), N("tb")], writes=[dname + "lo"])
            P.op("dve", lambda e, hh=hh, t0=t0, tg=tg: e.tensor_reduce(
                out=km32[:, hh, 2 * tg:2 * tg + 2], in_=KT[hh][:, t0:t0 + 512].rearrange("p (a b) -> p a b", a=2),
                axis=AX.X, op=ALU.add),
                reads=[N(f"KT{hh}_{tg}hi"), N(f"KT{hh}_{tg}lo")], writes=[N(f"km32_{hh}_{tg}")])
            P.op("act", lambda e, hh=hh, tg=tg: e.activation(out=kmT[:, hh, 2 * tg:2 * tg + 2],
                                                            in_=km32[:, hh, 2 * tg:2 * tg + 2], func=AF.Copy,
                                                            scale=1.0 / 256.0),
                 reads=[N(f"km32_{hh}_{tg}"), N("kmT_init")], writes=[N(f"kmT{hh}_{tg}")])
        for tt in range(4):
            tile = tg * 4 + tt

            def mv(e, tt=tt, s=s):
                for kc in range(8):
                    ins = e.matmul(out=pv[:, 0:256], lhsT=hnT[s][:, kc, tt * 128:(tt + 1) * 128], rhs=wv_bf[:, kc, :],
                                   start=(kc == 0), stop=(kc == 7))
                return ins
            P.op("pe", mv, reads=[N("wv_bf"), XN[tt]], writes=[N("pv")])
            P.op("act", lambda e, tile=tile: e.activation(out=Vext[:, :, tile, 0:128],
                                                         in_=pv[:, 0:256].rearrange("p (a b) -> p a b", a=2), func=AF.Copy),
                 reads=[N("pv"), N("Vext_init")], writes=[N(f"V{tile}")])
    gts = [gt, C.sb("gt1", [128, 32], F32)]
    mxs = [mx, C.sb("mx1", [128, 8], F32)]
    for qt in range(2, NQT):
        for hh in range(2):
            j = qt // 2
            g_, m_, pg_, pgn_ = gts[hh], mxs[hh], pO[hh], N(f"pO{hh}")
            kn = [N(f"kmT{hh}_{tg}") for tg in range((j - 1) // 2 + 1)]
            P.op("pe", lambda e, hh=hh, qt=qt, pg_=pg_: e.matmul(out=pg_[:, 0:32], lhsT=QT[hh][:, qt * 128:(qt + 1) * 128],
                                                                 rhs=kmT[:, hh, :], start=True, stop=True),
                 reads=[N(f"QT{hh}_{qt // 4}hi"), N(f"QT{hh}_{qt // 4}lo"), N("kmT_init")] + kn, writes=[pgn_])
            P.op("dve", lambda e, g_=g_, pg_=pg_: e.tensor_copy(out=g_[:], in_=pg_[:, 0:32]), reads=[pgn_], writes=[N(f"gt{hh}")])
            P.op("dve", lambda e, j=j, g_=g_: e.memset(g_[:, j:32], -1e30), reads=[N(f"gt{hh}")], writes=[N(f"gt{hh}")])
            P.op("dve", lambda e, g_=g_, m_=m_: e.max(out=m_[:], in_=g_[:]), reads=[N(f"gt{hh}")], writes=[N(f"mx{hh}")])
            P.op("dve", lambda e, hh=hh, qt=qt, g_=g_, m_=m_: e.tensor_scalar(out=Msel[:, hh, qt, :], in0=g_[:],
                                                                             scalar1=m_[:, 2:3], scalar2=None, op0=ALU.is_ge),
                 reads=[N(f"gt{hh}"), N(f"mx{hh}"), N("Msel_init")], writes=[N(f"M{hh}_{qt}")])

    pS3 = [pS[0][:], pS[1][:],
           psT[:].bitcast(F32).rearrange("p a b -> p (a b)").rearrange("p (k q) -> p k q", k=2)]
    pSn = [N("pS0"), N("pS1"), N("psT")]
    ND = 3
    pOb = [[pO[0], pm], [pO[1], prot]]
    pOn = [[N("pO0"), N("pm")], [N("pO1"), N("prot")]]
    pairs = []
    for j in range(NB):
        for hh in range(2):
            order = [j] + list(range(j))
            for idx, n in enumerate(order):
                pairs.append((j, hh, n, idx == len(order) - 1))
    oc = [0]

    def emit_s(i):
        j, hh, n, last = pairs[i]
        b = i % ND
        qs = slice(j * 256, (j + 1) * 256)
        qn = [N(f"QT{hh}_{j // 2}hi"), N(f"QT{hh}_{j // 2}lo")]

        def mS(e):
            for kt in range(2):
                k0 = (2 * n + kt) * 128
                ins = e.matmul(out=pS3[b][:, kt, :], lhsT=KT[hh][:, k0:k0 + 128], rhs=QT[hh][:, qs],
                               start=True, stop=True)
            return ins
        P.op("pe", mS, reads=qn + [N(f"KT{hh}_{n // 2}hi"), N(f"KT{hh}_{n // 2}lo")], writes=[pSn[b]])
        P.op("act", lambda e: e.activation(out=PT[b][:], in_=pS3[b], func=AF.Exp),
             reads=[pSn[b]], writes=[N(f"PT{b}")])
        if n == j:
            P.op("pool", lambda e: e.tensor_tensor(out=PT[b][:], in0=PT[b][:], in1=cmask[:], op=ALU.mult),
                 reads=[N(f"PT{b}"), N("cmask")], writes=[N(f"PT{b}")])

    def emit_o(i):
        j, hh, n, last = pairs[i]
        b = i % ND
        own = (n == j)
        for qi in range(2):
            po_, pn_ = pOb[qi][i % 2], pOn[qi][i % 2]

            def mO(e, qi=qi, po_=po_):
                for kt in range(2):
                    ins = e.matmul(out=po_[:, 0:129], lhsT=PT[b][:, kt, qi * 128:(qi + 1) * 128],
                                   rhs=Vext[:, hh, 2 * n + kt, 0:129], start=(kt == 0), stop=(kt == 1))
                return ins
            P.op("pe", mO, reads=[N(f"PT{b}"), N(f"V{2 * n}"), N(f"V{2 * n + 1}")], writes=[pn_])
            if own:
                P.op("dve", lambda e, qi=qi, po_=po_: e.tensor_copy(out=acc[qi][:, 0:129], in_=po_[:, 0:129]),
                     reads=[pn_], writes=[N(f"acc{qi}")])
            else:
                P.op("dve", lambda e, qi=qi, po_=po_: e.scalar_tensor_tensor(
                    out=acc[qi][:, 0:129], in0=po_[:, 0:129], scalar=Msel[:, hh, 2 * j + qi, n:n + 1],
                    in1=acc[qi][:, 0:129], op0=ALU.mult, op1=ALU.add),
                    reads=[pn_, N(f"M{hh}_{2 * j + qi}"), N(f"acc{qi}")], writes=[N(f"acc{qi}")])
        if last:
            for qi in range(2):
                o_ = oc[0] % 4
                oc[0] += 1
                qt = 2 * j + qi
                P.op("dve", lambda e, qi=qi: e.reciprocal(out=rec[:, qi:qi + 1], in_=acc[qi][:, 128:129]),
                     reads=[N(f"acc{qi}")], writes=[N(f"rec{qi}")])
                P.op("dve", lambda e, qi=qi, o_=o_: e.tensor_scalar(out=ob[o_][:], in0=acc[qi][:, 0:128],
                                                                    scalar1=rec[:, qi:qi + 1], scalar2=None, op0=ALU.mult),
                     reads=[N(f"acc{qi}"), N(f"rec{qi}")], writes=[N(f"ob{o_}")])
                P.dma("sp", lambda e, qt=qt, o_=o_: e.dma_start(
                    out=D["o_out"][qt * 128:(qt + 1) * 128, hh * 128:(hh + 1) * 128], in_=ob[o_][:]),
                    reads=[N(f"ob{o_}")], writes=[f"d:C_o{j // 4}"], lane="S:" + N(f"ob{o_}"))
            if hh == 1 and "after_block" in D:
                D["after_block"](j)

    for i in range(ND - 1):
        emit_s(i)
    for i in range(len(pairs)):
        if i + ND - 1 < len(pairs):
            emit_s(i + ND - 1)
        emit_o(i)


def moba_tables(S=8192):
    inv = (1.0 / (np.float32(500000.0) ** (np.arange(16, dtype=np.float32) / np.float32(16)))).astype(np.float32)
    ang = (np.arange(S, dtype=np.float32)[None, :] * inv[:, None]).astype(np.float32)
    cos = np.cos(ang).astype(np.float32)
    sin = np.sin(ang).astype(np.float32)
    k = np.arange(128)[:, None, None] + 128 * np.arange(2)[None, :, None]
    q = np.arange(256)[None, None, :]
    cmask = (k <= q).astype(np.float32).astype(ml_dtypes.bfloat16)
    return (np.ascontiguousarray(np.concatenate([cos, cos], 0)), np.ascontiguousarray(np.concatenate([sin, sin], 0)),
            np.ascontiguousarray(cmask))


def build_moba(NT=8192):
    nc = bass.Bass("TRN2", target_bir_lowering=False)
    P = Prog(nc)
    D = {}

    def inp(name, shape, dt=F32):
        D[name] = nc.dram_tensor(name, list(shape), dt, kind="ExternalInput").ap()
    inp("hb", [NT, 1024]); inp("wq", [1024, 256]); inp("wk", [1024, 256]); inp("wv", [1024, 256])
    inp("gq", [128, 8]); inp("gkv", [128, 8]); inp("cos32", [32, NT]); inp("sin32", [32, NT])
    inp("cmask", [128, 2, 256], BF16); inp("ident", [128, 128], BF16)
    D["o_out"] = nc.dram_tensor("o_out", [NT, 256], BF16, kind="ExternalOutput").ap()
    phase_moba(P, nc, D, NT)
    finish(P)
    P.build()
    return nc, P


def moba_maps(h_list, attn_norm, kv_norm, w_q, w_kv, NT=8192):
    cos32, sin32, cmask = moba_tables()
    ident = np.eye(128, dtype=np.float32).astype(ml_dtypes.bfloat16)
    gq = np.ascontiguousarray(attn_norm.reshape(8, 128).T)
    gkv = np.ascontiguousarray(kv_norm.reshape(8, 128).T)
    maps = []
    for c in range(8):
        b, p = c // 4, c % 4
        cs = slice(p * 256, (p + 1) * 256)
        maps.append(dict(hb=np.ascontiguousarray(h_list[b][:NT]), wq=np.ascontiguousarray(w_q[:, cs]),
                         wk=np.ascontiguousarray(w_kv[:, cs]), wv=np.ascontiguousarray(w_kv[:, 1024 + p * 256:1024 + (p + 1) * 256]),
                         gq=gq, gkv=gkv, cos32=np.ascontiguousarray(cos32[:, :NT]), sin32=np.ascontiguousarray(sin32[:, :NT]),
                         cmask=cmask, ident=ident))
    return maps


def finish(P, names=None):
    P.finish()


def ret_tables(S=8192):
    half = 128
    inv = (1.0 / (np.float32(10000.0) ** (np.arange(half, dtype=np.float32) / np.float32(half)))).astype(np.float32)
    ang = (np.arange(S, dtype=np.float32)[None, :] * inv[:, None]).astype(np.float32)
    return np.cos(ang).astype(np.float32), np.sin(ang).astype(np.float32)


def ret_decay(h):
    Cn = 128
    lg = np.log1p(-np.float32(2.0) ** np.float32(-5.0 - h)).astype(np.float32)
    idx = np.arange(Cn, dtype=np.float32)
    diff = idx[:, None] - idx[None, :]
    dm = np.where(diff >= 0, np.exp(lg * np.maximum(diff, 0.0)), 0.0).astype(np.float32)
    dmaskT = (dm.T * np.float32(256 ** -0.5)).astype(np.float32)
    qdec = np.exp(lg * (idx + 1.0)).astype(np.float32)
    kdec = (np.exp(lg * (Cn - 1.0 - idx)) * np.float32(256 ** -0.5)).astype(np.float32)
    cdec = np.exp(lg * np.float32(Cn)).astype(np.float32)
    qdec4 = np.ascontiguousarray(np.broadcast_to(np.tile(qdec, 4)[None, :], (128, 512))).astype(np.float32)
    return dict(dmask=np.ascontiguousarray(dmaskT), qdec4=qdec4,
                kdec=np.ascontiguousarray(kdec[:, None]),
                cdec=np.full((128, 1), cdec, np.float32))


def build_ret(NT=8192):
    nc = bass.Bass("TRN2", target_bir_lowering=False)
    P = Prog(nc)
    D = {}

    def inp(name, shape, dt=F32):
        D[name] = nc.dram_tensor(name, list(shape), dt, kind="ExternalInput").ap()
    inp("xb", [NT, 1024]); inp("w", [1024, 1536]); inp("gain", [128, 8])
    inp("cosT", [128, NT]); inp("sinT", [128, NT]); inp("dmask", [128, 128]); inp("qdec4", [128, 512])
    inp("kdec", [128, 1]); inp("cdec", [128, 1]); inp("ident", [128, 128], BF16)
    D["y_out"] = nc.dram_tensor("y_out", [NT, 512], BF16, kind="ExternalOutput").ap()
    phase_ret(P, nc, D, NT)
    finish(P, ["d:y_out"])
    P.build()
    return nc, P


def run_ret(x, ret_norm, ret_w_in):
    nc, P = build_ret()
    cosT, sinT = ret_tables()
    ident = np.eye(128, dtype=np.float32).astype(ml_dtypes.bfloat16)
    gain = np.ascontiguousarray(ret_norm[0].reshape(8, 128).T)
    maps = []
    for c in range(8):
        b, h = c // 4, c % 4
        w = ret_w_in[0]
        wh = np.concatenate([w[:, h * 256:(h + 1) * 256], w[:, 1024 + h * 256:1024 + (h + 1) * 256],
                             w[:, 2048 + h * 512:2048 + (h + 1) * 512],
                             w[:, 4096 + h * 512:4096 + (h + 1) * 512]], axis=1)
        m = dict(xb=np.ascontiguousarray(x[b]), w=np.ascontiguousarray(wh), gain=gain, cosT=cosT, sinT=sinT,
                 ident=ident)
        m.update(ret_decay(h))
        maps.append(m)
    res = run_bass_kernel_spmd(nc, maps, core_ids=list(range(8)))
    return [r["y_out"] for r in res.results]


GROUPS = [[0, 1, 2, 3], [4, 5, 6, 7]]


def build_fused(upto=4):
    nc = bass.Bass("TRN2", target_bir_lowering=False)
    P = Prog(nc)
    P.want_pid = True

    def inp(name, shape, dt=F32):
        return nc.dram_tensor("in_" + name, list(shape), dt, kind="ExternalInput").ap()

    def scr(name, shape, dt):
        return nc.dram_tensor(name, list(shape), dt).ap()

    def state():
        return (nc.sbuf_base, nc.sbuf_top, nc.psum_base, nc.psum_top)

    def restore(st):
        nc.sbuf_base, nc.sbuf_top, nc.psum_base, nc.psum_top = st

    def allgather(src, dst, reads, wname, lane):
        P.cc(lambda e: e.collective_compute("AllGather", ALU.bypass, replica_groups=GROUPS, ins=[src], outs=[dst]),
             reads=reads, writes=[wname], lane=lane)

    ident = inp("ident", [128, 128], BF16)
    identf = inp("identf", [128, 128])
    y_dram = scr("y_dram", [8192, 512], BF16)
    yall = scr("yall", [8 * 4 * 1024, 512], BF16)
    if upto == 2:
        h1_dram = nc.dram_tensor("h1_dram", [2048, 1024], F32, kind="ExternalOutput").ap()
    else:
        h1_dram = scr("h1_dram", [2048, 1024], F32)
    hn_dram = scr("hn_dram", [2048, 1024], BF16)
    hnall = scr("hnall", [4 * 4 * 512, 1024], BF16)
    o_dram = scr("o_dram", [8192, 256], BF16)
    oall = scr("oall", [8 * 4 * 1024, 256], BF16)
    out = nc.dram_tensor("out", [2048, 1024], F32, kind="ExternalOutput").ap()
    st0 = state()

    def ag_y(i):
        allgather(y_dram[i * 1024:(i + 1) * 1024, :], yall[i * 4096:(i + 1) * 4096, :], [f"d:A_y{i}"],
                  f"d:yall{i}", f"CC:y{i}")

    def after_group(tg):
        if tg >= 2 and tg % 2 == 0:
            ag_y(tg // 2 - 1)
    DA = dict(xb=inp("xb", [8192, 1024]), w=inp("A_w", [1024, 1536]), gain=inp("A_gain", [128, 8]),
              cosT=inp("A_cosT", [128, 8192]), sinT=inp("A_sinT", [128, 8192]), dmask=inp("A_dmask", [128, 128]),
              qdec4=inp("A_qdec4", [128, 512]), kdec=inp("A_kdec", [128, 1]), cdec=inp("A_cdec", [128, 1]),
              ident=ident, y_out=y_dram, after_group=after_group)
    phase_ret(P, nc, DA)
    ag_y(7)
    restore(st0)
    P.barrier()
    if upto == 1:
        finish(P)
        P.build()
        return nc, P

    def ys_B(e, t, dst):
        j, s0 = t // 8, (t % 8) * 128
        src = yall[j * 4096:, :][bass.ds(P.pid["sp"], 4096), :].rearrange("(r s) f -> s r f", r=4)[s0:s0 + 128, :, :]
        return e.dma_start(out=dst[:, 0:2048].rearrange("p (h f) -> p h f", h=4), in_=src)

    def ys_D(e, t, dst):
        j, s0 = t // 8, (t % 8) * 128
        src = oall[j * 4096:, :][bass.ds(P.pid["pool"], 4096), :].rearrange("(r s) f -> s r f", r=4)[s0:s0 + 128, :, :]
        return e.dma_start(out=dst[:, 0:1024].rearrange("p (h f) -> p h f", h=4), in_=src)

    def ffn_inputs(pfx, F, final):
        d = dict(wo=inp(pfx + "wo", [F, 1024]), fgain=inp(pfx + "fgain", [128, 1024]), wr=inp(pfx + "wr", [1024, 20]),
                 rbias=inp(pfx + "rbias", [128, 20]), wg=inp(pfx + "wg", [16, 1024, 512]),
                 wu=inp(pfx + "wu", [16, 1024, 512]), wd=inp(pfx + "wd", [16, 512, 1024]), ident=ident, identf=identf)
        if final:
            d["ngain"] = inp(pfx + "ngain", [128, 1024])
        return d

    DB = ffn_inputs("B_", 2048, False)
    DB.update(xs=inp("xs", [2048, 1024]), ys_fn=ys_B, ys_reads=lambda t: [f"d:yall{2 * q + t // 8}" for q in range(4)],
              h_out=h1_dram, hn_out=hn_dram)
    phase_ffn(P, nc, DB, 2048, False, "B_")
    for i in range(4):
        allgather(hn_dram[i * 512:(i + 1) * 512, :], hnall[i * 2048:(i + 1) * 2048, :], [f"d:B_hn{i}"],
                  f"d:hnall{i}", f"CC:hn{i}")
    restore(st0)
    P.barrier()
    if upto == 2:
        finish(P)
        P.build()
        return nc, P

    def hn_src(r0):
        r, i, s_ = r0 // 2048, (r0 % 2048) // 512, r0 % 512
        row = (i * 4 + r) * 512 + s_
        return hnall[row:row + 128, :]

    def after_block(j):
        if j % 4 == 3:
            i = j // 4
            allgather(o_dram[i * 1024:(i + 1) * 1024, :], oall[i * 4096:(i + 1) * 4096, :], [f"d:C_o{i}"],
                      f"d:oall{i}", f"CC:o{i}")
    DC = dict(wq=inp("C_wq", [1024, 256]), wk=inp("C_wk", [1024, 256]), wv=inp("C_wv", [1024, 256]),
              gq=inp("C_gq", [128, 8]), gkv=inp("C_gkv", [128, 8]), cos32=inp("C_cos32", [32, 8192]),
              sin32=inp("C_sin32", [32, 8192]), cmask=inp("C_cmask", [128, 2, 256], BF16), ident=ident,
              hn_src=hn_src, hn_reads=lambda r0: [f"d:hnall{(r0 % 2048) // 512}"], o_out=o_dram,
              after_block=after_block)
    phase_moba(P, nc, DC)
    restore(st0)
    P.barrier()
    if upto == 3:
        finish(P)
        P.build()
        return nc, P

    DD = ffn_inputs("D_", 1024, True)
    DD.update(xs=h1_dram, xs_reads=["d:B_h_out"], ys_fn=ys_D, ys_eng="pool", ys_reads=lambda t: [f"d:oall{2 * q + t // 8}" for q in range(4)],
              h_out=out)
    phase_ffn(P, nc, DD, 1024, True, "D_")
    finish(P)
    P.build()
    return nc, P


def fused_maps(x, ret_norm, ret_w_in, ret_w_out, kv_norm, w_kv, attn_norm, w_q, w_o, ffn_norm,
               router_group_w, router_group_b, router_expert_w, router_expert_b,
               expert_w_gate, expert_w_up, expert_w_down, final_norm):
    cosT, sinT = ret_tables()
    cos32, sin32, cmask = moba_tables()
    ident = np.eye(128, dtype=np.float32).astype(ml_dtypes.bfloat16)
    identf = np.eye(128, dtype=np.float32)
    bc = lambda v, n: np.ascontiguousarray(np.broadcast_to(v[None, :], (128, n)))
    common = dict(ident=ident, identf=identf, A_gain=np.ascontiguousarray(ret_norm[0].reshape(8, 128).T),
                  A_cosT=cosT, A_sinT=sinT, C_cos32=cos32, C_sin32=sin32, C_cmask=cmask,
                  C_gq=np.ascontiguousarray(attn_norm[0].reshape(8, 128).T),
                  C_gkv=np.ascontiguousarray(kv_norm.reshape(8, 128).T))
    for pfx, l, wo in (("B_", 0, ret_w_out[0]), ("D_", 1, w_o[0])):
        rb = np.concatenate([router_group_b[l], router_expert_b[l].reshape(-1)])
        common.update({
            pfx + "wo": np.ascontiguousarray(wo), pfx + "fgain": bc(ffn_norm[l], 1024),
            pfx + "wr": np.ascontiguousarray(np.concatenate([router_group_w[l]] + [router_expert_w[l][g] for g in range(4)], axis=1)),
            pfx + "rbias": bc(rb, 20),
            pfx + "wg": np.ascontiguousarray(expert_w_gate[l].reshape(16, 1024, 512)),
            pfx + "wu": np.ascontiguousarray(expert_w_up[l].reshape(16, 1024, 512)),
            pfx + "wd": np.ascontiguousarray(expert_w_down[l].reshape(16, 512, 1024))})
    common["D_ngain"] = bc(final_norm, 1024)
    dec = [ret_decay(h) for h in range(4)]
    maps = []
    w = ret_w_in[0]
    for c in range(8):
        b, r = c // 4, c % 4
        wh = np.concatenate([w[:, r * 256:(r + 1) * 256], w[:, 1024 + r * 256:1024 + (r + 1) * 256],
                             w[:, 2048 + r * 512:2048 + (r + 1) * 512], w[:, 4096 + r * 512:4096 + (r + 1) * 512]], axis=1)
        cs = slice(r * 256, (r + 1) * 256)
        m = dict(common, xb=np.ascontiguousarray(x[b]), xs=np.ascontiguousarray(x[b, r * 2048:(r + 1) * 2048]),
                 A_w=np.ascontiguousarray(wh), A_dmask=dec[r]["dmask"], A_qdec4=dec[r]["qdec4"], A_kdec=dec[r]["kdec"],
                 A_cdec=dec[r]["cdec"], C_wq=np.ascontiguousarray(w_q[0][:, cs]), C_wk=np.ascontiguousarray(w_kv[:, cs]),
                 C_wv=np.ascontiguousarray(w_kv[:, 1024 + r * 256:1024 + (r + 1) * 256]))
        maps.append({"in_" + k: v for k, v in m.items()})
    return maps


_CACHE = {}


def kernel(**inputs):
    a = {k: np.asarray(v, dtype=np.float32) for k, v in inputs.items()}
    if "nc" not in _CACHE:
        _CACHE["nc"] = build_fused()[0]
    maps = fused_maps(**a)
    res = run_bass_kernel_spmd(_CACHE["nc"], maps, core_ids=list(range(8)))
    out = np.stack([np.concatenate([np.asarray(res.results[b * 4 + q]["out"]) for q in range(4)], axis=0)
                    for b in range(2)])
    return out.astype(np.float32)
```

```python
import numpy as np
import ml_dtypes
import concourse.bass as bass
import concourse.mybir as mybir
from concourse.bass_utils import run_bass_kernel_spmd

F32 = mybir.dt.float32
BF16 = mybir.dt.bfloat16
AF = mybir.ActivationFunctionType
ALU = mybir.AluOpType
AX = mybir.AxisListType

EPS = 1e-6
ENGS = ("pe", "act", "dve", "pool", "sp")


class _First:
    def __init__(self, eng, w):
        self._e, self._w = eng, w

    def __getattr__(self, name):
        real = getattr(self._e, name)
        if not callable(real):
            return real

        def call(*a, **k):
            ins = real(*a, **k)
            if self._w is not None and hasattr(ins, "_wait_ge"):
                ins._wait_ge(*self._w)
                self._w = None
            return ins
        return call


class _Dummy:
    def then_inc(self, *a, **k):
        return self

    def _wait_ge(self, *a, **k):
        return self


class _Rec:
    def __init__(self):
        self.calls = []

    def __getattr__(self, name):
        def call(*a, **k):
            self.calls.append((name, a, k))
            return _Dummy()
        return call


class Prog:
    def __init__(self, nc):
        self.nc = nc
        self.ops = []
        self.want_pid = False
        self.reorder = True
        self.debug = None
        self.fifo = FIFO_Q
        self.psum = set()

    def op(self, eng, fn, reads=(), writes=()):
        self.ops.append(dict(eng=eng, fn=fn, r=tuple(reads), w=tuple(writes), dma=None))

    def dma(self, eng, fn, reads=(), writes=(), lane=None):
        assert lane is not None
        self.ops.append(dict(eng=eng, fn=fn, r=tuple(reads), w=tuple(writes), dma=lane))

    def cc(self, fn, reads=(), writes=(), lane=None):
        self.ops.append(dict(eng="pool", fn=fn, r=tuple(reads), w=tuple(writes), dma=lane, cc=True))

    def barrier(self):
        self.ops.append(dict(eng="barrier", fn=None, r=(), w=(), dma=None))

    def finish(self):
        self.ops.append(dict(eng="sp", fn=lambda e: e.nop(), r=(), w=(), dma=None, fin=True))

    def _cost(self, o):
        E = o["eng"]
        rec = _Rec()
        try:
            o["fn"](rec)
        except Exception:
            return (50.0, 2000.0) if o["dma"] is not None else (500.0, 0.0)
        busy, lat = 0.0, 0.0
        for name, a, k in rec.calls:
            out = k.get("out", a[0] if a else None)
            if name == "dma_start":
                src = k.get("in_")
                nb = 1
                for d_ in out.shape:
                    nb *= d_
                nb *= max(mybir.dt.size(out.dtype), mybir.dt.size(src.dtype))
                busy += 50.0
                lat += nb / 250.0
            elif name in ("matmul", "transpose"):
                src = k.get("lhsT") if name == "matmul" else k.get("in_")
                f = 4.0 if src.dtype == F32 else 1.0
                busy += (16.0 + 0.46 * out.free_size()) * f
            elif name == "collective_compute":
                busy += 100.0
            elif out is None or not hasattr(out, "free_size"):
                busy += 50.0
            else:
                nf = out.free_size()
                busy += {"act": 200.0 + 1.05 * nf, "dve": 100.0 + 1.4 * nf, "pool": 150.0 + 4.6 * nf}.get(E, 100.0 + nf)
        return busy, lat

    def _sched_seg(self, seg):
        n = len(seg)
        if n < 3:
            return seg
        deps = [set() for _ in range(n)]
        last_w, readers = {}, {}
        for i, o in enumerate(seg):
            d = deps[i]
            for b in o["r"]:
                if b in last_w:
                    d.add(last_w[b])
                if b in self.psum:
                    for r in readers.get(b, ()):
                        if seg[r]["eng"] != o["eng"]:
                            d.add(r)
            for b in o["w"]:
                if b in last_w:
                    d.add(last_w[b])
                d.update(readers.get(b, ()))
            d.discard(i)
            for b in o["w"]:
                last_w[b] = i
                readers[b] = []
            for b in o["r"]:
                readers.setdefault(b, []).append(i)
        succ = [[] for _ in range(n)]
        indeg = [len(d) for d in deps]
        for i, d in enumerate(deps):
            for x in d:
                succ[x].append(i)
        cost = [self._cost(o) for o in seg]
        free_at = dict.fromkeys(ENGS, 0.0)
        dma_free = 0.0
        ready = [0.0] * n
        ext = getattr(self, "_ext", {})
        for i, o in enumerate(seg):
            for b in o["r"]:
                if b in ext:
                    ready[i] = max(ready[i], ext[b])
        avail = {E: [] for E in ENGS}
        for i in range(n):
            if indeg[i] == 0:
                avail[seg[i]["eng"]].append(i)
        order = []
        SLACK = 150.0
        while len(order) < n:
            best = None
            for E in ENGS:
                av = avail[E]
                if not av:
                    continue
                t0 = max(free_at[E], min(ready[i] for i in av))
                if self.fifo:
                    cand = min((i for i in av if ready[i] <= t0 + SLACK), key=lambda i: (int(ready[i] / self.fifo), i))
                else:
                    cand = min(i for i in av if ready[i] <= t0 + SLACK)
                st = max(free_at[E], ready[cand])
                if best is None or (st, cand) < best[:2]:
                    best = (st, cand, E)
            st, i, E = best
            avail[E].remove(i)
            busy, lat = cost[i]
            free_at[E] = st + busy
            if seg[i].get("cc"):
                fin = st + 150000.0
            elif seg[i]["dma"] is not None:
                dma_free = max(st + busy, dma_free) + lat
                fin = dma_free + 2000.0
            else:
                fin = st + busy + 100.0
            order.append(i)
            if self.debug is not None:
                self.debug.append((seg[i], st, fin, E))
            for j in succ[i]:
                if fin > ready[j]:
                    ready[j] = fin
                indeg[j] -= 1
                if indeg[j] == 0:
                    avail[seg[j]["eng"]].append(j)
        self.sim_time = getattr(self, "sim_time", 0.0) + max(max(free_at.values()), dma_free)
        self._ext = {}
        k = 0
        for i in order:
            if seg[i].get("cc"):
                k += 1
        j = 0
        for i in order:
            if seg[i].get("cc"):
                j += 1
                late = max(0.0, 60000.0 * (j - (k - 4))) if j > k - 4 else 0.0
                for b in seg[i]["w"]:
                    self._ext[b] = late
        return [seg[i] for i in order]

    def _schedule(self, ops):
        out, seg = [], []
        for o in ops:
            if o["eng"] == "barrier" or o.get("fin"):
                out += self._sched_seg(seg)
                seg = []
                out.append(o)
            else:
                seg.append(o)
        return out + self._sched_seg(seg)

    def build(self):
        nc = self.nc
        if self.reorder:
            self.ops = self._schedule(self.ops)
        ops = self.ops
        n = len(ops)
        last_w = {}
        readers = {}
        deps = [None] * n
        bar_deps = set()
        last_on_eng = {}
        lanes_last = {}
        first_after_bar = {}
        for i, o in enumerate(ops):
            if o["eng"] == "barrier":
                bar_deps = set(last_on_eng.values()) | {v for k, v in lanes_last.items() if not k.startswith("CC:")}
                first_after_bar = {}
                last_w = {k: v for k, v in last_w.items() if k.startswith("d:")}
                readers = {k: v for k, v in readers.items() if k.startswith("d:")}
                deps[i] = set()
                continue
            raw = set()
            oth = set()
            for b in o["r"]:
                if b in last_w:
                    raw.add(last_w[b])
                if b in self.psum:
                    for r in readers.get(b, ()):
                        if ops[r]["eng"] != o["eng"]:
                            raw.add(r)
            for b in o["w"]:
                if b in last_w:
                    oth.add(last_w[b])
                for r in readers.get(b, ()):
                    oth.add(r)
            E = o["eng"]
            d = set()
            for x in raw | oth:
                if x == i:
                    continue
                ox = ops[x]
                if ox["dma"] is None and o["dma"] is None and ox["eng"] == E:
                    if E == "pe":
                        continue
                    if x not in raw:
                        continue
                d.add(x)
            if o.get("fin"):
                d |= set(last_on_eng.values()) | set(lanes_last.values())
            if bar_deps and E not in first_after_bar:
                d |= bar_deps
                first_after_bar[E] = i
            deps[i] = d
            for b in o["w"]:
                last_w[b] = i
                readers[b] = []
            for b in o["r"]:
                lst = readers.setdefault(b, [])
                if o["dma"] is None:
                    lst[:] = [r for r in lst if not (ops[r]["dma"] is None and ops[r]["eng"] == E)]
                lst.append(i)
            if o["dma"] is None:
                last_on_eng[E] = i
            else:
                lanes_last[o["dma"]] = i

        pos = [0] * n
        eng_count = {e: 0 for e in ENGS}
        lane_count = {}
        seen = {e: {} for e in ENGS}
        snap = [None] * n
        waits = [None] * n
        signals = [False] * n
        for i, o in enumerate(ops):
            if o["eng"] == "barrier":
                continue
            E = o["eng"]
            w = []
            for d in sorted(deps[i], reverse=True):
                od = ops[d]
                key = ("L", od["dma"]) if od["dma"] is not None else ("E", od["eng"])
                need = pos[d]
                if od["dma"] is not None and od["dma"].startswith("G:"):
                    need = lane_count[od["dma"]]
                if seen[E].get(key, 0) >= need:
                    continue
                w.append((key, d, need))
                if od["dma"] is None:
                    signals[d] = True
                seen[E][key] = need
                for k2, v2 in snap[d].items():
                    if seen[E].get(k2, 0) < v2:
                        seen[E][k2] = v2
            waits[i] = w
            if o["dma"] is not None:
                lane_count[o["dma"]] = lane_count.get(o["dma"], 0) + 1
                pos[i] = lane_count[o["dma"]]
            else:
                eng_count[E] += 1
                pos[i] = eng_count[E]
            snap[i] = dict(seen[E])

        sigval = [0] * n
        cnt = {e: 0 for e in ENGS}
        for i, o in enumerate(ops):
            if o["eng"] == "barrier" or o["dma"] is not None:
                continue
            if signals[i]:
                cnt[o["eng"]] += 1
            sigval[i] = cnt[o["eng"]]

        sems = {e: nc.alloc_semaphore(f"s_{e}") for e in ENGS}
        lane_sems = {ln: nc.alloc_semaphore(f"l{j}") for j, ln in enumerate(sorted(lane_count))}
        per_eng = {e: [i for i, o in enumerate(ops) if o["eng"] == e] for e in ENGS}
        self.stats = dict(n_ops=n, lanes=len(lane_sems), sig=dict(cnt),
                          nwaits=sum(len(w) for w in waits if w))

        self.pid = {}

        def run(E, eng):
            if E in ("sp", "pool") and self.want_pid:
                r = eng.alloc_register("qoff")
                r2 = eng.alloc_register("qoff2")
                eng.reg_mod(r, eng.partition_id(), 4)
                eng.reg_mul(r, r, 2048)
                eng.reg_mod(r2, eng.partition_id(), 2)
                eng.reg_mul(r2, r2, 1536)
                eng.reg_sub(r, r, r2)
                self.pid[E] = eng.snap(r, min_val=0, max_val=4608)
            for i in per_eng[E]:
                o = ops[i]
                wl = []
                for key, d, need in waits[i]:
                    if key[0] == "L":
                        wl.append((lane_sems[key[1]], 1 if ops[d].get("cc") else 16 * need))
                    else:
                        wl.append((sems[key[1]], sigval[d]))
                attach = bool(wl) and not o.get("cc") and not o.get("fin")
                for sem_, val_ in (wl[:-1] if attach else wl):
                    eng.wait_ge(sem_, val_)
                ins = o["fn"](_First(eng, wl[-1]) if attach else eng)
                if o.get("cc"):
                    ins.then_inc(lane_sems[o["dma"]])
                elif o["dma"] is not None:
                    ins.then_inc(lane_sems[o["dma"]], 16)
                elif signals[i]:
                    ins.then_inc(sems[E], 1)

        with nc.Block() as block:
            @block.tensor
            def _(e):
                run("pe", e)

            @block.scalar
            def _(e):
                run("act", e)

            @block.vector
            def _(e):
                run("dve", e)

            @block.gpsimd
            def _(e):
                run("pool", e)

            @block.sync
            def _(e):
                run("sp", e)


class Ctx:
    def __init__(self, nc, P, pfx):
        self.nc, self.P, self.pfx = nc, P, pfx

    def sb(self, name, shape, dt):
        return self.nc.alloc_sbuf_tensor(f"{self.pfx}{name}", list(shape), dt)

    def ps(self, name, shape, dt=F32):
        return self.nc.alloc_psum_tensor(f"{self.pfx}{name}", list(shape), dt)


def rstd_ops(P, ss, tmp, rstd, n, rd, wr):
    P.op("act", lambda e: e.activation(out=tmp, in_=ss, func=AF.Ln, scale=1.0 / n, bias=EPS),
         reads=rd, writes=[wr + "_t"])
    P.op("act", lambda e: e.activation(out=rstd, in_=tmp, func=AF.Exp, scale=-0.5),
         reads=[wr + "_t"], writes=[wr])


A_ENGS = ("dve", "pool", "pool")
FIFO_Q = 0.0
NXT = 6


def phase_ret(P, nc, D, NT=8192, NS=2):
    C = Ctx(nc, P, "A_")
    RENG, QENG, GENG = A_ENGS
    NG = NT // 512
    ident = C.sb("ident", [128, 128], BF16)
    w_bf = C.sb("w_bf", [128, 8, 1536], BF16)
    wst = [C.sb(f"wst{i}", [128, 1536], F32) for i in range(2)]
    gain = C.sb("gain", [128, 8], F32)
    dmask = C.sb("dmask", [128, 128], F32)
    qdec4 = C.sb("qdec4", [128, 512], F32)
    kdec = C.sb("kdec", [128, 1], F32)
    cdec = C.sb("cdec", [128, 1], F32)
    xt = [C.sb(f"xt{i}", [128, 1024], F32) for i in range(NXT)]
    junk = C.sb("junk", [128, 1024], BF16)
    xn = [C.sb(f"xn{i}", [128, 1024], BF16) for i in range(2)]
    xnT = [C.sb(f"xnT{i}", [128, 8, 512], BF16) for i in range(NS)]
    cosg = [C.sb(f"cos{i}", [128, 512], F32) for i in range(NS)]
    sing = [C.sb(f"sin{i}", [128, 512], F32) for i in range(NS)]
    qrot = [C.sb(f"qrot{i}", [128, 2, 512], BF16) for i in range(NS)]
    qd = [C.sb(f"qd{i}", [128, 2, 512], BF16) for i in range(NS)]
    krot = [C.sb(f"krot{i}", [128, 2, 512], BF16) for i in range(NS)]
    vv = [C.sb(f"v{i}", [128, 4, 512], BF16) for i in range(NS)]
    sg = [C.sb(f"sg{i}", [128, 4, 512], F32) for i in range(NS)]
    ktm = [C.sb(f"ktm{i}", [128, 4, 256], BF16) for i in range(NS)]
    tmp = [C.sb(f"tmp{i}", [128, 512], F32) for i in range(4)]
    st32 = C.sb("st32", [128, 2, 512], F32)
    stbf = [C.sb(f"stbf{i}", [128, 2, 512], BF16) for i in range(2)]
    stm = [C.sb(f"stm{i}", [128, 128], BF16) for i in range(2)]
    ybuf = [C.sb(f"ybuf{i}", [128, 512], BF16) for i in range(4)]
    stat = C.sb("stat", [128, 24], F32)
    junk2 = C.sb("junk2", [128, 512], BF16)
    ge = C.sb("ge", [128, 512], F32)
    gc = C.sb("gc", [128, 512], F32)

    psT = C.ps("psT", [128, 8, 128], BF16)
    pq = [C.ps(f"pq{i}", [128, 512]) for i in range(2)]
    pv = C.ps("pv", [128, 512])
    pg = C.ps("pg", [128, 512])
    pst = C.ps("pst", [128, 128])
    po = C.ps("po", [128, 512])
    pu = C.ps("pu", [128, 512])

    P.psum |= {"A_psT", "A_pq0", "A_pq1", "A_pv", "A_pg", "A_pst", "A_po", "A_pu"}

    def ld(dst, src, name, eng="sp", lane=None):
        P.dma(eng, lambda e: e.dma_start(out=dst, in_=src), writes=[name], lane=lane or ("L:" + name))

    ld(ident[:], D["ident"], "A_ident", lane="G:const")
    ld(gain[:], D["gain"], "A_gain", lane="G:const")
    ld(dmask[:], D["dmask"], "A_dmask", lane="G:const")
    ld(qdec4[:], D["qdec4"], "A_qdec4", lane="G:const")
    ld(kdec[:], D["kdec"], "A_kdec", lane="G:const")
    ld(cdec[:], D["cdec"], "A_cdec", lane="G:const")
    wv_ = D["w"].rearrange("(kc p) f -> p kc f", p=128)
    for kc in range(8):
        s = kc % 2
        ld(wst[s][:], wv_[:, kc, :], f"A_wst{s}")
        P.op("dve", lambda e, kc=kc, s=s: e.tensor_scalar(
            out=w_bf[:, kc, :], in0=wst[s][:], scalar1=gain[:, kc:kc + 1], scalar2=None, op0=ALU.mult),
            reads=[f"A_wst{s}", "A_gain"], writes=[f"A_w{kc}"])
    WN = [f"A_w{kc}" for kc in range(8)]
    P.op("dve", lambda e: e.memset(st32[:], 0.0), writes=["A_st32_0", "A_st32_1"])
    P.op("pool", lambda e: e.memset(stbf[0][:], 0.0), writes=["A_stbf0_0", "A_stbf0_1"])

    xb = D["xb"]
    for tg in range(NG):
        s = tg % NS
        t0 = tg * 512
        ld(cosg[s][:], D["cosT"][:, t0:t0 + 512], f"A_cos{s}")
        ld(sing[s][:], D["sinT"][:, t0:t0 + 512], f"A_sin{s}")
        for tt in range(4):
            xs = (tg * 4 + tt) % NXT
            r0 = t0 + tt * 128
            ld(xt[xs][:], xb[r0:r0 + 128, :], f"A_xt{xs}")
            c0 = 3 * tt
            P.op("act", lambda e, xs=xs, c0=c0: e.activation(out=junk[:], in_=xt[xs][:], func=AF.Square,
                                                            accum_out=stat[:, c0:c0 + 1]),
                 reads=[f"A_xt{xs}"], writes=[f"A_ss{tt}"])
            rstd_ops(P, stat[:, c0:c0 + 1], stat[:, c0 + 1:c0 + 2], stat[:, c0 + 2:c0 + 3], 1024, [f"A_ss{tt}"],
                     f"A_rstd{tt}")
            xq = tt % 2
            P.op("act", lambda e, xs=xs, xq=xq, c0=c0: e.activation(out=xn[xq][:], in_=xt[xs][:], func=AF.Copy,
                                                                  scale=stat[:, c0 + 2:c0 + 3]),
                 reads=[f"A_xt{xs}", f"A_rstd{tt}"], writes=[f"A_xn{xq}"])

            def tr(e, xq=xq):
                for kc in range(8):
                    ins = e.transpose(out=psT[:, kc, :], in_=xn[xq][:, kc * 128:(kc + 1) * 128],
                                      identity=ident[:])
                return ins
            P.op("pe", tr, reads=[f"A_xn{xq}", "A_ident"], writes=["A_psT"])
            P.op("dve", lambda e, s=s, tt=tt: e.tensor_copy(out=xnT[s][:, :, tt * 128:(tt + 1) * 128],
                                                            in_=psT[:]),
                 reads=["A_psT"], writes=[f"A_xnT{s}_{tt}"])
        XN = [f"A_xnT{s}_{tt}" for tt in range(4)]

        for which, dst, c0 in (("q", qrot, 0), ("k", krot, 256)):
            for hh in range(2):
                def mm(e, hh=hh, c0=c0, s=s):
                    for kc in range(8):
                        ins = e.matmul(out=pq[hh][:], lhsT=w_bf[:, kc, c0 + hh * 128:c0 + (hh + 1) * 128],
                                       rhs=xnT[s][:, kc, :], start=(kc == 0), stop=(kc == 7))
                    return ins
                P.op("pe", mm, reads=WN + XN, writes=[f"A_pq{hh}"])
            cs, sn = cosg[s], sing[s]
            P.op("dve", lambda e, cs=cs: e.tensor_tensor(out=tmp[0][:], in0=pq[0][:], in1=cs[:], op=ALU.mult),
                 reads=["A_pq0", f"A_cos{s}"], writes=["A_tmp0"])
            P.op("dve", lambda e, sn=sn: e.tensor_tensor(out=tmp[1][:], in0=pq[1][:], in1=sn[:], op=ALU.mult),
                 reads=["A_pq1", f"A_sin{s}"], writes=["A_tmp1"])
            P.op("dve", lambda e, cs=cs: e.tensor_tensor(out=tmp[2][:], in0=pq[1][:], in1=cs[:], op=ALU.mult),
                 reads=["A_pq1", f"A_cos{s}"], writes=["A_tmp2"])
            P.op("dve", lambda e, sn=sn: e.tensor_tensor(out=tmp[3][:], in0=pq[0][:], in1=sn[:], op=ALU.mult),
                 reads=["A_pq0", f"A_sin{s}"], writes=["A_tmp3"])
            P.op(RENG, lambda e, dst=dst, s=s: e.tensor_tensor(out=dst[s][:, 0, :], in0=tmp[0][:], in1=tmp[1][:],
                                                                 op=ALU.subtract),
                 reads=["A_tmp0", "A_tmp1"], writes=[f"A_{which}rot{s}_0"])
            P.op(RENG, lambda e, dst=dst, s=s: e.tensor_tensor(out=dst[s][:, 1, :], in0=tmp[2][:], in1=tmp[3][:],
                                                                 op=ALU.add),
                 reads=["A_tmp2", "A_tmp3"], writes=[f"A_{which}rot{s}_1"])
            if which == "q":
                for dc in range(2):
                    P.op(QENG, lambda e, dc=dc, s=s: e.tensor_tensor(out=qd[s][:, dc, :], in0=qrot[s][:, dc, :],
                                                                       in1=qdec4[:], op=ALU.mult),
                         reads=[f"A_qrot{s}_{dc}", "A_qdec4"], writes=[f"A_qd{s}_{dc}"])

        for tt in range(4):
            def mv(e, tt=tt, s=s):
                for kc in range(8):
                    ins = e.matmul(out=pv[:], lhsT=xnT[s][:, kc, tt * 128:(tt + 1) * 128],
                                   rhs=w_bf[:, kc, 512:1024], start=(kc == 0), stop=(kc == 7))
                return ins
            P.op("pe", mv, reads=WN + [XN[tt]], writes=["A_pv"])
            P.op("act", lambda e, tt=tt, s=s: e.activation(out=vv[s][:, tt, :], in_=pv[:], func=AF.Copy),
                 reads=["A_pv"], writes=[f"A_v{s}_{tt}"])

            def mg(e, tt=tt, s=s):
                for kc in range(8):
                    ins = e.matmul(out=pg[:], lhsT=xnT[s][:, kc, tt * 128:(tt + 1) * 128],
                                   rhs=w_bf[:, kc, 1024:1536], start=(kc == 0), stop=(kc == 7))
                return ins
            P.op("pe", mg, reads=WN + [XN[tt]], writes=["A_pg"])
            P.op("act", lambda e: e.activation(out=ge[:], in_=pg[:], func=AF.Exp, scale=-1.0),
                 reads=["A_pg"], writes=["A_ge"])
            P.op("act", lambda e: e.activation(out=gc[:], in_=pg[:], func=AF.Copy),
                 reads=["A_pg"], writes=["A_gc"])
            P.op("dve", lambda e: e.tensor_scalar(out=ge[:], in0=ge[:], scalar1=1.0, scalar2=None, op0=ALU.add),
                 reads=["A_ge"], writes=["A_ge"])
            P.op("dve", lambda e: e.reciprocal(out=ge[:], in_=ge[:]), reads=["A_ge"], writes=["A_ge"])
            P.op(GENG, lambda e, tt=tt, s=s: e.tensor_tensor(out=sg[s][:, tt, :], in0=gc[:], in1=ge[:], op=ALU.mult),
                 reads=["A_ge", "A_gc"], writes=[f"A_sg{s}_{tt}"])

            def tk(e, tt=tt, s=s):
                for dc in range(2):
                    ins = e.transpose(out=psT[:, dc, :], in_=krot[s][:, dc, tt * 128:(tt + 1) * 128],
                                      identity=ident[:])
                return ins
            P.op("pe", tk, reads=[f"A_krot{s}_0", f"A_krot{s}_1", "A_ident"], writes=["A_psT"])
            P.op("dve", lambda e, tt=tt, s=s: e.tensor_scalar(
                out=ktm[s][:, tt, :].rearrange("p (a b) -> p a b", a=2), in0=psT[:, 0:2, :],
                scalar1=kdec[:, 0:1], scalar2=None, op0=ALU.mult),
                reads=["A_psT", "A_kdec"], writes=[f"A_ktm{s}_{tt}"])

        for tt in range(4):
            c = tg * 4 + tt
            sp_ = c % 2
            sl = slice(tt * 128, (tt + 1) * 128)

            def ms(e, s=s, sl=sl):
                for dc in range(2):
                    ins = e.matmul(out=pst[:], lhsT=krot[s][:, dc, sl], rhs=qrot[s][:, dc, sl],
                                   start=(dc == 0), stop=(dc == 1))
                return ins
            P.op("pe", ms, reads=[f"A_krot{s}_0", f"A_krot{s}_1", f"A_qrot{s}_0", f"A_qrot{s}_1"],
                 writes=["A_pst"])
            P.op("dve", lambda e, sp_=sp_: e.tensor_tensor(out=stm[sp_][:], in0=pst[:], in1=dmask[:], op=ALU.mult),
                 reads=["A_pst", "A_dmask"], writes=[f"A_stm{sp_}"])

            def mo(e, s=s, sl=sl, sp_=sp_, tt=tt):
                e.matmul(out=po[:], lhsT=stm[sp_][:], rhs=vv[s][:, tt, :], start=True, stop=False)
                for dc in range(2):
                    ins = e.matmul(out=po[:], lhsT=qd[s][:, dc, sl], rhs=stbf[sp_][:, dc, :],
                                   start=False, stop=(dc == 1))
                return ins
            P.op("pe", mo, reads=[f"A_stm{sp_}", f"A_v{s}_{tt}", f"A_qd{s}_0", f"A_qd{s}_1",
                                  f"A_stbf{sp_}_0", f"A_stbf{sp_}_1"], writes=["A_po"])
            for dc in range(2):
                P.op("pe", lambda e, s=s, tt=tt, dc=dc: e.matmul(
                    out=pu[:], lhsT=ktm[s][:, tt, dc * 128:(dc + 1) * 128], rhs=vv[s][:, tt, :],
                    start=True, stop=True),
                    reads=[f"A_ktm{s}_{tt}", f"A_v{s}_{tt}"], writes=["A_pu"])
                P.op("dve", lambda e, dc=dc: e.scalar_tensor_tensor(
                    out=st32[:, dc, :], in0=st32[:, dc, :], scalar=cdec[:, 0:1], in1=pu[:],
                    op0=ALU.mult, op1=ALU.add),
                    reads=["A_pu", "A_cdec", f"A_st32_{dc}"], writes=[f"A_st32_{dc}"])
                P.op("act", lambda e, dc=dc, sp_=sp_: e.activation(out=stbf[1 - sp_][:, dc, :], in_=st32[:, dc, :],
                                                                   func=AF.Copy),
                     reads=[f"A_st32_{dc}"], writes=[f"A_stbf{1 - sp_}_{dc}"])
            g0 = 12 + 3 * (c % 2)
            P.op("act", lambda e, g0=g0: e.activation(out=junk2[:], in_=po[:], func=AF.Square,
                                                      accum_out=stat[:, g0:g0 + 1]),
                 reads=["A_po"], writes=[f"A_ssq{c % 2}"])
            rstd_ops(P, stat[:, g0:g0 + 1], stat[:, g0 + 1:g0 + 2], stat[:, g0 + 2:g0 + 3], 512, [f"A_ssq{c % 2}"],
                     f"A_rs{c % 2}")
            yb = c % 4
            P.op("dve", lambda e, yb=yb, s=s, tt=tt, g0=g0: e.scalar_tensor_tensor(
                out=ybuf[yb][:], in0=po[:], scalar=stat[:, g0 + 2:g0 + 3], in1=sg[s][:, tt, :],
                op0=ALU.mult, op1=ALU.mult),
                reads=["A_po", f"A_rs{c % 2}", f"A_sg{s}_{tt}"], writes=[f"A_ybuf{yb}"])
            P.dma("sp", lambda e, yb=yb, c=c: e.dma_start(out=D["y_out"][c * 128:(c + 1) * 128, :], in_=ybuf[yb][:]),
                  reads=[f"A_ybuf{yb}"], writes=[f"d:A_y{c // 8}"], lane=f"S:A_ybuf{yb}")
        if "after_group" in D:
            D["after_group"](tg)


def phase_ffn(P, nc, D, F, final, pfx, NTOK=2048, NE=16, stage=3):
    C = Ctx(nc, P, pfx)
    assert F <= 2048
    N = lambda s: pfx + s
    NTILE = NTOK // 128
    FC = F // 128
    NGRP = NTOK // 512
    ident = C.sb("ident", [128, 128], BF16)
    identf = C.sb("identf", [128, 128], F32)
    h = C.sb("h", [128, NTILE, 1024], F32)
    hnT = C.sb("hnT", [128, 8, NTOK], BF16)
    wbuf = C.sb("wbuf", [128, 24576], BF16)
    wo_v = wbuf[:, 0:FC * 1024].rearrange("p (f n) -> p f n", f=FC)

    def wslot(s):
        b = s * 12288
        return (wbuf[:, b:b + 4096].rearrange("p (k f) -> p k f", k=8),
                wbuf[:, b + 4096:b + 8192].rearrange("p (k f) -> p k f", k=8),
                wbuf[:, b + 8192:b + 12288].rearrange("p (k f) -> p k f", k=4))
    yt = [C.sb(f"yt{i}", [128, F], BF16) for i in range(2)]
    yT = [C.sb(f"yT{i}", [128, FC, 128], BF16) for i in range(2)]
    hn32 = [C.sb(f"hn32_{i}", [128, 1024], F32) for i in range(2)]
    hnT32 = [C.sb(f"hnT32_{i}", [128, 8, 128], F32) for i in range(2)]
    fgain = C.sb("fgain", [128, 1024], F32)
    wr = C.sb("wr", [128, 8, 20], F32)
    rbias = C.sb("rbias", [128, 20], F32)
    comb = C.sb("comb", [128, NTILE, 16], F32)
    rts = [C.sb(f"rt{i}", [128, 64], F32) for i in range(4)]
    st = C.sb("st", [128, 3, NTILE], F32)
    junk = C.sb("junk", [128, 1024], BF16)
    sgl = [C.sb(f"sgl{i}", [128, 512], F32) for i in range(2)]
    hT = [C.sb(f"hT{i}", [128, 4, 512], BF16) for i in range(2)]
    if final:
        ngain = C.sb("ngain", [128, 1024], F32)
        ob = [C.sb(f"ob{i}", [128, 1024], F32) for i in range(2)]

    pyT = C.ps("pyT", [128, 8, 128], BF16)
    pT32 = C.ps("pT32", [128, 8, 128], F32)
    pT32v = pT32[:].rearrange("p a b -> p (a b)")
    pg0 = C.ps("pg", [128, 512])
    pg = [pg0[:], pT32v[:, 0:512]]
    pyTs = [pyT[:], pg0[:].bitcast(BF16).rearrange("p (a b) -> p a b", a=8)]
    pyTn = [N("pyT"), N("pg")]
    pu = [C.ps("pu", [128, 512])[:], pT32v[:, 512:1024]]
    pgn = [N("pg"), N("pT32a")]
    pun = [N("pu"), N("pT32b")]
    pd = C.ps("pd", [128, 2, 512])
    pr = C.ps("pr", [128, 32])

    P.psum |= {N(x) for x in ("pyT", "pT32a", "pT32b", "pg", "pu", "pd0", "pd1", "pr")}

    def ld(dst, src, name, eng="sp", lane=None):
        P.dma(eng, lambda e: e.dma_start(out=dst, in_=src), writes=[name], lane=lane or ("L:" + name))

    ld(ident[:], D["ident"], N("ident"), lane="G:const")
    ld(identf[:], D["identf"], N("identf"), lane="G:const")
    ld(fgain[:], D["fgain"], N("fgain"), lane="G:const")
    ld(wr[:], D["wr"].rearrange("(kc p) n -> p kc n", p=128), N("wr"), lane="G:const")
    ld(rbias[:], D["rbias"], N("rbias"), lane="G:const")
    if final:
        ld(ngain[:], D["ngain"], N("ngain"), lane="G:const")
    xs_v = D["xs"].rearrange("(t p) f -> p t f", p=128)
    def load_hq(q):
        tq = NTILE // 4
        P.dma("sp", lambda e, q=q, tq=tq: e.dma_start(out=h[:, q * tq:(q + 1) * tq, :], in_=xs_v[:, q * tq:(q + 1) * tq, :]),
              reads=D.get("xs_reads", ()), writes=[N(f"hq{q}")], lane="G:hq")
    HQ = lambda t: N(f"hq{t // (NTILE // 4)}")
    wo_d = D["wo"].rearrange("(fc p) n -> p fc n", p=128)
    nq = FC // 4
    for q in range(nq):
        P.dma("pool", lambda e, q=q: e.dma_start(out=wo_v[:, q * 4:(q + 1) * 4, :], in_=wo_d[:, q * 4:(q + 1) * 4, :]),
              writes=[N(f"wo{q}")], lane="G:wo")
    WO = [N(f"wo{q}") for q in range(nq)]
    SL = [N("ws0"), N("ws1")]

    for t in range(NTILE):
        s = t % 2
        if "ys_fn" in D:
            P.dma(D.get("ys_eng", "sp"), lambda e, t=t, s=s: D["ys_fn"](e, t, yt[s]), reads=D["ys_reads"](t), writes=[N(f"yt{s}")],
                  lane="L:" + N(f"yt{s}"))
        else:
            ld(yt[s][:], D["ys"][t * 128:(t + 1) * 128, :], N(f"yt{s}"))
        if t % (NTILE // 4) == 0:
            load_hq(t // (NTILE // 4))
        for half in range(FC // 8):
            pb = (t * (FC // 8) + half) % 2

            def tr(e, s=s, half=half, pb=pb):
                for j in range(8):
                    fc = half * 8 + j
                    ins = e.transpose(out=pyTs[pb][:, j, :], in_=yt[s][:, fc * 128:(fc + 1) * 128], identity=ident[:])
                return ins
            P.op("pe", tr, reads=[N(f"yt{s}"), N("ident")], writes=[pyTn[pb]])
            P.op("act", lambda e, half=half, s=s, pb=pb: e.activation(out=yT[s][:, half * 8:(half + 1) * 8, :], in_=pyTs[pb],
                                                                func=AF.Copy),
                 reads=[pyTn[pb]], writes=[N(f"yT{s}_{half}")])
        for hh in range(2):
            def mm(e, hh=hh, s=s):
                for fc in range(FC):
                    ins = e.matmul(out=pd[:, hh, :], lhsT=yT[s][:, fc, :], rhs=wo_v[:, fc, hh * 512:(hh + 1) * 512],
                                   start=(fc == 0), stop=(fc == FC - 1))
                return ins
            P.op("pe", mm, reads=[N(f"yT{s}_{i}") for i in range(FC // 8)] + WO + SL, writes=[N(f"pd{hh}")])
            P.op("dve", lambda e, t=t, hh=hh: e.tensor_tensor(out=h[:, t, hh * 512:(hh + 1) * 512],
                                                              in0=pd[:, hh, :], in1=h[:, t, hh * 512:(hh + 1) * 512],
                                                              op=ALU.add),
                 reads=[N(f"pd{hh}"), HQ(t), N(f"h{t}")], writes=[N(f"h{t}")])

    for t in range(NTILE if stage >= 2 else 0):
        P.op("act", lambda e, t=t: e.activation(out=junk[:], in_=h[:, t, :], func=AF.Square, accum_out=st[:, 0, t:t + 1]),
             reads=[N(f"h{t}")], writes=[N(f"ss{t}")])
    for t in range(NTILE if stage >= 2 else 0):
        rstd_ops(P, st[:, 0, t:t + 1], st[:, 1, t:t + 1], st[:, 2, t:t + 1], 1024, [N(f"ss{t}")], N(f"rstd{t}"))
    for t in range(NTILE if stage >= 2 else 0):
        u = t % 2
        P.op("dve", lambda e, t=t, u=u: e.scalar_tensor_tensor(out=hn32[u][:], in0=h[:, t, :], scalar=st[:, 2, t:t + 1],
                                                               in1=fgain[:], op0=ALU.mult, op1=ALU.mult),
             reads=[N(f"h{t}"), N(f"rstd{t}"), N("fgain")], writes=[N(f"hn32_{u}")])

        def trf(e, u=u):
            for kc in range(8):
                ins = e.transpose(out=pT32[:, kc, :], in_=hn32[u][:, kc * 128:(kc + 1) * 128], identity=identf[:])
            return ins
        P.op("pe", trf, reads=[N(f"hn32_{u}"), N("identf")], writes=[N("pT32a"), N("pT32b")])
        P.op("act", lambda e, t=t: e.activation(out=hnT[:, :, t * 128:(t + 1) * 128], in_=pT32[:], func=AF.Copy),
             reads=[N("pT32a"), N("pT32b")], writes=[N(f"hnT{t}")])
        P.op("dve", lambda e, u=u: e.tensor_copy(out=hnT32[u][:], in_=pT32[:]),
             reads=[N("pT32a"), N("pT32b")], writes=[N(f"hnT32_{u}")])

        def mr(e, u=u):
            for kc in range(8):
                ins = e.matmul(out=pr[:, 0:20], lhsT=hnT32[u][:, kc, :], rhs=wr[:, kc, :], start=(kc == 0), stop=(kc == 7))
            return ins
        P.op("pe", mr, reads=[N(f"hnT32_{u}"), N("wr")], writes=[N("pr")])
        rt = rts[t % 4]
        R = N(f"rt{t % 4}")
        k = [0]

        def dv(fn, rd=(), eng="dve"):
            P.op(eng, fn, reads=[R] + list(rd), writes=[R])
        dv(lambda e, rt=rt: e.tensor_tensor(out=rt[:, 0:20], in0=pr[:, 0:20], in1=rbias[:], op=ALU.add), [N("pr"), N("rbias")])
        dv(lambda e, rt=rt: e.tensor_reduce(out=rt[:, 20:21], in_=rt[:, 0:4], axis=AX.X, op=ALU.max))
        dv(lambda e, rt=rt: e.tensor_scalar(out=rt[:, 24:28], in0=rt[:, 0:4], scalar1=rt[:, 20:21], scalar2=None, op0=ALU.is_equal))
        dv(lambda e, rt=rt: e.tensor_scalar(out=rt[:, 28:32], in0=rt[:, 0:4], scalar1=rt[:, 20:21], scalar2=None, op0=ALU.subtract))
        dv(lambda e, rt=rt: e.activation(out=rt[:, 28:32], in_=rt[:, 28:32], func=AF.Exp, accum_out=rt[:, 21:22]), eng="act")
        dv(lambda e, rt=rt: e.reciprocal(out=rt[:, 22:23], in_=rt[:, 21:22]))
        dv(lambda e, rt=rt: e.tensor_scalar(out=rt[:, 32:36], in0=rt[:, 4:8], scalar1=rt[:, 24:25], scalar2=None, op0=ALU.mult))
        for g in range(1, 4):
            dv(lambda e, g=g, rt=rt: e.scalar_tensor_tensor(out=rt[:, 32:36], in0=rt[:, 4 + 4 * g:8 + 4 * g],
                                                     scalar=rt[:, 24 + g:25 + g], in1=rt[:, 32:36],
                                                     op0=ALU.mult, op1=ALU.add))
        dv(lambda e, rt=rt: e.tensor_reduce(out=rt[:, 36:37], in_=rt[:, 32:36], axis=AX.X, op=ALU.max))
        dv(lambda e, rt=rt: e.tensor_scalar(out=rt[:, 40:44], in0=rt[:, 32:36], scalar1=rt[:, 36:37], scalar2=None, op0=ALU.is_equal))
        dv(lambda e, rt=rt: e.scalar_tensor_tensor(out=rt[:, 44:48], in0=rt[:, 40:44], scalar=-1e30, in1=rt[:, 32:36],
                                            op0=ALU.mult, op1=ALU.add))
        dv(lambda e, rt=rt: e.tensor_reduce(out=rt[:, 37:38], in_=rt[:, 44:48], axis=AX.X, op=ALU.max))
        dv(lambda e, rt=rt: e.tensor_scalar(out=rt[:, 48:52], in0=rt[:, 44:48], scalar1=rt[:, 37:38], scalar2=None, op0=ALU.is_equal))
        dv(lambda e, rt=rt: e.tensor_tensor(out=rt[:, 38:39], in0=rt[:, 37:38], in1=rt[:, 36:37], op=ALU.subtract))
        dv(lambda e, rt=rt: e.activation(out=rt[:, 39:40], in_=rt[:, 38:39], func=AF.Exp), eng="act")
        dv(lambda e, rt=rt: e.tensor_scalar(out=rt[:, 52:53], in0=rt[:, 39:40], scalar1=1.0, scalar2=None, op0=ALU.add))
        dv(lambda e, rt=rt: e.reciprocal(out=rt[:, 52:53], in_=rt[:, 52:53]))
        dv(lambda e, rt=rt: e.tensor_tensor(out=rt[:, 53:54], in0=rt[:, 39:40], in1=rt[:, 52:53], op=ALU.mult))
        dv(lambda e, rt=rt: e.tensor_scalar(out=rt[:, 56:60], in0=rt[:, 40:44], scalar1=rt[:, 52:53], scalar2=None, op0=ALU.mult))
        dv(lambda e, rt=rt: e.scalar_tensor_tensor(out=rt[:, 56:60], in0=rt[:, 48:52], scalar=rt[:, 53:54], in1=rt[:, 56:60],
                                            op0=ALU.mult, op1=ALU.add))
        dv(lambda e, rt=rt: e.tensor_scalar(out=rt[:, 60:64], in0=rt[:, 24:28], scalar1=rt[:, 22:23], scalar2=None, op0=ALU.mult))
        for g in range(4):
            P.op("dve", lambda e, g=g, t=t, rt=rt: e.tensor_scalar(out=comb[:, t, 4 * g:4 * g + 4], in0=rt[:, 56:60],
                                                            scalar1=rt[:, 60 + g:61 + g], scalar2=None, op0=ALU.mult),
                 reads=[R], writes=[N(f"comb{t}")])

    HNT = [N(f"hnT{t}") for t in range(NTILE)]
    pend = None
    it = 0
    wl_cnt = [0]
    passes = D.get("tg_passes", [list(range(NGRP))])
    for ex, tgs in [(ex, tgs) for tgs in (passes if stage >= 3 else []) for ex in range(NE)]:
        s = wl_cnt[0] % 2
        wl_cnt[0] += 1
        wg_s, wu_s, wd_s = wslot(s)
        wg_d = D["wg"][ex].rearrange("(kc p) f -> p kc f", p=128)
        wu_d = D["wu"][ex].rearrange("(kc p) f -> p kc f", p=128)
        wd_d = D["wd"][ex].rearrange("(fc p) n -> p fc n", p=128)
        for (dst, src) in ((wg_s, wg_d), (wu_s, wu_d), (wd_s, wd_d)):
            P.dma("pool", lambda e, dst=dst, src=src: e.dma_start(out=dst, in_=src),
                  writes=[SL[s]], lane="L:" + SL[s])
        for tg in tgs:
            b = it % 2
            for fc in range(4):
                pb = (it * 4 + fc) % 2

                def mg(e, fc=fc, tg=tg, pb=pb, wg_s=wg_s):
                    for kc in range(8):
                        ins = e.matmul(out=pg[pb], lhsT=wg_s[:, kc, fc * 128:(fc + 1) * 128],
                                       rhs=hnT[:, kc, tg * 512:(tg + 1) * 512], start=(kc == 0), stop=(kc == 7))
                    return ins

                def mu(e, fc=fc, tg=tg, pb=pb, wu_s=wu_s):
                    for kc in range(8):
                        ins = e.matmul(out=pu[pb], lhsT=wu_s[:, kc, fc * 128:(fc + 1) * 128],
                                       rhs=hnT[:, kc, tg * 512:(tg + 1) * 512], start=(kc == 0), stop=(kc == 7))
                    return ins
                hn_names = HNT[tg * 4:(tg + 1) * 4]
                P.op("pe", mg, reads=[SL[s]] + hn_names, writes=[pgn[pb]])
                P.op("pe", mu, reads=[SL[s]] + hn_names, writes=[pun[pb]])
                P.op("act", lambda e, pb=pb: e.activation(out=sgl[pb][:], in_=pg[pb], func=AF.Silu),
                     reads=[pgn[pb]], writes=[N(f"sgl{pb}")])
                P.op("dve", lambda e, pb=pb, b=b, fc=fc: e.tensor_tensor(out=hT[b][:, fc, :], in0=pu[pb], in1=sgl[pb][:],
                                                                       op=ALU.mult),
                     reads=[pun[pb], N(f"sgl{pb}")], writes=[N(f"hT{b}_{fc}")])

            def down(ex=ex, tg=tg, b=b, s=s, wd_s=wd_s):
                for tt in range(4):
                    t = tg * 4 + tt
                    for hh in range(2):
                        def md(e, tt=tt, hh=hh):
                            for fc in range(4):
                                ins = e.matmul(out=pd[:, hh, :], lhsT=hT[b][:, fc, tt * 128:(tt + 1) * 128],
                                               rhs=wd_s[:, fc, hh * 512:(hh + 1) * 512], start=(fc == 0), stop=(fc == 3))
                            return ins
                        P.op("pe", md, reads=[SL[s]] + [N(f"hT{b}_{fc}") for fc in range(4)], writes=[N(f"pd{hh}")])
                        P.op("dve", lambda e, t=t, hh=hh: e.scalar_tensor_tensor(
                            out=h[:, t, hh * 512:(hh + 1) * 512], in0=pd[:, hh, :], scalar=comb[:, t, ex:ex + 1],
                            in1=h[:, t, hh * 512:(hh + 1) * 512], op0=ALU.mult, op1=ALU.add),
                            reads=[N(f"pd{hh}"), N(f"comb{t}"), N(f"h{t}")], writes=[N(f"h{t}")])
            if pend is not None:
                pend()
            pend = down
            it += 1
    if pend is not None:
        pend()

    if not final:
        for t in range(NTILE):
            P.dma("sp", lambda e, t=t: e.dma_start(out=D["h_out"][t * 128:(t + 1) * 128, :], in_=h[:, t, :]),
                  reads=[N(f"h{t}")], writes=["d:" + N("h_out")], lane="S:" + N(f"h{t % 4}"))
        if "hn_out" in D:
            for t in range(NTILE):
                P.op("act", lambda e, t=t: e.activation(out=junk[:], in_=h[:, t, :], func=AF.Square,
                                                       accum_out=st[:, 0, t:t + 1]),
                     reads=[N(f"h{t}")], writes=[N(f"nss{t}")])
            rstd_ops(P, st[:, 0, :], st[:, 1, :], st[:, 2, :], 1024, [N(f"nss{t}") for t in range(NTILE)], N("nrstd"))
            for t in range(NTILE):
                s2 = t % 2
                P.op("act", lambda e, t=t, s2=s2: e.activation(out=yt[s2][:, 0:1024], in_=h[:, t, :], func=AF.Copy,
                                                              scale=st[:, 2, t:t + 1]),
                     reads=[N(f"h{t}"), N("nrstd")], writes=[N(f"yt{s2}")])
                P.dma("sp", lambda e, t=t, s2=s2: e.dma_start(out=D["hn_out"][t * 128:(t + 1) * 128, :],
                                                             in_=yt[s2][:, 0:1024]),
                      reads=[N(f"yt{s2}")], writes=["d:" + N(f"hn{t // 4}")], lane="S:" + N(f"yt{s2}"))
    else:
        for t in range(NTILE):
            P.op("act", lambda e, t=t: e.activation(out=junk[:], in_=h[:, t, :], func=AF.Square, accum_out=st[:, 0, t:t + 1]),
                 reads=[N(f"h{t}")], writes=[N(f"fss{t}")])
        for t in range(NTILE):
            rstd_ops(P, st[:, 0, t:t + 1], st[:, 1, t:t + 1], st[:, 2, t:t + 1], 1024, [N(f"fss{t}")], N(f"frstd{t}"))
        for t in range(NTILE):
            s = t % 2
            P.op("dve", lambda e, t=t, s=s: e.scalar_tensor_tensor(out=ob[s][:], in0=h[:, t, :], scalar=st[:, 2, t:t + 1],
                                                                  in1=ngain[:], op0=ALU.mult, op1=ALU.mult),
                 reads=[N(f"h{t}"), N(f"frstd{t}"), N("ngain")], writes=[N(f"ob{s}")])
            P.dma("sp", lambda e, t=t, s=s: e.dma_start(out=D["h_out"][t * 128:(t + 1) * 128, :], in_=ob[s][:]),
                  reads=[N(f"ob{s}")], writes=["d:" + N("h_out")], lane="S:" + N(f"ob{s}"))


def build_ffn(F, final, NTOK=2048, NE=16, stage=3):
    nc = bass.Bass("TRN2", target_bir_lowering=False)
    P = Prog(nc)
    D = {}

    def inp(name, shape, dt=F32):
        D[name] = nc.dram_tensor(name, list(shape), dt, kind="ExternalInput").ap()
    inp("xs", [NTOK, 1024]); inp("ys", [NTOK, F], BF16); inp("wo", [F, 1024]); inp("fgain", [128, 1024])
    inp("wr", [1024, 20]); inp("rbias", [128, 20]); inp("wg", [NE, 1024, 512]); inp("wu", [NE, 1024, 512])
    inp("wd", [NE, 512, 1024]); inp("ident", [128, 128], BF16); inp("identf", [128, 128])
    if final:
        inp("ngain", [128, 1024])
    D["h_out"] = nc.dram_tensor("h_out", [NTOK, 1024], F32, kind="ExternalOutput").ap()
    phase_ffn(P, nc, D, F, final, "B_", NTOK, NE, stage)
    finish(P, ["d:h_out"])
    P.build()
    return nc, P


def ffn_maps(xs_list, ys_list, wo, fgain, rgw, rgb, rew, reb, wg, wu, wd, ngain=None):
    ident = np.eye(128, dtype=np.float32).astype(ml_dtypes.bfloat16)
    identf = np.eye(128, dtype=np.float32)
    wr = np.ascontiguousarray(np.concatenate([rgw] + [rew[g] for g in range(4)], axis=1))
    rb = np.concatenate([rgb, reb.reshape(-1)])[None, :]
    common = dict(wo=np.ascontiguousarray(wo), fgain=np.ascontiguousarray(np.broadcast_to(fgain[None, :], (128, 1024))),
                  wr=wr, rbias=np.ascontiguousarray(np.broadcast_to(rb, (128, 20))),
                  wg=np.ascontiguousarray(wg.reshape(16, 1024, 512)), wu=np.ascontiguousarray(wu.reshape(16, 1024, 512)),
                  wd=np.ascontiguousarray(wd.reshape(16, 512, 1024)), ident=ident, identf=identf)
    if ngain is not None:
        common["ngain"] = np.ascontiguousarray(np.broadcast_to(ngain[None, :], (128, 1024)))
    return [dict(common, xs=np.ascontiguousarray(xs_list[c]), ys=np.ascontiguousarray(ys_list[c])) for c in range(8)]


def phase_moba(P, nc, D, NT=8192):
    C = Ctx(nc, P, "C_")
    N = lambda s: "C_" + s
    NG = NT // 512
    NQT = NT // 128
    NB = NT // 256
    ident = C.sb("ident", [128, 128], BF16)
    cmask = C.sb("cmask", [128, 2, 256], BF16)
    gq = C.sb("gq", [128, 8], F32)
    gkv = C.sb("gkv", [128, 8], F32)
    wst = C.sb("wst", [128, 8, 256], F32)
    wq_bf = C.sb("wq_bf", [128, 8, 256], BF16)
    wk_bf = C.sb("wk_bf", [128, 8, 256], BF16)
    wv_bf = C.sb("wv_bf", [128, 8, 256], BF16)
    wq_rot = C.sb("wq_rot", [128, 8, 2, 32], BF16)
    wk_rot = C.sb("wk_rot", [128, 8, 2, 32], BF16)
    QT = [C.sb(f"QT{i}", [128, NT], BF16) for i in range(2)]
    KT = [C.sb(f"KT{i}", [128, NT], BF16) for i in range(2)]
    Vext = C.sb("Vext", [128, 2, NQT, 130], BF16)
    Msel = C.sb("Msel", [128, 2, NQT, 32], F32)
    km32 = C.sb("km32", [128, 2, 32], F32)
    kmT = C.sb("kmT", [128, 2, 32], BF16)
    ht = [C.sb(f"ht{i}", [128, 1024], F32) for i in range(2)]
    junk = C.sb("junk", [128, 1024], BF16)
    hn = [C.sb(f"hn{i}", [128, 1024], BF16) for i in range(2)]
    hnT = [C.sb(f"hnT{i}", [128, 8, 512], BF16) for i in range(2)]
    cosg = [C.sb(f"cos{i}", [32, 512], F32) for i in range(2)]
    sing = [C.sb(f"sin{i}", [32, 512], F32) for i in range(2)]
    ta = C.sb("ta", [32, 512], F32)
    tb = C.sb("tb", [32, 512], F32)
    st = C.sb("st", [128, 3, 4], F32)
    gt = C.sb("gt", [128, 32], F32)
    mx = C.sb("mx", [128, 8], F32)
    PT = [C.sb(f"PT{i}", [128, 2, 256], BF16) for i in range(3)]
    acc = [C.sb(f"acc{i}", [128, 130], F32) for i in range(2)]
    rec = C.sb("rec", [128, 2], F32)
    ob = [C.sb(f"ob{i}", [128, 128], BF16) for i in range(4)]

    psT = C.ps("psT", [128, 8, 128], BF16)
    pm = C.ps("pm", [128, 512])
    prot = C.ps("prot", [128, 512])
    pv = C.ps("pv", [128, 512])
    pS = [C.ps(f"pS{i}", [128, 2, 256]) for i in range(2)]
    pO = [C.ps(f"pO{i}", [128, 512]) for i in range(2)]
    P.psum |= {N(x) for x in ("psT", "pm", "prot", "pv", "pS0", "pS1", "pO0", "pO1")}

    def ld(dst, src, name, eng="sp", lane=None):
        P.dma(eng, lambda e: e.dma_start(out=dst, in_=src), writes=[name], lane=lane or ("L:" + name))

    ld(ident[:], D["ident"], N("ident"), lane="G:const")
    ld(cmask[:], D["cmask"], N("cmask"), lane="G:const")
    ld(gq[:], D["gq"], N("gq"), lane="G:const")
    ld(gkv[:], D["gkv"], N("gkv"), lane="G:const")
    qscale = float(128 ** -0.5)
    for (wname, wdst, g, sc) in (("wq", wq_bf, gq, qscale), ("wk", wk_bf, gkv, 1.0), ("wv", wv_bf, gkv, 1.0)):
        ld(wst[:], D[wname].rearrange("(kc p) f -> p kc f", p=128), N("wst"))
        for kc in range(8):
            P.op("dve", lambda e, kc=kc, wdst=wdst, g=g, sc=sc: e.tensor_scalar(
                out=wdst[:, kc, :], in0=wst[:, kc, :], scalar1=g[:, kc:kc + 1], scalar2=sc, op0=ALU.mult, op1=ALU.mult),
                reads=[N("wst"), N("gq"), N("gkv")], writes=[N(wname + "_bf")])
    for (wsrc, wrot, nm) in ((wq_bf, wq_rot, "wq"), (wk_bf, wk_rot, "wk")):
        for hh in range(2):
            P.op("dve", lambda e, wsrc=wsrc, wrot=wrot, hh=hh: e.tensor_scalar(
                out=wrot[:, :, hh, 0:16], in0=wsrc[:, :, hh * 128 + 16:hh * 128 + 32], scalar1=-1.0, scalar2=None,
                op0=ALU.mult), reads=[N(nm + "_bf")], writes=[N(nm + "_rot")])
            P.op("dve", lambda e, wsrc=wsrc, wrot=wrot, hh=hh: e.tensor_copy(
                out=wrot[:, :, hh, 16:32], in_=wsrc[:, :, hh * 128:hh * 128 + 16]),
                reads=[N(nm + "_bf")], writes=[N(nm + "_rot")])
    P.op("pool", lambda e: e.memset(Vext[:], 1.0), writes=[N("Vext_init")])
    P.op("pool", lambda e: e.memset(Msel[:], 0.0), writes=[N("Msel_init")])
    P.op("pool", lambda e: e.memset(kmT[:], 0.0), writes=[N("kmT_init")])

    hb = D.get("hb")
    for tg in range(NG):
        s = tg % 2
        t0 = tg * 512
        ld(cosg[s][:], D["cos32"][:, t0:t0 + 512], N(f"cos{s}"))
        ld(sing[s][:], D["sin32"][:, t0:t0 + 512], N(f"sin{s}"))
        for tt in range(4):
            xs = tt % 2
            r0 = t0 + tt * 128
            if "hn_src" in D:
                P.dma("sp", lambda e, xs=xs, r0=r0: e.dma_start(out=hn[xs][:], in_=D["hn_src"](r0)),
                      reads=D["hn_reads"](r0), writes=[N(f"hn{xs}")], lane="L:" + N(f"hn{xs}"))
            else:
                ld(ht[xs][:], hb[r0:r0 + 128, :], N(f"ht{xs}"))
                P.op("act", lambda e, xs=xs, tt=tt: e.activation(out=junk[:], in_=ht[xs][:], func=AF.Square,
                                                                accum_out=st[:, 0, tt:tt + 1]),
                     reads=[N(f"ht{xs}")], writes=[N("ss")])
                rstd_ops(P, st[:, 0, tt:tt + 1], st[:, 1, tt:tt + 1], st[:, 2, tt:tt + 1], 1024, [N("ss")], N("rstd"))
                P.op("act", lambda e, xs=xs, tt=tt: e.activation(out=hn[xs][:], in_=ht[xs][:], func=AF.Copy,
                                                                scale=st[:, 2, tt:tt + 1]),
                     reads=[N(f"ht{xs}"), N("rstd")], writes=[N(f"hn{xs}")])

            def tr(e, xs=xs):
                for kc in range(8):
                    ins = e.transpose(out=psT[:, kc, :], in_=hn[xs][:, kc * 128:(kc + 1) * 128], identity=ident[:])
                return ins
            P.op("pe", tr, reads=[N(f"hn{xs}"), N("ident")], writes=[N("psT")])
            P.op("dve", lambda e, s=s, tt=tt: e.tensor_copy(out=hnT[s][:, :, tt * 128:(tt + 1) * 128], in_=psT[:]),
                 reads=[N("psT")], writes=[N(f"hnT{s}_{tt}")])
        XN = [N(f"hnT{s}_{tt}") for tt in range(4)]
        for hh in range(2):
            for (w_bf, w_rot, dst, nm) in ((wq_bf, wq_rot, QT, "Q"), (wk_bf, wk_rot, KT, "K")):
                wn = "wq" if nm == "Q" else "wk"

                def mm(e, w_bf=w_bf, hh=hh, s=s):
                    for kc in range(8):
                        ins = e.matmul(out=pm[:], lhsT=w_bf[:, kc, hh * 128:(hh + 1) * 128], rhs=hnT[s][:, kc, :],
                                       start=(kc == 0), stop=(kc == 7))
                    return ins

                def mr(e, w_rot=w_rot, hh=hh, s=s):
                    for kc in range(8):
                        ins = e.matmul(out=prot[0:32, :], lhsT=w_rot[:, kc, hh, :], rhs=hnT[s][:, kc, :],
                                       start=(kc == 0), stop=(kc == 7))
                    return ins
                P.op("pe", mm, reads=[N(wn + "_bf")] + XN, writes=[N("pm")])
                P.op("pe", mr, reads=[N(wn + "_rot")] + XN, writes=[N("prot")])
                dname = N(f"{nm}T{hh}_{tg}")
                P.op("act", lambda e, dst=dst, hh=hh, t0=t0: e.activation(out=dst[hh][32:64, t0:t0 + 512],
                                                                         in_=pm[32:64, :], func=AF.Copy),
                     reads=[N("pm")], writes=[dname + "mid"])
                P.op("act", lambda e, dst=dst, hh=hh, t0=t0: e.activation(out=dst[hh][64:128, t0:t0 + 512],
                                                                         in_=pm[64:128, :], func=AF.Copy),
                     reads=[N("pm")], writes=[dname + "hi"])
                P.op("dve", lambda e, s=s: e.tensor_tensor(out=ta[:], in0=pm[0:32, :], in1=cosg[s][:], op=ALU.mult),
                     reads=[N("pm"), N(f"cos{s}")], writes=[N("ta")])
                P.op("dve", lambda e, s=s: e.tensor_tensor(out=tb[:], in0=prot[0:32, :], in1=sing[s][:], op=ALU.mult),
                     reads=[N("prot"), N(f"sin{s}")], writes=[N("tb")])
                P.op("pool", lambda e, dst=dst, hh=hh, t0=t0: e.tensor_tensor(out=dst[hh][0:32, t0:t0 + 512], in0=ta[:],
                                                                             in1=tb[:], op=ALU.add),
                     reads=[N("ta"), N("tb")], writes=[dname + "lo"])
            P.op("dve", lambda e, hh=hh, t0=t0, tg=tg: e.tensor_reduce(
                out=km32[:, hh, 2 * tg:2 * tg + 2], in_=KT[hh][:, t0:t0 + 512].rearrange("p (a b) -> p a b", a=2),
                axis=AX.X, op=ALU.add),
                reads=[N(f"KT{hh}_{tg}hi"), N(f"KT{hh}_{tg}lo")], writes=[N(f"km32_{hh}_{tg}")])
            P.op("act", lambda e, hh=hh, tg=tg: e.activation(out=kmT[:, hh, 2 * tg:2 * tg + 2],
                                                            in_=km32[:, hh, 2 * tg:2 * tg + 2], func=AF.Copy,
                                                            scale=1.0 / 256.0),
                 reads=[N(f"km32_{hh}_{tg}"), N("kmT_init")], writes=[N(f"kmT{hh}_{tg}")])
        for tt in range(4):
            tile = tg * 4 + tt

            def mv(e, tt=tt, s=s):
                for kc in range(8):
                    ins = e.matmul(out=pv[:, 0:256], lhsT=hnT[s][:, kc, tt * 128:(tt + 1) * 128], rhs=wv_bf[:, kc, :],
                                   start=(kc == 0), stop=(kc == 7))
                return ins
            P.op("pe", mv, reads=[N("wv_bf"), XN[tt]], writes=[N("pv")])
            P.op("act", lambda e, tile=tile: e.activation(out=Vext[:, :, tile, 0:128],
                                                         in_=pv[:, 0:256].rearrange("p (a b) -> p a b", a=2), func=AF.Copy),
                 reads=[N("pv"), N("Vext_init")], writes=[N(f"V{tile}")])
    gts = [gt, C.sb("gt1", [128, 32], F32)]
    mxs = [mx, C.sb("mx1", [128, 8], F32)]
    for qt in range(2, NQT):
        for hh in range(2):
            j = qt // 2
            g_, m_, pg_, pgn_ = gts[hh], mxs[hh], pO[hh], N(f"pO{hh}")
            kn = [N(f"kmT{hh}_{tg}") for tg in range((j - 1) // 2 + 1)]
            P.op("pe", lambda e, hh=hh, qt=qt, pg_=pg_: e.matmul(out=pg_[:, 0:32], lhsT=QT[hh][:, qt * 128:(qt + 1) * 128],
                                                                 rhs=kmT[:, hh, :], start=True, stop=True),
                 reads=[N(f"QT{hh}_{qt // 4}hi"), N(f"QT{hh}_{qt // 4}lo"), N("kmT_init")] + kn, writes=[pgn_])
            P.op("dve", lambda e, g_=g_, pg_=pg_: e.tensor_copy(out=g_[:], in_=pg_[:, 0:32]), reads=[pgn_], writes=[N(f"gt{hh}")])
            P.op("dve", lambda e, j=j, g_=g_: e.memset(g_[:, j:32], -1e30), reads=[N(f"gt{hh}")], writes=[N(f"gt{hh}")])
            P.op("dve", lambda e, g_=g_, m_=m_: e.max(out=m_[:], in_=g_[:]), reads=[N(f"gt{hh}")], writes=[N(f"mx{hh}")])
            P.op("dve", lambda e, hh=hh, qt=qt, g_=g_, m_=m_: e.tensor_scalar(out=Msel[:, hh, qt, :], in0=g_[:],
                                                                             scalar1=m_[:, 2:3], scalar2=None, op0=ALU.is_ge),
                 reads=[N(f"gt{hh}"), N(f"mx{hh}"), N("Msel_init")], writes=[N(f"M{hh}_{qt}")])

    pS3 = [pS[0][:], pS[1][:],
           psT[:].bitcast(F32).rearrange("p a b -> p (a b)").rearrange("p (k q) -> p k q", k=2)]
    pSn = [N("pS0"), N("pS1"), N("psT")]
    ND = 3
    pOb = [[pO[0], pm], [pO[1], prot]]
    pOn = [[N("pO0"), N("pm")], [N("pO1"), N("prot")]]
    pairs = []
    for j in range(NB):
        for hh in range(2):
            order = [j] + list(range(j))
            for idx, n in enumerate(order):
                pairs.append((j, hh, n, idx == len(order) - 1))
    oc = [0]

    def emit_s(i):
        j, hh, n, last = pairs[i]
        b = i % ND
        qs = slice(j * 256, (j + 1) * 256)
        qn = [N(f"QT{hh}_{j // 2}hi"), N(f"QT{hh}_{j // 2}lo")]

        def mS(e):
            for kt in range(2):
                k0 = (2 * n + kt) * 128
                ins = e.matmul(out=pS3[b][:, kt, :], lhsT=KT[hh][:, k0:k0 + 128], rhs=QT[hh][:, qs],
                               start=True, stop=True)
            return ins
        P.op("pe", mS, reads=qn + [N(f"KT{hh}_{n // 2}hi"), N(f"KT{hh}_{n // 2}lo")], writes=[pSn[b]])
        P.op("act", lambda e: e.activation(out=PT[b][:], in_=pS3[b], func=AF.Exp),
             reads=[pSn[b]], writes=[N(f"PT{b}")])
        if n == j:
            P.op("pool", lambda e: e.tensor_tensor(out=PT[b][:], in0=PT[b][:], in1=cmask[:], op=ALU.mult),
                 reads=[N(f"PT{b}"), N("cmask")], writes=[N(f"PT{b}")])

    def emit_o(i):
        j, hh, n, last = pairs[i]
        b = i % ND
        own = (n == j)
        for qi in range(2):
            po_, pn_ = pOb[qi][i % 2], pOn[qi][i % 2]

            def mO(e, qi=qi, po_=po_):
                for kt in range(2):
                    ins = e.matmul(out=po_[:, 0:129], lhsT=PT[b][:, kt, qi * 128:(qi + 1) * 128],
                                   rhs=Vext[:, hh, 2 * n + kt, 0:129], start=(kt == 0), stop=(kt == 1))
                return ins
            P.op("pe", mO, reads=[N(f"PT{b}"), N(f"V{2 * n}"), N(f"V{2 * n + 1}")], writes=[pn_])
            if own:
                P.op("dve", lambda e, qi=qi, po_=po_: e.tensor_copy(out=acc[qi][:, 0:129], in_=po_[:, 0:129]),
                     reads=[pn_], writes=[N(f"acc{qi}")])
            else:
                P.op("dve", lambda e, qi=qi, po_=po_: e.scalar_tensor_tensor(
                    out=acc[qi][:, 0:129], in0=po_[:, 0:129], scalar=Msel[:, hh, 2 * j + qi, n:n + 1],
                    in1=acc[qi][:, 0:129], op0=ALU.mult, op1=ALU.add),
                    reads=[pn_, N(f"M{hh}_{2 * j + qi}"), N(f"acc{qi}")], writes=[N(f"acc{qi}")])
        if last:
            for qi in range(2):
                o_ = oc[0] % 4
                oc[0] += 1
                qt = 2 * j + qi
                P.op("dve", lambda e, qi=qi: e.reciprocal(out=rec[:, qi:qi + 1], in_=acc[qi][:, 128:129]),
                     reads=[N(f"acc{qi}")], writes=[N(f"rec{qi}")])
                P.op("dve", lambda e, qi=qi, o_=o_: e.tensor_scalar(out=ob[o_][:], in0=acc[qi][:, 0:128],
                                                                    scalar1=rec[:, qi:qi + 1], scalar2=None, op0=ALU.mult),
                     reads=[N(f"acc{qi}"), N(f"rec{qi}")], writes=[N(f"ob{o_}")])
                P.dma("sp", lambda e, qt=qt, o_=o_: e.dma_start(
                    out=D["o_out"][qt * 128:(qt + 1) * 128, hh * 128:(hh + 1) * 128], in_=ob[o_][:]),
                    reads=[N(f"ob{o_}")], writes=[f"d:C_o{j // 4}"], lane="S:" + N(f"ob{o_}"))
            if hh == 1 and "after_block" in D:
                D["after_block"](j)

    for i in range(ND - 1):
        emit_s(i)
    for i in range(len(pairs)):
        if i + ND - 1 < len(pairs):
            emit_s(i + ND - 1)
        emit_o(i)


def moba_tables(S=8192):
    inv = (1.0 / (np.float32(500000.0) ** (np.arange(16, dtype=np.float32) / np.float32(16)))).astype(np.float32)
    ang = (np.arange(S, dtype=np.float32)[None, :] * inv[:, None]).astype(np.float32)
    cos = np.cos(ang).astype(np.float32)
    sin = np.sin(ang).astype(np.float32)
    k = np.arange(128)[:, None, None] + 128 * np.arange(2)[None, :, None]
    q = np.arange(256)[None, None, :]
    cmask = (k <= q).astype(np.float32).astype(ml_dtypes.bfloat16)
    return (np.ascontiguousarray(np.concatenate([cos, cos], 0)), np.ascontiguousarray(np.concatenate([sin, sin], 0)),
            np.ascontiguousarray(cmask))


def build_moba(NT=8192):
    nc = bass.Bass("TRN2", target_bir_lowering=False)
    P = Prog(nc)
    D = {}

    def inp(name, shape, dt=F32):
        D[name] = nc.dram_tensor(name, list(shape), dt, kind="ExternalInput").ap()
    inp("hb", [NT, 1024]); inp("wq", [1024, 256]); inp("wk", [1024, 256]); inp("wv", [1024, 256])
    inp("gq", [128, 8]); inp("gkv", [128, 8]); inp("cos32", [32, NT]); inp("sin32", [32, NT])
    inp("cmask", [128, 2, 256], BF16); inp("ident", [128, 128], BF16)
    D["o_out"] = nc.dram_tensor("o_out", [NT, 256], BF16, kind="ExternalOutput").ap()
    phase_moba(P, nc, D, NT)
    finish(P)
    P.build()
    return nc, P


def moba_maps(h_list, attn_norm, kv_norm, w_q, w_kv, NT=8192):
    cos32, sin32, cmask = moba_tables()
    ident = np.eye(128, dtype=np.float32).astype(ml_dtypes.bfloat16)
    gq = np.ascontiguousarray(attn_norm.reshape(8, 128).T)
    gkv = np.ascontiguousarray(kv_norm.reshape(8, 128).T)
    maps = []
    for c in range(8):
        b, p = c // 4, c % 4
        cs = slice(p * 256, (p + 1) * 256)
        maps.append(dict(hb=np.ascontiguousarray(h_list[b][:NT]), wq=np.ascontiguousarray(w_q[:, cs]),
                         wk=np.ascontiguousarray(w_kv[:, cs]), wv=np.ascontiguousarray(w_kv[:, 1024 + p * 256:1024 + (p + 1) * 256]),
                         gq=gq, gkv=gkv, cos32=np.ascontiguousarray(cos32[:, :NT]), sin32=np.ascontiguousarray(sin32[:, :NT]),
                         cmask=cmask, ident=ident))
    return maps


def finish(P, names=None):
    P.finish()


def ret_tables(S=8192):
    half = 128
    inv = (1.0 / (np.float32(10000.0) ** (np.arange(half, dtype=np.float32) / np.float32(half)))).astype(np.float32)
    ang = (np.arange(S, dtype=np.float32)[None, :] * inv[:, None]).astype(np.float32)
    return np.cos(ang).astype(np.float32), np.sin(ang).astype(np.float32)


def ret_decay(h):
    Cn = 128
    lg = np.log1p(-np.float32(2.0) ** np.float32(-5.0 - h)).astype(np.float32)
    idx = np.arange(Cn, dtype=np.float32)
    diff = idx[:, None] - idx[None, :]
    dm = np.where(diff >= 0, np.exp(lg * np.maximum(diff, 0.0)), 0.0).astype(np.float32)
    dmaskT = (dm.T * np.float32(256 ** -0.5)).astype(np.float32)
    qdec = np.exp(lg * (idx + 1.0)).astype(np.float32)
    kdec = (np.exp(lg * (Cn - 1.0 - idx)) * np.float32(256 ** -0.5)).astype(np.float32)
    cdec = np.exp(lg * np.float32(Cn)).astype(np.float32)
    qdec4 = np.ascontiguousarray(np.broadcast_to(np.tile(qdec, 4)[None, :], (128, 512))).astype(np.float32)
    return dict(dmask=np.ascontiguousarray(dmaskT), qdec4=qdec4,
                kdec=np.ascontiguousarray(kdec[:, None]),
                cdec=np.full((128, 1), cdec, np.float32))


def build_ret(NT=8192):
    nc = bass.Bass("TRN2", target_bir_lowering=False)
    P = Prog(nc)
    D = {}

    def inp(name, shape, dt=F32):
        D[name] = nc.dram_tensor(name, list(shape), dt, kind="ExternalInput").ap()
    inp("xb", [NT, 1024]); inp("w", [1024, 1536]); inp("gain", [128, 8])
    inp("cosT", [128, NT]); inp("sinT", [128, NT]); inp("dmask", [128, 128]); inp("qdec4", [128, 512])
    inp("kdec", [128, 1]); inp("cdec", [128, 1]); inp("ident", [128, 128], BF16)
    D["y_out"] = nc.dram_tensor("y_out", [NT, 512], BF16, kind="ExternalOutput").ap()
    phase_ret(P, nc, D, NT)
    finish(P, ["d:y_out"])
    P.build()
    return nc, P


def run_ret(x, ret_norm, ret_w_in):
    nc, P = build_ret()
    cosT, sinT = ret_tables()
    ident = np.eye(128, dtype=np.float32).astype(ml_dtypes.bfloat16)
    gain = np.ascontiguousarray(ret_norm[0].reshape(8, 128).T)
    maps = []
    for c in range(8):
        b, h = c // 4, c % 4
        w = ret_w_in[0]
        wh = np.concatenate([w[:, h * 256:(h + 1) * 256], w[:, 1024 + h * 256:1024 + (h + 1) * 256],
                             w[:, 2048 + h * 512:2048 + (h + 1) * 512],
                             w[:, 4096 + h * 512:4096 + (h + 1) * 512]], axis=1)
        m = dict(xb=np.ascontiguousarray(x[b]), w=np.ascontiguousarray(wh), gain=gain, cosT=cosT, sinT=sinT,
                 ident=ident)
        m.update(ret_decay(h))
        maps.append(m)
    res = run_bass_kernel_spmd(nc, maps, core_ids=list(range(8)))
    return [r["y_out"] for r in res.results]


GROUPS = [[0, 1, 2, 3], [4, 5, 6, 7]]


def build_fused(upto=4):
    nc = bass.Bass("TRN2", target_bir_lowering=False)
    P = Prog(nc)
    P.want_pid = True

    def inp(name, shape, dt=F32):
        return nc.dram_tensor("in_" + name, list(shape), dt, kind="ExternalInput").ap()

    def scr(name, shape, dt):
        return nc.dram_tensor(name, list(shape), dt).ap()

    def state():
        return (nc.sbuf_base, nc.sbuf_top, nc.psum_base, nc.psum_top)

    def restore(st):
        nc.sbuf_base, nc.sbuf_top, nc.psum_base, nc.psum_top = st

    def allgather(src, dst, reads, wname, lane):
        P.cc(lambda e: e.collective_compute("AllGather", ALU.bypass, replica_groups=GROUPS, ins=[src], outs=[dst]),
             reads=reads, writes=[wname], lane=lane)

    ident = inp("ident", [128, 128], BF16)
    identf = inp("identf", [128, 128])
    y_dram = scr("y_dram", [8192, 512], BF16)
    yall = scr("yall", [8 * 4 * 1024 + 1024, 512], BF16)
    if upto == 2:
        h1_dram = nc.dram_tensor("h1_dram", [2048, 1024], F32, kind="ExternalOutput").ap()
    else:
        h1_dram = scr("h1_dram", [2048, 1024], F32)
    hn_dram = scr("hn_dram", [2048, 1024], BF16)
    hnall = scr("hnall", [4 * 4 * 512, 1024], BF16)
    o_dram = scr("o_dram", [8192, 256], BF16)
    oall = scr("oall", [8 * 4 * 1024 + 1024, 256], BF16)
    out = nc.dram_tensor("out", [2048, 1024], F32, kind="ExternalOutput").ap()
    st0 = state()

    def ag_y(i):
        allgather(y_dram[i * 1024:(i + 1) * 1024, :], yall[i * 4096:(i + 1) * 4096, :], [f"d:A_y{i}"],
                  f"d:yall{i}", f"CC:y{i}")

    def after_group(tg):
        if tg >= 2 and tg % 2 == 0:
            ag_y(tg // 2 - 1)
    DA = dict(xb=inp("xb", [8192, 1024]), w=inp("A_w", [1024, 1536]), gain=inp("A_gain", [128, 8]),
              cosT=inp("A_cosT", [128, 8192]), sinT=inp("A_sinT", [128, 8192]), dmask=inp("A_dmask", [128, 128]),
              qdec4=inp("A_qdec4", [128, 512]), kdec=inp("A_kdec", [128, 1]), cdec=inp("A_cdec", [128, 1]),
              ident=ident, y_out=y_dram, after_group=after_group)
    phase_ret(P, nc, DA)
    ag_y(7)
    restore(st0)
    P.barrier()
    if upto == 1:
        finish(P)
        P.build()
        return nc, P

    def ys_B(e, t, dst):
        p, w = t // 4, t % 4
        src = yall[p * 8192 + w * 128:, :][bass.ds(P.pid["sp"], 4096), :].rearrange("(r s) f -> s r f", r=4)[0:128, :, :]
        return e.dma_start(out=dst[:, 0:2048].rearrange("p (h f) -> p h f", h=4), in_=src)

    def ys_D(e, t, dst):
        p, w = t // 4, t % 4
        src = oall[p * 8192 + w * 128:, :][bass.ds(P.pid["pool"], 4096), :].rearrange("(r s) f -> s r f", r=4)[0:128, :, :]
        return e.dma_start(out=dst[:, 0:1024].rearrange("p (h f) -> p h f", h=4), in_=src)

    def ffn_inputs(pfx, F, final):
        d = dict(wo=inp(pfx + "wo", [F, 1024]), fgain=inp(pfx + "fgain", [128, 1024]), wr=inp(pfx + "wr", [1024, 20]),
                 rbias=inp(pfx + "rbias", [128, 20]), wg=inp(pfx + "wg", [16, 1024, 512]),
                 wu=inp(pfx + "wu", [16, 1024, 512]), wd=inp(pfx + "wd", [16, 512, 1024]), ident=ident, identf=identf)
        if final:
            d["ngain"] = inp(pfx + "ngain", [128, 1024])
        return d

    DB = ffn_inputs("B_", 2048, False)
    DB.update(xs=inp("xs", [2048, 1024]), ys_fn=ys_B, ys_reads=lambda t: [f"d:yall{2 * (t // 4)}", f"d:yall{2 * (t // 4) + 1}"],
              h_out=h1_dram, hn_out=hn_dram)
    phase_ffn(P, nc, DB, 2048, False, "B_")
    for i in range(4):
        allgather(hn_dram[i * 512:(i + 1) * 512, :], hnall[i * 2048:(i + 1) * 2048, :], [f"d:B_hn{i}"],
                  f"d:hnall{i}", f"CC:hn{i}")
    restore(st0)
    P.barrier()
    if upto == 2:
        finish(P)
        P.build()
        return nc, P

    def hn_src(r0):
        return hnall[r0:r0 + 128, :]

    def after_block(j):
        if j % 4 == 3:
            i = j // 4
            allgather(o_dram[i * 1024:(i + 1) * 1024, :], oall[i * 4096:(i + 1) * 4096, :], [f"d:C_o{i}"],
                      f"d:oall{i}", f"CC:o{i}")
    DC = dict(wq=inp("C_wq", [1024, 256]), wk=inp("C_wk", [1024, 256]), wv=inp("C_wv", [1024, 256]),
              gq=inp("C_gq", [128, 8]), gkv=inp("C_gkv", [128, 8]), cos32=inp("C_cos32", [32, 8192]),
              sin32=inp("C_sin32", [32, 8192]), cmask=inp("C_cmask", [128, 2, 256], BF16), ident=ident,
              hn_src=hn_src, hn_reads=lambda r0: [f"d:hnall{r0 // 2048}"], o_out=o_dram,
              after_block=after_block)
    phase_moba(P, nc, DC)
    restore(st0)
    P.barrier()
    if upto == 3:
        finish(P)
        P.build()
        return nc, P

    DD = ffn_inputs("D_", 1024, True)
    DD.update(xs=h1_dram, xs_reads=["d:B_h_out"], ys_fn=ys_D, ys_eng="pool", ys_reads=lambda t: [f"d:oall{2 * (t // 4)}", f"d:oall{2 * (t // 4) + 1}"],
              h_out=out)
    phase_ffn(P, nc, DD, 1024, True, "D_")
    finish(P)
    P.build()
    return nc, P


def fused_maps(x, ret_norm, ret_w_in, ret_w_out, kv_norm, w_kv, attn_norm, w_q, w_o, ffn_norm,
               router_group_w, router_group_b, router_expert_w, router_expert_b,
               expert_w_gate, expert_w_up, expert_w_down, final_norm):
    cosT, sinT = ret_tables()
    cos32, sin32, cmask = moba_tables()
    ident = np.eye(128, dtype=np.float32).astype(ml_dtypes.bfloat16)
    identf = np.eye(128, dtype=np.float32)
    bc = lambda v, n: np.ascontiguousarray(np.broadcast_to(v[None, :], (128, n)))
    common = dict(ident=ident, identf=identf, A_gain=np.ascontiguousarray(ret_norm[0].reshape(8, 128).T),
                  A_cosT=cosT, A_sinT=sinT, C_cos32=cos32, C_sin32=sin32, C_cmask=cmask,
                  C_gq=np.ascontiguousarray(attn_norm[0].reshape(8, 128).T),
                  C_gkv=np.ascontiguousarray(kv_norm.reshape(8, 128).T))
    for pfx, l, wo in (("B_", 0, ret_w_out[0]), ("D_", 1, w_o[0])):
        rb = np.concatenate([router_group_b[l], router_expert_b[l].reshape(-1)])
        common.update({
            pfx + "wo": np.ascontiguousarray(wo), pfx + "fgain": bc(ffn_norm[l], 1024),
            pfx + "wr": np.ascontiguousarray(np.concatenate([router_group_w[l]] + [router_expert_w[l][g] for g in range(4)], axis=1)),
            pfx + "rbias": bc(rb, 20),
            pfx + "wg": np.ascontiguousarray(expert_w_gate[l].reshape(16, 1024, 512)),
            pfx + "wu": np.ascontiguousarray(expert_w_up[l].reshape(16, 1024, 512)),
            pfx + "wd": np.ascontiguousarray(expert_w_down[l].reshape(16, 512, 1024))})
    common["D_ngain"] = bc(final_norm, 1024)
    dec = [ret_decay(h) for h in range(4)]
    maps = []
    w = ret_w_in[0]
    for c in range(8):
        b, r = c // 4, c % 4
        wh = np.concatenate([w[:, r * 256:(r + 1) * 256], w[:, 1024 + r * 256:1024 + (r + 1) * 256],
                             w[:, 2048 + r * 512:2048 + (r + 1) * 512], w[:, 4096 + r * 512:4096 + (r + 1) * 512]], axis=1)
        cs = slice(r * 256, (r + 1) * 256)
        m = dict(common, xb=np.ascontiguousarray(x[b]), xs=np.ascontiguousarray(x[b].reshape(4, 4, 512, 1024)[:, r].reshape(2048, 1024)),
                 A_w=np.ascontiguousarray(wh), A_dmask=dec[r]["dmask"], A_qdec4=dec[r]["qdec4"], A_kdec=dec[r]["kdec"],
                 A_cdec=dec[r]["cdec"], C_wq=np.ascontiguousarray(w_q[0][:, cs]), C_wk=np.ascontiguousarray(w_kv[:, cs]),
                 C_wv=np.ascontiguousarray(w_kv[:, 1024 + r * 256:1024 + (r + 1) * 256]))
        maps.append({"in_" + k: v for k, v in m.items()})
    return maps


_CACHE = {}


def kernel(**inputs):
    a = {k: np.asarray(v, dtype=np.float32) for k, v in inputs.items()}
    if "nc" not in _CACHE:
        _CACHE["nc"] = build_fused()[0]
    maps = fused_maps(**a)
    res = run_bass_kernel_spmd(_CACHE["nc"], maps, core_ids=list(range(8)))
    out = np.empty((2, 4, 4, 512, 1024), np.float32)
    for b in range(2):
        for q in range(4):
            out[b, :, q] = np.asarray(res.results[b * 4 + q]["out"]).reshape(4, 512, 1024)
    return out.reshape(2, 8192, 1024)
```

```python
import numpy as np
import ml_dtypes
import concourse.bass as bass
import concourse.mybir as mybir
from concourse.bass_utils import run_bass_kernel_spmd

F32 = mybir.dt.float32
BF16 = mybir.dt.bfloat16
AF = mybir.ActivationFunctionType
ALU = mybir.AluOpType
AX = mybir.AxisListType

EPS = 1e-6
ENGS = ("pe", "act", "dve", "pool", "sp")


class _First:
    def __init__(self, eng, w):
        self._e, self._w = eng, w

    def __getattr__(self, name):
        real = getattr(self._e, name)
        if not callable(real):
            return real

        def call(*a, **k):
            ins = real(*a, **k)
            if self._w is not None and hasattr(ins, "_wait_ge"):
                ins._wait_ge(*self._w)
                self._w = None
            return ins
        return call


class _Dummy:
    def then_inc(self, *a, **k):
        return self

    def _wait_ge(self, *a, **k):
        return self


class _Rec:
    def __init__(self):
        self.calls = []

    def __getattr__(self, name):
        def call(*a, **k):
            self.calls.append((name, a, k))
            return _Dummy()
        return call


class Prog:
    def __init__(self, nc):
        self.nc = nc
        self.ops = []
        self.want_pid = False
        self.reorder = True
        self.debug = None
        self.fifo = FIFO_Q
        self.psum = set()

    def op(self, eng, fn, reads=(), writes=()):
        self.ops.append(dict(eng=eng, fn=fn, r=tuple(reads), w=tuple(writes), dma=None))

    def dma(self, eng, fn, reads=(), writes=(), lane=None):
        assert lane is not None
        self.ops.append(dict(eng=eng, fn=fn, r=tuple(reads), w=tuple(writes), dma=lane))

    def cc(self, fn, reads=(), writes=(), lane=None):
        self.ops.append(dict(eng="pool", fn=fn, r=tuple(reads), w=tuple(writes), dma=lane, cc=True))

    def barrier(self):
        self.ops.append(dict(eng="barrier", fn=None, r=(), w=(), dma=None))

    def finish(self):
        self.ops.append(dict(eng="sp", fn=lambda e: e.nop(), r=(), w=(), dma=None, fin=True))

    def _cost(self, o):
        E = o["eng"]
        rec = _Rec()
        try:
            o["fn"](rec)
        except Exception:
            return (50.0, 2000.0) if o["dma"] is not None else (500.0, 0.0)
        busy, lat = 0.0, 0.0
        for name, a, k in rec.calls:
            out = k.get("out", a[0] if a else None)
            if name == "dma_start":
                src = k.get("in_")
                nb = 1
                for d_ in out.shape:
                    nb *= d_
                nb *= max(mybir.dt.size(out.dtype), mybir.dt.size(src.dtype))
                busy += 50.0
                lat += nb / 250.0
            elif name in ("matmul", "transpose"):
                src = k.get("lhsT") if name == "matmul" else k.get("in_")
                f = 4.0 if src.dtype == F32 else 1.0
                busy += (16.0 + 0.46 * out.free_size()) * f
            elif name == "collective_compute":
                busy += 100.0
            elif out is None or not hasattr(out, "free_size"):
                busy += 50.0
            else:
                nf = out.free_size()
                busy += {"act": 200.0 + 1.05 * nf, "dve": 100.0 + 1.4 * nf, "pool": 150.0 + 4.6 * nf}.get(E, 100.0 + nf)
        return busy, lat

    def _sched_seg(self, seg):
        n = len(seg)
        if n < 3:
            return seg
        deps = [set() for _ in range(n)]
        last_w, readers = {}, {}
        for i, o in enumerate(seg):
            d = deps[i]
            for b in o["r"]:
                if b in last_w:
                    d.add(last_w[b])
                if b in self.psum:
                    for r in readers.get(b, ()):
                        if seg[r]["eng"] != o["eng"]:
                            d.add(r)
            for b in o["w"]:
                if b in last_w:
                    d.add(last_w[b])
                d.update(readers.get(b, ()))
            d.discard(i)
            for b in o["w"]:
                last_w[b] = i
                readers[b] = []
            for b in o["r"]:
                readers.setdefault(b, []).append(i)
        succ = [[] for _ in range(n)]
        indeg = [len(d) for d in deps]
        for i, d in enumerate(deps):
            for x in d:
                succ[x].append(i)
        cost = [self._cost(o) for o in seg]
        free_at = dict.fromkeys(ENGS, 0.0)
        dma_free = 0.0
        ready = [0.0] * n
        ext = getattr(self, "_ext", {})
        for i, o in enumerate(seg):
            for b in o["r"]:
                if b in ext:
                    ready[i] = max(ready[i], ext[b])
        avail = {E: [] for E in ENGS}
        for i in range(n):
            if indeg[i] == 0:
                avail[seg[i]["eng"]].append(i)
        order = []
        SLACK = 150.0
        while len(order) < n:
            best = None
            for E in ENGS:
                av = avail[E]
                if not av:
                    continue
                t0 = max(free_at[E], min(ready[i] for i in av))
                if self.fifo:
                    cand = min((i for i in av if ready[i] <= t0 + SLACK), key=lambda i: (int(ready[i] / self.fifo), i))
                else:
                    cand = min(i for i in av if ready[i] <= t0 + SLACK)
                st = max(free_at[E], ready[cand])
                if best is None or (st, cand) < best[:2]:
                    best = (st, cand, E)
            st, i, E = best
            avail[E].remove(i)
            busy, lat = cost[i]
            free_at[E] = st + busy
            if seg[i].get("cc"):
                fin = st + 150000.0
            elif seg[i]["dma"] is not None:
                dma_free = max(st + busy, dma_free) + lat
                fin = dma_free + 2000.0
            else:
                fin = st + busy + 100.0
            order.append(i)
            if self.debug is not None:
                self.debug.append((seg[i], st, fin, E))
            for j in succ[i]:
                if fin > ready[j]:
                    ready[j] = fin
                indeg[j] -= 1
                if indeg[j] == 0:
                    avail[seg[j]["eng"]].append(j)
        self.sim_time = getattr(self, "sim_time", 0.0) + max(max(free_at.values()), dma_free)
        self._ext = {}
        k = 0
        for i in order:
            if seg[i].get("cc"):
                k += 1
        j = 0
        for i in order:
            if seg[i].get("cc"):
                j += 1
                late = max(0.0, 60000.0 * (j - (k - 4))) if j > k - 4 else 0.0
                for b in seg[i]["w"]:
                    self._ext[b] = late
        return [seg[i] for i in order]

    def _schedule(self, ops):
        out, seg = [], []
        for o in ops:
            if o["eng"] == "barrier" or o.get("fin"):
                out += self._sched_seg(seg)
                seg = []
                out.append(o)
            else:
                seg.append(o)
        return out + self._sched_seg(seg)

    def build(self):
        nc = self.nc
        if self.reorder:
            self.ops = self._schedule(self.ops)
        ops = self.ops
        n = len(ops)
        last_w = {}
        readers = {}
        deps = [None] * n
        bar_deps = set()
        last_on_eng = {}
        lanes_last = {}
        first_after_bar = {}
        for i, o in enumerate(ops):
            if o["eng"] == "barrier":
                bar_deps = set(last_on_eng.values()) | {v for k, v in lanes_last.items() if not k.startswith("CC:")}
                first_after_bar = {}
                last_w = {k: v for k, v in last_w.items() if k.startswith("d:")}
                readers = {k: v for k, v in readers.items() if k.startswith("d:")}
                deps[i] = set()
                continue
            raw = set()
            oth = set()
            for b in o["r"]:
                if b in last_w:
                    raw.add(last_w[b])
                if b in self.psum:
                    for r in readers.get(b, ()):
                        if ops[r]["eng"] != o["eng"]:
                            raw.add(r)
            for b in o["w"]:
                if b in last_w:
                    oth.add(last_w[b])
                for r in readers.get(b, ()):
                    oth.add(r)
            E = o["eng"]
            d = set()
            for x in raw | oth:
                if x == i:
                    continue
                ox = ops[x]
                if ox["dma"] is None and o["dma"] is None and ox["eng"] == E:
                    if E == "pe":
                        continue
                    if x not in raw:
                        continue
                d.add(x)
            if o.get("fin"):
                d |= set(last_on_eng.values()) | set(lanes_last.values())
            if bar_deps and E not in first_after_bar:
                d |= bar_deps
                first_after_bar[E] = i
            deps[i] = d
            for b in o["w"]:
                last_w[b] = i
                readers[b] = []
            for b in o["r"]:
                lst = readers.setdefault(b, [])
                if o["dma"] is None:
                    lst[:] = [r for r in lst if not (ops[r]["dma"] is None and ops[r]["eng"] == E)]
                lst.append(i)
            if o["dma"] is None:
                last_on_eng[E] = i
            else:
                lanes_last[o["dma"]] = i

        pos = [0] * n
        eng_count = {e: 0 for e in ENGS}
        lane_count = {}
        seen = {e: {} for e in ENGS}
        snap = [None] * n
        waits = [None] * n
        signals = [False] * n
        for i, o in enumerate(ops):
            if o["eng"] == "barrier":
                continue
            E = o["eng"]
            w = []
            for d in sorted(deps[i], reverse=True):
                od = ops[d]
                key = ("L", od["dma"]) if od["dma"] is not None else ("E", od["eng"])
                need = pos[d]
                if od["dma"] is not None and od["dma"].startswith("G:"):
                    need = lane_count[od["dma"]]
                if seen[E].get(key, 0) >= need:
                    continue
                w.append((key, d, need))
                if od["dma"] is None:
                    signals[d] = True
                seen[E][key] = need
                for k2, v2 in snap[d].items():
                    if seen[E].get(k2, 0) < v2:
                        seen[E][k2] = v2
            waits[i] = w
            if o["dma"] is not None:
                lane_count[o["dma"]] = lane_count.get(o["dma"], 0) + 1
                pos[i] = lane_count[o["dma"]]
            else:
                eng_count[E] += 1
                pos[i] = eng_count[E]
            snap[i] = dict(seen[E])

        sigval = [0] * n
        cnt = {e: 0 for e in ENGS}
        for i, o in enumerate(ops):
            if o["eng"] == "barrier" or o["dma"] is not None:
                continue
            if signals[i]:
                cnt[o["eng"]] += 1
            sigval[i] = cnt[o["eng"]]

        sems = {e: nc.alloc_semaphore(f"s_{e}") for e in ENGS}
        lane_sems = {ln: nc.alloc_semaphore(f"l{j}") for j, ln in enumerate(sorted(lane_count))}
        per_eng = {e: [i for i, o in enumerate(ops) if o["eng"] == e] for e in ENGS}
        self.stats = dict(n_ops=n, lanes=len(lane_sems), sig=dict(cnt),
                          nwaits=sum(len(w) for w in waits if w))

        self.pid = {}

        def run(E, eng):
            if E in ("sp", "pool") and self.want_pid:
                r = eng.alloc_register("qoff")
                eng.reg_mod(r, eng.partition_id(), 4)
                eng.reg_mul(r, r, 8192)
                self.pid[E] = eng.snap(r, min_val=0, max_val=3 * 8192)
            for i in per_eng[E]:
                o = ops[i]
                wl = []
                for key, d, need in waits[i]:
                    if key[0] == "L":
                        wl.append((lane_sems[key[1]], 1 if ops[d].get("cc") else 16 * need))
                    else:
                        wl.append((sems[key[1]], sigval[d]))
                attach = bool(wl) and not o.get("cc") and not o.get("fin")
                for sem_, val_ in (wl[:-1] if attach else wl):
                    eng.wait_ge(sem_, val_)
                ins = o["fn"](_First(eng, wl[-1]) if attach else eng)
                if o.get("cc"):
                    ins.then_inc(lane_sems[o["dma"]])
                elif o["dma"] is not None:
                    ins.then_inc(lane_sems[o["dma"]], 16)
                elif signals[i]:
                    ins.then_inc(sems[E], 1)

        with nc.Block() as block:
            @block.tensor
            def _(e):
                run("pe", e)

            @block.scalar
            def _(e):
                run("act", e)

            @block.vector
            def _(e):
                run("dve", e)

            @block.gpsimd
            def _(e):
                run("pool", e)

            @block.sync
            def _(e):
                run("sp", e)


class Ctx:
    def __init__(self, nc, P, pfx):
        self.nc, self.P, self.pfx = nc, P, pfx

    def sb(self, name, shape, dt):
        return self.nc.alloc_sbuf_tensor(f"{self.pfx}{name}", list(shape), dt)

    def ps(self, name, shape, dt=F32):
        return self.nc.alloc_psum_tensor(f"{self.pfx}{name}", list(shape), dt)


def rstd_ops(P, ss, tmp, rstd, n, rd, wr):
    P.op("act", lambda e: e.activation(out=tmp, in_=ss, func=AF.Ln, scale=1.0 / n, bias=EPS),
         reads=rd, writes=[wr + "_t"])
    P.op("act", lambda e: e.activation(out=rstd, in_=tmp, func=AF.Exp, scale=-0.5),
         reads=[wr + "_t"], writes=[wr])


A_ENGS = ("dve", "pool", "pool")
FIFO_Q = 0.0
NXT = 6


def phase_ret(P, nc, D, NT=8192, NS=2):
    C = Ctx(nc, P, "A_")
    RENG, QENG, GENG = A_ENGS
    NG = NT // 512
    ident = C.sb("ident", [128, 128], BF16)
    w_bf = C.sb("w_bf", [128, 8, 1536], BF16)
    wst = [C.sb(f"wst{i}", [128, 1536], F32) for i in range(2)]
    gain = C.sb("gain", [128, 8], F32)
    dmask = C.sb("dmask", [128, 128], F32)
    qdec4 = C.sb("qdec4", [128, 512], F32)
    kdec = C.sb("kdec", [128, 1], F32)
    cdec = C.sb("cdec", [128, 1], F32)
    xt = [C.sb(f"xt{i}", [128, 1024], F32) for i in range(NXT)]
    junk = C.sb("junk", [128, 1024], BF16)
    xn = [C.sb(f"xn{i}", [128, 1024], BF16) for i in range(2)]
    xnT = [C.sb(f"xnT{i}", [128, 8, 512], BF16) for i in range(NS)]
    cosg = [C.sb(f"cos{i}", [128, 512], F32) for i in range(NS)]
    sing = [C.sb(f"sin{i}", [128, 512], F32) for i in range(NS)]
    qrot = [C.sb(f"qrot{i}", [128, 2, 512], BF16) for i in range(NS)]
    qd = [C.sb(f"qd{i}", [128, 2, 512], BF16) for i in range(NS)]
    krot = [C.sb(f"krot{i}", [128, 2, 512], BF16) for i in range(NS)]
    vv = [C.sb(f"v{i}", [128, 4, 512], BF16) for i in range(NS)]
    sg = [C.sb(f"sg{i}", [128, 4, 512], F32) for i in range(NS)]
    ktm = [C.sb(f"ktm{i}", [128, 4, 256], BF16) for i in range(NS)]
    tmp = [C.sb(f"tmp{i}", [128, 512], F32) for i in range(4)]
    st32 = C.sb("st32", [128, 2, 512], F32)
    stbf = [C.sb(f"stbf{i}", [128, 2, 512], BF16) for i in range(2)]
    stm = [C.sb(f"stm{i}", [128, 128], BF16) for i in range(2)]
    ybuf = [C.sb(f"ybuf{i}", [128, 512], BF16) for i in range(4)]
    stat = C.sb("stat", [128, 24], F32)
    junk2 = C.sb("junk2", [128, 512], BF16)
    ge = C.sb("ge", [128, 512], F32)
    gc = C.sb("gc", [128, 512], F32)

    psT = C.ps("psT", [128, 8, 128], BF16)
    pq = [C.ps(f"pq{i}", [128, 512]) for i in range(2)]
    pv = C.ps("pv", [128, 512])
    pg = C.ps("pg", [128, 512])
    pst = C.ps("pst", [128, 128])
    po = C.ps("po", [128, 512])
    pu = C.ps("pu", [128, 512])

    P.psum |= {"A_psT", "A_pq0", "A_pq1", "A_pv", "A_pg", "A_pst", "A_po", "A_pu"}

    def ld(dst, src, name, eng="sp", lane=None):
        P.dma(eng, lambda e: e.dma_start(out=dst, in_=src), writes=[name], lane=lane or ("L:" + name))

    ld(ident[:], D["ident"], "A_ident", lane="G:const")
    ld(gain[:], D["gain"], "A_gain", lane="G:const")
    ld(dmask[:], D["dmask"], "A_dmask", lane="G:const")
    ld(qdec4[:], D["qdec4"], "A_qdec4", lane="G:const")
    ld(kdec[:], D["kdec"], "A_kdec", lane="G:const")
    ld(cdec[:], D["cdec"], "A_cdec", lane="G:const")
    wv_ = D["w"].rearrange("(kc p) f -> p kc f", p=128)
    for kc in range(8):
        s = kc % 2
        ld(wst[s][:], wv_[:, kc, :], f"A_wst{s}")
        P.op("dve", lambda e, kc=kc, s=s: e.tensor_scalar(
            out=w_bf[:, kc, :], in0=wst[s][:], scalar1=gain[:, kc:kc + 1], scalar2=None, op0=ALU.mult),
            reads=[f"A_wst{s}", "A_gain"], writes=[f"A_w{kc}"])
    WN = [f"A_w{kc}" for kc in range(8)]
    P.op("dve", lambda e: e.memset(st32[:], 0.0), writes=["A_st32_0", "A_st32_1"])
    P.op("pool", lambda e: e.memset(stbf[0][:], 0.0), writes=["A_stbf0_0", "A_stbf0_1"])

    xb = D["xb"]
    for tg in range(NG):
        s = tg % NS
        t0 = tg * 512
        ld(cosg[s][:], D["cosT"][:, t0:t0 + 512], f"A_cos{s}")
        ld(sing[s][:], D["sinT"][:, t0:t0 + 512], f"A_sin{s}")
        for tt in range(4):
            xs = (tg * 4 + tt) % NXT
            r0 = t0 + tt * 128
            ld(xt[xs][:], xb[r0:r0 + 128, :], f"A_xt{xs}")
            c0 = 3 * tt
            P.op("act", lambda e, xs=xs, c0=c0: e.activation(out=junk[:], in_=xt[xs][:], func=AF.Square,
                                                            accum_out=stat[:, c0:c0 + 1]),
                 reads=[f"A_xt{xs}"], writes=[f"A_ss{tt}"])
            rstd_ops(P, stat[:, c0:c0 + 1], stat[:, c0 + 1:c0 + 2], stat[:, c0 + 2:c0 + 3], 1024, [f"A_ss{tt}"],
                     f"A_rstd{tt}")
            xq = tt % 2
            P.op("act", lambda e, xs=xs, xq=xq, c0=c0: e.activation(out=xn[xq][:], in_=xt[xs][:], func=AF.Copy,
                                                                  scale=stat[:, c0 + 2:c0 + 3]),
                 reads=[f"A_xt{xs}", f"A_rstd{tt}"], writes=[f"A_xn{xq}"])

            def tr(e, xq=xq):
                for kc in range(8):
                    ins = e.transpose(out=psT[:, kc, :], in_=xn[xq][:, kc * 128:(kc + 1) * 128],
                                      identity=ident[:])
                return ins
            P.op("pe", tr, reads=[f"A_xn{xq}", "A_ident"], writes=["A_psT"])
            P.op("dve", lambda e, s=s, tt=tt: e.tensor_copy(out=xnT[s][:, :, tt * 128:(tt + 1) * 128],
                                                            in_=psT[:]),
                 reads=["A_psT"], writes=[f"A_xnT{s}_{tt}"])
        XN = [f"A_xnT{s}_{tt}" for tt in range(4)]

        for which, dst, c0 in (("q", qrot, 0), ("k", krot, 256)):
            for hh in range(2):
                def mm(e, hh=hh, c0=c0, s=s):
                    for kc in range(8):
                        ins = e.matmul(out=pq[hh][:], lhsT=w_bf[:, kc, c0 + hh * 128:c0 + (hh + 1) * 128],
                                       rhs=xnT[s][:, kc, :], start=(kc == 0), stop=(kc == 7))
                    return ins
                P.op("pe", mm, reads=WN + XN, writes=[f"A_pq{hh}"])
            cs, sn = cosg[s], sing[s]
            P.op("dve", lambda e, cs=cs: e.tensor_tensor(out=tmp[0][:], in0=pq[0][:], in1=cs[:], op=ALU.mult),
                 reads=["A_pq0", f"A_cos{s}"], writes=["A_tmp0"])
            P.op("dve", lambda e, sn=sn: e.tensor_tensor(out=tmp[1][:], in0=pq[1][:], in1=sn[:], op=ALU.mult),
                 reads=["A_pq1", f"A_sin{s}"], writes=["A_tmp1"])
            P.op("dve", lambda e, cs=cs: e.tensor_tensor(out=tmp[2][:], in0=pq[1][:], in1=cs[:], op=ALU.mult),
                 reads=["A_pq1", f"A_cos{s}"], writes=["A_tmp2"])
            P.op("dve", lambda e, sn=sn: e.tensor_tensor(out=tmp[3][:], in0=pq[0][:], in1=sn[:], op=ALU.mult),
                 reads=["A_pq0", f"A_sin{s}"], writes=["A_tmp3"])
            P.op(RENG, lambda e, dst=dst, s=s: e.tensor_tensor(out=dst[s][:, 0, :], in0=tmp[0][:], in1=tmp[1][:],
                                                                 op=ALU.subtract),
                 reads=["A_tmp0", "A_tmp1"], writes=[f"A_{which}rot{s}_0"])
            P.op(RENG, lambda e, dst=dst, s=s: e.tensor_tensor(out=dst[s][:, 1, :], in0=tmp[2][:], in1=tmp[3][:],
                                                                 op=ALU.add),
                 reads=["A_tmp2", "A_tmp3"], writes=[f"A_{which}rot{s}_1"])
            if which == "q":
                for dc in range(2):
                    P.op(QENG, lambda e, dc=dc, s=s: e.tensor_tensor(out=qd[s][:, dc, :], in0=qrot[s][:, dc, :],
                                                                       in1=qdec4[:], op=ALU.mult),
                         reads=[f"A_qrot{s}_{dc}", "A_qdec4"], writes=[f"A_qd{s}_{dc}"])

        for tt in range(4):
            def mv(e, tt=tt, s=s):
                for kc in range(8):
                    ins = e.matmul(out=pv[:], lhsT=xnT[s][:, kc, tt * 128:(tt + 1) * 128],
                                   rhs=w_bf[:, kc, 512:1024], start=(kc == 0), stop=(kc == 7))
                return ins
            P.op("pe", mv, reads=WN + [XN[tt]], writes=["A_pv"])
            P.op("act", lambda e, tt=tt, s=s: e.activation(out=vv[s][:, tt, :], in_=pv[:], func=AF.Copy),
                 reads=["A_pv"], writes=[f"A_v{s}_{tt}"])

            def mg(e, tt=tt, s=s):
                for kc in range(8):
                    ins = e.matmul(out=pg[:], lhsT=xnT[s][:, kc, tt * 128:(tt + 1) * 128],
                                   rhs=w_bf[:, kc, 1024:1536], start=(kc == 0), stop=(kc == 7))
                return ins
            P.op("pe", mg, reads=WN + [XN[tt]], writes=["A_pg"])
            P.op("act", lambda e: e.activation(out=ge[:], in_=pg[:], func=AF.Exp, scale=-1.0),
                 reads=["A_pg"], writes=["A_ge"])
            P.op("act", lambda e: e.activation(out=gc[:], in_=pg[:], func=AF.Copy),
                 reads=["A_pg"], writes=["A_gc"])
            P.op("dve", lambda e: e.tensor_scalar(out=ge[:], in0=ge[:], scalar1=1.0, scalar2=None, op0=ALU.add),
                 reads=["A_ge"], writes=["A_ge"])
            P.op("dve", lambda e: e.reciprocal(out=ge[:], in_=ge[:]), reads=["A_ge"], writes=["A_ge"])
            P.op(GENG, lambda e, tt=tt, s=s: e.tensor_tensor(out=sg[s][:, tt, :], in0=gc[:], in1=ge[:], op=ALU.mult),
                 reads=["A_ge", "A_gc"], writes=[f"A_sg{s}_{tt}"])

            def tk(e, tt=tt, s=s):
                for dc in range(2):
                    ins = e.transpose(out=psT[:, dc, :], in_=krot[s][:, dc, tt * 128:(tt + 1) * 128],
                                      identity=ident[:])
                return ins
            P.op("pe", tk, reads=[f"A_krot{s}_0", f"A_krot{s}_1", "A_ident"], writes=["A_psT"])
            P.op("dve", lambda e, tt=tt, s=s: e.tensor_scalar(
                out=ktm[s][:, tt, :].rearrange("p (a b) -> p a b", a=2), in0=psT[:, 0:2, :],
                scalar1=kdec[:, 0:1], scalar2=None, op0=ALU.mult),
                reads=["A_psT", "A_kdec"], writes=[f"A_ktm{s}_{tt}"])

        for tt in range(4):
            c = tg * 4 + tt
            sp_ = c % 2
            sl = slice(tt * 128, (tt + 1) * 128)

            def ms(e, s=s, sl=sl):
                for dc in range(2):
                    ins = e.matmul(out=pst[:], lhsT=krot[s][:, dc, sl], rhs=qrot[s][:, dc, sl],
                                   start=(dc == 0), stop=(dc == 1))
                return ins
            P.op("pe", ms, reads=[f"A_krot{s}_0", f"A_krot{s}_1", f"A_qrot{s}_0", f"A_qrot{s}_1"],
                 writes=["A_pst"])
            P.op("dve", lambda e, sp_=sp_: e.tensor_tensor(out=stm[sp_][:], in0=pst[:], in1=dmask[:], op=ALU.mult),
                 reads=["A_pst", "A_dmask"], writes=[f"A_stm{sp_}"])

            def mo(e, s=s, sl=sl, sp_=sp_, tt=tt):
                e.matmul(out=po[:], lhsT=stm[sp_][:], rhs=vv[s][:, tt, :], start=True, stop=False)
                for dc in range(2):
                    ins = e.matmul(out=po[:], lhsT=qd[s][:, dc, sl], rhs=stbf[sp_][:, dc, :],
                                   start=False, stop=(dc == 1))
                return ins
            P.op("pe", mo, reads=[f"A_stm{sp_}", f"A_v{s}_{tt}", f"A_qd{s}_0", f"A_qd{s}_1",
                                  f"A_stbf{sp_}_0", f"A_stbf{sp_}_1"], writes=["A_po"])
            for dc in range(2):
                P.op("pe", lambda e, s=s, tt=tt, dc=dc: e.matmul(
                    out=pu[:], lhsT=ktm[s][:, tt, dc * 128:(dc + 1) * 128], rhs=vv[s][:, tt, :],
                    start=True, stop=True),
                    reads=[f"A_ktm{s}_{tt}", f"A_v{s}_{tt}"], writes=["A_pu"])
                P.op("dve", lambda e, dc=dc: e.scalar_tensor_tensor(
                    out=st32[:, dc, :], in0=st32[:, dc, :], scalar=cdec[:, 0:1], in1=pu[:],
                    op0=ALU.mult, op1=ALU.add),
                    reads=["A_pu", "A_cdec", f"A_st32_{dc}"], writes=[f"A_st32_{dc}"])
                P.op("act", lambda e, dc=dc, sp_=sp_: e.activation(out=stbf[1 - sp_][:, dc, :], in_=st32[:, dc, :],
                                                                   func=AF.Copy),
                     reads=[f"A_st32_{dc}"], writes=[f"A_stbf{1 - sp_}_{dc}"])
            g0 = 12 + 3 * (c % 2)
            P.op("act", lambda e, g0=g0: e.activation(out=junk2[:], in_=po[:], func=AF.Square,
                                                      accum_out=stat[:, g0:g0 + 1]),
                 reads=["A_po"], writes=[f"A_ssq{c % 2}"])
            rstd_ops(P, stat[:, g0:g0 + 1], stat[:, g0 + 1:g0 + 2], stat[:, g0 + 2:g0 + 3], 512, [f"A_ssq{c % 2}"],
                     f"A_rs{c % 2}")
            yb = c % 4
            P.op("dve", lambda e, yb=yb, s=s, tt=tt, g0=g0: e.scalar_tensor_tensor(
                out=ybuf[yb][:], in0=po[:], scalar=stat[:, g0 + 2:g0 + 3], in1=sg[s][:, tt, :],
                op0=ALU.mult, op1=ALU.mult),
                reads=["A_po", f"A_rs{c % 2}", f"A_sg{s}_{tt}"], writes=[f"A_ybuf{yb}"])
            P.dma("sp", lambda e, yb=yb, c=c: e.dma_start(out=D["y_out"][c * 128:(c + 1) * 128, :], in_=ybuf[yb][:]),
                  reads=[f"A_ybuf{yb}"], writes=[f"d:A_y{c // 8}"], lane=f"S:A_ybuf{yb}")
        if "after_group" in D:
            D["after_group"](tg)


def phase_ffn(P, nc, D, F, final, pfx, NTOK=2048, NE=16, stage=3):
    C = Ctx(nc, P, pfx)
    assert F <= 2048
    N = lambda s: pfx + s
    NTILE = NTOK // 128
    FC = F // 128
    NGRP = NTOK // 512
    ident = C.sb("ident", [128, 128], BF16)
    identf = C.sb("identf", [128, 128], F32)
    h = C.sb("h", [128, NTILE, 1024], F32)
    hnT = C.sb("hnT", [128, 8, NTOK], BF16)
    wbuf = C.sb("wbuf", [128, 24576], BF16)
    wo_v = wbuf[:, 0:FC * 1024].rearrange("p (f n) -> p f n", f=FC)

    def wslot(s):
        b = s * 12288
        return (wbuf[:, b:b + 4096].rearrange("p (k f) -> p k f", k=8),
                wbuf[:, b + 4096:b + 8192].rearrange("p (k f) -> p k f", k=8),
                wbuf[:, b + 8192:b + 12288].rearrange("p (k f) -> p k f", k=4))
    yt = [C.sb(f"yt{i}", [128, F], BF16) for i in range(2)]
    yT = [C.sb(f"yT{i}", [128, FC, 128], BF16) for i in range(2)]
    hn32 = [C.sb(f"hn32_{i}", [128, 1024], F32) for i in range(2)]
    hnT32 = [C.sb(f"hnT32_{i}", [128, 8, 128], F32) for i in range(2)]
    fgain = C.sb("fgain", [128, 1024], F32)
    wr = C.sb("wr", [128, 8, 20], F32)
    rbias = C.sb("rbias", [128, 20], F32)
    comb = C.sb("comb", [128, NTILE, 16], F32)
    rts = [C.sb(f"rt{i}", [128, 64], F32) for i in range(4)]
    st = C.sb("st", [128, 3, NTILE], F32)
    junk = C.sb("junk", [128, 1024], BF16)
    sgl = [C.sb(f"sgl{i}", [128, 512], F32) for i in range(2)]
    hT = [C.sb(f"hT{i}", [128, 4, 512], BF16) for i in range(2)]
    if final:
        ngain = C.sb("ngain", [128, 1024], F32)
        ob = [C.sb(f"ob{i}", [128, 1024], F32) for i in range(2)]

    pyT = C.ps("pyT", [128, 8, 128], BF16)
    pT32 = C.ps("pT32", [128, 8, 128], F32)
    pT32v = pT32[:].rearrange("p a b -> p (a b)")
    pg0 = C.ps("pg", [128, 512])
    pg = [pg0[:], pT32v[:, 0:512]]
    pyTs = [pyT[:], pg0[:].bitcast(BF16).rearrange("p (a b) -> p a b", a=8)]
    pyTn = [N("pyT"), N("pg")]
    pu = [C.ps("pu", [128, 512])[:], pT32v[:, 512:1024]]
    pgn = [N("pg"), N("pT32a")]
    pun = [N("pu"), N("pT32b")]
    pd = C.ps("pd", [128, 2, 512])
    pr = C.ps("pr", [128, 32])

    P.psum |= {N(x) for x in ("pyT", "pT32a", "pT32b", "pg", "pu", "pd0", "pd1", "pr")}

    def ld(dst, src, name, eng="sp", lane=None):
        P.dma(eng, lambda e: e.dma_start(out=dst, in_=src), writes=[name], lane=lane or ("L:" + name))

    ld(ident[:], D["ident"], N("ident"), lane="G:const")
    ld(identf[:], D["identf"], N("identf"), lane="G:const")
    ld(fgain[:], D["fgain"], N("fgain"), lane="G:const")
    ld(wr[:], D["wr"].rearrange("(kc p) n -> p kc n", p=128), N("wr"), lane="G:const")
    ld(rbias[:], D["rbias"], N("rbias"), lane="G:const")
    if final:
        ld(ngain[:], D["ngain"], N("ngain"), lane="G:const")
    xs_v = D["xs"].rearrange("(t p) f -> p t f", p=128)
    def load_hq(q):
        tq = NTILE // 4
        P.dma("sp", lambda e, q=q, tq=tq: e.dma_start(out=h[:, q * tq:(q + 1) * tq, :], in_=xs_v[:, q * tq:(q + 1) * tq, :]),
              reads=D.get("xs_reads", ()), writes=[N(f"hq{q}")], lane="G:hq")
    HQ = lambda t: N(f"hq{t // (NTILE // 4)}")
    wo_d = D["wo"].rearrange("(fc p) n -> p fc n", p=128)
    nq = FC // 4
    for q in range(nq):
        P.dma("pool", lambda e, q=q: e.dma_start(out=wo_v[:, q * 4:(q + 1) * 4, :], in_=wo_d[:, q * 4:(q + 1) * 4, :]),
              writes=[N(f"wo{q}")], lane="G:wo")
    WO = [N(f"wo{q}") for q in range(nq)]
    SL = [N("ws0"), N("ws1")]

    for t in range(NTILE):
        s = t % 2
        if "ys_fn" in D:
            P.dma(D.get("ys_eng", "sp"), lambda e, t=t, s=s: D["ys_fn"](e, t, yt[s]), reads=D["ys_reads"](t), writes=[N(f"yt{s}")],
                  lane="L:" + N(f"yt{s}"))
        else:
            ld(yt[s][:], D["ys"][t * 128:(t + 1) * 128, :], N(f"yt{s}"))
        if t % (NTILE // 4) == 0:
            load_hq(t // (NTILE // 4))
        for half in range(FC // 8):
            pb = (t * (FC // 8) + half) % 2

            def tr(e, s=s, half=half, pb=pb):
                for j in range(8):
                    fc = half * 8 + j
                    ins = e.transpose(out=pyTs[pb][:, j, :], in_=yt[s][:, fc * 128:(fc + 1) * 128], identity=ident[:])
                return ins
            P.op("pe", tr, reads=[N(f"yt{s}"), N("ident")], writes=[pyTn[pb]])
            P.op("act", lambda e, half=half, s=s, pb=pb: e.activation(out=yT[s][:, half * 8:(half + 1) * 8, :], in_=pyTs[pb],
                                                                func=AF.Copy),
                 reads=[pyTn[pb]], writes=[N(f"yT{s}_{half}")])
        for hh in range(2):
            def mm(e, hh=hh, s=s):
                for fc in range(FC):
                    ins = e.matmul(out=pd[:, hh, :], lhsT=yT[s][:, fc, :], rhs=wo_v[:, fc, hh * 512:(hh + 1) * 512],
                                   start=(fc == 0), stop=(fc == FC - 1))
                return ins
            P.op("pe", mm, reads=[N(f"yT{s}_{i}") for i in range(FC // 8)] + WO + SL, writes=[N(f"pd{hh}")])
            P.op("dve", lambda e, t=t, hh=hh: e.tensor_tensor(out=h[:, t, hh * 512:(hh + 1) * 512],
                                                              in0=pd[:, hh, :], in1=h[:, t, hh * 512:(hh + 1) * 512],
                                                              op=ALU.add),
                 reads=[N(f"pd{hh}"), HQ(t), N(f"h{t}")], writes=[N(f"h{t}")])

    for t in range(NTILE if stage >= 2 else 0):
        P.op("act", lambda e, t=t: e.activation(out=junk[:], in_=h[:, t, :], func=AF.Square, accum_out=st[:, 0, t:t + 1]),
             reads=[N(f"h{t}")], writes=[N(f"ss{t}")])
    for t in range(NTILE if stage >= 2 else 0):
        rstd_ops(P, st[:, 0, t:t + 1], st[:, 1, t:t + 1], st[:, 2, t:t + 1], 1024, [N(f"ss{t}")], N(f"rstd{t}"))
    for t in range(NTILE if stage >= 2 else 0):
        u = t % 2
        P.op("dve", lambda e, t=t, u=u: e.scalar_tensor_tensor(out=hn32[u][:], in0=h[:, t, :], scalar=st[:, 2, t:t + 1],
                                                               in1=fgain[:], op0=ALU.mult, op1=ALU.mult),
             reads=[N(f"h{t}"), N(f"rstd{t}"), N("fgain")], writes=[N(f"hn32_{u}")])

        def trf(e, u=u):
            for kc in range(8):
                ins = e.transpose(out=pT32[:, kc, :], in_=hn32[u][:, kc * 128:(kc + 1) * 128], identity=identf[:])
            return ins
        P.op("pe", trf, reads=[N(f"hn32_{u}"), N("identf")], writes=[N("pT32a"), N("pT32b")])
        P.op("act", lambda e, t=t: e.activation(out=hnT[:, :, t * 128:(t + 1) * 128], in_=pT32[:], func=AF.Copy),
             reads=[N("pT32a"), N("pT32b")], writes=[N(f"hnT{t}")])
        P.op("dve", lambda e, u=u: e.tensor_copy(out=hnT32[u][:], in_=pT32[:]),
             reads=[N("pT32a"), N("pT32b")], writes=[N(f"hnT32_{u}")])

        def mr(e, u=u):
            for kc in range(8):
                ins = e.matmul(out=pr[:, 0:20], lhsT=hnT32[u][:, kc, :], rhs=wr[:, kc, :], start=(kc == 0), stop=(kc == 7))
            return ins
        P.op("pe", mr, reads=[N(f"hnT32_{u}"), N("wr")], writes=[N("pr")])
        rt = rts[t % 4]
        R = N(f"rt{t % 4}")
        k = [0]

        def dv(fn, rd=(), eng="dve"):
            P.op(eng, fn, reads=[R] + list(rd), writes=[R])
        dv(lambda e, rt=rt: e.tensor_tensor(out=rt[:, 0:20], in0=pr[:, 0:20], in1=rbias[:], op=ALU.add), [N("pr"), N("rbias")])
        dv(lambda e, rt=rt: e.tensor_reduce(out=rt[:, 20:21], in_=rt[:, 0:4], axis=AX.X, op=ALU.max))
        dv(lambda e, rt=rt: e.tensor_scalar(out=rt[:, 24:28], in0=rt[:, 0:4], scalar1=rt[:, 20:21], scalar2=None, op0=ALU.is_equal))
        dv(lambda e, rt=rt: e.tensor_scalar(out=rt[:, 28:32], in0=rt[:, 0:4], scalar1=rt[:, 20:21], scalar2=None, op0=ALU.subtract))
        dv(lambda e, rt=rt: e.activation(out=rt[:, 28:32], in_=rt[:, 28:32], func=AF.Exp, accum_out=rt[:, 21:22]), eng="act")
        dv(lambda e, rt=rt: e.reciprocal(out=rt[:, 22:23], in_=rt[:, 21:22]))
        dv(lambda e, rt=rt: e.tensor_scalar(out=rt[:, 32:36], in0=rt[:, 4:8], scalar1=rt[:, 24:25], scalar2=None, op0=ALU.mult))
        for g in range(1, 4):
            dv(lambda e, g=g, rt=rt: e.scalar_tensor_tensor(out=rt[:, 32:36], in0=rt[:, 4 + 4 * g:8 + 4 * g],
                                                     scalar=rt[:, 24 + g:25 + g], in1=rt[:, 32:36],
                                                     op0=ALU.mult, op1=ALU.add))
        dv(lambda e, rt=rt: e.tensor_reduce(out=rt[:, 36:37], in_=rt[:, 32:36], axis=AX.X, op=ALU.max))
        dv(lambda e, rt=rt: e.tensor_scalar(out=rt[:, 40:44], in0=rt[:, 32:36], scalar1=rt[:, 36:37], scalar2=None, op0=ALU.is_equal))
        dv(lambda e, rt=rt: e.scalar_tensor_tensor(out=rt[:, 44:48], in0=rt[:, 40:44], scalar=-1e30, in1=rt[:, 32:36],
                                            op0=ALU.mult, op1=ALU.add))
        dv(lambda e, rt=rt: e.tensor_reduce(out=rt[:, 37:38], in_=rt[:, 44:48], axis=AX.X, op=ALU.max))
        dv(lambda e, rt=rt: e.tensor_scalar(out=rt[:, 48:52], in0=rt[:, 44:48], scalar1=rt[:, 37:38], scalar2=None, op0=ALU.is_equal))
        dv(lambda e, rt=rt: e.tensor_tensor(out=rt[:, 38:39], in0=rt[:, 37:38], in1=rt[:, 36:37], op=ALU.subtract))
        dv(lambda e, rt=rt: e.activation(out=rt[:, 39:40], in_=rt[:, 38:39], func=AF.Exp), eng="act")
        dv(lambda e, rt=rt: e.tensor_scalar(out=rt[:, 52:53], in0=rt[:, 39:40], scalar1=1.0, scalar2=None, op0=ALU.add))
        dv(lambda e, rt=rt: e.reciprocal(out=rt[:, 52:53], in_=rt[:, 52:53]))
        dv(lambda e, rt=rt: e.tensor_tensor(out=rt[:, 53:54], in0=rt[:, 39:40], in1=rt[:, 52:53], op=ALU.mult))
        dv(lambda e, rt=rt: e.tensor_scalar(out=rt[:, 56:60], in0=rt[:, 40:44], scalar1=rt[:, 52:53], scalar2=None, op0=ALU.mult))
        dv(lambda e, rt=rt: e.scalar_tensor_tensor(out=rt[:, 56:60], in0=rt[:, 48:52], scalar=rt[:, 53:54], in1=rt[:, 56:60],
                                            op0=ALU.mult, op1=ALU.add))
        dv(lambda e, rt=rt: e.tensor_scalar(out=rt[:, 60:64], in0=rt[:, 24:28], scalar1=rt[:, 22:23], scalar2=None, op0=ALU.mult))
        for g in range(4):
            P.op("dve", lambda e, g=g, t=t, rt=rt: e.tensor_scalar(out=comb[:, t, 4 * g:4 * g + 4], in0=rt[:, 56:60],
                                                            scalar1=rt[:, 60 + g:61 + g], scalar2=None, op0=ALU.mult),
                 reads=[R], writes=[N(f"comb{t}")])

    HNT = [N(f"hnT{t}") for t in range(NTILE)]
    pend = None
    it = 0
    wl_cnt = [0]
    passes = D.get("tg_passes", [list(range(NGRP))])
    for ex, tgs in [(ex, tgs) for tgs in (passes if stage >= 3 else []) for ex in range(NE)]:
        s = wl_cnt[0] % 2
        wl_cnt[0] += 1
        wg_s, wu_s, wd_s = wslot(s)
        wg_d = D["wg"][ex].rearrange("(kc p) f -> p kc f", p=128)
        wu_d = D["wu"][ex].rearrange("(kc p) f -> p kc f", p=128)
        wd_d = D["wd"][ex].rearrange("(fc p) n -> p fc n", p=128)
        for (dst, src) in ((wg_s, wg_d), (wu_s, wu_d), (wd_s, wd_d)):
            P.dma("pool", lambda e, dst=dst, src=src: e.dma_start(out=dst, in_=src),
                  writes=[SL[s]], lane="L:" + SL[s])
        for tg in tgs:
            b = it % 2
            for fc in range(4):
                pb = (it * 4 + fc) % 2

                def mg(e, fc=fc, tg=tg, pb=pb, wg_s=wg_s):
                    for kc in range(8):
                        ins = e.matmul(out=pg[pb], lhsT=wg_s[:, kc, fc * 128:(fc + 1) * 128],
                                       rhs=hnT[:, kc, tg * 512:(tg + 1) * 512], start=(kc == 0), stop=(kc == 7))
                    return ins

                def mu(e, fc=fc, tg=tg, pb=pb, wu_s=wu_s):
                    for kc in range(8):
                        ins = e.matmul(out=pu[pb], lhsT=wu_s[:, kc, fc * 128:(fc + 1) * 128],
                                       rhs=hnT[:, kc, tg * 512:(tg + 1) * 512], start=(kc == 0), stop=(kc == 7))
                    return ins
                hn_names = HNT[tg * 4:(tg + 1) * 4]
                P.op("pe", mg, reads=[SL[s]] + hn_names, writes=[pgn[pb]])
                P.op("pe", mu, reads=[SL[s]] + hn_names, writes=[pun[pb]])
                P.op("act", lambda e, pb=pb: e.activation(out=sgl[pb][:], in_=pg[pb], func=AF.Silu),
                     reads=[pgn[pb]], writes=[N(f"sgl{pb}")])
                P.op("dve", lambda e, pb=pb, b=b, fc=fc: e.tensor_tensor(out=hT[b][:, fc, :], in0=pu[pb], in1=sgl[pb][:],
                                                                       op=ALU.mult),
                     reads=[pun[pb], N(f"sgl{pb}")], writes=[N(f"hT{b}_{fc}")])

            def down(ex=ex, tg=tg, b=b, s=s, wd_s=wd_s):
                for tt in range(4):
                    t = tg * 4 + tt
                    for hh in range(2):
                        def md(e, tt=tt, hh=hh):
                            for fc in range(4):
                                ins = e.matmul(out=pd[:, hh, :], lhsT=hT[b][:, fc, tt * 128:(tt + 1) * 128],
                                               rhs=wd_s[:, fc, hh * 512:(hh + 1) * 512], start=(fc == 0), stop=(fc == 3))
                            return ins
                        P.op("pe", md, reads=[SL[s]] + [N(f"hT{b}_{fc}") for fc in range(4)], writes=[N(f"pd{hh}")])
                        P.op("dve", lambda e, t=t, hh=hh: e.scalar_tensor_tensor(
                            out=h[:, t, hh * 512:(hh + 1) * 512], in0=pd[:, hh, :], scalar=comb[:, t, ex:ex + 1],
                            in1=h[:, t, hh * 512:(hh + 1) * 512], op0=ALU.mult, op1=ALU.add),
                            reads=[N(f"pd{hh}"), N(f"comb{t}"), N(f"h{t}")], writes=[N(f"h{t}")])
            if pend is not None:
                pend()
            pend = down
            it += 1
    if pend is not None:
        pend()

    if not final:
        for t in range(NTILE):
            P.dma("sp", lambda e, t=t: e.dma_start(out=D["h_out"][t * 128:(t + 1) * 128, :], in_=h[:, t, :]),
                  reads=[N(f"h{t}")], writes=["d:" + N("h_out")], lane="S:" + N(f"h{t % 4}"))
        if "hn_out" in D:
            for t in range(NTILE):
                P.op("act", lambda e, t=t: e.activation(out=junk[:], in_=h[:, t, :], func=AF.Square,
                                                       accum_out=st[:, 0, t:t + 1]),
                     reads=[N(f"h{t}")], writes=[N(f"nss{t}")])
            rstd_ops(P, st[:, 0, :], st[:, 1, :], st[:, 2, :], 1024, [N(f"nss{t}") for t in range(NTILE)], N("nrstd"))
            for t in range(NTILE):
                s2 = t % 2
                P.op("act", lambda e, t=t, s2=s2: e.activation(out=yt[s2][:, 0:1024], in_=h[:, t, :], func=AF.Copy,
                                                              scale=st[:, 2, t:t + 1]),
                     reads=[N(f"h{t}"), N("nrstd")], writes=[N(f"yt{s2}")])
                P.dma("sp", lambda e, t=t, s2=s2: e.dma_start(out=D["hn_out"][t * 128:(t + 1) * 128, :],
                                                             in_=yt[s2][:, 0:1024]),
                      reads=[N(f"yt{s2}")], writes=["d:" + N(f"hn{t // 4}")], lane="S:" + N(f"yt{s2}"))
    else:
        for t in range(NTILE):
            P.op("act", lambda e, t=t: e.activation(out=junk[:], in_=h[:, t, :], func=AF.Square, accum_out=st[:, 0, t:t + 1]),
                 reads=[N(f"h{t}")], writes=[N(f"fss{t}")])
        for t in range(NTILE):
            rstd_ops(P, st[:, 0, t:t + 1], st[:, 1, t:t + 1], st[:, 2, t:t + 1], 1024, [N(f"fss{t}")], N(f"frstd{t}"))
        for t in range(NTILE):
            s = t % 2
            P.op("dve", lambda e, t=t, s=s: e.scalar_tensor_tensor(out=ob[s][:], in0=h[:, t, :], scalar=st[:, 2, t:t + 1],
                                                                  in1=ngain[:], op0=ALU.mult, op1=ALU.mult),
                 reads=[N(f"h{t}"), N(f"frstd{t}"), N("ngain")], writes=[N(f"ob{s}")])
            P.dma("sp", lambda e, t=t, s=s: e.dma_start(out=D["h_out"][t * 128:(t + 1) * 128, :], in_=ob[s][:]),
                  reads=[N(f"ob{s}")], writes=["d:" + N("h_out")], lane="S:" + N(f"ob{s}"))


def build_ffn(F, final, NTOK=2048, NE=16, stage=3):
    nc = bass.Bass("TRN2", target_bir_lowering=False)
    P = Prog(nc)
    D = {}

    def inp(name, shape, dt=F32):
        D[name] = nc.dram_tensor(name, list(shape), dt, kind="ExternalInput").ap()
    inp("xs", [NTOK, 1024]); inp("ys", [NTOK, F], BF16); inp("wo", [F, 1024]); inp("fgain", [128, 1024])
    inp("wr", [1024, 20]); inp("rbias", [128, 20]); inp("wg", [NE, 1024, 512]); inp("wu", [NE, 1024, 512])
    inp("wd", [NE, 512, 1024]); inp("ident", [128, 128], BF16); inp("identf", [128, 128])
    if final:
        inp("ngain", [128, 1024])
    D["h_out"] = nc.dram_tensor("h_out", [NTOK, 1024], F32, kind="ExternalOutput").ap()
    phase_ffn(P, nc, D, F, final, "B_", NTOK, NE, stage)
    finish(P, ["d:h_out"])
    P.build()
    return nc, P


def ffn_maps(xs_list, ys_list, wo, fgain, rgw, rgb, rew, reb, wg, wu, wd, ngain=None):
    ident = np.eye(128, dtype=np.float32).astype(ml_dtypes.bfloat16)
    identf = np.eye(128, dtype=np.float32)
    wr = np.ascontiguousarray(np.concatenate([rgw] + [rew[g] for g in range(4)], axis=1))
    rb = np.concatenate([rgb, reb.reshape(-1)])[None, :]
    common = dict(wo=np.ascontiguousarray(wo), fgain=np.ascontiguousarray(np.broadcast_to(fgain[None, :], (128, 1024))),
                  wr=wr, rbias=np.ascontiguousarray(np.broadcast_to(rb, (128, 20))),
                  wg=np.ascontiguousarray(wg.reshape(16, 1024, 512)), wu=np.ascontiguousarray(wu.reshape(16, 1024, 512)),
                  wd=np.ascontiguousarray(wd.reshape(16, 512, 1024)), ident=ident, identf=identf)
    if ngain is not None:
        common["ngain"] = np.ascontiguousarray(np.broadcast_to(ngain[None, :], (128, 1024)))
    return [dict(common, xs=np.ascontiguousarray(xs_list[c]), ys=np.ascontiguousarray(ys_list[c])) for c in range(8)]


def phase_moba(P, nc, D, NT=8192):
    C = Ctx(nc, P, "C_")
    N = lambda s: "C_" + s
    NG = NT // 512
    NQT = NT // 128
    NB = NT // 256
    ident = C.sb("ident", [128, 128], BF16)
    cmask = C.sb("cmask", [128, 2, 256], BF16)
    gq = C.sb("gq", [128, 8], F32)
    gkv = C.sb("gkv", [128, 8], F32)
    wst = C.sb("wst", [128, 8, 256], F32)
    wq_bf = C.sb("wq_bf", [128, 8, 256], BF16)
    wk_bf = C.sb("wk_bf", [128, 8, 256], BF16)
    wv_bf = C.sb("wv_bf", [128, 8, 256], BF16)
    wq_rot = C.sb("wq_rot", [128, 8, 2, 32], BF16)
    wk_rot = C.sb("wk_rot", [128, 8, 2, 32], BF16)
    QT = [C.sb(f"QT{i}", [128, NT], BF16) for i in range(2)]
    KT = [C.sb(f"KT{i}", [128, NT], BF16) for i in range(2)]
    Vext = C.sb("Vext", [128, 2, NQT, 130], BF16)
    Msel = C.sb("Msel", [128, 2, NQT, 32], F32)
    km32 = C.sb("km32", [128, 2, 32], F32)
    kmT = C.sb("kmT", [128, 2, 32], BF16)
    ht = [C.sb(f"ht{i}", [128, 1024], F32) for i in range(2)]
    junk = C.sb("junk", [128, 1024], BF16)
    hn = [C.sb(f"hn{i}", [128, 1024], BF16) for i in range(2)]
    hnT = [C.sb(f"hnT{i}", [128, 8, 512], BF16) for i in range(2)]
    cosg = [C.sb(f"cos{i}", [32, 512], F32) for i in range(2)]
    sing = [C.sb(f"sin{i}", [32, 512], F32) for i in range(2)]
    ta = C.sb("ta", [32, 512], F32)
    tb = C.sb("tb", [32, 512], F32)
    xr = [C.sb(f"xr{i}", [32, 512], F32) for i in range(2)]
    xsw = [C.sb(f"xsw{i}", [32, 512], F32) for i in range(2)]
    rc = [0]
    st = C.sb("st", [128, 3, 4], F32)
    gt = C.sb("gt", [128, 32], F32)
    mx = C.sb("mx", [128, 8], F32)
    PT = [C.sb(f"PT{i}", [128, 2, 256], BF16) for i in range(3)]
    acc = [C.sb(f"acc{i}", [128, 130], F32) for i in range(2)]
    rec = C.sb("rec", [128, 2], F32)
    ob = [C.sb(f"ob{i}", [128, 128], BF16) for i in range(4)]

    psT = C.ps("psT", [128, 8, 128], BF16)
    pm = C.ps("pm", [128, 512])
    prot = C.ps("prot", [128, 512])
    pv = C.ps("pv", [128, 512])
    pS = [C.ps(f"pS{i}", [128, 2, 256]) for i in range(2)]
    pO = [C.ps(f"pO{i}", [128, 512]) for i in range(2)]
    P.psum |= {N(x) for x in ("psT", "pm", "prot", "pv", "pS0", "pS1", "pO0", "pO1")}

    def ld(dst, src, name, eng="sp", lane=None):
        P.dma(eng, lambda e: e.dma_start(out=dst, in_=src), writes=[name], lane=lane or ("L:" + name))

    ld(ident[:], D["ident"], N("ident"), lane="G:const")
    ld(cmask[:], D["cmask"], N("cmask"), lane="G:const")
    ld(gq[:], D["gq"], N("gq"), lane="G:const")
    ld(gkv[:], D["gkv"], N("gkv"), lane="G:const")
    qscale = float(128 ** -0.5)
    for (wname, wdst, g, sc) in (("wq", wq_bf, gq, qscale), ("wk", wk_bf, gkv, 1.0), ("wv", wv_bf, gkv, 1.0)):
        ld(wst[:], D[wname].rearrange("(kc p) f -> p kc f", p=128), N("wst"))
        for kc in range(8):
            P.op("dve", lambda e, kc=kc, wdst=wdst, g=g, sc=sc: e.tensor_scalar(
                out=wdst[:, kc, :], in0=wst[:, kc, :], scalar1=g[:, kc:kc + 1], scalar2=sc, op0=ALU.mult, op1=ALU.mult),
                reads=[N("wst"), N("gq"), N("gkv")], writes=[N(wname + "_bf")])
    for (wsrc, wrot, nm) in ((wq_bf, wq_rot, "wq"), (wk_bf, wk_rot, "wk")):
        for hh in range(2):
            P.op("dve", lambda e, wsrc=wsrc, wrot=wrot, hh=hh: e.tensor_scalar(
                out=wrot[:, :, hh, 0:16], in0=wsrc[:, :, hh * 128 + 16:hh * 128 + 32], scalar1=-1.0, scalar2=None,
                op0=ALU.mult), reads=[N(nm + "_bf")], writes=[N(nm + "_rot")])
            P.op("dve", lambda e, wsrc=wsrc, wrot=wrot, hh=hh: e.tensor_copy(
                out=wrot[:, :, hh, 16:32], in_=wsrc[:, :, hh * 128:hh * 128 + 16]),
                reads=[N(nm + "_bf")], writes=[N(nm + "_rot")])
    P.op("pool", lambda e: e.memset(Vext[:], 1.0), writes=[N("Vext_init")])
    P.op("pool", lambda e: e.memset(Msel[:], 0.0), writes=[N("Msel_init")])
    P.op("pool", lambda e: e.memset(kmT[:], 0.0), writes=[N("kmT_init")])

    hb = D.get("hb")
    for tg in range(NG):
        s = tg % 2
        t0 = tg * 512
        ld(cosg[s][:], D["cos32"][:, t0:t0 + 512], N(f"cos{s}"))
        ld(sing[s][:], D["sin32"][:, t0:t0 + 512], N(f"sin{s}"))
        for tt in range(4):
            xs = tt % 2
            r0 = t0 + tt * 128
            if "hn_src" in D:
                P.dma("sp", lambda e, xs=xs, r0=r0: e.dma_start(out=hn[xs][:], in_=D["hn_src"](r0)),
                      reads=D["hn_reads"](r0), writes=[N(f"hn{xs}")], lane="L:" + N(f"hn{xs}"))
            else:
                ld(ht[xs][:], hb[r0:r0 + 128, :], N(f"ht{xs}"))
                P.op("act", lambda e, xs=xs, tt=tt: e.activation(out=junk[:], in_=ht[xs][:], func=AF.Square,
                                                                accum_out=st[:, 0, tt:tt + 1]),
                     reads=[N(f"ht{xs}")], writes=[N("ss")])
                rstd_ops(P, st[:, 0, tt:tt + 1], st[:, 1, tt:tt + 1], st[:, 2, tt:tt + 1], 1024, [N("ss")], N("rstd"))
                P.op("act", lambda e, xs=xs, tt=tt: e.activation(out=hn[xs][:], in_=ht[xs][:], func=AF.Copy,
                                                                scale=st[:, 2, tt:tt + 1]),
                     reads=[N(f"ht{xs}"), N("rstd")], writes=[N(f"hn{xs}")])

            def tr(e, xs=xs):
                for kc in range(8):
                    ins = e.transpose(out=psT[:, kc, :], in_=hn[xs][:, kc * 128:(kc + 1) * 128], identity=ident[:])
                return ins
            P.op("pe", tr, reads=[N(f"hn{xs}"), N("ident")], writes=[N("psT")])
            P.op("dve", lambda e, s=s, tt=tt: e.tensor_copy(out=hnT[s][:, :, tt * 128:(tt + 1) * 128], in_=psT[:]),
                 reads=[N("psT")], writes=[N(f"hnT{s}_{tt}")])
        XN = [N(f"hnT{s}_{tt}") for tt in range(4)]
        for hh in range(2):
            for (w_bf, w_rot, dst, nm) in ((wq_bf, wq_rot, QT, "Q"), (wk_bf, wk_rot, KT, "K")):
                wn = "wq" if nm == "Q" else "wk"

                def mm(e, w_bf=w_bf, hh=hh, s=s):
                    for kc in range(8):
                        ins = e.matmul(out=pm[:], lhsT=w_bf[:, kc, hh * 128:(hh + 1) * 128], rhs=hnT[s][:, kc, :],
                                       start=(kc == 0), stop=(kc == 7))
                    return ins

                def mr(e, w_rot=w_rot, hh=hh, s=s):
                    for kc in range(8):
                        ins = e.matmul(out=prot[0:32, :], lhsT=w_rot[:, kc, hh, :], rhs=hnT[s][:, kc, :],
                                       start=(kc == 0), stop=(kc == 7))
                    return ins
                P.op("pe", mm, reads=[N(wn + "_bf")] + XN, writes=[N("pm")])
                dname = N(f"{nm}T{hh}_{tg}")
                u = rc[0] % 2
                rc[0] += 1
                P.op("act", lambda e, u=u: e.activation(out=xr[u][:], in_=pm[0:32, :], func=AF.Copy),
                     reads=[N("pm")], writes=[N(f"xr{u}")])
                P.dma("sp", lambda e, u=u: e.dma_start(out=xsw[u][0:16, :], in_=xr[u][16:32, :]),
                      reads=[N(f"xr{u}")], writes=[N(f"xsw{u}")], lane="L:" + N(f"xsw{u}"))
                P.dma("sp", lambda e, u=u: e.dma_start(out=xsw[u][16:32, :], in_=xr[u][0:16, :]),
                      reads=[N(f"xr{u}")], writes=[N(f"xsw{u}")], lane="L:" + N(f"xsw{u}"))
                P.op("act", lambda e, dst=dst, hh=hh, t0=t0: e.activation(out=dst[hh][32:64, t0:t0 + 512],
                                                                         in_=pm[32:64, :], func=AF.Copy),
                     reads=[N("pm")], writes=[dname + "mid"])
                P.op("act", lambda e, dst=dst, hh=hh, t0=t0: e.activation(out=dst[hh][64:128, t0:t0 + 512],
                                                                         in_=pm[64:128, :], func=AF.Copy),
                     reads=[N("pm")], writes=[dname + "hi"])
                P.op("dve", lambda e, s=s, u=u: e.tensor_tensor(out=ta[:], in0=xr[u][:], in1=cosg[s][:], op=ALU.mult),
                     reads=[N(f"xr{u}"), N(f"cos{s}")], writes=[N("ta")])
                P.op("dve", lambda e, s=s, u=u: e.tensor_tensor(out=tb[:], in0=xsw[u][:], in1=sing[s][:], op=ALU.mult),
                     reads=[N(f"xsw{u}"), N(f"sin{s}")], writes=[N("tb")])
                P.op("pool", lambda e, dst=dst, hh=hh, t0=t0: e.tensor_tensor(out=dst[hh][0:32, t0:t0 + 512], in0=ta[:],
                                                                             in1=tb[:], op=ALU.add),
                     reads=[N("ta"), N("tb")], writes=[dname + "lo"])
            P.op("dve", lambda e, hh=hh, t0=t0, tg=tg: e.tensor_reduce(
                out=km32[:, hh, 2 * tg:2 * tg + 2], in_=KT[hh][:, t0:t0 + 512].rearrange("p (a b) -> p a b", a=2),
                axis=AX.X, op=ALU.add),
                reads=[N(f"KT{hh}_{tg}hi"), N(f"KT{hh}_{tg}lo")], writes=[N(f"km32_{hh}_{tg}")])
            P.op("act", lambda e, hh=hh, tg=tg: e.activation(out=kmT[:, hh, 2 * tg:2 * tg + 2],
                                                            in_=km32[:, hh, 2 * tg:2 * tg + 2], func=AF.Copy,
                                                            scale=1.0 / 256.0),
                 reads=[N(f"km32_{hh}_{tg}"), N("kmT_init")], writes=[N(f"kmT{hh}_{tg}")])
        for tt in range(4):
            tile = tg * 4 + tt

            def mv(e, tt=tt, s=s):
                for kc in range(8):
                    ins = e.matmul(out=pv[:, 0:256], lhsT=hnT[s][:, kc, tt * 128:(tt + 1) * 128], rhs=wv_bf[:, kc, :],
                                   start=(kc == 0), stop=(kc == 7))
                return ins
            P.op("pe", mv, reads=[N("wv_bf"), XN[tt]], writes=[N("pv")])
            P.op("act", lambda e, tile=tile: e.activation(out=Vext[:, :, tile, 0:128],
                                                         in_=pv[:, 0:256].rearrange("p (a b) -> p a b", a=2), func=AF.Copy),
                 reads=[N("pv"), N("Vext_init")], writes=[N(f"V{tile}")])
    gts = [gt, C.sb("gt1", [128, 32], F32)]
    mxs = [mx, C.sb("mx1", [128, 8], F32)]
    for qt in range(2, NQT):
        for hh in range(2):
            j = qt // 2
            g_, m_, pg_, pgn_ = gts[hh], mxs[hh], pO[hh], N(f"pO{hh}")
            kn = [N(f"kmT{hh}_{tg}") for tg in range((j - 1) // 2 + 1)]
            P.op("pe", lambda e, hh=hh, qt=qt, pg_=pg_: e.matmul(out=pg_[:, 0:32], lhsT=QT[hh][:, qt * 128:(qt + 1) * 128],
                                                                 rhs=kmT[:, hh, :], start=True, stop=True),
                 reads=[N(f"QT{hh}_{qt // 4}hi"), N(f"QT{hh}_{qt // 4}lo"), N("kmT_init")] + kn, writes=[pgn_])
            P.op("dve", lambda e, g_=g_, pg_=pg_: e.tensor_copy(out=g_[:], in_=pg_[:, 0:32]), reads=[pgn_], writes=[N(f"gt{hh}")])
            P.op("dve", lambda e, j=j, g_=g_: e.memset(g_[:, j:32], -1e30), reads=[N(f"gt{hh}")], writes=[N(f"gt{hh}")])
            P.op("dve", lambda e, g_=g_, m_=m_: e.max(out=m_[:], in_=g_[:]), reads=[N(f"gt{hh}")], writes=[N(f"mx{hh}")])
            P.op("dve", lambda e, hh=hh, qt=qt, g_=g_, m_=m_: e.tensor_scalar(out=Msel[:, hh, qt, :], in0=g_[:],
                                                                             scalar1=m_[:, 2:3], scalar2=None, op0=ALU.is_ge),
                 reads=[N(f"gt{hh}"), N(f"mx{hh}"), N("Msel_init")], writes=[N(f"M{hh}_{qt}")])

    pS3 = [pS[0][:], pS[1][:],
           psT[:].bitcast(F32).rearrange("p a b -> p (a b)").rearrange("p (k q) -> p k q", k=2)]
    pSn = [N("pS0"), N("pS1"), N("psT")]
    ND = 3
    pOb = [[pO[0], pm], [pO[1], prot]]
    pOn = [[N("pO0"), N("pm")], [N("pO1"), N("prot")]]
    pairs = []
    for j in range(NB):
        for hh in range(2):
            order = [j] + list(range(j))
            for idx, n in enumerate(order):
                pairs.append((j, hh, n, idx == len(order) - 1))
    oc = [0]

    def emit_s(i):
        j, hh, n, last = pairs[i]
        b = i % ND
        qs = slice(j * 256, (j + 1) * 256)
        qn = [N(f"QT{hh}_{j // 2}hi"), N(f"QT{hh}_{j // 2}lo")]

        def mS(e):
            for kt in range(2):
                k0 = (2 * n + kt) * 128
                ins = e.matmul(out=pS3[b][:, kt, :], lhsT=KT[hh][:, k0:k0 + 128], rhs=QT[hh][:, qs],
                               start=True, stop=True)
            return ins
        P.op("pe", mS, reads=qn + [N(f"KT{hh}_{n // 2}hi"), N(f"KT{hh}_{n // 2}lo")], writes=[pSn[b]])
        P.op("act", lambda e: e.activation(out=PT[b][:], in_=pS3[b], func=AF.Exp),
             reads=[pSn[b]], writes=[N(f"PT{b}")])
        if n == j:
            P.op("pool", lambda e: e.tensor_tensor(out=PT[b][:], in0=PT[b][:], in1=cmask[:], op=ALU.mult),
                 reads=[N(f"PT{b}"), N("cmask")], writes=[N(f"PT{b}")])

    def emit_o(i):
        j, hh, n, last = pairs[i]
        b = i % ND
        own = (n == j)
        for qi in range(2):
            po_, pn_ = pOb[qi][i % 2], pOn[qi][i % 2]

            def mO(e, qi=qi, po_=po_):
                for kt in range(2):
                    ins = e.matmul(out=po_[:, 0:129], lhsT=PT[b][:, kt, qi * 128:(qi + 1) * 128],
                                   rhs=Vext[:, hh, 2 * n + kt, 0:129], start=(kt == 0), stop=(kt == 1))
                return ins
            P.op("pe", mO, reads=[N(f"PT{b}"), N(f"V{2 * n}"), N(f"V{2 * n + 1}")], writes=[pn_])
            if own:
                P.op("dve", lambda e, qi=qi, po_=po_: e.tensor_copy(out=acc[qi][:, 0:129], in_=po_[:, 0:129]),
                     reads=[pn_], writes=[N(f"acc{qi}")])
            else:
                P.op("dve", lambda e, qi=qi, po_=po_: e.scalar_tensor_tensor(
                    out=acc[qi][:, 0:129], in0=po_[:, 0:129], scalar=Msel[:, hh, 2 * j + qi, n:n + 1],
                    in1=acc[qi][:, 0:129], op0=ALU.mult, op1=ALU.add),
                    reads=[pn_, N(f"M{hh}_{2 * j + qi}"), N(f"acc{qi}")], writes=[N(f"acc{qi}")])
        if last:
            for qi in range(2):
                o_ = oc[0] % 4
                oc[0] += 1
                qt = 2 * j + qi
                P.op("dve", lambda e, qi=qi: e.reciprocal(out=rec[:, qi:qi + 1], in_=acc[qi][:, 128:129]),
                     reads=[N(f"acc{qi}")], writes=[N(f"rec{qi}")])
                P.op("dve", lambda e, qi=qi, o_=o_: e.tensor_scalar(out=ob[o_][:], in0=acc[qi][:, 0:128],
                                                                    scalar1=rec[:, qi:qi + 1], scalar2=None, op0=ALU.mult),
                     reads=[N(f"acc{qi}"), N(f"rec{qi}")], writes=[N(f"ob{o_}")])
                P.dma("sp", lambda e, qt=qt, o_=o_: e.dma_start(
                    out=D["o_out"][qt * 128:(qt + 1) * 128, hh * 128:(hh + 1) * 128], in_=ob[o_][:]),
                    reads=[N(f"ob{o_}")], writes=[f"d:C_o{j // 4}"], lane="S:" + N(f"ob{o_}"))
            if hh == 1 and "after_block" in D:
                D["after_block"](j)

    for i in range(ND - 1):
        emit_s(i)
    for i in range(len(pairs)):
        if i + ND - 1 < len(pairs):
            emit_s(i + ND - 1)
        emit_o(i)


def moba_tables(S=8192):
    inv = (1.0 / (np.float32(500000.0) ** (np.arange(16, dtype=np.float32) / np.float32(16)))).astype(np.float32)
    ang = (np.arange(S, dtype=np.float32)[None, :] * inv[:, None]).astype(np.float32)
    cos = np.cos(ang).astype(np.float32)
    sin = np.sin(ang).astype(np.float32)
    k = np.arange(128)[:, None, None] + 128 * np.arange(2)[None, :, None]
    q = np.arange(256)[None, None, :]
    cmask = (k <= q).astype(np.float32).astype(ml_dtypes.bfloat16)
    return (np.ascontiguousarray(np.concatenate([cos, cos], 0)), np.ascontiguousarray(np.concatenate([-sin, sin], 0)),
            np.ascontiguousarray(cmask))


def build_moba(NT=8192):
    nc = bass.Bass("TRN2", target_bir_lowering=False)
    P = Prog(nc)
    D = {}

    def inp(name, shape, dt=F32):
        D[name] = nc.dram_tensor(name, list(shape), dt, kind="ExternalInput").ap()
    inp("hb", [NT, 1024]); inp("wq", [1024, 256]); inp("wk", [1024, 256]); inp("wv", [1024, 256])
    inp("gq", [128, 8]); inp("gkv", [128, 8]); inp("cos32", [32, NT]); inp("sin32", [32, NT])
    inp("cmask", [128, 2, 256], BF16); inp("ident", [128, 128], BF16)
    D["o_out"] = nc.dram_tensor("o_out", [NT, 256], BF16, kind="ExternalOutput").ap()
    phase_moba(P, nc, D, NT)
    finish(P)
    P.build()
    return nc, P


def moba_maps(h_list, attn_norm, kv_norm, w_q, w_kv, NT=8192):
    cos32, sin32, cmask = moba_tables()
    ident = np.eye(128, dtype=np.float32).astype(ml_dtypes.bfloat16)
    gq = np.ascontiguousarray(attn_norm.reshape(8, 128).T)
    gkv = np.ascontiguousarray(kv_norm.reshape(8, 128).T)
    maps = []
    for c in range(8):
        b, p = c // 4, c % 4
        cs = slice(p * 256, (p + 1) * 256)
        maps.append(dict(hb=np.ascontiguousarray(h_list[b][:NT]), wq=np.ascontiguousarray(w_q[:, cs]),
                         wk=np.ascontiguousarray(w_kv[:, cs]), wv=np.ascontiguousarray(w_kv[:, 1024 + p * 256:1024 + (p + 1) * 256]),
                         gq=gq, gkv=gkv, cos32=np.ascontiguousarray(cos32[:, :NT]), sin32=np.ascontiguousarray(sin32[:, :NT]),
                         cmask=cmask, ident=ident))
    return maps


def finish(P, names=None):
    P.finish()


def ret_tables(S=8192):
    half = 128
    inv = (1.0 / (np.float32(10000.0) ** (np.arange(half, dtype=np.float32) / np.float32(half)))).astype(np.float32)
    ang = (np.arange(S, dtype=np.float32)[None, :] * inv[:, None]).astype(np.float32)
    return np.cos(ang).astype(np.float32), np.sin(ang).astype(np.float32)


def ret_decay(h):
    Cn = 128
    lg = np.log1p(-np.float32(2.0) ** np.float32(-5.0 - h)).astype(np.float32)
    idx = np.arange(Cn, dtype=np.float32)
    diff = idx[:, None] - idx[None, :]
    dm = np.where(diff >= 0, np.exp(lg * np.maximum(diff, 0.0)), 0.0).astype(np.float32)
    dmaskT = (dm.T * np.float32(256 ** -0.5)).astype(np.float32)
    qdec = np.exp(lg * (idx + 1.0)).astype(np.float32)
    kdec = (np.exp(lg * (Cn - 1.0 - idx)) * np.float32(256 ** -0.5)).astype(np.float32)
    cdec = np.exp(lg * np.float32(Cn)).astype(np.float32)
    qdec4 = np.ascontiguousarray(np.broadcast_to(np.tile(qdec, 4)[None, :], (128, 512))).astype(np.float32)
    return dict(dmask=np.ascontiguousarray(dmaskT), qdec4=qdec4,
                kdec=np.ascontiguousarray(kdec[:, None]),
                cdec=np.full((128, 1), cdec, np.float32))


def build_ret(NT=8192):
    nc = bass.Bass("TRN2", target_bir_lowering=False)
    P = Prog(nc)
    D = {}

    def inp(name, shape, dt=F32):
        D[name] = nc.dram_tensor(name, list(shape), dt, kind="ExternalInput").ap()
    inp("xb", [NT, 1024]); inp("w", [1024, 1536]); inp("gain", [128, 8])
    inp("cosT", [128, NT]); inp("sinT", [128, NT]); inp("dmask", [128, 128]); inp("qdec4", [128, 512])
    inp("kdec", [128, 1]); inp("cdec", [128, 1]); inp("ident", [128, 128], BF16)
    D["y_out"] = nc.dram_tensor("y_out", [NT, 512], BF16, kind="ExternalOutput").ap()
    phase_ret(P, nc, D, NT)
    finish(P, ["d:y_out"])
    P.build()
    return nc, P


def run_ret(x, ret_norm, ret_w_in):
    nc, P = build_ret()
    cosT, sinT = ret_tables()
    ident = np.eye(128, dtype=np.float32).astype(ml_dtypes.bfloat16)
    gain = np.ascontiguousarray(ret_norm[0].reshape(8, 128).T)
    maps = []
    for c in range(8):
        b, h = c // 4, c % 4
        w = ret_w_in[0]
        wh = np.concatenate([w[:, h * 256:(h + 1) * 256], w[:, 1024 + h * 256:1024 + (h + 1) * 256],
                             w[:, 2048 + h * 512:2048 + (h + 1) * 512],
                             w[:, 4096 + h * 512:4096 + (h + 1) * 512]], axis=1)
        m = dict(xb=np.ascontiguousarray(x[b]), w=np.ascontiguousarray(wh), gain=gain, cosT=cosT, sinT=sinT,
                 ident=ident)
        m.update(ret_decay(h))
        maps.append(m)
    res = run_bass_kernel_spmd(nc, maps, core_ids=list(range(8)))
    return [r["y_out"] for r in res.results]


GROUPS = [[0, 1, 2, 3], [4, 5, 6, 7]]


def build_fused(upto=4):
    nc = bass.Bass("TRN2", target_bir_lowering=False)
    P = Prog(nc)
    P.want_pid = True

    def inp(name, shape, dt=F32):
        return nc.dram_tensor("in_" + name, list(shape), dt, kind="ExternalInput").ap()

    def scr(name, shape, dt):
        return nc.dram_tensor(name, list(shape), dt).ap()

    def state():
        return (nc.sbuf_base, nc.sbuf_top, nc.psum_base, nc.psum_top)

    def restore(st):
        nc.sbuf_base, nc.sbuf_top, nc.psum_base, nc.psum_top = st

    def allgather(src, dst, reads, wname, lane):
        P.cc(lambda e: e.collective_compute("AllGather", ALU.bypass, replica_groups=GROUPS, ins=[src], outs=[dst]),
             reads=reads, writes=[wname], lane=lane)

    ident = inp("ident", [128, 128], BF16)
    identf = inp("identf", [128, 128])
    y_dram = scr("y_dram", [8192, 512], BF16)
    yall = scr("yall", [8 * 4 * 1024, 512], BF16)
    if upto == 2:
        h1_dram = nc.dram_tensor("h1_dram", [2048, 1024], F32, kind="ExternalOutput").ap()
    else:
        h1_dram = scr("h1_dram", [2048, 1024], F32)
    hn_dram = scr("hn_dram", [2048, 1024], BF16)
    hnall = scr("hnall", [4 * 4 * 512, 1024], BF16)
    o_dram = scr("o_dram", [8192, 256], BF16)
    oall = scr("oall", [8 * 4 * 1024, 256], BF16)
    out = nc.dram_tensor("out", [2048, 1024], F32, kind="ExternalOutput").ap()
    st0 = state()

    def ag_y(i):
        allgather(y_dram[i * 1024:(i + 1) * 1024, :], yall[i * 4096:(i + 1) * 4096, :], [f"d:A_y{i}"],
                  f"d:yall{i}", f"CC:y{i}")

    def after_group(tg):
        if tg >= 2 and tg % 2 == 0:
            ag_y(tg // 2 - 1)
    DA = dict(xb=inp("xb", [8192, 1024]), w=inp("A_w", [1024, 1536]), gain=inp("A_gain", [128, 8]),
              cosT=inp("A_cosT", [128, 8192]), sinT=inp("A_sinT", [128, 8192]), dmask=inp("A_dmask", [128, 128]),
              qdec4=inp("A_qdec4", [128, 512]), kdec=inp("A_kdec", [128, 1]), cdec=inp("A_cdec", [128, 1]),
              ident=ident, y_out=y_dram, after_group=after_group)
    phase_ret(P, nc, DA)
    ag_y(7)
    restore(st0)
    P.barrier()
    if upto == 1:
        finish(P)
        P.build()
        return nc, P

    def ys_B(e, t, dst):
        j, s0 = t // 8, (t % 8) * 128
        src = yall[j * 4096:, :][bass.ds(P.pid["sp"], 4096), :].rearrange("(r s) f -> s r f", r=4)[s0:s0 + 128, :, :]
        return e.dma_start(out=dst[:, 0:2048].rearrange("p (h f) -> p h f", h=4), in_=src)

    def ys_D(e, t, dst):
        j, s0 = t // 8, (t % 8) * 128
        src = oall[j * 4096:, :][bass.ds(P.pid["pool"], 4096), :].rearrange("(r s) f -> s r f", r=4)[s0:s0 + 128, :, :]
        return e.dma_start(out=dst[:, 0:1024].rearrange("p (h f) -> p h f", h=4), in_=src)

    def ffn_inputs(pfx, F, final):
        d = dict(wo=inp(pfx + "wo", [F, 1024]), fgain=inp(pfx + "fgain", [128, 1024]), wr=inp(pfx + "wr", [1024, 20]),
                 rbias=inp(pfx + "rbias", [128, 20]), wg=inp(pfx + "wg", [16, 1024, 512]),
                 wu=inp(pfx + "wu", [16, 1024, 512]), wd=inp(pfx + "wd", [16, 512, 1024]), ident=ident, identf=identf)
        if final:
            d["ngain"] = inp(pfx + "ngain", [128, 1024])
        return d

    DB = ffn_inputs("B_", 2048, False)
    DB.update(xs=inp("xs", [2048, 1024]), ys_fn=ys_B, ys_reads=lambda t: [f"d:yall{2 * q + t // 8}" for q in range(4)],
              h_out=h1_dram, hn_out=hn_dram)
    phase_ffn(P, nc, DB, 2048, False, "B_")
    for i in range(4):
        allgather(hn_dram[i * 512:(i + 1) * 512, :], hnall[i * 2048:(i + 1) * 2048, :], [f"d:B_hn{i}"],
                  f"d:hnall{i}", f"CC:hn{i}")
    restore(st0)
    P.barrier()
    if upto == 2:
        finish(P)
        P.build()
        return nc, P

    def hn_src(r0):
        r, i, s_ = r0 // 2048, (r0 % 2048) // 512, r0 % 512
        row = (i * 4 + r) * 512 + s_
        return hnall[row:row + 128, :]

    def after_block(j):
        if j % 4 == 3:
            i = j // 4
            allgather(o_dram[i * 1024:(i + 1) * 1024, :], oall[i * 4096:(i + 1) * 4096, :], [f"d:C_o{i}"],
                      f"d:oall{i}", f"CC:o{i}")
    DC = dict(wq=inp("C_wq", [1024, 256]), wk=inp("C_wk", [1024, 256]), wv=inp("C_wv", [1024, 256]),
              gq=inp("C_gq", [128, 8]), gkv=inp("C_gkv", [128, 8]), cos32=inp("C_cos32", [32, 8192]),
              sin32=inp("C_sin32", [32, 8192]), cmask=inp("C_cmask", [128, 2, 256], BF16), ident=ident,
              hn_src=hn_src, hn_reads=lambda r0: [f"d:hnall{(r0 % 2048) // 512}"], o_out=o_dram,
              after_block=after_block)
    phase_moba(P, nc, DC)
    restore(st0)
    P.barrier()
    if upto == 3:
        finish(P)
        P.build()
        return nc, P

    DD = ffn_inputs("D_", 1024, True)
    DD.update(xs=h1_dram, xs_reads=["d:B_h_out"], ys_fn=ys_D, ys_eng="pool", ys_reads=lambda t: [f"d:oall{2 * q + t // 8}" for q in range(4)],
              h_out=out)
    phase_ffn(P, nc, DD, 1024, True, "D_")
    finish(P)
    P.build()
    return nc, P


def fused_maps(x, ret_norm, ret_w_in, ret_w_out, kv_norm, w_kv, attn_norm, w_q, w_o, ffn_norm,
               router_group_w, router_group_b, router_expert_w, router_expert_b,
               expert_w_gate, expert_w_up, expert_w_down, final_norm):
    cosT, sinT = ret_tables()
    cos32, sin32, cmask = moba_tables()
    ident = np.eye(128, dtype=np.float32).astype(ml_dtypes.bfloat16)
    identf = np.eye(128, dtype=np.float32)
    bc = lambda v, n: np.ascontiguousarray(np.broadcast_to(v[None, :], (128, n)))
    common = dict(ident=ident, identf=identf, A_gain=np.ascontiguousarray(ret_norm[0].reshape(8, 128).T),
                  A_cosT=cosT, A_sinT=sinT, C_cos32=cos32, C_sin32=sin32, C_cmask=cmask,
                  C_gq=np.ascontiguousarray(attn_norm[0].reshape(8, 128).T),
                  C_gkv=np.ascontiguousarray(kv_norm.reshape(8, 128).T))
    for pfx, l, wo in (("B_", 0, ret_w_out[0]), ("D_", 1, w_o[0])):
        rb = np.concatenate([router_group_b[l], router_expert_b[l].reshape(-1)])
        common.update({
            pfx + "wo": np.ascontiguousarray(wo), pfx + "fgain": bc(ffn_norm[l], 1024),
            pfx + "wr": np.ascontiguousarray(np.concatenate([router_group_w[l]] + [router_expert_w[l][g] for g in range(4)], axis=1)),
            pfx + "rbias": bc(rb, 20),
            pfx + "wg": np.ascontiguousarray(expert_w_gate[l].reshape(16, 1024, 512)),
            pfx + "wu": np.ascontiguousarray(expert_w_up[l].reshape(16, 1024, 512)),
            pfx + "wd": np.ascontiguousarray(expert_w_down[l].reshape(16, 512, 1024))})
    common["D_ngain"] = bc(final_norm, 1024)
    dec = [ret_decay(h) for h in range(4)]
    maps = []
    w = ret_w_in[0]
    for c in range(8):
        b, r = c // 4, c % 4
        wh = np.concatenate([w[:, r * 256:(r + 1) * 256], w[:, 1024 + r * 256:1024 + (r + 1) * 256],
                             w[:, 2048 + r * 512:2048 + (r + 1) * 512], w[:, 4096 + r * 512:4096 + (r + 1) * 512]], axis=1)
        cs = slice(r * 256, (r + 1) * 256)
        m = dict(common, xb=np.ascontiguousarray(x[b]), xs=np.ascontiguousarray(x[b, r * 2048:(r + 1) * 2048]),
                 A_w=np.ascontiguousarray(wh), A_dmask=dec[r]["dmask"], A_qdec4=dec[r]["qdec4"], A_kdec=dec[r]["kdec"],
                 A_cdec=dec[r]["cdec"], C_wq=np.ascontiguousarray(w_q[0][:, cs]), C_wk=np.ascontiguousarray(w_kv[:, cs]),
                 C_wv=np.ascontiguousarray(w_kv[:, 1024 + r * 256:1024 + (r + 1) * 256]))
        maps.append({"in_" + k: v for k, v in m.items()})
    return maps


_CACHE = {}


def kernel(**inputs):
    a = {k: np.asarray(v, dtype=np.float32) for k, v in inputs.items()}
    if "nc" not in _CACHE:
        _CACHE["nc"] = build_fused()[0]
    maps = fused_maps(**a)
    res = run_bass_kernel_spmd(_CACHE["nc"], maps, core_ids=list(range(8)))
    out = np.stack([np.concatenate([np.asarray(res.results[b * 4 + q]["out"]) for q in range(4)], axis=0)
                    for b in range(2)])
    return out.astype(np.float32)
```

```python
import numpy as np
import ml_dtypes
import concourse.bass as bass
import concourse.mybir as mybir
from concourse.bass_utils import run_bass_kernel_spmd

F32 = mybir.dt.float32
BF16 = mybir.dt.bfloat16
AF = mybir.ActivationFunctionType
ALU = mybir.AluOpType
AX = mybir.AxisListType

EPS = 1e-6
ENGS = ("pe", "act", "dve", "pool", "sp")


class _First:
    def __init__(self, eng, w):
        self._e, self._w = eng, w

    def __getattr__(self, name):
        real = getattr(self._e, name)
        if not callable(real):
            return real

        def call(*a, **k):
            ins = real(*a, **k)
            if self._w is not None and hasattr(ins, "_wait_ge"):
                ins._wait_ge(*self._w)
                self._w = None
            return ins
        return call


class _Dummy:
    def then_inc(self, *a, **k):
        return self

    def _wait_ge(self, *a, **k):
        return self


class _Rec:
    def __init__(self):
        self.calls = []

    def __getattr__(self, name):
        def call(*a, **k):
            self.calls.append((name, a, k))
            return _Dummy()
        return call


class Prog:
    def __init__(self, nc):
        self.nc = nc
        self.ops = []
        self.want_pid = False
        self.reorder = True
        self.debug = None
        self.fifo = FIFO_Q
        self.psum = set()

    def op(self, eng, fn, reads=(), writes=()):
        self.ops.append(dict(eng=eng, fn=fn, r=tuple(reads), w=tuple(writes), dma=None))

    def dma(self, eng, fn, reads=(), writes=(), lane=None):
        assert lane is not None
        self.ops.append(dict(eng=eng, fn=fn, r=tuple(reads), w=tuple(writes), dma=lane))

    def cc(self, fn, reads=(), writes=(), lane=None):
        self.ops.append(dict(eng="pool", fn=fn, r=tuple(reads), w=tuple(writes), dma=lane, cc=True))

    def barrier(self):
        self.ops.append(dict(eng="barrier", fn=None, r=(), w=(), dma=None))

    def finish(self):
        self.ops.append(dict(eng="sp", fn=lambda e: e.nop(), r=(), w=(), dma=None, fin=True))

    def _cost(self, o):
        E = o["eng"]
        rec = _Rec()
        try:
            o["fn"](rec)
        except Exception:
            return (50.0, 2000.0) if o["dma"] is not None else (500.0, 0.0)
        busy, lat = 0.0, 0.0
        for name, a, k in rec.calls:
            out = k.get("out", a[0] if a else None)
            if name == "dma_start":
                src = k.get("in_")
                nb = 1
                for d_ in out.shape:
                    nb *= d_
                nb *= max(mybir.dt.size(out.dtype), mybir.dt.size(src.dtype))
                busy += 50.0
                lat += nb / 250.0
            elif name in ("matmul", "transpose"):
                src = k.get("lhsT") if name == "matmul" else k.get("in_")
                f = 4.0 if src.dtype == F32 else 1.0
                busy += (16.0 + 0.46 * out.free_size()) * f
            elif name == "collective_compute":
                busy += 100.0
            elif out is None or not hasattr(out, "free_size"):
                busy += 50.0
            else:
                nf = out.free_size()
                busy += {"act": 200.0 + 1.05 * nf, "dve": 100.0 + 1.4 * nf, "pool": 150.0 + 4.6 * nf}.get(E, 100.0 + nf)
        return busy, lat

    def _sched_seg(self, seg):
        n = len(seg)
        if n < 3:
            return seg
        deps = [set() for _ in range(n)]
        last_w, readers = {}, {}
        for i, o in enumerate(seg):
            d = deps[i]
            for b in o["r"]:
                if b in last_w:
                    d.add(last_w[b])
                if b in self.psum:
                    for r in readers.get(b, ()):
                        if seg[r]["eng"] != o["eng"]:
                            d.add(r)
            for b in o["w"]:
                if b in last_w:
                    d.add(last_w[b])
                d.update(readers.get(b, ()))
            d.discard(i)
            for b in o["w"]:
                last_w[b] = i
                readers[b] = []
            for b in o["r"]:
                readers.setdefault(b, []).append(i)
        succ = [[] for _ in range(n)]
        indeg = [len(d) for d in deps]
        for i, d in enumerate(deps):
            for x in d:
                succ[x].append(i)
        cost = [self._cost(o) for o in seg]
        free_at = dict.fromkeys(ENGS, 0.0)
        dma_free = 0.0
        ready = [0.0] * n
        ext = getattr(self, "_ext", {})
        for i, o in enumerate(seg):
            for b in o["r"]:
                if b in ext:
                    ready[i] = max(ready[i], ext[b])
        avail = {E: [] for E in ENGS}
        for i in range(n):
            if indeg[i] == 0:
                avail[seg[i]["eng"]].append(i)
        order = []
        SLACK = 150.0
        while len(order) < n:
            best = None
            for E in ENGS:
                av = avail[E]
                if not av:
                    continue
                t0 = max(free_at[E], min(ready[i] for i in av))
                if self.fifo:
                    cand = min((i for i in av if ready[i] <= t0 + SLACK), key=lambda i: (int(ready[i] / self.fifo), i))
                else:
                    cand = min(i for i in av if ready[i] <= t0 + SLACK)
                st = max(free_at[E], ready[cand])
                if best is None or (st, cand) < best[:2]:
                    best = (st, cand, E)
            st, i, E = best
            avail[E].remove(i)
            busy, lat = cost[i]
            free_at[E] = st + busy
            if seg[i].get("cc"):
                fin = st + 150000.0
            elif seg[i]["dma"] is not None:
                dma_free = max(st + busy, dma_free) + lat
                fin = dma_free + 2000.0
            else:
                fin = st + busy + 100.0
            order.append(i)
            if self.debug is not None:
                self.debug.append((seg[i], st, fin, E))
            for j in succ[i]:
                if fin > ready[j]:
                    ready[j] = fin
                indeg[j] -= 1
                if indeg[j] == 0:
                    avail[seg[j]["eng"]].append(j)
        self.sim_time = getattr(self, "sim_time", 0.0) + max(max(free_at.values()), dma_free)
        self._ext = {}
        k = 0
        for i in order:
            if seg[i].get("cc"):
                k += 1
        j = 0
        for i in order:
            if seg[i].get("cc"):
                j += 1
                late = max(0.0, 60000.0 * (j - (k - 4))) if j > k - 4 else 0.0
                for b in seg[i]["w"]:
                    self._ext[b] = late
        return [seg[i] for i in order]

    def _schedule(self, ops):
        out, seg = [], []
        for o in ops:
            if o["eng"] == "barrier" or o.get("fin"):
                out += self._sched_seg(seg)
                seg = []
                out.append(o)
            else:
                seg.append(o)
        return out + self._sched_seg(seg)

    def build(self):
        nc = self.nc
        if self.reorder:
            self.ops = self._schedule(self.ops)
        ops = self.ops
        n = len(ops)
        last_w = {}
        readers = {}
        deps = [None] * n
        bar_deps = set()
        last_on_eng = {}
        lanes_last = {}
        first_after_bar = {}
        for i, o in enumerate(ops):
            if o["eng"] == "barrier":
                bar_deps = set(last_on_eng.values()) | {v for k, v in lanes_last.items() if not k.startswith("CC:")}
                first_after_bar = {}
                last_w = {k: v for k, v in last_w.items() if k.startswith("d:")}
                readers = {k: v for k, v in readers.items() if k.startswith("d:")}
                deps[i] = set()
                continue
            raw = set()
            oth = set()
            for b in o["r"]:
                if b in last_w:
                    raw.add(last_w[b])
                if b in self.psum:
                    for r in readers.get(b, ()):
                        if ops[r]["eng"] != o["eng"]:
                            raw.add(r)
            for b in o["w"]:
                if b in last_w:
                    oth.add(last_w[b])
                for r in readers.get(b, ()):
                    oth.add(r)
            E = o["eng"]
            d = set()
            for x in raw | oth:
                if x == i:
                    continue
                ox = ops[x]
                if ox["dma"] is None and o["dma"] is None and ox["eng"] == E:
                    if E == "pe":
                        continue
                    if x not in raw:
                        continue
                d.add(x)
            if o.get("fin"):
                d |= set(last_on_eng.values()) | set(lanes_last.values())
            if bar_deps and E not in first_after_bar:
                d |= bar_deps
                first_after_bar[E] = i
            deps[i] = d
            for b in o["w"]:
                last_w[b] = i
                readers[b] = []
            for b in o["r"]:
                lst = readers.setdefault(b, [])
                if o["dma"] is None:
                    lst[:] = [r for r in lst if not (ops[r]["dma"] is None and ops[r]["eng"] == E)]
                lst.append(i)
            if o["dma"] is None:
                last_on_eng[E] = i
            else:
                lanes_last[o["dma"]] = i

        pos = [0] * n
        eng_count = {e: 0 for e in ENGS}
        lane_count = {}
        seen = {e: {} for e in ENGS}
        snap = [None] * n
        waits = [None] * n
        signals = [False] * n
        for i, o in enumerate(ops):
            if o["eng"] == "barrier":
                continue
            E = o["eng"]
            w = []
            for d in sorted(deps[i], reverse=True):
                od = ops[d]
                key = ("L", od["dma"]) if od["dma"] is not None else ("E", od["eng"])
                need = pos[d]
                if od["dma"] is not None and od["dma"].startswith("G:"):
                    need = lane_count[od["dma"]]
                if seen[E].get(key, 0) >= need:
                    continue
                w.append((key, d, need))
                if od["dma"] is None:
                    signals[d] = True
                seen[E][key] = need
                for k2, v2 in snap[d].items():
                    if seen[E].get(k2, 0) < v2:
                        seen[E][k2] = v2
            waits[i] = w
            if o["dma"] is not None:
                lane_count[o["dma"]] = lane_count.get(o["dma"], 0) + 1
                pos[i] = lane_count[o["dma"]]
            else:
                eng_count[E] += 1
                pos[i] = eng_count[E]
            snap[i] = dict(seen[E])

        sigval = [0] * n
        cnt = {e: 0 for e in ENGS}
        for i, o in enumerate(ops):
            if o["eng"] == "barrier" or o["dma"] is not None:
                continue
            if signals[i]:
                cnt[o["eng"]] += 1
            sigval[i] = cnt[o["eng"]]

        sems = {e: nc.alloc_semaphore(f"s_{e}") for e in ENGS}
        lane_sems = {ln: nc.alloc_semaphore(f"l{j}") for j, ln in enumerate(sorted(lane_count))}
        per_eng = {e: [i for i, o in enumerate(ops) if o["eng"] == e] for e in ENGS}
        self.stats = dict(n_ops=n, lanes=len(lane_sems), sig=dict(cnt),
                          nwaits=sum(len(w) for w in waits if w))

        self.pid = {}

        def run(E, eng):
            if E in ("sp", "pool") and self.want_pid:
                r = eng.alloc_register("qoff")
                eng.reg_mod(r, eng.partition_id(), 4)
                eng.reg_mul(r, r, 8192)
                self.pid[E] = eng.snap(r, min_val=0, max_val=3 * 8192)
            for i in per_eng[E]:
                o = ops[i]
                wl = []
                for key, d, need in waits[i]:
                    if key[0] == "L":
                        wl.append((lane_sems[key[1]], 1 if ops[d].get("cc") else 16 * need))
                    else:
                        wl.append((sems[key[1]], sigval[d]))
                attach = bool(wl) and not o.get("cc") and not o.get("fin")
                for sem_, val_ in (wl[:-1] if attach else wl):
                    eng.wait_ge(sem_, val_)
                ins = o["fn"](_First(eng, wl[-1]) if attach else eng)
                if o.get("cc"):
                    ins.then_inc(lane_sems[o["dma"]])
                elif o["dma"] is not None:
                    ins.then_inc(lane_sems[o["dma"]], 16)
                elif signals[i]:
                    ins.then_inc(sems[E], 1)

        with nc.Block() as block:
            @block.tensor
            def _(e):
                run("pe", e)

            @block.scalar
            def _(e):
                run("act", e)

            @block.vector
            def _(e):
                run("dve", e)

            @block.gpsimd
            def _(e):
                run("pool", e)

            @block.sync
            def _(e):
                run("sp", e)


class Ctx:
    def __init__(self, nc, P, pfx):
        self.nc, self.P, self.pfx = nc, P, pfx

    def sb(self, name, shape, dt):
        return self.nc.alloc_sbuf_tensor(f"{self.pfx}{name}", list(shape), dt)

    def ps(self, name, shape, dt=F32):
        return self.nc.alloc_psum_tensor(f"{self.pfx}{name}", list(shape), dt)


def rstd_ops(P, ss, tmp, rstd, n, rd, wr):
    P.op("act", lambda e: e.activation(out=tmp, in_=ss, func=AF.Ln, scale=1.0 / n, bias=EPS),
         reads=rd, writes=[wr + "_t"])
    P.op("act", lambda e: e.activation(out=rstd, in_=tmp, func=AF.Exp, scale=-0.5),
         reads=[wr + "_t"], writes=[wr])


A_ENGS = ("dve", "pool", "pool")
FIFO_Q = 0.0
NXT = 6


def phase_ret(P, nc, D, NT=8192, NS=2):
    C = Ctx(nc, P, "A_")
    RENG, QENG, GENG = A_ENGS
    NG = NT // 512
    ident = C.sb("ident", [128, 128], BF16)
    w_bf = C.sb("w_bf", [128, 8, 1536], BF16)
    wst = [C.sb(f"wst{i}", [128, 1536], F32) for i in range(2)]
    gain = C.sb("gain", [128, 8], F32)
    dmask = C.sb("dmask", [128, 128], F32)
    qdec4 = C.sb("qdec4", [128, 512], F32)
    kdec = C.sb("kdec", [128, 1], F32)
    cdec = C.sb("cdec", [128, 1], F32)
    xt = [C.sb(f"xt{i}", [128, 1024], F32) for i in range(NXT)]
    junk = C.sb("junk", [128, 1024], BF16)
    xn = [C.sb(f"xn{i}", [128, 1024], BF16) for i in range(2)]
    xnT = [C.sb(f"xnT{i}", [128, 8, 512], BF16) for i in range(NS)]
    cosg = [C.sb(f"cos{i}", [128, 512], F32) for i in range(NS)]
    sing = [C.sb(f"sin{i}", [128, 512], F32) for i in range(NS)]
    qrot = [C.sb(f"qrot{i}", [128, 2, 512], BF16) for i in range(NS)]
    qd = [C.sb(f"qd{i}", [128, 2, 512], BF16) for i in range(NS)]
    krot = [C.sb(f"krot{i}", [128, 2, 512], BF16) for i in range(NS)]
    vv = [C.sb(f"v{i}", [128, 4, 512], BF16) for i in range(NS)]
    sg = [C.sb(f"sg{i}", [128, 4, 512], F32) for i in range(NS)]
    ktm = [C.sb(f"ktm{i}", [128, 4, 256], BF16) for i in range(NS)]
    tmp = [C.sb(f"tmp{i}", [128, 512], F32) for i in range(4)]
    st32 = C.sb("st32", [128, 2, 512], F32)
    stbf = [C.sb(f"stbf{i}", [128, 2, 512], BF16) for i in range(2)]
    stm = [C.sb(f"stm{i}", [128, 128], BF16) for i in range(2)]
    ybuf = [C.sb(f"ybuf{i}", [128, 512], BF16) for i in range(4)]
    stat = C.sb("stat", [128, 24], F32)
    junk2 = C.sb("junk2", [128, 512], BF16)
    ge = C.sb("ge", [128, 512], F32)
    gc = C.sb("gc", [128, 512], F32)

    psT = C.ps("psT", [128, 8, 128], BF16)
    pq = [C.ps(f"pq{i}", [128, 512]) for i in range(2)]
    pv = C.ps("pv", [128, 512])
    pg = C.ps("pg", [128, 512])
    pst = C.ps("pst", [128, 128])
    po = C.ps("po", [128, 512])
    pu = C.ps("pu", [128, 512])

    P.psum |= {"A_psT", "A_pq0", "A_pq1", "A_pv", "A_pg", "A_pst", "A_po", "A_pu"}

    def ld(dst, src, name, eng="sp", lane=None):
        P.dma(eng, lambda e: e.dma_start(out=dst, in_=src), writes=[name], lane=lane or ("L:" + name))

    ld(ident[:], D["ident"], "A_ident", lane="G:const")
    ld(gain[:], D["gain"], "A_gain", lane="G:const")
    ld(dmask[:], D["dmask"], "A_dmask", lane="G:const")
    ld(qdec4[:], D["qdec4"], "A_qdec4", lane="G:const")
    ld(kdec[:], D["kdec"], "A_kdec", lane="G:const")
    ld(cdec[:], D["cdec"], "A_cdec", lane="G:const")
    wv_ = D["w"].rearrange("(kc p) f -> p kc f", p=128)
    for kc in range(8):
        s = kc % 2
        ld(wst[s][:], wv_[:, kc, :], f"A_wst{s}")
        P.op("dve", lambda e, kc=kc, s=s: e.tensor_scalar(
            out=w_bf[:, kc, :], in0=wst[s][:], scalar1=gain[:, kc:kc + 1], scalar2=None, op0=ALU.mult),
            reads=[f"A_wst{s}", "A_gain"], writes=[f"A_w{kc}"])
    WN = [f"A_w{kc}" for kc in range(8)]
    P.op("dve", lambda e: e.memset(st32[:], 0.0), writes=["A_st32_0", "A_st32_1"])
    P.op("pool", lambda e: e.memset(stbf[0][:], 0.0), writes=["A_stbf0_0", "A_stbf0_1"])

    xb = D["xb"]
    for tg in range(NG):
        s = tg % NS
        t0 = tg * 512
        ld(cosg[s][:], D["cosT"][:, t0:t0 + 512], f"A_cos{s}")
        ld(sing[s][:], D["sinT"][:, t0:t0 + 512], f"A_sin{s}")
        for tt in range(4):
            xs = (tg * 4 + tt) % NXT
            r0 = t0 + tt * 128
            ld(xt[xs][:], xb[r0:r0 + 128, :], f"A_xt{xs}")
            c0 = 3 * tt
            P.op("act", lambda e, xs=xs, c0=c0: e.activation(out=junk[:], in_=xt[xs][:], func=AF.Square,
                                                            accum_out=stat[:, c0:c0 + 1]),
                 reads=[f"A_xt{xs}"], writes=[f"A_ss{tt}"])
            rstd_ops(P, stat[:, c0:c0 + 1], stat[:, c0 + 1:c0 + 2], stat[:, c0 + 2:c0 + 3], 1024, [f"A_ss{tt}"],
                     f"A_rstd{tt}")
            xq = tt % 2
            P.op("act", lambda e, xs=xs, xq=xq, c0=c0: e.activation(out=xn[xq][:], in_=xt[xs][:], func=AF.Copy,
                                                                  scale=stat[:, c0 + 2:c0 + 3]),
                 reads=[f"A_xt{xs}", f"A_rstd{tt}"], writes=[f"A_xn{xq}"])

            def tr(e, xq=xq):
                for kc in range(8):
                    ins = e.transpose(out=psT[:, kc, :], in_=xn[xq][:, kc * 128:(kc + 1) * 128],
                                      identity=ident[:])
                return ins
            P.op("pe", tr, reads=[f"A_xn{xq}", "A_ident"], writes=["A_psT"])
            P.op("dve", lambda e, s=s, tt=tt: e.tensor_copy(out=xnT[s][:, :, tt * 128:(tt + 1) * 128],
                                                            in_=psT[:]),
                 reads=["A_psT"], writes=[f"A_xnT{s}_{tt}"])
        XN = [f"A_xnT{s}_{tt}" for tt in range(4)]

        for which, dst, c0 in (("q", qrot, 0), ("k", krot, 256)):
            for hh in range(2):
                def mm(e, hh=hh, c0=c0, s=s):
                    for kc in range(8):
                        ins = e.matmul(out=pq[hh][:], lhsT=w_bf[:, kc, c0 + hh * 128:c0 + (hh + 1) * 128],
                                       rhs=xnT[s][:, kc, :], start=(kc == 0), stop=(kc == 7))
                    return ins
                P.op("pe", mm, reads=WN + XN, writes=[f"A_pq{hh}"])
            cs, sn = cosg[s], sing[s]
            P.op("dve", lambda e, cs=cs: e.tensor_tensor(out=tmp[0][:], in0=pq[0][:], in1=cs[:], op=ALU.mult),
                 reads=["A_pq0", f"A_cos{s}"], writes=["A_tmp0"])
            P.op("dve", lambda e, sn=sn: e.tensor_tensor(out=tmp[1][:], in0=pq[1][:], in1=sn[:], op=ALU.mult),
                 reads=["A_pq1", f"A_sin{s}"], writes=["A_tmp1"])
            P.op("dve", lambda e, cs=cs: e.tensor_tensor(out=tmp[2][:], in0=pq[1][:], in1=cs[:], op=ALU.mult),
                 reads=["A_pq1", f"A_cos{s}"], writes=["A_tmp2"])
            P.op("dve", lambda e, sn=sn: e.tensor_tensor(out=tmp[3][:], in0=pq[0][:], in1=sn[:], op=ALU.mult),
                 reads=["A_pq0", f"A_sin{s}"], writes=["A_tmp3"])
            P.op(RENG, lambda e, dst=dst, s=s: e.tensor_tensor(out=dst[s][:, 0, :], in0=tmp[0][:], in1=tmp[1][:],
                                                                 op=ALU.subtract),
                 reads=["A_tmp0", "A_tmp1"], writes=[f"A_{which}rot{s}_0"])
            P.op(RENG, lambda e, dst=dst, s=s: e.tensor_tensor(out=dst[s][:, 1, :], in0=tmp[2][:], in1=tmp[3][:],
                                                                 op=ALU.add),
                 reads=["A_tmp2", "A_tmp3"], writes=[f"A_{which}rot{s}_1"])
            if which == "q":
                for dc in range(2):
                    P.op(QENG, lambda e, dc=dc, s=s: e.tensor_tensor(out=qd[s][:, dc, :], in0=qrot[s][:, dc, :],
                                                                       in1=qdec4[:], op=ALU.mult),
                         reads=[f"A_qrot{s}_{dc}", "A_qdec4"], writes=[f"A_qd{s}_{dc}"])

        for tt in range(4):
            def mv(e, tt=tt, s=s):
                for kc in range(8):
                    ins = e.matmul(out=pv[:], lhsT=xnT[s][:, kc, tt * 128:(tt + 1) * 128],
                                   rhs=w_bf[:, kc, 512:1024], start=(kc == 0), stop=(kc == 7))
                return ins
            P.op("pe", mv, reads=WN + [XN[tt]], writes=["A_pv"])
            P.op("act", lambda e, tt=tt, s=s: e.activation(out=vv[s][:, tt, :], in_=pv[:], func=AF.Copy),
                 reads=["A_pv"], writes=[f"A_v{s}_{tt}"])

            def mg(e, tt=tt, s=s):
                for kc in range(8):
                    ins = e.matmul(out=pg[:], lhsT=xnT[s][:, kc, tt * 128:(tt + 1) * 128],
                                   rhs=w_bf[:, kc, 1024:1536], start=(kc == 0), stop=(kc == 7))
                return ins
            P.op("pe", mg, reads=WN + [XN[tt]], writes=["A_pg"])
            P.op("act", lambda e: e.activation(out=ge[:], in_=pg[:], func=AF.Exp, scale=-1.0),
                 reads=["A_pg"], writes=["A_ge"])
            P.op("act", lambda e: e.activation(out=gc[:], in_=pg[:], func=AF.Copy),
                 reads=["A_pg"], writes=["A_gc"])
            P.op("dve", lambda e: e.tensor_scalar(out=ge[:], in0=ge[:], scalar1=1.0, scalar2=None, op0=ALU.add),
                 reads=["A_ge"], writes=["A_ge"])
            P.op("dve", lambda e: e.reciprocal(out=ge[:], in_=ge[:]), reads=["A_ge"], writes=["A_ge"])
            P.op(GENG, lambda e, tt=tt, s=s: e.tensor_tensor(out=sg[s][:, tt, :], in0=gc[:], in1=ge[:], op=ALU.mult),
                 reads=["A_ge", "A_gc"], writes=[f"A_sg{s}_{tt}"])

            def tk(e, tt=tt, s=s):
                for dc in range(2):
                    ins = e.transpose(out=psT[:, dc, :], in_=krot[s][:, dc, tt * 128:(tt + 1) * 128],
                                      identity=ident[:])
                return ins
            P.op("pe", tk, reads=[f"A_krot{s}_0", f"A_krot{s}_1", "A_ident"], writes=["A_psT"])
            P.op("dve", lambda e, tt=tt, s=s: e.tensor_scalar(
                out=ktm[s][:, tt, :].rearrange("p (a b) -> p a b", a=2), in0=psT[:, 0:2, :],
                scalar1=kdec[:, 0:1], scalar2=None, op0=ALU.mult),
                reads=["A_psT", "A_kdec"], writes=[f"A_ktm{s}_{tt}"])

        for tt in range(4):
            c = tg * 4 + tt
            sp_ = c % 2
            sl = slice(tt * 128, (tt + 1) * 128)

            def ms(e, s=s, sl=sl):
                for dc in range(2):
                    ins = e.matmul(out=pst[:], lhsT=krot[s][:, dc, sl], rhs=qrot[s][:, dc, sl],
                                   start=(dc == 0), stop=(dc == 1))
                return ins
            P.op("pe", ms, reads=[f"A_krot{s}_0", f"A_krot{s}_1", f"A_qrot{s}_0", f"A_qrot{s}_1"],
                 writes=["A_pst"])
            P.op("dve", lambda e, sp_=sp_: e.tensor_tensor(out=stm[sp_][:], in0=pst[:], in1=dmask[:], op=ALU.mult),
                 reads=["A_pst", "A_dmask"], writes=[f"A_stm{sp_}"])

            def mo(e, s=s, sl=sl, sp_=sp_, tt=tt):
                e.matmul(out=po[:], lhsT=stm[sp_][:], rhs=vv[s][:, tt, :], start=True, stop=False)
                for dc in range(2):
                    ins = e.matmul(out=po[:], lhsT=qd[s][:, dc, sl], rhs=stbf[sp_][:, dc, :],
                                   start=False, stop=(dc == 1))
                return ins
            P.op("pe", mo, reads=[f"A_stm{sp_}", f"A_v{s}_{tt}", f"A_qd{s}_0", f"A_qd{s}_1",
                                  f"A_stbf{sp_}_0", f"A_stbf{sp_}_1"], writes=["A_po"])
            for dc in range(2):
                P.op("pe", lambda e, s=s, tt=tt, dc=dc: e.matmul(
                    out=pu[:], lhsT=ktm[s][:, tt, dc * 128:(dc + 1) * 128], rhs=vv[s][:, tt, :],
                    start=True, stop=True),
                    reads=[f"A_ktm{s}_{tt}", f"A_v{s}_{tt}"], writes=["A_pu"])
                P.op("dve", lambda e, dc=dc: e.scalar_tensor_tensor(
                    out=st32[:, dc, :], in0=st32[:, dc, :], scalar=cdec[:, 0:1], in1=pu[:],
                    op0=ALU.mult, op1=ALU.add),
                    reads=["A_pu", "A_cdec", f"A_st32_{dc}"], writes=[f"A_st32_{dc}"])
                P.op("act", lambda e, dc=dc, sp_=sp_: e.activation(out=stbf[1 - sp_][:, dc, :], in_=st32[:, dc, :],
                                                                   func=AF.Copy),
                     reads=[f"A_st32_{dc}"], writes=[f"A_stbf{1 - sp_}_{dc}"])
            g0 = 12 + 3 * (c % 2)
            P.op("act", lambda e, g0=g0: e.activation(out=junk2[:], in_=po[:], func=AF.Square,
                                                      accum_out=stat[:, g0:g0 + 1]),
                 reads=["A_po"], writes=[f"A_ssq{c % 2}"])
            rstd_ops(P, stat[:, g0:g0 + 1], stat[:, g0 + 1:g0 + 2], stat[:, g0 + 2:g0 + 3], 512, [f"A_ssq{c % 2}"],
                     f"A_rs{c % 2}")
            yb = c % 4
            P.op("dve", lambda e, yb=yb, s=s, tt=tt, g0=g0: e.scalar_tensor_tensor(
                out=ybuf[yb][:], in0=po[:], scalar=stat[:, g0 + 2:g0 + 3], in1=sg[s][:, tt, :],
                op0=ALU.mult, op1=ALU.mult),
                reads=["A_po", f"A_rs{c % 2}", f"A_sg{s}_{tt}"], writes=[f"A_ybuf{yb}"])
            P.dma("sp", lambda e, yb=yb, c=c: e.dma_start(out=D["y_out"][c * 128:(c + 1) * 128, :], in_=ybuf[yb][:]),
                  reads=[f"A_ybuf{yb}"], writes=[f"d:A_y{c // 8}"], lane=f"S:A_ybuf{yb}")
        if "after_group" in D:
            D["after_group"](tg)


def phase_ffn(P, nc, D, F, final, pfx, NTOK=2048, NE=16, stage=3):
    C = Ctx(nc, P, pfx)
    assert F <= 2048
    N = lambda s: pfx + s
    NTILE = NTOK // 128
    FC = F // 128
    NGRP = NTOK // 512
    ident = C.sb("ident", [128, 128], BF16)
    identf = C.sb("identf", [128, 128], F32)
    h = C.sb("h", [128, NTILE, 1024], F32)
    hnT = C.sb("hnT", [128, 8, NTOK], BF16)
    wbuf = C.sb("wbuf", [128, 24576], BF16)
    wo_v = wbuf[:, 0:FC * 1024].rearrange("p (f n) -> p f n", f=FC)

    def wslot(s):
        b = s * 12288
        return (wbuf[:, b:b + 4096].rearrange("p (k f) -> p k f", k=8),
                wbuf[:, b + 4096:b + 8192].rearrange("p (k f) -> p k f", k=8),
                wbuf[:, b + 8192:b + 12288].rearrange("p (k f) -> p k f", k=4))
    yt = [C.sb(f"yt{i}", [128, F], BF16) for i in range(2)]
    yT = [C.sb(f"yT{i}", [128, FC, 128], BF16) for i in range(2)]
    hn32 = [C.sb(f"hn32_{i}", [128, 1024], F32) for i in range(2)]
    hnT32 = [C.sb(f"hnT32_{i}", [128, 8, 128], F32) for i in range(2)]
    fgain = C.sb("fgain", [128, 1024], F32)
    wr = C.sb("wr", [128, 8, 20], F32)
    rbias = C.sb("rbias", [128, 20], F32)
    comb = C.sb("comb", [128, NTILE, 16], F32)
    rts = [C.sb(f"rt{i}", [128, 64], F32) for i in range(4)]
    st = C.sb("st", [128, 3, NTILE], F32)
    junk = C.sb("junk", [128, 1024], BF16)
    sgl = [C.sb(f"sgl{i}", [128, 512], F32) for i in range(2)]
    hT = [C.sb(f"hT{i}", [128, 4, 512], BF16) for i in range(2)]
    if final:
        ngain = C.sb("ngain", [128, 1024], F32)
        ob = [C.sb(f"ob{i}", [128, 1024], F32) for i in range(2)]

    pyT = C.ps("pyT", [128, 8, 128], BF16)
    pT32 = C.ps("pT32", [128, 8, 128], F32)
    pT32v = pT32[:].rearrange("p a b -> p (a b)")
    pg0 = C.ps("pg", [128, 512])
    pg = [pg0[:], pT32v[:, 0:512]]
    pyTs = [pyT[:], pg0[:].bitcast(BF16).rearrange("p (a b) -> p a b", a=8)]
    pyTn = [N("pyT"), N("pg")]
    pu = [C.ps("pu", [128, 512])[:], pT32v[:, 512:1024]]
    pgn = [N("pg"), N("pT32a")]
    pun = [N("pu"), N("pT32b")]
    pd = C.ps("pd", [128, 2, 512])
    pr = C.ps("pr", [128, 32])

    P.psum |= {N(x) for x in ("pyT", "pT32a", "pT32b", "pg", "pu", "pd0", "pd1", "pr")}

    def ld(dst, src, name, eng="sp", lane=None):
        P.dma(eng, lambda e: e.dma_start(out=dst, in_=src), writes=[name], lane=lane or ("L:" + name))

    ld(ident[:], D["ident"], N("ident"), lane="G:const")
    ld(identf[:], D["identf"], N("identf"), lane="G:const")
    ld(fgain[:], D["fgain"], N("fgain"), lane="G:const")
    ld(wr[:], D["wr"].rearrange("(kc p) n -> p kc n", p=128), N("wr"), lane="G:const")
    ld(rbias[:], D["rbias"], N("rbias"), lane="G:const")
    if final:
        ld(ngain[:], D["ngain"], N("ngain"), lane="G:const")
    xs_v = D["xs"].rearrange("(t p) f -> p t f", p=128)
    def load_hq(q):
        tq = NTILE // 4
        P.dma("sp", lambda e, q=q, tq=tq: e.dma_start(out=h[:, q * tq:(q + 1) * tq, :], in_=xs_v[:, q * tq:(q + 1) * tq, :]),
              reads=D.get("xs_reads", ()), writes=[N(f"hq{q}")], lane="G:hq")
    HQ = lambda t: N(f"hq{t // (NTILE // 4)}")
    wo_d = D["wo"].rearrange("(fc p) n -> p fc n", p=128)
    nq = FC // 4
    for q in range(nq):
        P.dma("pool", lambda e, q=q: e.dma_start(out=wo_v[:, q * 4:(q + 1) * 4, :], in_=wo_d[:, q * 4:(q + 1) * 4, :]),
              writes=[N(f"wo{q}")], lane="G:wo")
    WO = [N(f"wo{q}") for q in range(nq)]
    SL = [N("ws0"), N("ws1")]

    for t in range(NTILE):
        s = t % 2
        if "ys_fn" in D:
            P.dma(D.get("ys_eng", "sp"), lambda e, t=t, s=s: D["ys_fn"](e, t, yt[s]), reads=D["ys_reads"](t), writes=[N(f"yt{s}")],
                  lane="L:" + N(f"yt{s}"))
        else:
            ld(yt[s][:], D["ys"][t * 128:(t + 1) * 128, :], N(f"yt{s}"))
        if t % (NTILE // 4) == 0:
            load_hq(t // (NTILE // 4))
        for half in range(FC // 8):
            pb = (t * (FC // 8) + half) % 2

            def tr(e, s=s, half=half, pb=pb):
                for j in range(8):
                    fc = half * 8 + j
                    ins = e.transpose(out=pyTs[pb][:, j, :], in_=yt[s][:, fc * 128:(fc + 1) * 128], identity=ident[:])
                return ins
            P.op("pe", tr, reads=[N(f"yt{s}"), N("ident")], writes=[pyTn[pb]])
            P.op("act", lambda e, half=half, s=s, pb=pb: e.activation(out=yT[s][:, half * 8:(half + 1) * 8, :], in_=pyTs[pb],
                                                                func=AF.Copy),
                 reads=[pyTn[pb]], writes=[N(f"yT{s}_{half}")])
        for hh in range(2):
            def mm(e, hh=hh, s=s):
                for fc in range(FC):
                    ins = e.matmul(out=pd[:, hh, :], lhsT=yT[s][:, fc, :], rhs=wo_v[:, fc, hh * 512:(hh + 1) * 512],
                                   start=(fc == 0), stop=(fc == FC - 1))
                return ins
            P.op("pe", mm, reads=[N(f"yT{s}_{i}") for i in range(FC // 8)] + WO + SL, writes=[N(f"pd{hh}")])
            P.op("dve", lambda e, t=t, hh=hh: e.tensor_tensor(out=h[:, t, hh * 512:(hh + 1) * 512],
                                                              in0=pd[:, hh, :], in1=h[:, t, hh * 512:(hh + 1) * 512],
                                                              op=ALU.add),
                 reads=[N(f"pd{hh}"), HQ(t), N(f"h{t}")], writes=[N(f"h{t}")])

    for t in range(NTILE if stage >= 2 else 0):
        P.op("act", lambda e, t=t: e.activation(out=junk[:], in_=h[:, t, :], func=AF.Square, accum_out=st[:, 0, t:t + 1]),
             reads=[N(f"h{t}")], writes=[N(f"ss{t}")])
    for t in range(NTILE if stage >= 2 else 0):
        rstd_ops(P, st[:, 0, t:t + 1], st[:, 1, t:t + 1], st[:, 2, t:t + 1], 1024, [N(f"ss{t}")], N(f"rstd{t}"))
    for t in range(NTILE if stage >= 2 else 0):
        u = t % 2
        P.op("dve", lambda e, t=t, u=u: e.scalar_tensor_tensor(out=hn32[u][:], in0=h[:, t, :], scalar=st[:, 2, t:t + 1],
                                                               in1=fgain[:], op0=ALU.mult, op1=ALU.mult),
             reads=[N(f"h{t}"), N(f"rstd{t}"), N("fgain")], writes=[N(f"hn32_{u}")])

        def trf(e, u=u):
            for kc in range(8):
                ins = e.transpose(out=pT32[:, kc, :], in_=hn32[u][:, kc * 128:(kc + 1) * 128], identity=identf[:])
            return ins
        P.op("pe", trf, reads=[N(f"hn32_{u}"), N("identf")], writes=[N("pT32a"), N("pT32b")])
        P.op("act", lambda e, t=t: e.activation(out=hnT[:, :, t * 128:(t + 1) * 128], in_=pT32[:], func=AF.Copy),
             reads=[N("pT32a"), N("pT32b")], writes=[N(f"hnT{t}")])
        P.op("dve", lambda e, u=u: e.tensor_copy(out=hnT32[u][:], in_=pT32[:]),
             reads=[N("pT32a"), N("pT32b")], writes=[N(f"hnT32_{u}")])

        def mr(e, u=u):
            for kc in range(8):
                ins = e.matmul(out=pr[:, 0:20], lhsT=hnT32[u][:, kc, :], rhs=wr[:, kc, :], start=(kc == 0), stop=(kc == 7))
            return ins
        P.op("pe", mr, reads=[N(f"hnT32_{u}"), N("wr")], writes=[N("pr")])
        rt = rts[t % 4]
        R = N(f"rt{t % 4}")
        k = [0]

        def dv(fn, rd=(), eng="dve"):
            P.op(eng, fn, reads=[R] + list(rd), writes=[R])
        dv(lambda e, rt=rt: e.tensor_tensor(out=rt[:, 0:20], in0=pr[:, 0:20], in1=rbias[:], op=ALU.add), [N("pr"), N("rbias")])
        dv(lambda e, rt=rt: e.tensor_reduce(out=rt[:, 20:21], in_=rt[:, 0:4], axis=AX.X, op=ALU.max))
        dv(lambda e, rt=rt: e.tensor_scalar(out=rt[:, 24:28], in0=rt[:, 0:4], scalar1=rt[:, 20:21], scalar2=None, op0=ALU.is_equal))
        dv(lambda e, rt=rt: e.tensor_scalar(out=rt[:, 28:32], in0=rt[:, 0:4], scalar1=rt[:, 20:21], scalar2=None, op0=ALU.subtract))
        dv(lambda e, rt=rt: e.activation(out=rt[:, 28:32], in_=rt[:, 28:32], func=AF.Exp, accum_out=rt[:, 21:22]), eng="act")
        dv(lambda e, rt=rt: e.reciprocal(out=rt[:, 22:23], in_=rt[:, 21:22]))
        dv(lambda e, rt=rt: e.tensor_scalar(out=rt[:, 32:36], in0=rt[:, 4:8], scalar1=rt[:, 24:25], scalar2=None, op0=ALU.mult))
        for g in range(1, 4):
            dv(lambda e, g=g, rt=rt: e.scalar_tensor_tensor(out=rt[:, 32:36], in0=rt[:, 4 + 4 * g:8 + 4 * g],
                                                     scalar=rt[:, 24 + g:25 + g], in1=rt[:, 32:36],
                                                     op0=ALU.mult, op1=ALU.add))
        dv(lambda e, rt=rt: e.tensor_reduce(out=rt[:, 36:37], in_=rt[:, 32:36], axis=AX.X, op=ALU.max))
        dv(lambda e, rt=rt: e.tensor_scalar(out=rt[:, 40:44], in0=rt[:, 32:36], scalar1=rt[:, 36:37], scalar2=None, op0=ALU.is_equal))
        dv(lambda e, rt=rt: e.scalar_tensor_tensor(out=rt[:, 44:48], in0=rt[:, 40:44], scalar=-1e30, in1=rt[:, 32:36],
                                            op0=ALU.mult, op1=ALU.add))
        dv(lambda e, rt=rt: e.tensor_reduce(out=rt[:, 37:38], in_=rt[:, 44:48], axis=AX.X, op=ALU.max))
        dv(lambda e, rt=rt: e.tensor_scalar(out=rt[:, 48:52], in0=rt[:, 44:48], scalar1=rt[:, 37:38], scalar2=None, op0=ALU.is_equal))
        dv(lambda e, rt=rt: e.tensor_tensor(out=rt[:, 38:39], in0=rt[:, 37:38], in1=rt[:, 36:37], op=ALU.subtract))
        dv(lambda e, rt=rt: e.activation(out=rt[:, 39:40], in_=rt[:, 38:39], func=AF.Exp), eng="act")
        dv(lambda e, rt=rt: e.tensor_scalar(out=rt[:, 52:53], in0=rt[:, 39:40], scalar1=1.0, scalar2=None, op0=ALU.add))
        dv(lambda e, rt=rt: e.reciprocal(out=rt[:, 52:53], in_=rt[:, 52:53]))
        dv(lambda e, rt=rt: e.tensor_tensor(out=rt[:, 53:54], in0=rt[:, 39:40], in1=rt[:, 52:53], op=ALU.mult))
        dv(lambda e, rt=rt: e.tensor_scalar(out=rt[:, 56:60], in0=rt[:, 40:44], scalar1=rt[:, 52:53], scalar2=None, op0=ALU.mult))
        dv(lambda e, rt=rt: e.scalar_tensor_tensor(out=rt[:, 56:60], in0=rt[:, 48:52], scalar=rt[:, 53:54], in1=rt[:, 56:60],
                                            op0=ALU.mult, op1=ALU.add))
        dv(lambda e, rt=rt: e.tensor_scalar(out=rt[:, 60:64], in0=rt[:, 24:28], scalar1=rt[:, 22:23], scalar2=None, op0=ALU.mult))
        for g in range(4):
            P.op("dve", lambda e, g=g, t=t, rt=rt: e.tensor_scalar(out=comb[:, t, 4 * g:4 * g + 4], in0=rt[:, 56:60],
                                                            scalar1=rt[:, 60 + g:61 + g], scalar2=None, op0=ALU.mult),
                 reads=[R], writes=[N(f"comb{t}")])

    HNT = [N(f"hnT{t}") for t in range(NTILE)]
    pend = None
    it = 0
    wl_cnt = [0]
    if final:
        steps = [(ex, tg) for ex in range(NE) for tg in range(NGRP)]
    else:
        steps = [(ex, tg) for ex in range(NE - 2) for tg in range(NGRP)] + \
                [(ex, tg) for tg in range(NGRP) for ex in (NE - 2, NE - 1)]
    loaded = set()
    for ex, tg in (steps if stage >= 3 else []):
        s = ex % 2
        wg_s, wu_s, wd_s = wslot(s)
        if ex not in loaded:
            loaded.add(ex)
            wg_d = D["wg"][ex].rearrange("(kc p) f -> p kc f", p=128)
            wu_d = D["wu"][ex].rearrange("(kc p) f -> p kc f", p=128)
            wd_d = D["wd"][ex].rearrange("(fc p) n -> p fc n", p=128)
            for (dst, src) in ((wg_s, wg_d), (wu_s, wu_d), (wd_s, wd_d)):
                P.dma("pool", lambda e, dst=dst, src=src: e.dma_start(out=dst, in_=src),
                      writes=[SL[s]], lane="L:" + SL[s])
        if True:
            b = it % 2
            for fc in range(4):
                pb = (it * 4 + fc) % 2

                def mg(e, fc=fc, tg=tg, pb=pb, wg_s=wg_s):
                    for kc in range(8):
                        ins = e.matmul(out=pg[pb], lhsT=wg_s[:, kc, fc * 128:(fc + 1) * 128],
                                       rhs=hnT[:, kc, tg * 512:(tg + 1) * 512], start=(kc == 0), stop=(kc == 7))
                    return ins

                def mu(e, fc=fc, tg=tg, pb=pb, wu_s=wu_s):
                    for kc in range(8):
                        ins = e.matmul(out=pu[pb], lhsT=wu_s[:, kc, fc * 128:(fc + 1) * 128],
                                       rhs=hnT[:, kc, tg * 512:(tg + 1) * 512], start=(kc == 0), stop=(kc == 7))
                    return ins
                hn_names = HNT[tg * 4:(tg + 1) * 4]
                P.op("pe", mg, reads=[SL[s]] + hn_names, writes=[pgn[pb]])
                P.op("pe", mu, reads=[SL[s]] + hn_names, writes=[pun[pb]])
                P.op("act", lambda e, pb=pb: e.activation(out=sgl[pb][:], in_=pg[pb], func=AF.Silu),
                     reads=[pgn[pb]], writes=[N(f"sgl{pb}")])
                P.op("dve", lambda e, pb=pb, b=b, fc=fc: e.tensor_tensor(out=hT[b][:, fc, :], in0=pu[pb], in1=sgl[pb][:],
                                                                       op=ALU.mult),
                     reads=[pun[pb], N(f"sgl{pb}")], writes=[N(f"hT{b}_{fc}")])

            def down(ex=ex, tg=tg, b=b, s=s, wd_s=wd_s):
                for tt in range(4):
                    t = tg * 4 + tt
                    for hh in range(2):
                        def md(e, tt=tt, hh=hh):
                            for fc in range(4):
                                ins = e.matmul(out=pd[:, hh, :], lhsT=hT[b][:, fc, tt * 128:(tt + 1) * 128],
                                               rhs=wd_s[:, fc, hh * 512:(hh + 1) * 512], start=(fc == 0), stop=(fc == 3))
                            return ins
                        P.op("pe", md, reads=[SL[s]] + [N(f"hT{b}_{fc}") for fc in range(4)], writes=[N(f"pd{hh}")])
                        P.op("dve", lambda e, t=t, hh=hh: e.scalar_tensor_tensor(
                            out=h[:, t, hh * 512:(hh + 1) * 512], in0=pd[:, hh, :], scalar=comb[:, t, ex:ex + 1],
                            in1=h[:, t, hh * 512:(hh + 1) * 512], op0=ALU.mult, op1=ALU.add),
                            reads=[N(f"pd{hh}"), N(f"comb{t}"), N(f"h{t}")], writes=[N(f"h{t}")])
            if pend is not None:
                pend()
            pend = down
            it += 1
    if pend is not None:
        pend()

    if not final:
        for t in range(NTILE):
            P.dma("sp", lambda e, t=t: e.dma_start(out=D["h_out"][t * 128:(t + 1) * 128, :], in_=h[:, t, :]),
                  reads=[N(f"h{t}")], writes=["d:" + N("h_out")], lane="S:" + N(f"h{t % 4}"))
        if "hn_out" in D:
            for t in range(NTILE):
                P.op("act", lambda e, t=t: e.activation(out=junk[:], in_=h[:, t, :], func=AF.Square,
                                                       accum_out=st[:, 0, t:t + 1]),
                     reads=[N(f"h{t}")], writes=[N(f"nss{t}")])
            for t in range(NTILE):
                rstd_ops(P, st[:, 0, t:t + 1], st[:, 1, t:t + 1], st[:, 2, t:t + 1], 1024, [N(f"nss{t}")], N(f"nrstd{t}"))
            for t in range(NTILE):
                s2 = t % 2
                P.op("act", lambda e, t=t, s2=s2: e.activation(out=yt[s2][:, 0:1024], in_=h[:, t, :], func=AF.Copy,
                                                              scale=st[:, 2, t:t + 1]),
                     reads=[N(f"h{t}"), N(f"nrstd{t}")], writes=[N(f"yt{s2}")])
                P.dma("sp", lambda e, t=t, s2=s2: e.dma_start(out=D["hn_out"][t * 128:(t + 1) * 128, :],
                                                             in_=yt[s2][:, 0:1024]),
                      reads=[N(f"yt{s2}")], writes=["d:" + N(f"hn{t // 4}")], lane="S:" + N(f"yt{s2}"))
    else:
        for t in range(NTILE):
            P.op("act", lambda e, t=t: e.activation(out=junk[:], in_=h[:, t, :], func=AF.Square, accum_out=st[:, 0, t:t + 1]),
                 reads=[N(f"h{t}")], writes=[N(f"fss{t}")])
        for t in range(NTILE):
            rstd_ops(P, st[:, 0, t:t + 1], st[:, 1, t:t + 1], st[:, 2, t:t + 1], 1024, [N(f"fss{t}")], N(f"frstd{t}"))
        for t in range(NTILE):
            s = t % 2
            P.op("dve", lambda e, t=t, s=s: e.scalar_tensor_tensor(out=ob[s][:], in0=h[:, t, :], scalar=st[:, 2, t:t + 1],
                                                                  in1=ngain[:], op0=ALU.mult, op1=ALU.mult),
                 reads=[N(f"h{t}"), N(f"frstd{t}"), N("ngain")], writes=[N(f"ob{s}")])
            P.dma("sp", lambda e, t=t, s=s: e.dma_start(out=D["h_out"][t * 128:(t + 1) * 128, :], in_=ob[s][:]),
                  reads=[N(f"ob{s}")], writes=["d:" + N("h_out")], lane="S:" + N(f"ob{s}"))


def build_ffn(F, final, NTOK=2048, NE=16, stage=3):
    nc = bass.Bass("TRN2", target_bir_lowering=False)
    P = Prog(nc)
    D = {}

    def inp(name, shape, dt=F32):
        D[name] = nc.dram_tensor(name, list(shape), dt, kind="ExternalInput").ap()
    inp("xs", [NTOK, 1024]); inp("ys", [NTOK, F], BF16); inp("wo", [F, 1024]); inp("fgain", [128, 1024])
    inp("wr", [1024, 20]); inp("rbias", [128, 20]); inp("wg", [NE, 1024, 512]); inp("wu", [NE, 1024, 512])
    inp("wd", [NE, 512, 1024]); inp("ident", [128, 128], BF16); inp("identf", [128, 128])
    if final:
        inp("ngain", [128, 1024])
    D["h_out"] = nc.dram_tensor("h_out", [NTOK, 1024], F32, kind="ExternalOutput").ap()
    phase_ffn(P, nc, D, F, final, "B_", NTOK, NE, stage)
    finish(P, ["d:h_out"])
    P.build()
    return nc, P


def ffn_maps(xs_list, ys_list, wo, fgain, rgw, rgb, rew, reb, wg, wu, wd, ngain=None):
    ident = np.eye(128, dtype=np.float32).astype(ml_dtypes.bfloat16)
    identf = np.eye(128, dtype=np.float32)
    wr = np.ascontiguousarray(np.concatenate([rgw] + [rew[g] for g in range(4)], axis=1))
    rb = np.concatenate([rgb, reb.reshape(-1)])[None, :]
    common = dict(wo=np.ascontiguousarray(wo), fgain=np.ascontiguousarray(np.broadcast_to(fgain[None, :], (128, 1024))),
                  wr=wr, rbias=np.ascontiguousarray(np.broadcast_to(rb, (128, 20))),
                  wg=np.ascontiguousarray(wg.reshape(16, 1024, 512)), wu=np.ascontiguousarray(wu.reshape(16, 1024, 512)),
                  wd=np.ascontiguousarray(wd.reshape(16, 512, 1024)), ident=ident, identf=identf)
    if ngain is not None:
        common["ngain"] = np.ascontiguousarray(np.broadcast_to(ngain[None, :], (128, 1024)))
    return [dict(common, xs=np.ascontiguousarray(xs_list[c]), ys=np.ascontiguousarray(ys_list[c])) for c in range(8)]


def phase_moba(P, nc, D, NT=8192):
    C = Ctx(nc, P, "C_")
    N = lambda s: "C_" + s
    NG = NT // 512
    NQT = NT // 128
    NB = NT // 256
    ident = C.sb("ident", [128, 128], BF16)
    cmask = C.sb("cmask", [128, 2, 256], BF16)
    gq = C.sb("gq", [128, 8], F32)
    gkv = C.sb("gkv", [128, 8], F32)
    wst = C.sb("wst", [128, 8, 256], F32)
    wq_bf = C.sb("wq_bf", [128, 8, 256], BF16)
    wk_bf = C.sb("wk_bf", [128, 8, 256], BF16)
    wv_bf = C.sb("wv_bf", [128, 8, 256], BF16)
    wq_rot = C.sb("wq_rot", [128, 8, 2, 32], BF16)
    wk_rot = C.sb("wk_rot", [128, 8, 2, 32], BF16)
    QT = [C.sb(f"QT{i}", [128, NT], BF16) for i in range(2)]
    KT = [C.sb(f"KT{i}", [128, NT], BF16) for i in range(2)]
    Vext = C.sb("Vext", [128, 2, NQT, 130], BF16)
    Msel = C.sb("Msel", [128, 2, NQT, 32], F32)
    km32 = C.sb("km32", [128, 2, 32], F32)
    kmT = C.sb("kmT", [128, 2, 32], BF16)
    ht = [C.sb(f"ht{i}", [128, 1024], F32) for i in range(2)]
    junk = C.sb("junk", [128, 1024], BF16)
    hn = [C.sb(f"hn{i}", [128, 1024], BF16) for i in range(2)]
    hnT = [C.sb(f"hnT{i}", [128, 8, 512], BF16) for i in range(2)]
    cosg = [C.sb(f"cos{i}", [32, 512], F32) for i in range(2)]
    sing = [C.sb(f"sin{i}", [32, 512], F32) for i in range(2)]
    ta = C.sb("ta", [32, 512], F32)
    tb = C.sb("tb", [32, 512], F32)
    xr = [C.sb(f"xr{i}", [32, 512], F32) for i in range(2)]
    xsw = [C.sb(f"xsw{i}", [32, 512], F32) for i in range(2)]
    rc = [0]
    st = C.sb("st", [128, 3, 4], F32)
    gt = C.sb("gt", [128, 32], F32)
    mx = C.sb("mx", [128, 8], F32)
    PT = [C.sb(f"PT{i}", [128, 2, 256], BF16) for i in range(3)]
    acc = [C.sb(f"acc{i}", [128, 130], F32) for i in range(2)]
    rec = C.sb("rec", [128, 2], F32)
    ob = [C.sb(f"ob{i}", [128, 128], BF16) for i in range(4)]

    psT = C.ps("psT", [128, 8, 128], BF16)
    pm = C.ps("pm", [128, 512])
    prot = C.ps("prot", [128, 512])
    pv = C.ps("pv", [128, 512])
    pS = [C.ps(f"pS{i}", [128, 2, 256]) for i in range(2)]
    pO = [C.ps(f"pO{i}", [128, 512]) for i in range(2)]
    P.psum |= {N(x) for x in ("psT", "pm", "prot", "pv", "pS0", "pS1", "pO0", "pO1")}

    def ld(dst, src, name, eng="sp", lane=None):
        P.dma(eng, lambda e: e.dma_start(out=dst, in_=src), writes=[name], lane=lane or ("L:" + name))

    ld(ident[:], D["ident"], N("ident"), lane="G:const")
    ld(cmask[:], D["cmask"], N("cmask"), lane="G:const")
    ld(gq[:], D["gq"], N("gq"), lane="G:const")
    ld(gkv[:], D["gkv"], N("gkv"), lane="G:const")
    qscale = float(128 ** -0.5)
    for (wname, wdst, g, sc) in (("wq", wq_bf, gq, qscale), ("wk", wk_bf, gkv, 1.0), ("wv", wv_bf, gkv, 1.0)):
        ld(wst[:], D[wname].rearrange("(kc p) f -> p kc f", p=128), N("wst"))
        for kc in range(8):
            P.op("dve", lambda e, kc=kc, wdst=wdst, g=g, sc=sc: e.tensor_scalar(
                out=wdst[:, kc, :], in0=wst[:, kc, :], scalar1=g[:, kc:kc + 1], scalar2=sc, op0=ALU.mult, op1=ALU.mult),
                reads=[N("wst"), N("gq"), N("gkv")], writes=[N(wname + "_bf")])
    for (wsrc, wrot, nm) in ((wq_bf, wq_rot, "wq"), (wk_bf, wk_rot, "wk")):
        for hh in range(2):
            P.op("dve", lambda e, wsrc=wsrc, wrot=wrot, hh=hh: e.tensor_scalar(
                out=wrot[:, :, hh, 0:16], in0=wsrc[:, :, hh * 128 + 16:hh * 128 + 32], scalar1=-1.0, scalar2=None,
                op0=ALU.mult), reads=[N(nm + "_bf")], writes=[N(nm + "_rot")])
            P.op("dve", lambda e, wsrc=wsrc, wrot=wrot, hh=hh: e.tensor_copy(
                out=wrot[:, :, hh, 16:32], in_=wsrc[:, :, hh * 128:hh * 128 + 16]),
                reads=[N(nm + "_bf")], writes=[N(nm + "_rot")])
    P.op("pool", lambda e: e.memset(Vext[:], 1.0), writes=[N("Vext_init")])
    P.op("pool", lambda e: e.memset(Msel[:], 0.0), writes=[N("Msel_init")])
    P.op("pool", lambda e: e.memset(kmT[:], 0.0), writes=[N("kmT_init")])

    hb = D.get("hb")
    for tg in range(NG):
        s = tg % 2
        t0 = tg * 512
        ld(cosg[s][:], D["cos32"][:, t0:t0 + 512], N(f"cos{s}"))
        ld(sing[s][:], D["sin32"][:, t0:t0 + 512], N(f"sin{s}"))
        for tt in range(4):
            xs = tt % 2
            r0 = t0 + tt * 128
            if "hn_src" in D:
                P.dma("sp", lambda e, xs=xs, r0=r0: e.dma_start(out=hn[xs][:], in_=D["hn_src"](r0)),
                      reads=D["hn_reads"](r0), writes=[N(f"hn{xs}")], lane="L:" + N(f"hn{xs}"))
            else:
                ld(ht[xs][:], hb[r0:r0 + 128, :], N(f"ht{xs}"))
                P.op("act", lambda e, xs=xs, tt=tt: e.activation(out=junk[:], in_=ht[xs][:], func=AF.Square,
                                                                accum_out=st[:, 0, tt:tt + 1]),
                     reads=[N(f"ht{xs}")], writes=[N("ss")])
                rstd_ops(P, st[:, 0, tt:tt + 1], st[:, 1, tt:tt + 1], st[:, 2, tt:tt + 1], 1024, [N("ss")], N("rstd"))
                P.op("act", lambda e, xs=xs, tt=tt: e.activation(out=hn[xs][:], in_=ht[xs][:], func=AF.Copy,
                                                                scale=st[:, 2, tt:tt + 1]),
                     reads=[N(f"ht{xs}"), N("rstd")], writes=[N(f"hn{xs}")])

            def tr(e, xs=xs):
                for kc in range(8):
                    ins = e.transpose(out=psT[:, kc, :], in_=hn[xs][:, kc * 128:(kc + 1) * 128], identity=ident[:])
                return ins
            P.op("pe", tr, reads=[N(f"hn{xs}"), N("ident")], writes=[N("psT")])
            P.op("dve", lambda e, s=s, tt=tt: e.tensor_copy(out=hnT[s][:, :, tt * 128:(tt + 1) * 128], in_=psT[:]),
                 reads=[N("psT")], writes=[N(f"hnT{s}_{tt}")])
        XN = [N(f"hnT{s}_{tt}") for tt in range(4)]
        for hh in range(2):
            for (w_bf, w_rot, dst, nm) in ((wq_bf, wq_rot, QT, "Q"), (wk_bf, wk_rot, KT, "K")):
                wn = "wq" if nm == "Q" else "wk"

                def mm(e, w_bf=w_bf, hh=hh, s=s):
                    for kc in range(8):
                        ins = e.matmul(out=pm[:], lhsT=w_bf[:, kc, hh * 128:(hh + 1) * 128], rhs=hnT[s][:, kc, :],
                                       start=(kc == 0), stop=(kc == 7))
                    return ins

                def mr(e, w_rot=w_rot, hh=hh, s=s):
                    for kc in range(8):
                        ins = e.matmul(out=prot[0:32, :], lhsT=w_rot[:, kc, hh, :], rhs=hnT[s][:, kc, :],
                                       start=(kc == 0), stop=(kc == 7))
                    return ins
                P.op("pe", mm, reads=[N(wn + "_bf")] + XN, writes=[N("pm")])
                dname = N(f"{nm}T{hh}_{tg}")
                u = rc[0] % 2
                rc[0] += 1
                P.op("act", lambda e, u=u: e.activation(out=xr[u][:], in_=pm[0:32, :], func=AF.Copy),
                     reads=[N("pm")], writes=[N(f"xr{u}")])
                P.dma("sp", lambda e, u=u: e.dma_start(out=xsw[u][0:16, :], in_=xr[u][16:32, :]),
                      reads=[N(f"xr{u}")], writes=[N(f"xsw{u}")], lane="L:" + N(f"xsw{u}"))
                P.dma("sp", lambda e, u=u: e.dma_start(out=xsw[u][16:32, :], in_=xr[u][0:16, :]),
                      reads=[N(f"xr{u}")], writes=[N(f"xsw{u}")], lane="L:" + N(f"xsw{u}"))
                P.op("act", lambda e, dst=dst, hh=hh, t0=t0: e.activation(out=dst[hh][32:64, t0:t0 + 512],
                                                                         in_=pm[32:64, :], func=AF.Copy),
                     reads=[N("pm")], writes=[dname + "mid"])
                P.op("act", lambda e, dst=dst, hh=hh, t0=t0: e.activation(out=dst[hh][64:128, t0:t0 + 512],
                                                                         in_=pm[64:128, :], func=AF.Copy),
                     reads=[N("pm")], writes=[dname + "hi"])
                P.op("dve", lambda e, s=s, u=u: e.tensor_tensor(out=ta[:], in0=xr[u][:], in1=cosg[s][:], op=ALU.mult),
                     reads=[N(f"xr{u}"), N(f"cos{s}")], writes=[N("ta")])
                P.op("dve", lambda e, s=s, u=u: e.tensor_tensor(out=tb[:], in0=xsw[u][:], in1=sing[s][:], op=ALU.mult),
                     reads=[N(f"xsw{u}"), N(f"sin{s}")], writes=[N("tb")])
                P.op("pool", lambda e, dst=dst, hh=hh, t0=t0: e.tensor_tensor(out=dst[hh][0:32, t0:t0 + 512], in0=ta[:],
                                                                             in1=tb[:], op=ALU.add),
                     reads=[N("ta"), N("tb")], writes=[dname + "lo"])
            P.op("dve", lambda e, hh=hh, t0=t0, tg=tg: e.tensor_reduce(
                out=km32[:, hh, 2 * tg:2 * tg + 2], in_=KT[hh][:, t0:t0 + 512].rearrange("p (a b) -> p a b", a=2),
                axis=AX.X, op=ALU.add),
                reads=[N(f"KT{hh}_{tg}hi"), N(f"KT{hh}_{tg}lo")], writes=[N(f"km32_{hh}_{tg}")])
            P.op("act", lambda e, hh=hh, tg=tg: e.activation(out=kmT[:, hh, 2 * tg:2 * tg + 2],
                                                            in_=km32[:, hh, 2 * tg:2 * tg + 2], func=AF.Copy,
                                                            scale=1.0 / 256.0),
                 reads=[N(f"km32_{hh}_{tg}"), N("kmT_init")], writes=[N(f"kmT{hh}_{tg}")])
        for tt in range(4):
            tile = tg * 4 + tt

            def mv(e, tt=tt, s=s):
                for kc in range(8):
                    ins = e.matmul(out=pv[:, 0:256], lhsT=hnT[s][:, kc, tt * 128:(tt + 1) * 128], rhs=wv_bf[:, kc, :],
                                   start=(kc == 0), stop=(kc == 7))
                return ins
            P.op("pe", mv, reads=[N("wv_bf"), XN[tt]], writes=[N("pv")])
            P.op("act", lambda e, tile=tile: e.activation(out=Vext[:, :, tile, 0:128],
                                                         in_=pv[:, 0:256].rearrange("p (a b) -> p a b", a=2), func=AF.Copy),
                 reads=[N("pv"), N("Vext_init")], writes=[N(f"V{tile}")])
    gts = [gt, C.sb("gt1", [128, 32], F32)]
    mxs = [mx, C.sb("mx1", [128, 8], F32)]
    for qt in range(2, NQT):
        for hh in range(2):
            j = qt // 2
            g_, m_, pg_, pgn_ = gts[hh], mxs[hh], pO[hh], N(f"pO{hh}")
            kn = [N(f"kmT{hh}_{tg}") for tg in range((j - 1) // 2 + 1)]
            P.op("pe", lambda e, hh=hh, qt=qt, pg_=pg_: e.matmul(out=pg_[:, 0:32], lhsT=QT[hh][:, qt * 128:(qt + 1) * 128],
                                                                 rhs=kmT[:, hh, :], start=True, stop=True),
                 reads=[N(f"QT{hh}_{qt // 4}hi"), N(f"QT{hh}_{qt // 4}lo"), N("kmT_init")] + kn, writes=[pgn_])
            P.op("dve", lambda e, g_=g_, pg_=pg_: e.tensor_copy(out=g_[:], in_=pg_[:, 0:32]), reads=[pgn_], writes=[N(f"gt{hh}")])
            P.op("dve", lambda e, j=j, g_=g_: e.memset(g_[:, j:32], -1e30), reads=[N(f"gt{hh}")], writes=[N(f"gt{hh}")])
            P.op("dve", lambda e, g_=g_, m_=m_: e.max(out=m_[:], in_=g_[:]), reads=[N(f"gt{hh}")], writes=[N(f"mx{hh}")])
            P.op("dve", lambda e, hh=hh, qt=qt, g_=g_, m_=m_: e.tensor_scalar(out=Msel[:, hh, qt, :], in0=g_[:],
                                                                             scalar1=m_[:, 2:3], scalar2=None, op0=ALU.is_ge),
                 reads=[N(f"gt{hh}"), N(f"mx{hh}"), N("Msel_init")], writes=[N(f"M{hh}_{qt}")])

    pS3 = [pS[0][:], pS[1][:],
           psT[:].bitcast(F32).rearrange("p a b -> p (a b)").rearrange("p (k q) -> p k q", k=2)]
    pSn = [N("pS0"), N("pS1"), N("psT")]
    ND = 3
    pOb = [[pO[0], pm], [pO[1], prot]]
    pOn = [[N("pO0"), N("pm")], [N("pO1"), N("prot")]]
    pairs = []
    for j in range(NB):
        for hh in range(2):
            order = [j] + list(range(j))
            for idx, n in enumerate(order):
                pairs.append((j, hh, n, idx == len(order) - 1))
    oc = [0]

    def emit_s(i):
        j, hh, n, last = pairs[i]
        b = i % ND
        qs = slice(j * 256, (j + 1) * 256)
        qn = [N(f"QT{hh}_{j // 2}hi"), N(f"QT{hh}_{j // 2}lo")]

        def mS(e):
            for kt in range(2):
                k0 = (2 * n + kt) * 128
                ins = e.matmul(out=pS3[b][:, kt, :], lhsT=KT[hh][:, k0:k0 + 128], rhs=QT[hh][:, qs],
                               start=True, stop=True)
            return ins
        P.op("pe", mS, reads=qn + [N(f"KT{hh}_{n // 2}hi"), N(f"KT{hh}_{n // 2}lo")], writes=[pSn[b]])
        P.op("act", lambda e: e.activation(out=PT[b][:], in_=pS3[b], func=AF.Exp),
             reads=[pSn[b]], writes=[N(f"PT{b}")])
        if n == j:
            P.op("pool", lambda e: e.tensor_tensor(out=PT[b][:], in0=PT[b][:], in1=cmask[:], op=ALU.mult),
                 reads=[N(f"PT{b}"), N("cmask")], writes=[N(f"PT{b}")])

    def emit_o(i):
        j, hh, n, last = pairs[i]
        b = i % ND
        own = (n == j)
        for qi in range(2):
            po_, pn_ = pOb[qi][i % 2], pOn[qi][i % 2]

            def mO(e, qi=qi, po_=po_):
                for kt in range(2):
                    ins = e.matmul(out=po_[:, 0:129], lhsT=PT[b][:, kt, qi * 128:(qi + 1) * 128],
                                   rhs=Vext[:, hh, 2 * n + kt, 0:129], start=(kt == 0), stop=(kt == 1))
                return ins
            P.op("pe", mO, reads=[N(f"PT{b}"), N(f"V{2 * n}"), N(f"V{2 * n + 1}")], writes=[pn_])
            if own:
                P.op("dve", lambda e, qi=qi, po_=po_: e.tensor_copy(out=acc[qi][:, 0:129], in_=po_[:, 0:129]),
                     reads=[pn_], writes=[N(f"acc{qi}")])
            else:
                P.op("dve", lambda e, qi=qi, po_=po_: e.scalar_tensor_tensor(
                    out=acc[qi][:, 0:129], in0=po_[:, 0:129], scalar=Msel[:, hh, 2 * j + qi, n:n + 1],
                    in1=acc[qi][:, 0:129], op0=ALU.mult, op1=ALU.add),
                    reads=[pn_, N(f"M{hh}_{2 * j + qi}"), N(f"acc{qi}")], writes=[N(f"acc{qi}")])
        if last:
            for qi in range(2):
                o_ = oc[0] % 4
                oc[0] += 1
                qt = 2 * j + qi
                P.op("dve", lambda e, qi=qi: e.reciprocal(out=rec[:, qi:qi + 1], in_=acc[qi][:, 128:129]),
                     reads=[N(f"acc{qi}")], writes=[N(f"rec{qi}")])
                P.op("dve", lambda e, qi=qi, o_=o_: e.tensor_scalar(out=ob[o_][:], in0=acc[qi][:, 0:128],
                                                                    scalar1=rec[:, qi:qi + 1], scalar2=None, op0=ALU.mult),
                     reads=[N(f"acc{qi}"), N(f"rec{qi}")], writes=[N(f"ob{o_}")])
                P.dma("sp", lambda e, qt=qt, o_=o_: e.dma_start(
                    out=D["o_out"][qt * 128:(qt + 1) * 128, hh * 128:(hh + 1) * 128], in_=ob[o_][:]),
                    reads=[N(f"ob{o_}")], writes=[f"d:C_o{j // 4}"], lane="S:" + N(f"ob{o_}"))
            if hh == 1 and "after_block" in D:
                D["after_block"](j)

    for i in range(ND - 1):
        emit_s(i)
    for i in range(len(pairs)):
        if i + ND - 1 < len(pairs):
            emit_s(i + ND - 1)
        emit_o(i)


def moba_tables(S=8192):
    inv = (1.0 / (np.float32(500000.0) ** (np.arange(16, dtype=np.float32) / np.float32(16)))).astype(np.float32)
    ang = (np.arange(S, dtype=np.float32)[None, :] * inv[:, None]).astype(np.float32)
    cos = np.cos(ang).astype(np.float32)
    sin = np.sin(ang).astype(np.float32)
    k = np.arange(128)[:, None, None] + 128 * np.arange(2)[None, :, None]
    q = np.arange(256)[None, None, :]
    cmask = (k <= q).astype(np.float32).astype(ml_dtypes.bfloat16)
    return (np.ascontiguousarray(np.concatenate([cos, cos], 0)), np.ascontiguousarray(np.concatenate([-sin, sin], 0)),
            np.ascontiguousarray(cmask))


def build_moba(NT=8192):
    nc = bass.Bass("TRN2", target_bir_lowering=False)
    P = Prog(nc)
    D = {}

    def inp(name, shape, dt=F32):
        D[name] = nc.dram_tensor(name, list(shape), dt, kind="ExternalInput").ap()
    inp("hb", [NT, 1024]); inp("wq", [1024, 256]); inp("wk", [1024, 256]); inp("wv", [1024, 256])
    inp("gq", [128, 8]); inp("gkv", [128, 8]); inp("cos32", [32, NT]); inp("sin32", [32, NT])
    inp("cmask", [128, 2, 256], BF16); inp("ident", [128, 128], BF16)
    D["o_out"] = nc.dram_tensor("o_out", [NT, 256], BF16, kind="ExternalOutput").ap()
    phase_moba(P, nc, D, NT)
    finish(P)
    P.build()
    return nc, P


def moba_maps(h_list, attn_norm, kv_norm, w_q, w_kv, NT=8192):
    cos32, sin32, cmask = moba_tables()
    ident = np.eye(128, dtype=np.float32).astype(ml_dtypes.bfloat16)
    gq = np.ascontiguousarray(attn_norm.reshape(8, 128).T)
    gkv = np.ascontiguousarray(kv_norm.reshape(8, 128).T)
    maps = []
    for c in range(8):
        b, p = c // 4, c % 4
        cs = slice(p * 256, (p + 1) * 256)
        maps.append(dict(hb=np.ascontiguousarray(h_list[b][:NT]), wq=np.ascontiguousarray(w_q[:, cs]),
                         wk=np.ascontiguousarray(w_kv[:, cs]), wv=np.ascontiguousarray(w_kv[:, 1024 + p * 256:1024 + (p + 1) * 256]),
                         gq=gq, gkv=gkv, cos32=np.ascontiguousarray(cos32[:, :NT]), sin32=np.ascontiguousarray(sin32[:, :NT]),
                         cmask=cmask, ident=ident))
    return maps


def finish(P, names=None):
    P.finish()


def ret_tables(S=8192):
    half = 128
    inv = (1.0 / (np.float32(10000.0) ** (np.arange(half, dtype=np.float32) / np.float32(half)))).astype(np.float32)
    ang = (np.arange(S, dtype=np.float32)[None, :] * inv[:, None]).astype(np.float32)
    return np.cos(ang).astype(np.float32), np.sin(ang).astype(np.float32)


def ret_decay(h):
    Cn = 128
    lg = np.log1p(-np.float32(2.0) ** np.float32(-5.0 - h)).astype(np.float32)
    idx = np.arange(Cn, dtype=np.float32)
    diff = idx[:, None] - idx[None, :]
    dm = np.where(diff >= 0, np.exp(lg * np.maximum(diff, 0.0)), 0.0).astype(np.float32)
    dmaskT = (dm.T * np.float32(256 ** -0.5)).astype(np.float32)
    qdec = np.exp(lg * (idx + 1.0)).astype(np.float32)
    kdec = (np.exp(lg * (Cn - 1.0 - idx)) * np.float32(256 ** -0.5)).astype(np.float32)
    cdec = np.exp(lg * np.float32(Cn)).astype(np.float32)
    qdec4 = np.ascontiguousarray(np.broadcast_to(np.tile(qdec, 4)[None, :], (128, 512))).astype(np.float32)
    return dict(dmask=np.ascontiguousarray(dmaskT), qdec4=qdec4,
                kdec=np.ascontiguousarray(kdec[:, None]),
                cdec=np.full((128, 1), cdec, np.float32))


def build_ret(NT=8192):
    nc = bass.Bass("TRN2", target_bir_lowering=False)
    P = Prog(nc)
    D = {}

    def inp(name, shape, dt=F32):
        D[name] = nc.dram_tensor(name, list(shape), dt, kind="ExternalInput").ap()
    inp("xb", [NT, 1024]); inp("w", [1024, 1536]); inp("gain", [128, 8])
    inp("cosT", [128, NT]); inp("sinT", [128, NT]); inp("dmask", [128, 128]); inp("qdec4", [128, 512])
    inp("kdec", [128, 1]); inp("cdec", [128, 1]); inp("ident", [128, 128], BF16)
    D["y_out"] = nc.dram_tensor("y_out", [NT, 512], BF16, kind="ExternalOutput").ap()
    phase_ret(P, nc, D, NT)
    finish(P, ["d:y_out"])
    P.build()
    return nc, P


def run_ret(x, ret_norm, ret_w_in):
    nc, P = build_ret()
    cosT, sinT = ret_tables()
    ident = np.eye(128, dtype=np.float32).astype(ml_dtypes.bfloat16)
    gain = np.ascontiguousarray(ret_norm[0].reshape(8, 128).T)
    maps = []
    for c in range(8):
        b, h = c // 4, c % 4
        w = ret_w_in[0]
        wh = np.concatenate([w[:, h * 256:(h + 1) * 256], w[:, 1024 + h * 256:1024 + (h + 1) * 256],
                             w[:, 2048 + h * 512:2048 + (h + 1) * 512],
                             w[:, 4096 + h * 512:4096 + (h + 1) * 512]], axis=1)
        m = dict(xb=np.ascontiguousarray(x[b]), w=np.ascontiguousarray(wh), gain=gain, cosT=cosT, sinT=sinT,
                 ident=ident)
        m.update(ret_decay(h))
        maps.append(m)
    res = run_bass_kernel_spmd(nc, maps, core_ids=list(range(8)))
    return [r["y_out"] for r in res.results]


GROUPS = [[0, 1, 2, 3], [4, 5, 6, 7]]


def build_fused(upto=4):
    nc = bass.Bass("TRN2", target_bir_lowering=False)
    P = Prog(nc)
    P.want_pid = True

    def inp(name, shape, dt=F32):
        return nc.dram_tensor("in_" + name, list(shape), dt, kind="ExternalInput").ap()

    def scr(name, shape, dt):
        return nc.dram_tensor(name, list(shape), dt).ap()

    def state():
        return (nc.sbuf_base, nc.sbuf_top, nc.psum_base, nc.psum_top)

    def restore(st):
        nc.sbuf_base, nc.sbuf_top, nc.psum_base, nc.psum_top = st

    def allgather(src, dst, reads, wname, lane):
        P.cc(lambda e: e.collective_compute("AllGather", ALU.bypass, replica_groups=GROUPS, ins=[src], outs=[dst]),
             reads=reads, writes=[wname], lane=lane)

    ident = inp("ident", [128, 128], BF16)
    identf = inp("identf", [128, 128])
    y_dram = scr("y_dram", [8192, 512], BF16)
    yall = scr("yall", [8 * 4 * 1024, 512], BF16)
    if upto == 2:
        h1_dram = nc.dram_tensor("h1_dram", [2048, 1024], F32, kind="ExternalOutput").ap()
    else:
        h1_dram = scr("h1_dram", [2048, 1024], F32)
    hn_dram = scr("hn_dram", [2048, 1024], BF16)
    hnall = scr("hnall", [4 * 4 * 512, 1024], BF16)
    o_dram = scr("o_dram", [8192, 256], BF16)
    oall = scr("oall", [8 * 4 * 1024, 256], BF16)
    out = nc.dram_tensor("out", [2048, 1024], F32, kind="ExternalOutput").ap()
    st0 = state()

    def ag_y(i):
        allgather(y_dram[i * 1024:(i + 1) * 1024, :], yall[i * 4096:(i + 1) * 4096, :], [f"d:A_y{i}"],
                  f"d:yall{i}", f"CC:y{i}")

    def after_group(tg):
        if tg >= 2 and tg % 2 == 0:
            ag_y(tg // 2 - 1)
    DA = dict(xb=inp("xb", [8192, 1024]), w=inp("A_w", [1024, 1536]), gain=inp("A_gain", [128, 8]),
              cosT=inp("A_cosT", [128, 8192]), sinT=inp("A_sinT", [128, 8192]), dmask=inp("A_dmask", [128, 128]),
              qdec4=inp("A_qdec4", [128, 512]), kdec=inp("A_kdec", [128, 1]), cdec=inp("A_cdec", [128, 1]),
              ident=ident, y_out=y_dram, after_group=after_group)
    phase_ret(P, nc, DA)
    ag_y(7)
    restore(st0)
    P.barrier()
    if upto == 1:
        finish(P)
        P.build()
        return nc, P

    def ys_B(e, t, dst):
        j, s0 = t // 8, (t % 8) * 128
        src = yall[j * 4096:, :][bass.ds(P.pid["sp"], 4096), :].rearrange("(r s) f -> s r f", r=4)[s0:s0 + 128, :, :]
        return e.dma_start(out=dst[:, 0:2048].rearrange("p (h f) -> p h f", h=4), in_=src)

    def ys_D(e, t, dst):
        j, s0 = t // 8, (t % 8) * 128
        src = oall[j * 4096:, :][bass.ds(P.pid["pool"], 4096), :].rearrange("(r s) f -> s r f", r=4)[s0:s0 + 128, :, :]
        return e.dma_start(out=dst[:, 0:1024].rearrange("p (h f) -> p h f", h=4), in_=src)

    def ffn_inputs(pfx, F, final):
        d = dict(wo=inp(pfx + "wo", [F, 1024]), fgain=inp(pfx + "fgain", [128, 1024]), wr=inp(pfx + "wr", [1024, 20]),
                 rbias=inp(pfx + "rbias", [128, 20]), wg=inp(pfx + "wg", [16, 1024, 512]),
                 wu=inp(pfx + "wu", [16, 1024, 512]), wd=inp(pfx + "wd", [16, 512, 1024]), ident=ident, identf=identf)
        if final:
            d["ngain"] = inp(pfx + "ngain", [128, 1024])
        return d

    DB = ffn_inputs("B_", 2048, False)
    DB.update(xs=inp("xs", [2048, 1024]), ys_fn=ys_B, ys_reads=lambda t: [f"d:yall{2 * q + t // 8}" for q in range(4)],
              h_out=h1_dram, hn_out=hn_dram)
    phase_ffn(P, nc, DB, 2048, False, "B_")
    for i in range(4):
        allgather(hn_dram[i * 512:(i + 1) * 512, :], hnall[i * 2048:(i + 1) * 2048, :], [f"d:B_hn{i}"],
                  f"d:hnall{i}", f"CC:hn{i}")
    restore(st0)
    P.barrier()
    if upto == 2:
        finish(P)
        P.build()
        return nc, P

    def hn_src(r0):
        r, i, s_ = r0 // 2048, (r0 % 2048) // 512, r0 % 512
        row = (i * 4 + r) * 512 + s_
        return hnall[row:row + 128, :]

    def after_block(j):
        if j % 4 == 3:
            i = j // 4
            allgather(o_dram[i * 1024:(i + 1) * 1024, :], oall[i * 4096:(i + 1) * 4096, :], [f"d:C_o{i}"],
                      f"d:oall{i}", f"CC:o{i}")
    DC = dict(wq=inp("C_wq", [1024, 256]), wk=inp("C_wk", [1024, 256]), wv=inp("C_wv", [1024, 256]),
              gq=inp("C_gq", [128, 8]), gkv=inp("C_gkv", [128, 8]), cos32=inp("C_cos32", [32, 8192]),
              sin32=inp("C_sin32", [32, 8192]), cmask=inp("C_cmask", [128, 2, 256], BF16), ident=ident,
              hn_src=hn_src, hn_reads=lambda r0: [f"d:hnall{(r0 % 2048) // 512}"], o_out=o_dram,
              after_block=after_block)
    phase_moba(P, nc, DC)
    restore(st0)
    P.barrier()
    if upto == 3:
        finish(P)
        P.build()
        return nc, P

    DD = ffn_inputs("D_", 1024, True)
    DD.update(xs=h1_dram, xs_reads=["d:B_h_out"], ys_fn=ys_D, ys_eng="pool", ys_reads=lambda t: [f"d:oall{2 * q + t // 8}" for q in range(4)],
              h_out=out)
    phase_ffn(P, nc, DD, 1024, True, "D_")
    finish(P)
    P.build()
    return nc, P


def fused_maps(x, ret_norm, ret_w_in, ret_w_out, kv_norm, w_kv, attn_norm, w_q, w_o, ffn_norm,
               router_group_w, router_group_b, router_expert_w, router_expert_b,
               expert_w_gate, expert_w_up, expert_w_down, final_norm):
    cosT, sinT = ret_tables()
    cos32, sin32, cmask = moba_tables()
    ident = np.eye(128, dtype=np.float32).astype(ml_dtypes.bfloat16)
    identf = np.eye(128, dtype=np.float32)
    bc = lambda v, n: np.ascontiguousarray(np.broadcast_to(v[None, :], (128, n)))
    common = dict(ident=ident, identf=identf, A_gain=np.ascontiguousarray(ret_norm[0].reshape(8, 128).T),
                  A_cosT=cosT, A_sinT=sinT, C_cos32=cos32, C_sin32=sin32, C_cmask=cmask,
                  C_gq=np.ascontiguousarray(attn_norm[0].reshape(8, 128).T),
                  C_gkv=np.ascontiguousarray(kv_norm.reshape(8, 128).T))
    for pfx, l, wo in (("B_", 0, ret_w_out[0]), ("D_", 1, w_o[0])):
        rb = np.concatenate([router_group_b[l], router_expert_b[l].reshape(-1)])
        common.update({
            pfx + "wo": np.ascontiguousarray(wo), pfx + "fgain": bc(ffn_norm[l], 1024),
            pfx + "wr": np.ascontiguousarray(np.concatenate([router_group_w[l]] + [router_expert_w[l][g] for g in range(4)], axis=1)),
            pfx + "rbias": bc(rb, 20),
            pfx + "wg": np.ascontiguousarray(expert_w_gate[l].reshape(16, 1024, 512)),
            pfx + "wu": np.ascontiguousarray(expert_w_up[l].reshape(16, 1024, 512)),
            pfx + "wd": np.ascontiguousarray(expert_w_down[l].reshape(16, 512, 1024))})
    common["D_ngain"] = bc(final_norm, 1024)
    dec = [ret_decay(h) for h in range(4)]
    maps = []
    w = ret_w_in[0]
    for c in range(8):
        b, r = c // 4, c % 4
        wh = np.concatenate([w[:, r * 256:(r + 1) * 256], w[:, 1024 + r * 256:1024 + (r + 1) * 256],
                             w[:, 2048 + r * 512:2048 + (r + 1) * 512], w[:, 4096 + r * 512:4096 + (r + 1) * 512]], axis=1)
        cs = slice(r * 256, (r + 1) * 256)
        m = dict(common, xb=np.ascontiguousarray(x[b]), xs=np.ascontiguousarray(x[b, r * 2048:(r + 1) * 2048]),
                 A_w=np.ascontiguousarray(wh), A_dmask=dec[r]["dmask"], A_qdec4=dec[r]["qdec4"], A_kdec=dec[r]["kdec"],
                 A_cdec=dec[r]["cdec"], C_wq=np.ascontiguousarray(w_q[0][:, cs]), C_wk=np.ascontiguousarray(w_kv[:, cs]),
                 C_wv=np.ascontiguousarray(w_kv[:, 1024 + r * 256:1024 + (r + 1) * 256]))
        maps.append({"in_" + k: v for k, v in m.items()})
    return maps


_CACHE = {}


def kernel(**inputs):
    a = {k: np.asarray(v, dtype=np.float32) for k, v in inputs.items()}
    if "nc" not in _CACHE:
        _CACHE["nc"] = build_fused()[0]
    maps = fused_maps(**a)
    res = run_bass_kernel_spmd(_CACHE["nc"], maps, core_ids=list(range(8)))
    out = np.stack([np.concatenate([np.asarray(res.results[b * 4 + q]["out"]) for q in range(4)], axis=0)
                    for b in range(2)])
    return out.astype(np.float32)
```

```python
import numpy as np
import ml_dtypes
import concourse.bass as bass
import concourse.mybir as mybir
from concourse.bass_utils import run_bass_kernel_spmd

F32 = mybir.dt.float32
BF16 = mybir.dt.bfloat16
AF = mybir.ActivationFunctionType
ALU = mybir.AluOpType
AX = mybir.AxisListType

EPS = 1e-6
ENGS = ("pe", "act", "dve", "pool", "sp")


class _First:
    def __init__(self, eng, w):
        self._e, self._w = eng, w

    def __getattr__(self, name):
        real = getattr(self._e, name)
        if not callable(real):
            return real

        def call(*a, **k):
            ins = real(*a, **k)
            if self._w is not None and hasattr(ins, "_wait_ge"):
                ins._wait_ge(*self._w)
                self._w = None
            return ins
        return call


class _Dummy:
    def then_inc(self, *a, **k):
        return self

    def _wait_ge(self, *a, **k):
        return self


class _Rec:
    def __init__(self):
        self.calls = []

    def __getattr__(self, name):
        def call(*a, **k):
            self.calls.append((name, a, k))
            return _Dummy()
        return call


class Prog:
    def __init__(self, nc):
        self.nc = nc
        self.ops = []
        self.want_pid = False
        self.reorder = True
        self.debug = None
        self.fifo = FIFO_Q
        self.psum = set()

    def op(self, eng, fn, reads=(), writes=()):
        self.ops.append(dict(eng=eng, fn=fn, r=tuple(reads), w=tuple(writes), dma=None))

    def dma(self, eng, fn, reads=(), writes=(), lane=None):
        assert lane is not None
        self.ops.append(dict(eng=eng, fn=fn, r=tuple(reads), w=tuple(writes), dma=lane))

    def cc(self, fn, reads=(), writes=(), lane=None):
        self.ops.append(dict(eng="pool", fn=fn, r=tuple(reads), w=tuple(writes), dma=lane, cc=True))

    def barrier(self):
        self.ops.append(dict(eng="barrier", fn=None, r=(), w=(), dma=None))

    def finish(self):
        self.ops.append(dict(eng="sp", fn=lambda e: e.nop(), r=(), w=(), dma=None, fin=True))

    def _cost(self, o):
        E = o["eng"]
        rec = _Rec()
        try:
            o["fn"](rec)
        except Exception:
            return (50.0, 2000.0) if o["dma"] is not None else (500.0, 0.0)
        busy, lat = 0.0, 0.0
        for name, a, k in rec.calls:
            out = k.get("out", a[0] if a else None)
            if name == "dma_start":
                src = k.get("in_")
                nb = 1
                for d_ in out.shape:
                    nb *= d_
                nb *= max(mybir.dt.size(out.dtype), mybir.dt.size(src.dtype))
                busy += 50.0
                lat += nb / 250.0
            elif name in ("matmul", "transpose"):
                src = k.get("lhsT") if name == "matmul" else k.get("in_")
                f = 4.0 if src.dtype == F32 else 1.0
                busy += (16.0 + 0.46 * out.free_size()) * f
            elif name == "collective_compute":
                busy += 100.0
            elif out is None or not hasattr(out, "free_size"):
                busy += 50.0
            else:
                nf = out.free_size()
                busy += {"act": 200.0 + 1.05 * nf, "dve": 100.0 + 1.4 * nf, "pool": 150.0 + 4.6 * nf}.get(E, 100.0 + nf)
        return busy, lat

    def _sched_seg(self, seg):
        n = len(seg)
        if n < 3:
            return seg
        deps = [set() for _ in range(n)]
        last_w, readers = {}, {}
        for i, o in enumerate(seg):
            d = deps[i]
            for b in o["r"]:
                if b in last_w:
                    d.add(last_w[b])
                if b in self.psum:
                    for r in readers.get(b, ()):
                        if seg[r]["eng"] != o["eng"]:
                            d.add(r)
            for b in o["w"]:
                if b in last_w:
                    d.add(last_w[b])
                d.update(readers.get(b, ()))
            d.discard(i)
            for b in o["w"]:
                last_w[b] = i
                readers[b] = []
            for b in o["r"]:
                readers.setdefault(b, []).append(i)
        succ = [[] for _ in range(n)]
        indeg = [len(d) for d in deps]
        for i, d in enumerate(deps):
            for x in d:
                succ[x].append(i)
        cost = [self._cost(o) for o in seg]
        free_at = dict.fromkeys(ENGS, 0.0)
        dma_free = 0.0
        ready = [0.0] * n
        ext = getattr(self, "_ext", {})
        for i, o in enumerate(seg):
            for b in o["r"]:
                if b in ext:
                    ready[i] = max(ready[i], ext[b])
        avail = {E: [] for E in ENGS}
        for i in range(n):
            if indeg[i] == 0:
                avail[seg[i]["eng"]].append(i)
        order = []
        SLACK = 150.0
        while len(order) < n:
            best = None
            for E in ENGS:
                av = avail[E]
                if not av:
                    continue
                t0 = max(free_at[E], min(ready[i] for i in av))
                if self.fifo:
                    cand = min((i for i in av if ready[i] <= t0 + SLACK), key=lambda i: (int(ready[i] / self.fifo), i))
                else:
                    cand = min(i for i in av if ready[i] <= t0 + SLACK)
                st = max(free_at[E], ready[cand])
                if best is None or (st, cand) < best[:2]:
                    best = (st, cand, E)
            st, i, E = best
            avail[E].remove(i)
            busy, lat = cost[i]
            free_at[E] = st + busy
            if seg[i].get("cc"):
                fin = st + 150000.0
            elif seg[i]["dma"] is not None:
                dma_free = max(st + busy, dma_free) + lat
                fin = dma_free + 2000.0
            else:
                fin = st + busy + 100.0
            order.append(i)
            if self.debug is not None:
                self.debug.append((seg[i], st, fin, E))
            for j in succ[i]:
                if fin > ready[j]:
                    ready[j] = fin
                indeg[j] -= 1
                if indeg[j] == 0:
                    avail[seg[j]["eng"]].append(j)
        self.sim_time = getattr(self, "sim_time", 0.0) + max(max(free_at.values()), dma_free)
        self._ext = {}
        k = 0
        for i in order:
            if seg[i].get("cc"):
                k += 1
        j = 0
        for i in order:
            if seg[i].get("cc"):
                j += 1
                late = max(0.0, 60000.0 * (j - (k - 4))) if j > k - 4 else 0.0
                for b in seg[i]["w"]:
                    self._ext[b] = late
        return [seg[i] for i in order]

    def _schedule(self, ops):
        out, seg = [], []
        for o in ops:
            if o["eng"] == "barrier" or o.get("fin"):
                out += self._sched_seg(seg)
                seg = []
                out.append(o)
            else:
                seg.append(o)
        return out + self._sched_seg(seg)

    def build(self):
        nc = self.nc
        if self.reorder:
            self.ops = self._schedule(self.ops)
        ops = self.ops
        n = len(ops)
        last_w = {}
        readers = {}
        deps = [None] * n
        bar_deps = set()
        last_on_eng = {}
        lanes_last = {}
        first_after_bar = {}
        for i, o in enumerate(ops):
            if o["eng"] == "barrier":
                bar_deps = set(last_on_eng.values()) | {v for k, v in lanes_last.items() if not k.startswith("CC:")}
                first_after_bar = {}
                last_w = {k: v for k, v in last_w.items() if k.startswith("d:")}
                readers = {k: v for k, v in readers.items() if k.startswith("d:")}
                deps[i] = set()
                continue
            raw = set()
            oth = set()
            for b in o["r"]:
                if b in last_w:
                    raw.add(last_w[b])
                if b in self.psum:
                    for r in readers.get(b, ()):
                        if ops[r]["eng"] != o["eng"]:
                            raw.add(r)
            for b in o["w"]:
                if b in last_w:
                    oth.add(last_w[b])
                for r in readers.get(b, ()):
                    oth.add(r)
            E = o["eng"]
            d = set()
            for x in raw | oth:
                if x == i:
                    continue
                ox = ops[x]
                if ox["dma"] is None and o["dma"] is None and ox["eng"] == E:
                    if E == "pe":
                        continue
                    if x not in raw:
                        continue
                d.add(x)
            if o.get("fin"):
                d |= set(last_on_eng.values()) | set(lanes_last.values())
            if bar_deps and E not in first_after_bar:
                d |= bar_deps
                first_after_bar[E] = i
            deps[i] = d
            for b in o["w"]:
                last_w[b] = i
                readers[b] = []
            for b in o["r"]:
                lst = readers.setdefault(b, [])
                if o["dma"] is None:
                    lst[:] = [r for r in lst if not (ops[r]["dma"] is None and ops[r]["eng"] == E)]
                lst.append(i)
            if o["dma"] is None:
                last_on_eng[E] = i
            else:
                lanes_last[o["dma"]] = i

        pos = [0] * n
        eng_count = {e: 0 for e in ENGS}
        lane_count = {}
        seen = {e: {} for e in ENGS}
        snap = [None] * n
        waits = [None] * n
        signals = [False] * n
        for i, o in enumerate(ops):
            if o["eng"] == "barrier":
                continue
            E = o["eng"]
            w = []
            for d in sorted(deps[i], reverse=True):
                od = ops[d]
                key = ("L", od["dma"]) if od["dma"] is not None else ("E", od["eng"])
                need = pos[d]
                if od["dma"] is not None and od["dma"].startswith("G:"):
                    need = lane_count[od["dma"]]
                if seen[E].get(key, 0) >= need:
                    continue
                w.append((key, d, need))
                if od["dma"] is None:
                    signals[d] = True
                seen[E][key] = need
                for k2, v2 in snap[d].items():
                    if seen[E].get(k2, 0) < v2:
                        seen[E][k2] = v2
            waits[i] = w
            if o["dma"] is not None:
                lane_count[o["dma"]] = lane_count.get(o["dma"], 0) + 1
                pos[i] = lane_count[o["dma"]]
            else:
                eng_count[E] += 1
                pos[i] = eng_count[E]
            snap[i] = dict(seen[E])

        sigval = [0] * n
        cnt = {e: 0 for e in ENGS}
        for i, o in enumerate(ops):
            if o["eng"] == "barrier" or o["dma"] is not None:
                continue
            if signals[i]:
                cnt[o["eng"]] += 1
            sigval[i] = cnt[o["eng"]]

        sems = {e: nc.alloc_semaphore(f"s_{e}") for e in ENGS}
        lane_sems = {ln: nc.alloc_semaphore(f"l{j}") for j, ln in enumerate(sorted(lane_count))}
        per_eng = {e: [i for i, o in enumerate(ops) if o["eng"] == e] for e in ENGS}
        self.stats = dict(n_ops=n, lanes=len(lane_sems), sig=dict(cnt),
                          nwaits=sum(len(w) for w in waits if w))

        self.pid = {}

        def run(E, eng):
            if E in ("sp", "pool") and self.want_pid:
                r = eng.alloc_register("qoff")
                eng.reg_mod(r, eng.partition_id(), 4)
                eng.reg_mul(r, r, 8192)
                self.pid[E] = eng.snap(r, min_val=0, max_val=3 * 8192)
            for i in per_eng[E]:
                o = ops[i]
                wl = []
                for key, d, need in waits[i]:
                    if key[0] == "L":
                        wl.append((lane_sems[key[1]], 1 if ops[d].get("cc") else 16 * need))
                    else:
                        wl.append((sems[key[1]], sigval[d]))
                attach = bool(wl) and not o.get("cc") and not o.get("fin")
                for sem_, val_ in (wl[:-1] if attach else wl):
                    eng.wait_ge(sem_, val_)
                ins = o["fn"](_First(eng, wl[-1]) if attach else eng)
                if o.get("cc"):
                    ins.then_inc(lane_sems[o["dma"]])
                elif o["dma"] is not None:
                    ins.then_inc(lane_sems[o["dma"]], 16)
                elif signals[i]:
                    ins.then_inc(sems[E], 1)

        with nc.Block() as block:
            @block.tensor
            def _(e):
                run("pe", e)

            @block.scalar
            def _(e):
                run("act", e)

            @block.vector
            def _(e):
                run("dve", e)

            @block.gpsimd
            def _(e):
                run("pool", e)

            @block.sync
            def _(e):
                run("sp", e)


class Ctx:
    def __init__(self, nc, P, pfx):
        self.nc, self.P, self.pfx = nc, P, pfx

    def sb(self, name, shape, dt):
        return self.nc.alloc_sbuf_tensor(f"{self.pfx}{name}", list(shape), dt)

    def ps(self, name, shape, dt=F32):
        return self.nc.alloc_psum_tensor(f"{self.pfx}{name}", list(shape), dt)


def rstd_ops(P, ss, tmp, rstd, n, rd, wr):
    P.op("act", lambda e: e.activation(out=tmp, in_=ss, func=AF.Ln, scale=1.0 / n, bias=EPS),
         reads=rd, writes=[wr + "_t"])
    P.op("act", lambda e: e.activation(out=rstd, in_=tmp, func=AF.Exp, scale=-0.5),
         reads=[wr + "_t"], writes=[wr])


A_ENGS = ("dve", "pool", "pool")
FIFO_Q = 0.0
NXT = 6


def phase_ret(P, nc, D, NT=8192, NS=2):
    C = Ctx(nc, P, "A_")
    RENG, QENG, GENG = A_ENGS
    NG = NT // 512
    ident = C.sb("ident", [128, 128], BF16)
    w_bf = C.sb("w_bf", [128, 8, 1536], BF16)
    wst = [C.sb(f"wst{i}", [128, 1536], F32) for i in range(2)]
    gain = C.sb("gain", [128, 8], F32)
    dmask = C.sb("dmask", [128, 128], F32)
    qdec4 = C.sb("qdec4", [128, 512], F32)
    kdec = C.sb("kdec", [128, 1], F32)
    cdec = C.sb("cdec", [128, 1], F32)
    xt = [C.sb(f"xt{i}", [128, 1024], F32) for i in range(NXT)]
    junk = C.sb("junk", [128, 1024], BF16)
    xn = [C.sb(f"xn{i}", [128, 1024], BF16) for i in range(2)]
    xnT = [C.sb(f"xnT{i}", [128, 8, 512], BF16) for i in range(NS)]
    cosg = [C.sb(f"cos{i}", [128, 512], F32) for i in range(NS)]
    sing = [C.sb(f"sin{i}", [128, 512], F32) for i in range(NS)]
    qrot = [C.sb(f"qrot{i}", [128, 2, 512], BF16) for i in range(NS)]
    qd = [C.sb(f"qd{i}", [128, 2, 512], BF16) for i in range(NS)]
    krot = [C.sb(f"krot{i}", [128, 2, 512], BF16) for i in range(NS)]
    vv = [C.sb(f"v{i}", [128, 4, 512], BF16) for i in range(NS)]
    sg = [C.sb(f"sg{i}", [128, 4, 512], F32) for i in range(NS)]
    ktm = [C.sb(f"ktm{i}", [128, 4, 256], BF16) for i in range(NS)]
    tmp = [C.sb(f"tmp{i}", [128, 512], F32) for i in range(4)]
    st32 = C.sb("st32", [128, 2, 512], F32)
    stbf = [C.sb(f"stbf{i}", [128, 2, 512], BF16) for i in range(2)]
    stm = [C.sb(f"stm{i}", [128, 128], BF16) for i in range(2)]
    ybuf = [C.sb(f"ybuf{i}", [128, 512], BF16) for i in range(4)]
    stat = C.sb("stat", [128, 24], F32)
    junk2 = C.sb("junk2", [128, 512], BF16)
    ge = C.sb("ge", [128, 512], F32)
    gc = C.sb("gc", [128, 512], F32)

    psT = C.ps("psT", [128, 8, 128], BF16)
    pq = [C.ps(f"pq{i}", [128, 512]) for i in range(2)]
    pv = C.ps("pv", [128, 512])
    pg = C.ps("pg", [128, 512])
    pst = C.ps("pst", [128, 128])
    po = C.ps("po", [128, 512])
    pu = C.ps("pu", [128, 512])

    P.psum |= {"A_psT", "A_pq0", "A_pq1", "A_pv", "A_pg", "A_pst", "A_po", "A_pu"}

    def ld(dst, src, name, eng="sp", lane=None):
        P.dma(eng, lambda e: e.dma_start(out=dst, in_=src), writes=[name], lane=lane or ("L:" + name))

    ld(ident[:], D["ident"], "A_ident", lane="G:const")
    ld(gain[:], D["gain"], "A_gain", lane="G:const")
    ld(dmask[:], D["dmask"], "A_dmask", lane="G:const")
    ld(qdec4[:], D["qdec4"], "A_qdec4", lane="G:const")
    ld(kdec[:], D["kdec"], "A_kdec", lane="G:const")
    ld(cdec[:], D["cdec"], "A_cdec", lane="G:const")
    wv_ = D["w"].rearrange("(kc p) f -> p kc f", p=128)
    for kc in range(8):
        s = kc % 2
        ld(wst[s][:], wv_[:, kc, :], f"A_wst{s}")
        P.op("dve", lambda e, kc=kc, s=s: e.tensor_scalar(
            out=w_bf[:, kc, :], in0=wst[s][:], scalar1=gain[:, kc:kc + 1], scalar2=None, op0=ALU.mult),
            reads=[f"A_wst{s}", "A_gain"], writes=[f"A_w{kc}"])
    WN = [f"A_w{kc}" for kc in range(8)]
    P.op("dve", lambda e: e.memset(st32[:], 0.0), writes=["A_st32_0", "A_st32_1"])
    P.op("pool", lambda e: e.memset(stbf[0][:], 0.0), writes=["A_stbf0_0", "A_stbf0_1"])

    xb = D["xb"]
    for tg in range(NG):
        s = tg % NS
        t0 = tg * 512
        ld(cosg[s][:], D["cosT"][:, t0:t0 + 512], f"A_cos{s}")
        ld(sing[s][:], D["sinT"][:, t0:t0 + 512], f"A_sin{s}")
        for tt in range(4):
            xs = (tg * 4 + tt) % NXT
            r0 = t0 + tt * 128
            ld(xt[xs][:], xb[r0:r0 + 128, :], f"A_xt{xs}")
            c0 = 3 * tt
            P.op("act", lambda e, xs=xs, c0=c0: e.activation(out=junk[:], in_=xt[xs][:], func=AF.Square,
                                                            accum_out=stat[:, c0:c0 + 1]),
                 reads=[f"A_xt{xs}"], writes=[f"A_ss{tt}"])
            rstd_ops(P, stat[:, c0:c0 + 1], stat[:, c0 + 1:c0 + 2], stat[:, c0 + 2:c0 + 3], 1024, [f"A_ss{tt}"],
                     f"A_rstd{tt}")
            xq = tt % 2
            P.op("act", lambda e, xs=xs, xq=xq, c0=c0: e.activation(out=xn[xq][:], in_=xt[xs][:], func=AF.Copy,
                                                                  scale=stat[:, c0 + 2:c0 + 3]),
                 reads=[f"A_xt{xs}", f"A_rstd{tt}"], writes=[f"A_xn{xq}"])

            def tr(e, xq=xq):
                for kc in range(8):
                    ins = e.transpose(out=psT[:, kc, :], in_=xn[xq][:, kc * 128:(kc + 1) * 128],
                                      identity=ident[:])
                return ins
            P.op("pe", tr, reads=[f"A_xn{xq}", "A_ident"], writes=["A_psT"])
            P.op("dve", lambda e, s=s, tt=tt: e.tensor_copy(out=xnT[s][:, :, tt * 128:(tt + 1) * 128],
                                                            in_=psT[:]),
                 reads=["A_psT"], writes=[f"A_xnT{s}_{tt}"])
        XN = [f"A_xnT{s}_{tt}" for tt in range(4)]

        for which, dst, c0 in (("q", qrot, 0), ("k", krot, 256)):
            for hh in range(2):
                def mm(e, hh=hh, c0=c0, s=s):
                    for kc in range(8):
                        ins = e.matmul(out=pq[hh][:], lhsT=w_bf[:, kc, c0 + hh * 128:c0 + (hh + 1) * 128],
                                       rhs=xnT[s][:, kc, :], start=(kc == 0), stop=(kc == 7))
                    return ins
                P.op("pe", mm, reads=WN + XN, writes=[f"A_pq{hh}"])
            cs, sn = cosg[s], sing[s]
            P.op("dve", lambda e, cs=cs: e.tensor_tensor(out=tmp[0][:], in0=pq[0][:], in1=cs[:], op=ALU.mult),
                 reads=["A_pq0", f"A_cos{s}"], writes=["A_tmp0"])
            P.op("dve", lambda e, sn=sn: e.tensor_tensor(out=tmp[1][:], in0=pq[1][:], in1=sn[:], op=ALU.mult),
                 reads=["A_pq1", f"A_sin{s}"], writes=["A_tmp1"])
            P.op("dve", lambda e, cs=cs: e.tensor_tensor(out=tmp[2][:], in0=pq[1][:], in1=cs[:], op=ALU.mult),
                 reads=["A_pq1", f"A_cos{s}"], writes=["A_tmp2"])
            P.op("dve", lambda e, sn=sn: e.tensor_tensor(out=tmp[3][:], in0=pq[0][:], in1=sn[:], op=ALU.mult),
                 reads=["A_pq0", f"A_sin{s}"], writes=["A_tmp3"])
            P.op(RENG, lambda e, dst=dst, s=s: e.tensor_tensor(out=dst[s][:, 0, :], in0=tmp[0][:], in1=tmp[1][:],
                                                                 op=ALU.subtract),
                 reads=["A_tmp0", "A_tmp1"], writes=[f"A_{which}rot{s}_0"])
            P.op(RENG, lambda e, dst=dst, s=s: e.tensor_tensor(out=dst[s][:, 1, :], in0=tmp[2][:], in1=tmp[3][:],
                                                                 op=ALU.add),
                 reads=["A_tmp2", "A_tmp3"], writes=[f"A_{which}rot{s}_1"])
            if which == "q":
                for dc in range(2):
                    P.op(QENG, lambda e, dc=dc, s=s: e.tensor_tensor(out=qd[s][:, dc, :], in0=qrot[s][:, dc, :],
                                                                       in1=qdec4[:], op=ALU.mult),
                         reads=[f"A_qrot{s}_{dc}", "A_qdec4"], writes=[f"A_qd{s}_{dc}"])

        for tt in range(4):
            def mv(e, tt=tt, s=s):
                for kc in range(8):
                    ins = e.matmul(out=pv[:], lhsT=xnT[s][:, kc, tt * 128:(tt + 1) * 128],
                                   rhs=w_bf[:, kc, 512:1024], start=(kc == 0), stop=(kc == 7))
                return ins
            P.op("pe", mv, reads=WN + [XN[tt]], writes=["A_pv"])
            P.op("act", lambda e, tt=tt, s=s: e.activation(out=vv[s][:, tt, :], in_=pv[:], func=AF.Copy),
                 reads=["A_pv"], writes=[f"A_v{s}_{tt}"])

            def mg(e, tt=tt, s=s):
                for kc in range(8):
                    ins = e.matmul(out=pg[:], lhsT=xnT[s][:, kc, tt * 128:(tt + 1) * 128],
                                   rhs=w_bf[:, kc, 1024:1536], start=(kc == 0), stop=(kc == 7))
                return ins
            P.op("pe", mg, reads=WN + [XN[tt]], writes=["A_pg"])
            P.op("act", lambda e: e.activation(out=ge[:], in_=pg[:], func=AF.Exp, scale=-1.0),
                 reads=["A_pg"], writes=["A_ge"])
            P.op("act", lambda e: e.activation(out=gc[:], in_=pg[:], func=AF.Copy),
                 reads=["A_pg"], writes=["A_gc"])
            P.op("dve", lambda e: e.tensor_scalar(out=ge[:], in0=ge[:], scalar1=1.0, scalar2=None, op0=ALU.add),
                 reads=["A_ge"], writes=["A_ge"])
            P.op("dve", lambda e: e.reciprocal(out=ge[:], in_=ge[:]), reads=["A_ge"], writes=["A_ge"])
            P.op(GENG, lambda e, tt=tt, s=s: e.tensor_tensor(out=sg[s][:, tt, :], in0=gc[:], in1=ge[:], op=ALU.mult),
                 reads=["A_ge", "A_gc"], writes=[f"A_sg{s}_{tt}"])

            def tk(e, tt=tt, s=s):
                for dc in range(2):
                    ins = e.transpose(out=psT[:, dc, :], in_=krot[s][:, dc, tt * 128:(tt + 1) * 128],
                                      identity=ident[:])
                return ins
            P.op("pe", tk, reads=[f"A_krot{s}_0", f"A_krot{s}_1", "A_ident"], writes=["A_psT"])
            P.op("dve", lambda e, tt=tt, s=s: e.tensor_scalar(
                out=ktm[s][:, tt, :].rearrange("p (a b) -> p a b", a=2), in0=psT[:, 0:2, :],
                scalar1=kdec[:, 0:1], scalar2=None, op0=ALU.mult),
                reads=["A_psT", "A_kdec"], writes=[f"A_ktm{s}_{tt}"])

        for tt in range(4):
            c = tg * 4 + tt
            sp_ = c % 2
            sl = slice(tt * 128, (tt + 1) * 128)

            def ms(e, s=s, sl=sl):
                for dc in range(2):
                    ins = e.matmul(out=pst[:], lhsT=krot[s][:, dc, sl], rhs=qrot[s][:, dc, sl],
                                   start=(dc == 0), stop=(dc == 1))
                return ins
            P.op("pe", ms, reads=[f"A_krot{s}_0", f"A_krot{s}_1", f"A_qrot{s}_0", f"A_qrot{s}_1"],
                 writes=["A_pst"])
            P.op("dve", lambda e, sp_=sp_: e.tensor_tensor(out=stm[sp_][:], in0=pst[:], in1=dmask[:], op=ALU.mult),
                 reads=["A_pst", "A_dmask"], writes=[f"A_stm{sp_}"])

            def mo(e, s=s, sl=sl, sp_=sp_, tt=tt):
                e.matmul(out=po[:], lhsT=stm[sp_][:], rhs=vv[s][:, tt, :], start=True, stop=False)
                for dc in range(2):
                    ins = e.matmul(out=po[:], lhsT=qd[s][:, dc, sl], rhs=stbf[sp_][:, dc, :],
                                   start=False, stop=(dc == 1))
                return ins
            P.op("pe", mo, reads=[f"A_stm{sp_}", f"A_v{s}_{tt}", f"A_qd{s}_0", f"A_qd{s}_1",
                                  f"A_stbf{sp_}_0", f"A_stbf{sp_}_1"], writes=["A_po"])
            for dc in range(2):
                P.op("pe", lambda e, s=s, tt=tt, dc=dc: e.matmul(
                    out=pu[:], lhsT=ktm[s][:, tt, dc * 128:(dc + 1) * 128], rhs=vv[s][:, tt, :],
                    start=True, stop=True),
                    reads=[f"A_ktm{s}_{tt}", f"A_v{s}_{tt}"], writes=["A_pu"])
                P.op("dve", lambda e, dc=dc: e.scalar_tensor_tensor(
                    out=st32[:, dc, :], in0=st32[:, dc, :], scalar=cdec[:, 0:1], in1=pu[:],
                    op0=ALU.mult, op1=ALU.add),
                    reads=["A_pu", "A_cdec", f"A_st32_{dc}"], writes=[f"A_st32_{dc}"])
                P.op("act", lambda e, dc=dc, sp_=sp_: e.activation(out=stbf[1 - sp_][:, dc, :], in_=st32[:, dc, :],
                                                                   func=AF.Copy),
                     reads=[f"A_st32_{dc}"], writes=[f"A_stbf{1 - sp_}_{dc}"])
            g0 = 12 + 3 * (c % 2)
            P.op("act", lambda e, g0=g0: e.activation(out=junk2[:], in_=po[:], func=AF.Square,
                                                      accum_out=stat[:, g0:g0 + 1]),
                 reads=["A_po"], writes=[f"A_ssq{c % 2}"])
            rstd_ops(P, stat[:, g0:g0 + 1], stat[:, g0 + 1:g0 + 2], stat[:, g0 + 2:g0 + 3], 512, [f"A_ssq{c % 2}"],
                     f"A_rs{c % 2}")
            yb = c % 4
            P.op("dve", lambda e, yb=yb, s=s, tt=tt, g0=g0: e.scalar_tensor_tensor(
                out=ybuf[yb][:], in0=po[:], scalar=stat[:, g0 + 2:g0 + 3], in1=sg[s][:, tt, :],
                op0=ALU.mult, op1=ALU.mult),
                reads=["A_po", f"A_rs{c % 2}", f"A_sg{s}_{tt}"], writes=[f"A_ybuf{yb}"])
            P.dma("sp", lambda e, yb=yb, c=c: e.dma_start(out=D["y_out"][c * 128:(c + 1) * 128, :], in_=ybuf[yb][:]),
                  reads=[f"A_ybuf{yb}"], writes=[f"d:A_y{c // 8}"], lane=f"S:A_ybuf{yb}")
        if "after_group" in D:
            D["after_group"](tg)


def phase_ffn(P, nc, D, F, final, pfx, NTOK=2048, NE=16, stage=3):
    C = Ctx(nc, P, pfx)
    assert F <= 2048
    N = lambda s: pfx + s
    NTILE = NTOK // 128
    FC = F // 128
    NGRP = NTOK // 512
    ident = C.sb("ident", [128, 128], BF16)
    identf = C.sb("identf", [128, 128], F32)
    h = C.sb("h", [128, NTILE, 1024], F32)
    hnT = C.sb("hnT", [128, 8, NTOK], BF16)
    wbuf = C.sb("wbuf", [128, 24576], BF16)
    wo_v = wbuf[:, 0:FC * 1024].rearrange("p (f n) -> p f n", f=FC)

    def wslot(s):
        b = s * 12288
        return (wbuf[:, b:b + 4096].rearrange("p (k f) -> p k f", k=8),
                wbuf[:, b + 4096:b + 8192].rearrange("p (k f) -> p k f", k=8),
                wbuf[:, b + 8192:b + 12288].rearrange("p (k f) -> p k f", k=4))
    yt = [C.sb(f"yt{i}", [128, F], BF16) for i in range(2)]
    yT = [C.sb(f"yT{i}", [128, FC, 128], BF16) for i in range(2)]
    hn32 = [C.sb(f"hn32_{i}", [128, 1024], F32) for i in range(2)]
    hnT32 = [C.sb(f"hnT32_{i}", [128, 8, 128], F32) for i in range(2)]
    fgain = C.sb("fgain", [128, 1024], F32)
    wr = C.sb("wr", [128, 8, 20], F32)
    rbias = C.sb("rbias", [128, 20], F32)
    comb = C.sb("comb", [128, NTILE, 16], F32)
    rts = [C.sb(f"rt{i}", [128, 64], F32) for i in range(4)]
    st = C.sb("st", [128, 3, NTILE], F32)
    junk = C.sb("junk", [128, 1024], BF16)
    sgl = [C.sb(f"sgl{i}", [128, 512], F32) for i in range(2)]
    hT = [C.sb(f"hT{i}", [128, 4, 512], BF16) for i in range(2)]
    if final:
        ngain = C.sb("ngain", [128, 1024], F32)
        ob = [C.sb(f"ob{i}", [128, 1024], F32) for i in range(2)]

    pyT = C.ps("pyT", [128, 8, 128], BF16)
    pT32 = C.ps("pT32", [128, 8, 128], F32)
    pT32v = pT32[:].rearrange("p a b -> p (a b)")
    pg0 = C.ps("pg", [128, 512])
    pg = [pg0[:], pT32v[:, 0:512]]
    pyTs = [pyT[:], pg0[:].bitcast(BF16).rearrange("p (a b) -> p a b", a=8)]
    pyTn = [N("pyT"), N("pg")]
    pu = [C.ps("pu", [128, 512])[:], pT32v[:, 512:1024]]
    pgn = [N("pg"), N("pT32a")]
    pun = [N("pu"), N("pT32b")]
    pd = C.ps("pd", [128, 2, 512])
    pr = C.ps("pr", [128, 32])

    P.psum |= {N(x) for x in ("pyT", "pT32a", "pT32b", "pg", "pu", "pd0", "pd1", "pr")}

    def ld(dst, src, name, eng="sp", lane=None):
        P.dma(eng, lambda e: e.dma_start(out=dst, in_=src), writes=[name], lane=lane or ("L:" + name))

    ld(ident[:], D["ident"], N("ident"), lane="G:const")
    ld(identf[:], D["identf"], N("identf"), lane="G:const")
    ld(fgain[:], D["fgain"], N("fgain"), lane="G:const")
    ld(wr[:], D["wr"].rearrange("(kc p) n -> p kc n", p=128), N("wr"), lane="G:const")
    ld(rbias[:], D["rbias"], N("rbias"), lane="G:const")
    if final:
        ld(ngain[:], D["ngain"], N("ngain"), lane="G:const")
    xs_v = D["xs"].rearrange("(t p) f -> p t f", p=128)
    def load_hq(q):
        tq = NTILE // 4
        P.dma("sp", lambda e, q=q, tq=tq: e.dma_start(out=h[:, q * tq:(q + 1) * tq, :], in_=xs_v[:, q * tq:(q + 1) * tq, :]),
              reads=D.get("xs_reads", ()), writes=[N(f"hq{q}")], lane="G:hq")
    HQ = lambda t: N(f"hq{t // (NTILE // 4)}")
    wo_d = D["wo"].rearrange("(fc p) n -> p fc n", p=128)
    nq = FC // 4
    for q in range(nq):
        P.dma("pool", lambda e, q=q: e.dma_start(out=wo_v[:, q * 4:(q + 1) * 4, :], in_=wo_d[:, q * 4:(q + 1) * 4, :]),
              writes=[N(f"wo{q}")], lane="G:wo")
    WO = [N(f"wo{q}") for q in range(nq)]
    SL = [N("ws0"), N("ws1")]

    for t in range(NTILE):
        s = t % 2
        if "ys_fn" in D:
            P.dma(D.get("ys_eng", "sp"), lambda e, t=t, s=s: D["ys_fn"](e, t, yt[s]), reads=D["ys_reads"](t), writes=[N(f"yt{s}")],
                  lane="L:" + N(f"yt{s}"))
        else:
            ld(yt[s][:], D["ys"][t * 128:(t + 1) * 128, :], N(f"yt{s}"))
        if t % (NTILE // 4) == 0:
            load_hq(t // (NTILE // 4))
        for half in range(FC // 8):
            pb = (t * (FC // 8) + half) % 2

            def tr(e, s=s, half=half, pb=pb):
                for j in range(8):
                    fc = half * 8 + j
                    ins = e.transpose(out=pyTs[pb][:, j, :], in_=yt[s][:, fc * 128:(fc + 1) * 128], identity=ident[:])
                return ins
            P.op("pe", tr, reads=[N(f"yt{s}"), N("ident")], writes=[pyTn[pb]])
            P.op("act", lambda e, half=half, s=s, pb=pb: e.activation(out=yT[s][:, half * 8:(half + 1) * 8, :], in_=pyTs[pb],
                                                                func=AF.Copy),
                 reads=[pyTn[pb]], writes=[N(f"yT{s}_{half}")])
        for hh in range(2):
            def mm(e, hh=hh, s=s):
                for fc in range(FC):
                    ins = e.matmul(out=pd[:, hh, :], lhsT=yT[s][:, fc, :], rhs=wo_v[:, fc, hh * 512:(hh + 1) * 512],
                                   start=(fc == 0), stop=(fc == FC - 1))
                return ins
            P.op("pe", mm, reads=[N(f"yT{s}_{i}") for i in range(FC // 8)] + WO + SL, writes=[N(f"pd{hh}")])
            P.op("dve", lambda e, t=t, hh=hh: e.tensor_tensor(out=h[:, t, hh * 512:(hh + 1) * 512],
                                                              in0=pd[:, hh, :], in1=h[:, t, hh * 512:(hh + 1) * 512],
                                                              op=ALU.add),
                 reads=[N(f"pd{hh}"), HQ(t), N(f"h{t}")], writes=[N(f"h{t}")])

    for t in range(NTILE if stage >= 2 else 0):
        P.op("act", lambda e, t=t: e.activation(out=junk[:], in_=h[:, t, :], func=AF.Square, accum_out=st[:, 0, t:t + 1]),
             reads=[N(f"h{t}")], writes=[N(f"ss{t}")])
    for t in range(NTILE if stage >= 2 else 0):
        rstd_ops(P, st[:, 0, t:t + 1], st[:, 1, t:t + 1], st[:, 2, t:t + 1], 1024, [N(f"ss{t}")], N(f"rstd{t}"))
    for t in range(NTILE if stage >= 2 else 0):
        u = t % 2
        P.op("dve", lambda e, t=t, u=u: e.scalar_tensor_tensor(out=hn32[u][:], in0=h[:, t, :], scalar=st[:, 2, t:t + 1],
                                                               in1=fgain[:], op0=ALU.mult, op1=ALU.mult),
             reads=[N(f"h{t}"), N(f"rstd{t}"), N("fgain")], writes=[N(f"hn32_{u}")])

        def trf(e, u=u):
            for kc in range(8):
                ins = e.transpose(out=pT32[:, kc, :], in_=hn32[u][:, kc * 128:(kc + 1) * 128], identity=identf[:])
            return ins
        P.op("pe", trf, reads=[N(f"hn32_{u}"), N("identf")], writes=[N("pT32a"), N("pT32b")])
        P.op("act", lambda e, t=t: e.activation(out=hnT[:, :, t * 128:(t + 1) * 128], in_=pT32[:], func=AF.Copy),
             reads=[N("pT32a"), N("pT32b")], writes=[N(f"hnT{t}")])
        P.op("dve", lambda e, u=u: e.tensor_copy(out=hnT32[u][:], in_=pT32[:]),
             reads=[N("pT32a"), N("pT32b")], writes=[N(f"hnT32_{u}")])

        def mr(e, u=u):
            for kc in range(8):
                ins = e.matmul(out=pr[:, 0:20], lhsT=hnT32[u][:, kc, :], rhs=wr[:, kc, :], start=(kc == 0), stop=(kc == 7))
            return ins
        P.op("pe", mr, reads=[N(f"hnT32_{u}"), N("wr")], writes=[N("pr")])
        rt = rts[t % 4]
        R = N(f"rt{t % 4}")
        k = [0]

        def dv(fn, rd=(), eng="dve"):
            P.op(eng, fn, reads=[R] + list(rd), writes=[R])
        dv(lambda e, rt=rt: e.tensor_tensor(out=rt[:, 0:20], in0=pr[:, 0:20], in1=rbias[:], op=ALU.add), [N("pr"), N("rbias")])
        dv(lambda e, rt=rt: e.tensor_reduce(out=rt[:, 20:21], in_=rt[:, 0:4], axis=AX.X, op=ALU.max))
        dv(lambda e, rt=rt: e.tensor_scalar(out=rt[:, 24:28], in0=rt[:, 0:4], scalar1=rt[:, 20:21], scalar2=None, op0=ALU.is_equal))
        dv(lambda e, rt=rt: e.tensor_scalar(out=rt[:, 28:32], in0=rt[:, 0:4], scalar1=rt[:, 20:21], scalar2=None, op0=ALU.subtract))
        dv(lambda e, rt=rt: e.activation(out=rt[:, 28:32], in_=rt[:, 28:32], func=AF.Exp, accum_out=rt[:, 21:22]), eng="act")
        dv(lambda e, rt=rt: e.reciprocal(out=rt[:, 22:23], in_=rt[:, 21:22]))
        dv(lambda e, rt=rt: e.tensor_scalar(out=rt[:, 32:36], in0=rt[:, 4:8], scalar1=rt[:, 24:25], scalar2=None, op0=ALU.mult))
        for g in range(1, 4):
            dv(lambda e, g=g, rt=rt: e.scalar_tensor_tensor(out=rt[:, 32:36], in0=rt[:, 4 + 4 * g:8 + 4 * g],
                                                     scalar=rt[:, 24 + g:25 + g], in1=rt[:, 32:36],
                                                     op0=ALU.mult, op1=ALU.add))
        dv(lambda e, rt=rt: e.tensor_reduce(out=rt[:, 36:37], in_=rt[:, 32:36], axis=AX.X, op=ALU.max))
        dv(lambda e, rt=rt: e.tensor_scalar(out=rt[:, 40:44], in0=rt[:, 32:36], scalar1=rt[:, 36:37], scalar2=None, op0=ALU.is_equal))
        dv(lambda e, rt=rt: e.scalar_tensor_tensor(out=rt[:, 44:48], in0=rt[:, 40:44], scalar=-1e30, in1=rt[:, 32:36],
                                            op0=ALU.mult, op1=ALU.add))
        dv(lambda e, rt=rt: e.tensor_reduce(out=rt[:, 37:38], in_=rt[:, 44:48], axis=AX.X, op=ALU.max))
        dv(lambda e, rt=rt: e.tensor_scalar(out=rt[:, 48:52], in0=rt[:, 44:48], scalar1=rt[:, 37:38], scalar2=None, op0=ALU.is_equal))
        dv(lambda e, rt=rt: e.tensor_tensor(out=rt[:, 38:39], in0=rt[:, 37:38], in1=rt[:, 36:37], op=ALU.subtract))
        dv(lambda e, rt=rt: e.activation(out=rt[:, 39:40], in_=rt[:, 38:39], func=AF.Exp), eng="act")
        dv(lambda e, rt=rt: e.tensor_scalar(out=rt[:, 52:53], in0=rt[:, 39:40], scalar1=1.0, scalar2=None, op0=ALU.add))
        dv(lambda e, rt=rt: e.reciprocal(out=rt[:, 52:53], in_=rt[:, 52:53]))
        dv(lambda e, rt=rt: e.tensor_tensor(out=rt[:, 53:54], in0=rt[:, 39:40], in1=rt[:, 52:53], op=ALU.mult))
        dv(lambda e, rt=rt: e.tensor_scalar(out=rt[:, 56:60], in0=rt[:, 40:44], scalar1=rt[:, 52:53], scalar2=None, op0=ALU.mult))
        dv(lambda e, rt=rt: e.scalar_tensor_tensor(out=rt[:, 56:60], in0=rt[:, 48:52], scalar=rt[:, 53:54], in1=rt[:, 56:60],
                                            op0=ALU.mult, op1=ALU.add))
        dv(lambda e, rt=rt: e.tensor_scalar(out=rt[:, 60:64], in0=rt[:, 24:28], scalar1=rt[:, 22:23], scalar2=None, op0=ALU.mult))
        for g in range(4):
            P.op("dve", lambda e, g=g, t=t, rt=rt: e.tensor_scalar(out=comb[:, t, 4 * g:4 * g + 4], in0=rt[:, 56:60],
                                                            scalar1=rt[:, 60 + g:61 + g], scalar2=None, op0=ALU.mult),
                 reads=[R], writes=[N(f"comb{t}")])

    HNT = [N(f"hnT{t}") for t in range(NTILE)]
    pend = None
    it = 0
    wl_cnt = [0]
    if final:
        steps = [(ex, tg) for ex in range(NE) for tg in range(NGRP)]
    else:
        steps = [(ex, tg) for ex in range(NE - 2) for tg in range(NGRP)] + \
                [(ex, tg) for tg in range(NGRP) for ex in (NE - 2, NE - 1)]
    loaded = set()
    for ex, tg in (steps if stage >= 3 else []):
        s = ex % 2
        wg_s, wu_s, wd_s = wslot(s)
        if ex not in loaded:
            loaded.add(ex)
            wg_d = D["wg"][ex].rearrange("(kc p) f -> p kc f", p=128)
            wu_d = D["wu"][ex].rearrange("(kc p) f -> p kc f", p=128)
            wd_d = D["wd"][ex].rearrange("(fc p) n -> p fc n", p=128)
            for (dst, src) in ((wg_s, wg_d), (wu_s, wu_d), (wd_s, wd_d)):
                P.dma("pool", lambda e, dst=dst, src=src: e.dma_start(out=dst, in_=src),
                      writes=[SL[s]], lane="L:" + SL[s])
        if True:
            b = it % 2
            for fc in range(4):
                pb = (it * 4 + fc) % 2

                def mg(e, fc=fc, tg=tg, pb=pb, wg_s=wg_s):
                    for kc in range(8):
                        ins = e.matmul(out=pg[pb], lhsT=wg_s[:, kc, fc * 128:(fc + 1) * 128],
                                       rhs=hnT[:, kc, tg * 512:(tg + 1) * 512], start=(kc == 0), stop=(kc == 7))
                    return ins

                def mu(e, fc=fc, tg=tg, pb=pb, wu_s=wu_s):
                    for kc in range(8):
                        ins = e.matmul(out=pu[pb], lhsT=wu_s[:, kc, fc * 128:(fc + 1) * 128],
                                       rhs=hnT[:, kc, tg * 512:(tg + 1) * 512], start=(kc == 0), stop=(kc == 7))
                    return ins
                hn_names = HNT[tg * 4:(tg + 1) * 4]
                P.op("pe", mg, reads=[SL[s]] + hn_names, writes=[pgn[pb]])
                P.op("pe", mu, reads=[SL[s]] + hn_names, writes=[pun[pb]])
                P.op("act", lambda e, pb=pb: e.activation(out=sgl[pb][:], in_=pg[pb], func=AF.Silu),
                     reads=[pgn[pb]], writes=[N(f"sgl{pb}")])
                P.op("dve", lambda e, pb=pb, b=b, fc=fc: e.tensor_tensor(out=hT[b][:, fc, :], in0=pu[pb], in1=sgl[pb][:],
                                                                       op=ALU.mult),
                     reads=[pun[pb], N(f"sgl{pb}")], writes=[N(f"hT{b}_{fc}")])

            def down(ex=ex, tg=tg, b=b, s=s, wd_s=wd_s):
                for tt in range(4):
                    t = tg * 4 + tt
                    for hh in range(2):
                        def md(e, tt=tt, hh=hh):
                            for fc in range(4):
                                ins = e.matmul(out=pd[:, hh, :], lhsT=hT[b][:, fc, tt * 128:(tt + 1) * 128],
                                               rhs=wd_s[:, fc, hh * 512:(hh + 1) * 512], start=(fc == 0), stop=(fc == 3))
                            return ins
                        P.op("pe", md, reads=[SL[s]] + [N(f"hT{b}_{fc}") for fc in range(4)], writes=[N(f"pd{hh}")])
                        P.op("dve", lambda e, t=t, hh=hh: e.scalar_tensor_tensor(
                            out=h[:, t, hh * 512:(hh + 1) * 512], in0=pd[:, hh, :], scalar=comb[:, t, ex:ex + 1],
                            in1=h[:, t, hh * 512:(hh + 1) * 512], op0=ALU.mult, op1=ALU.add),
                            reads=[N(f"pd{hh}"), N(f"comb{t}"), N(f"h{t}")], writes=[N(f"h{t}")])
            if pend is not None:
                pend()
            pend = down
            it += 1
    if pend is not None:
        pend()

    if not final:
        for t in range(NTILE):
            P.dma("sp", lambda e, t=t: e.dma_start(out=D["h_out"][t * 128:(t + 1) * 128, :], in_=h[:, t, :]),
                  reads=[N(f"h{t}")], writes=["d:" + N("h_out")], lane="S:" + N(f"h{t % 4}"))
        if "hn_out" in D:
            for t in range(NTILE):
                P.op("act", lambda e, t=t: e.activation(out=junk[:], in_=h[:, t, :], func=AF.Square,
                                                       accum_out=st[:, 0, t:t + 1]),
                     reads=[N(f"h{t}")], writes=[N(f"nss{t}")])
            for t in range(NTILE):
                rstd_ops(P, st[:, 0, t:t + 1], st[:, 1, t:t + 1], st[:, 2, t:t + 1], 1024, [N(f"nss{t}")], N(f"nrstd{t}"))
            for t in range(NTILE):
                s2 = t % 2
                P.op("act", lambda e, t=t, s2=s2: e.activation(out=yt[s2][:, 0:1024], in_=h[:, t, :], func=AF.Copy,
                                                              scale=st[:, 2, t:t + 1]),
                     reads=[N(f"h{t}"), N(f"nrstd{t}")], writes=[N(f"yt{s2}")])
                P.dma("sp", lambda e, t=t, s2=s2: e.dma_start(out=D["hn_out"][t * 128:(t + 1) * 128, :],
                                                             in_=yt[s2][:, 0:1024]),
                      reads=[N(f"yt{s2}")], writes=["d:" + N(f"hn{t // 4}")], lane="S:" + N(f"yt{s2}"))
    else:
        for t in range(NTILE):
            P.op("act", lambda e, t=t: e.activation(out=junk[:], in_=h[:, t, :], func=AF.Square, accum_out=st[:, 0, t:t + 1]),
                 reads=[N(f"h{t}")], writes=[N(f"fss{t}")])
        for t in range(NTILE):
            rstd_ops(P, st[:, 0, t:t + 1], st[:, 1, t:t + 1], st[:, 2, t:t + 1], 1024, [N(f"fss{t}")], N(f"frstd{t}"))
        for t in range(NTILE):
            s = t % 2
            P.op("dve", lambda e, t=t, s=s: e.scalar_tensor_tensor(out=ob[s][:], in0=h[:, t, :], scalar=st[:, 2, t:t + 1],
                                                                  in1=ngain[:], op0=ALU.mult, op1=ALU.mult),
                 reads=[N(f"h{t}"), N(f"frstd{t}"), N("ngain")], writes=[N(f"ob{s}")])
            P.dma("sp", lambda e, t=t, s=s: e.dma_start(out=D["h_out"][t * 128:(t + 1) * 128, :], in_=ob[s][:]),
                  reads=[N(f"ob{s}")], writes=["d:" + N("h_out")], lane="S:" + N(f"ob{s}"))


def build_ffn(F, final, NTOK=2048, NE=16, stage=3):
    nc = bass.Bass("TRN2", target_bir_lowering=False)
    P = Prog(nc)
    D = {}

    def inp(name, shape, dt=F32):
        D[name] = nc.dram_tensor(name, list(shape), dt, kind="ExternalInput").ap()
    inp("xs", [NTOK, 1024]); inp("ys", [NTOK, F], BF16); inp("wo", [F, 1024]); inp("fgain", [128, 1024])
    inp("wr", [1024, 20]); inp("rbias", [128, 20]); inp("wg", [NE, 1024, 512]); inp("wu", [NE, 1024, 512])
    inp("wd", [NE, 512, 1024]); inp("ident", [128, 128], BF16); inp("identf", [128, 128])
    if final:
        inp("ngain", [128, 1024])
    D["h_out"] = nc.dram_tensor("h_out", [NTOK, 1024], F32, kind="ExternalOutput").ap()
    phase_ffn(P, nc, D, F, final, "B_", NTOK, NE, stage)
    finish(P, ["d:h_out"])
    P.build()
    return nc, P


def ffn_maps(xs_list, ys_list, wo, fgain, rgw, rgb, rew, reb, wg, wu, wd, ngain=None):
    ident = np.eye(128, dtype=np.float32).astype(ml_dtypes.bfloat16)
    identf = np.eye(128, dtype=np.float32)
    wr = np.ascontiguousarray(np.concatenate([rgw] + [rew[g] for g in range(4)], axis=1))
    rb = np.concatenate([rgb, reb.reshape(-1)])[None, :]
    common = dict(wo=np.ascontiguousarray(wo), fgain=np.ascontiguousarray(np.broadcast_to(fgain[None, :], (128, 1024))),
                  wr=wr, rbias=np.ascontiguousarray(np.broadcast_to(rb, (128, 20))),
                  wg=np.ascontiguousarray(wg.reshape(16, 1024, 512)), wu=np.ascontiguousarray(wu.reshape(16, 1024, 512)),
                  wd=np.ascontiguousarray(wd.reshape(16, 512, 1024)), ident=ident, identf=identf)
    if ngain is not None:
        common["ngain"] = np.ascontiguousarray(np.broadcast_to(ngain[None, :], (128, 1024)))
    return [dict(common, xs=np.ascontiguousarray(xs_list[c]), ys=np.ascontiguousarray(ys_list[c])) for c in range(8)]


def phase_moba(P, nc, D, NT=8192):
    C = Ctx(nc, P, "C_")
    N = lambda s: "C_" + s
    NG = NT // 512
    NQT = NT // 128
    NB = NT // 256
    ident = C.sb("ident", [128, 128], BF16)
    cmask = C.sb("cmask", [128, 2, 256], BF16)
    gq = C.sb("gq", [128, 8], F32)
    gkv = C.sb("gkv", [128, 8], F32)
    wst = C.sb("wst", [128, 8, 256], F32)
    wq_bf = C.sb("wq_bf", [128, 8, 256], BF16)
    wk_bf = C.sb("wk_bf", [128, 8, 256], BF16)
    wv_bf = C.sb("wv_bf", [128, 8, 256], BF16)
    wq_rot = C.sb("wq_rot", [128, 8, 2, 32], BF16)
    wk_rot = C.sb("wk_rot", [128, 8, 2, 32], BF16)
    QT = [C.sb(f"QT{i}", [128, NT], BF16) for i in range(2)]
    KT = [C.sb(f"KT{i}", [128, NT], BF16) for i in range(2)]
    Vext = C.sb("Vext", [128, 2, NQT, 130], BF16)
    Msel = C.sb("Msel", [128, 2, NQT, 32], F32)
    km32 = C.sb("km32", [128, 2, 32], F32)
    kmT = C.sb("kmT", [128, 2, 32], BF16)
    ht = [C.sb(f"ht{i}", [128, 1024], F32) for i in range(2)]
    junk = C.sb("junk", [128, 1024], BF16)
    hn = [C.sb(f"hn{i}", [128, 1024], BF16) for i in range(2)]
    hnT = [C.sb(f"hnT{i}", [128, 8, 512], BF16) for i in range(2)]
    cosg = [C.sb(f"cos{i}", [32, 512], F32) for i in range(2)]
    sing = [C.sb(f"sin{i}", [32, 512], F32) for i in range(2)]
    ta = C.sb("ta", [32, 512], F32)
    tb = C.sb("tb", [32, 512], F32)
    xr = [C.sb(f"xr{i}", [32, 512], F32) for i in range(2)]
    xsw = [C.sb(f"xsw{i}", [32, 512], F32) for i in range(2)]
    rc = [0]
    st = C.sb("st", [128, 3, 4], F32)
    gt = C.sb("gt", [128, 32], F32)
    mx = C.sb("mx", [128, 8], F32)
    PT = [C.sb(f"PT{i}", [128, 2, 256], BF16) for i in range(3)]
    acc = [C.sb(f"acc{i}", [128, 130], F32) for i in range(2)]
    rec = C.sb("rec", [128, 2], F32)
    ob = [C.sb(f"ob{i}", [128, 128], BF16) for i in range(4)]

    psT = C.ps("psT", [128, 8, 128], BF16)
    pm = C.ps("pm", [128, 512])
    prot = C.ps("prot", [128, 512])
    pv = C.ps("pv", [128, 512])
    pS = [C.ps(f"pS{i}", [128, 2, 256]) for i in range(2)]
    pO = [C.ps(f"pO{i}", [128, 512]) for i in range(2)]
    P.psum |= {N(x) for x in ("psT", "pm", "prot", "pv", "pS0", "pS1", "pO0", "pO1")}

    def ld(dst, src, name, eng="sp", lane=None):
        P.dma(eng, lambda e: e.dma_start(out=dst, in_=src), writes=[name], lane=lane or ("L:" + name))

    ld(ident[:], D["ident"], N("ident"), lane="G:const")
    ld(cmask[:], D["cmask"], N("cmask"), lane="G:const")
    ld(gq[:], D["gq"], N("gq"), lane="G:const")
    ld(gkv[:], D["gkv"], N("gkv"), lane="G:const")
    qscale = float(128 ** -0.5)
    for (wname, wdst, g, sc) in (("wq", wq_bf, gq, qscale), ("wk", wk_bf, gkv, 1.0), ("wv", wv_bf, gkv, 1.0)):
        ld(wst[:], D[wname].rearrange("(kc p) f -> p kc f", p=128), N("wst"))
        for kc in range(8):
            P.op("dve", lambda e, kc=kc, wdst=wdst, g=g, sc=sc: e.tensor_scalar(
                out=wdst[:, kc, :], in0=wst[:, kc, :], scalar1=g[:, kc:kc + 1], scalar2=sc, op0=ALU.mult, op1=ALU.mult),
                reads=[N("wst"), N("gq"), N("gkv")], writes=[N(wname + "_bf")])
    for (wsrc, wrot, nm) in ((wq_bf, wq_rot, "wq"), (wk_bf, wk_rot, "wk")):
        for hh in range(2):
            P.op("dve", lambda e, wsrc=wsrc, wrot=wrot, hh=hh: e.tensor_scalar(
                out=wrot[:, :, hh, 0:16], in0=wsrc[:, :, hh * 128 + 16:hh * 128 + 32], scalar1=-1.0, scalar2=None,
                op0=ALU.mult), reads=[N(nm + "_bf")], writes=[N(nm + "_rot")])
            P.op("dve", lambda e, wsrc=wsrc, wrot=wrot, hh=hh: e.tensor_copy(
                out=wrot[:, :, hh, 16:32], in_=wsrc[:, :, hh * 128:hh * 128 + 16]),
                reads=[N(nm + "_bf")], writes=[N(nm + "_rot")])
    P.op("pool", lambda e: e.memset(Vext[:], 1.0), writes=[N("Vext_init")])
    P.op("pool", lambda e: e.memset(Msel[:], 0.0), writes=[N("Msel_init")])
    P.op("pool", lambda e: e.memset(kmT[:], 0.0), writes=[N("kmT_init")])

    psTb = [psT[:], pS[0][:].bitcast(BF16).rearrange("p a b -> p (a b)").rearrange("p (k c) -> p k c", k=8)]
    psTn = [N("psT"), N("pS0")]
    pmb = [pm, prot]
    pmn = [N("pm"), N("prot")]
    pvb = [pv[:, 0:256], pS[1][:].rearrange("p a b -> p (a b)")[:, 0:256]]
    pvn = [N("pv"), N("pS1")]
    cnt_t, cnt_m, cnt_v = [0], [0], [0]
    hb = D.get("hb")
    for tg in range(NG):
        s = tg % 2
        t0 = tg * 512
        ld(cosg[s][:], D["cos32"][:, t0:t0 + 512], N(f"cos{s}"))
        ld(sing[s][:], D["sin32"][:, t0:t0 + 512], N(f"sin{s}"))
        for tt in range(4):
            xs = tt % 2
            r0 = t0 + tt * 128
            if "hn_src" in D:
                P.dma("sp", lambda e, xs=xs, r0=r0: e.dma_start(out=hn[xs][:], in_=D["hn_src"](r0)),
                      reads=D["hn_reads"](r0), writes=[N(f"hn{xs}")], lane="L:" + N(f"hn{xs}"))
            else:
                ld(ht[xs][:], hb[r0:r0 + 128, :], N(f"ht{xs}"))
                P.op("act", lambda e, xs=xs, tt=tt: e.activation(out=junk[:], in_=ht[xs][:], func=AF.Square,
                                                                accum_out=st[:, 0, tt:tt + 1]),
                     reads=[N(f"ht{xs}")], writes=[N("ss")])
                rstd_ops(P, st[:, 0, tt:tt + 1], st[:, 1, tt:tt + 1], st[:, 2, tt:tt + 1], 1024, [N("ss")], N("rstd"))
                P.op("act", lambda e, xs=xs, tt=tt: e.activation(out=hn[xs][:], in_=ht[xs][:], func=AF.Copy,
                                                                scale=st[:, 2, tt:tt + 1]),
                     reads=[N(f"ht{xs}"), N("rstd")], writes=[N(f"hn{xs}")])

            bt = cnt_t[0] % 2
            cnt_t[0] += 1

            def tr(e, xs=xs, bt=bt):
                for kc in range(8):
                    ins = e.transpose(out=psTb[bt][:, kc, :], in_=hn[xs][:, kc * 128:(kc + 1) * 128], identity=ident[:])
                return ins
            P.op("pe", tr, reads=[N(f"hn{xs}"), N("ident")], writes=[psTn[bt]])
            P.op("dve", lambda e, s=s, tt=tt, bt=bt: e.tensor_copy(out=hnT[s][:, :, tt * 128:(tt + 1) * 128], in_=psTb[bt]),
                 reads=[psTn[bt]], writes=[N(f"hnT{s}_{tt}")])
        XN = [N(f"hnT{s}_{tt}") for tt in range(4)]
        for hh in range(2):
            for (w_bf, w_rot, dst, nm) in ((wq_bf, wq_rot, QT, "Q"), (wk_bf, wk_rot, KT, "K")):
                wn = "wq" if nm == "Q" else "wk"

                bm = cnt_m[0] % 2
                cnt_m[0] += 1
                pm_, pmn_ = pmb[bm], pmn[bm]

                def mm(e, w_bf=w_bf, hh=hh, s=s, pm_=pm_):
                    for kc in range(8):
                        ins = e.matmul(out=pm_[:], lhsT=w_bf[:, kc, hh * 128:(hh + 1) * 128], rhs=hnT[s][:, kc, :],
                                       start=(kc == 0), stop=(kc == 7))
                    return ins

                def mr(e, w_rot=w_rot, hh=hh, s=s):
                    for kc in range(8):
                        ins = e.matmul(out=prot[0:32, :], lhsT=w_rot[:, kc, hh, :], rhs=hnT[s][:, kc, :],
                                       start=(kc == 0), stop=(kc == 7))
                    return ins
                P.op("pe", mm, reads=[N(wn + "_bf")] + XN, writes=[pmn_])
                dname = N(f"{nm}T{hh}_{tg}")
                u = rc[0] % 2
                rc[0] += 1
                P.op("act", lambda e, u=u, pm_=pm_: e.activation(out=xr[u][:], in_=pm_[0:32, :], func=AF.Copy),
                     reads=[pmn_], writes=[N(f"xr{u}")])
                P.dma("sp", lambda e, u=u: e.dma_start(out=xsw[u][0:16, :], in_=xr[u][16:32, :]),
                      reads=[N(f"xr{u}")], writes=[N(f"xsw{u}")], lane="L:" + N(f"xsw{u}"))
                P.dma("sp", lambda e, u=u: e.dma_start(out=xsw[u][16:32, :], in_=xr[u][0:16, :]),
                      reads=[N(f"xr{u}")], writes=[N(f"xsw{u}")], lane="L:" + N(f"xsw{u}"))
                P.op("act", lambda e, dst=dst, hh=hh, t0=t0, pm_=pm_: e.activation(out=dst[hh][32:64, t0:t0 + 512],
                                                                                  in_=pm_[32:64, :], func=AF.Copy),
                     reads=[pmn_], writes=[dname + "mid"])
                P.op("act", lambda e, dst=dst, hh=hh, t0=t0, pm_=pm_: e.activation(out=dst[hh][64:128, t0:t0 + 512],
                                                                                  in_=pm_[64:128, :], func=AF.Copy),
                     reads=[pmn_], writes=[dname + "hi"])
                P.op("dve", lambda e, s=s, u=u: e.tensor_tensor(out=ta[:], in0=xr[u][:], in1=cosg[s][:], op=ALU.mult),
                     reads=[N(f"xr{u}"), N(f"cos{s}")], writes=[N("ta")])
                P.op("dve", lambda e, s=s, u=u: e.tensor_tensor(out=tb[:], in0=xsw[u][:], in1=sing[s][:], op=ALU.mult),
                     reads=[N(f"xsw{u}"), N(f"sin{s}")], writes=[N("tb")])
                P.op("pool", lambda e, dst=dst, hh=hh, t0=t0: e.tensor_tensor(out=dst[hh][0:32, t0:t0 + 512], in0=ta[:],
                                                                             in1=tb[:], op=ALU.add),
                     reads=[N("ta"), N("tb")], writes=[dname + "lo"])
            P.op("dve", lambda e, hh=hh, t0=t0, tg=tg: e.tensor_reduce(
                out=km32[:, hh, 2 * tg:2 * tg + 2], in_=KT[hh][:, t0:t0 + 512].rearrange("p (a b) -> p a b", a=2),
                axis=AX.X, op=ALU.add),
                reads=[N(f"KT{hh}_{tg}hi"), N(f"KT{hh}_{tg}lo")], writes=[N(f"km32_{hh}_{tg}")])
            P.op("act", lambda e, hh=hh, tg=tg: e.activation(out=kmT[:, hh, 2 * tg:2 * tg + 2],
                                                            in_=km32[:, hh, 2 * tg:2 * tg + 2], func=AF.Copy,
                                                            scale=1.0 / 256.0),
                 reads=[N(f"km32_{hh}_{tg}"), N("kmT_init")], writes=[N(f"kmT{hh}_{tg}")])
        for tt in range(4):
            tile = tg * 4 + tt

            bv = cnt_v[0] % 2
            cnt_v[0] += 1

            def mv(e, tt=tt, s=s, bv=bv):
                for kc in range(8):
                    ins = e.matmul(out=pvb[bv], lhsT=hnT[s][:, kc, tt * 128:(tt + 1) * 128], rhs=wv_bf[:, kc, :],
                                   start=(kc == 0), stop=(kc == 7))
                return ins
            P.op("pe", mv, reads=[N("wv_bf"), XN[tt]], writes=[pvn[bv]])
            P.op("act", lambda e, tile=tile, bv=bv: e.activation(out=Vext[:, :, tile, 0:128],
                                                                in_=pvb[bv].rearrange("p (a b) -> p a b", a=2), func=AF.Copy),
                 reads=[pvn[bv], N("Vext_init")], writes=[N(f"V{tile}")])
    gts = [gt, C.sb("gt1", [128, 32], F32)]
    mxs = [mx, C.sb("mx1", [128, 8], F32)]
    for qt in range(2, NQT):
        for hh in range(2):
            j = qt // 2
            g_, m_, pg_, pgn_ = gts[hh], mxs[hh], pO[hh], N(f"pO{hh}")
            kn = [N(f"kmT{hh}_{tg}") for tg in range((j - 1) // 2 + 1)]
            P.op("pe", lambda e, hh=hh, qt=qt, pg_=pg_: e.matmul(out=pg_[:, 0:32], lhsT=QT[hh][:, qt * 128:(qt + 1) * 128],
                                                                 rhs=kmT[:, hh, :], start=True, stop=True),
                 reads=[N(f"QT{hh}_{qt // 4}hi"), N(f"QT{hh}_{qt // 4}lo"), N("kmT_init")] + kn, writes=[pgn_])
            P.op("dve", lambda e, g_=g_, pg_=pg_: e.tensor_copy(out=g_[:], in_=pg_[:, 0:32]), reads=[pgn_], writes=[N(f"gt{hh}")])
            P.op("dve", lambda e, j=j, g_=g_: e.memset(g_[:, j:32], -1e30), reads=[N(f"gt{hh}")], writes=[N(f"gt{hh}")])
            P.op("dve", lambda e, g_=g_, m_=m_: e.max(out=m_[:], in_=g_[:]), reads=[N(f"gt{hh}")], writes=[N(f"mx{hh}")])
            P.op("dve", lambda e, hh=hh, qt=qt, g_=g_, m_=m_: e.tensor_scalar(out=Msel[:, hh, qt, :], in0=g_[:],
                                                                             scalar1=m_[:, 2:3], scalar2=None, op0=ALU.is_ge),
                 reads=[N(f"gt{hh}"), N(f"mx{hh}"), N("Msel_init")], writes=[N(f"M{hh}_{qt}")])

    pS3 = [pS[0][:], pS[1][:],
           psT[:].bitcast(F32).rearrange("p a b -> p (a b)").rearrange("p (k q) -> p k q", k=2)]
    pSn = [N("pS0"), N("pS1"), N("psT")]
    ND = 3
    pOb = [[pO[0], pm], [pO[1], prot]]
    pOn = [[N("pO0"), N("pm")], [N("pO1"), N("prot")]]
    pairs = []
    for j in range(NB):
        for hh in range(2):
            order = [j] + list(range(j))
            for idx, n in enumerate(order):
                pairs.append((j, hh, n, idx == len(order) - 1))
    oc = [0]

    def emit_s(i):
        j, hh, n, last = pairs[i]
        b = i % ND
        qs = slice(j * 256, (j + 1) * 256)
        qn = [N(f"QT{hh}_{j // 2}hi"), N(f"QT{hh}_{j // 2}lo")]

        def mS(e):
            for kt in range(2):
                k0 = (2 * n + kt) * 128
                ins = e.matmul(out=pS3[b][:, kt, :], lhsT=KT[hh][:, k0:k0 + 128], rhs=QT[hh][:, qs],
                               start=True, stop=True)
            return ins
        P.op("pe", mS, reads=qn + [N(f"KT{hh}_{n // 2}hi"), N(f"KT{hh}_{n // 2}lo")], writes=[pSn[b]])
        P.op("act", lambda e: e.activation(out=PT[b][:], in_=pS3[b], func=AF.Exp),
             reads=[pSn[b]], writes=[N(f"PT{b}")])
        if n == j:
            P.op("pool", lambda e: e.tensor_tensor(out=PT[b][:], in0=PT[b][:], in1=cmask[:], op=ALU.mult),
                 reads=[N(f"PT{b}"), N("cmask")], writes=[N(f"PT{b}")])

    def emit_o(i):
        j, hh, n, last = pairs[i]
        b = i % ND
        own = (n == j)
        for qi in range(2):
            po_, pn_ = pOb[qi][i % 2], pOn[qi][i % 2]

            def mO(e, qi=qi, po_=po_):
                for kt in range(2):
                    ins = e.matmul(out=po_[:, 0:129], lhsT=PT[b][:, kt, qi * 128:(qi + 1) * 128],
                                   rhs=Vext[:, hh, 2 * n + kt, 0:129], start=(kt == 0), stop=(kt == 1))
                return ins
            P.op("pe", mO, reads=[N(f"PT{b}"), N(f"V{2 * n}"), N(f"V{2 * n + 1}")], writes=[pn_])
            if own:
                P.op("dve", lambda e, qi=qi, po_=po_: e.tensor_copy(out=acc[qi][:, 0:129], in_=po_[:, 0:129]),
                     reads=[pn_], writes=[N(f"acc{qi}")])
            else:
                P.op("dve", lambda e, qi=qi, po_=po_: e.scalar_tensor_tensor(
                    out=acc[qi][:, 0:129], in0=po_[:, 0:129], scalar=Msel[:, hh, 2 * j + qi, n:n + 1],
                    in1=acc[qi][:, 0:129], op0=ALU.mult, op1=ALU.add),
                    reads=[pn_, N(f"M{hh}_{2 * j + qi}"), N(f"acc{qi}")], writes=[N(f"acc{qi}")])
        if last:
            for qi in range(2):
                o_ = oc[0] % 4
                oc[0] += 1
                qt = 2 * j + qi
                P.op("dve", lambda e, qi=qi: e.reciprocal(out=rec[:, qi:qi + 1], in_=acc[qi][:, 128:129]),
                     reads=[N(f"acc{qi}")], writes=[N(f"rec{qi}")])
                P.op("dve", lambda e, qi=qi, o_=o_: e.tensor_scalar(out=ob[o_][:], in0=acc[qi][:, 0:128],
                                                                    scalar1=rec[:, qi:qi + 1], scalar2=None, op0=ALU.mult),
                     reads=[N(f"acc{qi}"), N(f"rec{qi}")], writes=[N(f"ob{o_}")])
                P.dma("sp", lambda e, qt=qt, o_=o_: e.dma_start(
                    out=D["o_out"][qt * 128:(qt + 1) * 128, hh * 128:(hh + 1) * 128], in_=ob[o_][:]),
                    reads=[N(f"ob{o_}")], writes=[f"d:C_o{j // 4}"], lane="S:" + N(f"ob{o_}"))
            if hh == 1 and "after_block" in D:
                D["after_block"](j)

    for i in range(ND - 1):
        emit_s(i)
    for i in range(len(pairs)):
        if i + ND - 1 < len(pairs):
            emit_s(i + ND - 1)
        emit_o(i)


def moba_tables(S=8192):
    inv = (1.0 / (np.float32(500000.0) ** (np.arange(16, dtype=np.float32) / np.float32(16)))).astype(np.float32)
    ang = (np.arange(S, dtype=np.float32)[None, :] * inv[:, None]).astype(np.float32)
    cos = np.cos(ang).astype(np.float32)
    sin = np.sin(ang).astype(np.float32)
    k = np.arange(128)[:, None, None] + 128 * np.arange(2)[None, :, None]
    q = np.arange(256)[None, None, :]
    cmask = (k <= q).astype(np.float32).astype(ml_dtypes.bfloat16)
    return (np.ascontiguousarray(np.concatenate([cos, cos], 0)), np.ascontiguousarray(np.concatenate([-sin, sin], 0)),
            np.ascontiguousarray(cmask))


def build_moba(NT=8192):
    nc = bass.Bass("TRN2", target_bir_lowering=False)
    P = Prog(nc)
    D = {}

    def inp(name, shape, dt=F32):
        D[name] = nc.dram_tensor(name, list(shape), dt, kind="ExternalInput").ap()
    inp("hb", [NT, 1024]); inp("wq", [1024, 256]); inp("wk", [1024, 256]); inp("wv", [1024, 256])
    inp("gq", [128, 8]); inp("gkv", [128, 8]); inp("cos32", [32, NT]); inp("sin32", [32, NT])
    inp("cmask", [128, 2, 256], BF16); inp("ident", [128, 128], BF16)
    D["o_out"] = nc.dram_tensor("o_out", [NT, 256], BF16, kind="ExternalOutput").ap()
    phase_moba(P, nc, D, NT)
    finish(P)
    P.build()
    return nc, P


def moba_maps(h_list, attn_norm, kv_norm, w_q, w_kv, NT=8192):
    cos32, sin32, cmask = moba_tables()
    ident = np.eye(128, dtype=np.float32).astype(ml_dtypes.bfloat16)
    gq = np.ascontiguousarray(attn_norm.reshape(8, 128).T)
    gkv = np.ascontiguousarray(kv_norm.reshape(8, 128).T)
    maps = []
    for c in range(8):
        b, p = c // 4, c % 4
        cs = slice(p * 256, (p + 1) * 256)
        maps.append(dict(hb=np.ascontiguousarray(h_list[b][:NT]), wq=np.ascontiguousarray(w_q[:, cs]),
                         wk=np.ascontiguousarray(w_kv[:, cs]), wv=np.ascontiguousarray(w_kv[:, 1024 + p * 256:1024 + (p + 1) * 256]),
                         gq=gq, gkv=gkv, cos32=np.ascontiguousarray(cos32[:, :NT]), sin32=np.ascontiguousarray(sin32[:, :NT]),
                         cmask=cmask, ident=ident))
    return maps


def finish(P, names=None):
    P.finish()


def ret_tables(S=8192):
    half = 128
    inv = (1.0 / (np.float32(10000.0) ** (np.arange(half, dtype=np.float32) / np.float32(half)))).astype(np.float32)
    ang = (np.arange(S, dtype=np.float32)[None, :] * inv[:, None]).astype(np.float32)
    return np.cos(ang).astype(np.float32), np.sin(ang).astype(np.float32)


def ret_decay(h):
    Cn = 128
    lg = np.log1p(-np.float32(2.0) ** np.float32(-5.0 - h)).astype(np.float32)
    idx = np.arange(Cn, dtype=np.float32)
    diff = idx[:, None] - idx[None, :]
    dm = np.where(diff >= 0, np.exp(lg * np.maximum(diff, 0.0)), 0.0).astype(np.float32)
    dmaskT = (dm.T * np.float32(256 ** -0.5)).astype(np.float32)
    qdec = np.exp(lg * (idx + 1.0)).astype(np.float32)
    kdec = (np.exp(lg * (Cn - 1.0 - idx)) * np.float32(256 ** -0.5)).astype(np.float32)
    cdec = np.exp(lg * np.float32(Cn)).astype(np.float32)
    qdec4 = np.ascontiguousarray(np.broadcast_to(np.tile(qdec, 4)[None, :], (128, 512))).astype(np.float32)
    return dict(dmask=np.ascontiguousarray(dmaskT), qdec4=qdec4,
                kdec=np.ascontiguousarray(kdec[:, None]),
                cdec=np.full((128, 1), cdec, np.float32))


def build_ret(NT=8192):
    nc = bass.Bass("TRN2", target_bir_lowering=False)
    P = Prog(nc)
    D = {}

    def inp(name, shape, dt=F32):
        D[name] = nc.dram_tensor(name, list(shape), dt, kind="ExternalInput").ap()
    inp("xb", [NT, 1024]); inp("w", [1024, 1536]); inp("gain", [128, 8])
    inp("cosT", [128, NT]); inp("sinT", [128, NT]); inp("dmask", [128, 128]); inp("qdec4", [128, 512])
    inp("kdec", [128, 1]); inp("cdec", [128, 1]); inp("ident", [128, 128], BF16)
    D["y_out"] = nc.dram_tensor("y_out", [NT, 512], BF16, kind="ExternalOutput").ap()
    phase_ret(P, nc, D, NT)
    finish(P, ["d:y_out"])
    P.build()
    return nc, P


def run_ret(x, ret_norm, ret_w_in):
    nc, P = build_ret()
    cosT, sinT = ret_tables()
    ident = np.eye(128, dtype=np.float32).astype(ml_dtypes.bfloat16)
    gain = np.ascontiguousarray(ret_norm[0].reshape(8, 128).T)
    maps = []
    for c in range(8):
        b, h = c // 4, c % 4
        w = ret_w_in[0]
        wh = np.concatenate([w[:, h * 256:(h + 1) * 256], w[:, 1024 + h * 256:1024 + (h + 1) * 256],
                             w[:, 2048 + h * 512:2048 + (h + 1) * 512],
                             w[:, 4096 + h * 512:4096 + (h + 1) * 512]], axis=1)
        m = dict(xb=np.ascontiguousarray(x[b]), w=np.ascontiguousarray(wh), gain=gain, cosT=cosT, sinT=sinT,
                 ident=ident)
        m.update(ret_decay(h))
        maps.append(m)
    res = run_bass_kernel_spmd(nc, maps, core_ids=list(range(8)))
    return [r["y_out"] for r in res.results]


GROUPS = [[0, 1, 2, 3], [4, 5, 6, 7]]


def build_fused(upto=4):
    nc = bass.Bass("TRN2", target_bir_lowering=False)
    P = Prog(nc)
    P.want_pid = True

    def inp(name, shape, dt=F32):
        return nc.dram_tensor("in_" + name, list(shape), dt, kind="ExternalInput").ap()

    def scr(name, shape, dt):
        return nc.dram_tensor(name, list(shape), dt).ap()

    def state():
        return (nc.sbuf_base, nc.sbuf_top, nc.psum_base, nc.psum_top)

    def restore(st):
        nc.sbuf_base, nc.sbuf_top, nc.psum_base, nc.psum_top = st

    def allgather(src, dst, reads, wname, lane):
        P.cc(lambda e: e.collective_compute("AllGather", ALU.bypass, replica_groups=GROUPS, ins=[src], outs=[dst]),
             reads=reads, writes=[wname], lane=lane)

    ident = inp("ident", [128, 128], BF16)
    identf = inp("identf", [128, 128])
    y_dram = scr("y_dram", [8192, 512], BF16)
    yall = scr("yall", [8 * 4 * 1024, 512], BF16)
    if upto == 2:
        h1_dram = nc.dram_tensor("h1_dram", [2048, 1024], F32, kind="ExternalOutput").ap()
    else:
        h1_dram = scr("h1_dram", [2048, 1024], F32)
    hn_dram = scr("hn_dram", [2048, 1024], BF16)
    hnall = scr("hnall", [4 * 4 * 512, 1024], BF16)
    o_dram = scr("o_dram", [8192, 256], BF16)
    oall = scr("oall", [8 * 4 * 1024, 256], BF16)
    out = nc.dram_tensor("out", [2048, 1024], F32, kind="ExternalOutput").ap()
    st0 = state()

    def ag_y(i):
        allgather(y_dram[i * 1024:(i + 1) * 1024, :], yall[i * 4096:(i + 1) * 4096, :], [f"d:A_y{i}"],
                  f"d:yall{i}", f"CC:y{i}")

    def after_group(tg):
        if tg >= 2 and tg % 2 == 0:
            ag_y(tg // 2 - 1)
    DA = dict(xb=inp("xb", [8192, 1024]), w=inp("A_w", [1024, 1536]), gain=inp("A_gain", [128, 8]),
              cosT=inp("A_cosT", [128, 8192]), sinT=inp("A_sinT", [128, 8192]), dmask=inp("A_dmask", [128, 128]),
              qdec4=inp("A_qdec4", [128, 512]), kdec=inp("A_kdec", [128, 1]), cdec=inp("A_cdec", [128, 1]),
              ident=ident, y_out=y_dram, after_group=after_group)
    phase_ret(P, nc, DA)
    ag_y(7)
    restore(st0)
    P.barrier()
    if upto == 1:
        finish(P)
        P.build()
        return nc, P

    def ys_B(e, t, dst):
        j, s0 = t // 8, (t % 8) * 128
        src = yall[j * 4096:, :][bass.ds(P.pid["sp"], 4096), :].rearrange("(r s) f -> s r f", r=4)[s0:s0 + 128, :, :]
        return e.dma_start(out=dst[:, 0:2048].rearrange("p (h f) -> p h f", h=4), in_=src)

    def ys_D(e, t, dst):
        j, s0 = t // 8, (t % 8) * 128
        src = oall[j * 4096:, :][bass.ds(P.pid["pool"], 4096), :].rearrange("(r s) f -> s r f", r=4)[s0:s0 + 128, :, :]
        return e.dma_start(out=dst[:, 0:1024].rearrange("p (h f) -> p h f", h=4), in_=src)

    def ffn_inputs(pfx, F, final):
        d = dict(wo=inp(pfx + "wo", [F, 1024]), fgain=inp(pfx + "fgain", [128, 1024]), wr=inp(pfx + "wr", [1024, 20]),
                 rbias=inp(pfx + "rbias", [128, 20]), wg=inp(pfx + "wg", [16, 1024, 512]),
                 wu=inp(pfx + "wu", [16, 1024, 512]), wd=inp(pfx + "wd", [16, 512, 1024]), ident=ident, identf=identf)
        if final:
            d["ngain"] = inp(pfx + "ngain", [128, 1024])
        return d

    DB = ffn_inputs("B_", 2048, False)
    DB.update(xs=inp("xs", [2048, 1024]), ys_fn=ys_B, ys_reads=lambda t: [f"d:yall{2 * q + t // 8}" for q in range(4)],
              h_out=h1_dram, hn_out=hn_dram)
    phase_ffn(P, nc, DB, 2048, False, "B_")
    for i in range(4):
        allgather(hn_dram[i * 512:(i + 1) * 512, :], hnall[i * 2048:(i + 1) * 2048, :], [f"d:B_hn{i}"],
                  f"d:hnall{i}", f"CC:hn{i}")
    restore(st0)
    P.barrier()
    if upto == 2:
        finish(P)
        P.build()
        return nc, P

    def hn_src(r0):
        r, i, s_ = r0 // 2048, (r0 % 2048) // 512, r0 % 512
        row = (i * 4 + r) * 512 + s_
        return hnall[row:row + 128, :]

    def after_block(j):
        if j % 4 == 3:
            i = j // 4
            allgather(o_dram[i * 1024:(i + 1) * 1024, :], oall[i * 4096:(i + 1) * 4096, :], [f"d:C_o{i}"],
                      f"d:oall{i}", f"CC:o{i}")
    DC = dict(wq=inp("C_wq", [1024, 256]), wk=inp("C_wk", [1024, 256]), wv=inp("C_wv", [1024, 256]),
              gq=inp("C_gq", [128, 8]), gkv=inp("C_gkv", [128, 8]), cos32=inp("C_cos32", [32, 8192]),
              sin32=inp("C_sin32", [32, 8192]), cmask=inp("C_cmask", [128, 2, 256], BF16), ident=ident,
              hn_src=hn_src, hn_reads=lambda r0: [f"d:hnall{(r0 % 2048) // 512}"], o_out=o_dram,
              after_block=after_block)
    phase_moba(P, nc, DC)
    restore(st0)
    P.barrier()
    if upto == 3:
        finish(P)
        P.build()
        return nc, P

    DD = ffn_inputs("D_", 1024, True)
    DD.update(xs=h1_dram, xs_reads=["d:B_h_out"], ys_fn=ys_D, ys_eng="pool", ys_reads=lambda t: [f"d:oall{2 * q + t // 8}" for q in range(4)],
              h_out=out)
    phase_ffn(P, nc, DD, 1024, True, "D_")
    finish(P)
    P.build()
    return nc, P


def fused_maps(x, ret_norm, ret_w_in, ret_w_out, kv_norm, w_kv, attn_norm, w_q, w_o, ffn_norm,
               router_group_w, router_group_b, router_expert_w, router_expert_b,
               expert_w_gate, expert_w_up, expert_w_down, final_norm):
    cosT, sinT = ret_tables()
    cos32, sin32, cmask = moba_tables()
    ident = np.eye(128, dtype=np.float32).astype(ml_dtypes.bfloat16)
    identf = np.eye(128, dtype=np.float32)
    bc = lambda v, n: np.ascontiguousarray(np.broadcast_to(v[None, :], (128, n)))
    common = dict(ident=ident, identf=identf, A_gain=np.ascontiguousarray(ret_norm[0].reshape(8, 128).T),
                  A_cosT=cosT, A_sinT=sinT, C_cos32=cos32, C_sin32=sin32, C_cmask=cmask,
                  C_gq=np.ascontiguousarray(attn_norm[0].reshape(8, 128).T),
                  C_gkv=np.ascontiguousarray(kv_norm.reshape(8, 128).T))
    for pfx, l, wo in (("B_", 0, ret_w_out[0]), ("D_", 1, w_o[0])):
        rb = np.concatenate([router_group_b[l], router_expert_b[l].reshape(-1)])
        common.update({
            pfx + "wo": np.ascontiguousarray(wo), pfx + "fgain": bc(ffn_norm[l], 1024),
            pfx + "wr": np.ascontiguousarray(np.concatenate([router_group_w[l]] + [router_expert_w[l][g] for g in range(4)], axis=1)),
            pfx + "rbias": bc(rb, 20),
            pfx + "wg": np.ascontiguousarray(expert_w_gate[l].reshape(16, 1024, 512)),
            pfx + "wu": np.ascontiguousarray(expert_w_up[l].reshape(16, 1024, 512)),
            pfx + "wd": np.ascontiguousarray(expert_w_down[l].reshape(16, 512, 1024))})
    common["D_ngain"] = bc(final_norm, 1024)
    dec = [ret_decay(h) for h in range(4)]
    maps = []
    w = ret_w_in[0]
    for c in range(8):
        b, r = c // 4, c % 4
        wh = np.concatenate([w[:, r * 256:(r + 1) * 256], w[:, 1024 + r * 256:1024 + (r + 1) * 256],
                             w[:, 2048 + r * 512:2048 + (r + 1) * 512], w[:, 4096 + r * 512:4096 + (r + 1) * 512]], axis=1)
        cs = slice(r * 256, (r + 1) * 256)
        m = dict(common, xb=np.ascontiguousarray(x[b]), xs=np.ascontiguousarray(x[b, r * 2048:(r + 1) * 2048]),
                 A_w=np.ascontiguousarray(wh), A_dmask=dec[r]["dmask"], A_qdec4=dec[r]["qdec4"], A_kdec=dec[r]["kdec"],
                 A_cdec=dec[r]["cdec"], C_wq=np.ascontiguousarray(w_q[0][:, cs]), C_wk=np.ascontiguousarray(w_kv[:, cs]),
                 C_wv=np.ascontiguousarray(w_kv[:, 1024 + r * 256:1024 + (r + 1) * 256]))
        maps.append({"in_" + k: v for k, v in m.items()})
    return maps


_CACHE = {}


def kernel(**inputs):
    a = {k: np.asarray(v, dtype=np.float32) for k, v in inputs.items()}
    if "nc" not in _CACHE:
        _CACHE["nc"] = build_fused()[0]
    maps = fused_maps(**a)
    res = run_bass_kernel_spmd(_CACHE["nc"], maps, core_ids=list(range(8)))
    out = np.stack([np.concatenate([np.asarray(res.results[b * 4 + q]["out"]) for q in range(4)], axis=0)
                    for b in range(2)])
    return out.astype(np.float32)
```
